# Optimizing a Trainium2 kernel written in Bass

```python
import math
import jax, jax.numpy as jnp
from jax import lax
import numpy as np

D_MODEL = 1024
BATCH = 32
SEQ = 2048
DEPTH = 1

CTX_LEN = 256
GRID_W = 64
POS_BASE = 10000.0
EPS = 1e-6
N_MOD = 6
ML_HEADS = 4
ML_DV = (D_MODEL // 2) // ML_HEADS
ML_DK = ML_DV // 2
ML_QK = ML_HEADS * ML_DK
ML_V = ML_HEADS * ML_DV
ML_CONV = 3
ML_CHUNK = 64
ML_F_BIAS_LO = 3.0
ML_F_BIAS_HI = 6.0
GLA_HEADS = 4
GLA_DV = (D_MODEL - D_MODEL // 2) // GLA_HEADS
GLA_DK = GLA_DV // 2
GLA_QK = GLA_HEADS * GLA_DK
GLA_V = GLA_HEADS * GLA_DV
GLA_RANK = 16
GLA_NORMALIZER = 16.0
GLA_CHUNK = 64
MIX_WIDTH = ML_V + GLA_V
IN_SIZES = (ML_QK, ML_QK, ML_V, ML_V, 4 * ML_HEADS, GLA_QK, GLA_QK, GLA_V, GLA_V, 2 * GLA_RANK)
IN_WIDTH = 2 * ML_QK + 2 * ML_V + 4 * ML_HEADS + 2 * GLA_QK + 2 * GLA_V + 2 * GLA_RANK
N_EXPERTS = 32
TOP_K = 4
D_EXPERT = D_MODEL
SWIGLU_LIMIT = 7.0
SWIGLU_ALPHA = 1.702

kernel_name = 'hybrid_mlstm_gla_moe_dit_block'


def _rms(x, g):
    xf = x.astype(jnp.float32)
    y = xf * lax.rsqrt(jnp.mean(xf * xf, axis=-1, keepdims=True) + EPS)
    return (y * g.astype(jnp.float32)).astype(x.dtype)


def _modulate(h, shift, scale):
    return h * (1 + scale) + shift


def _adaln(cond, w, b):
    return (jax.nn.silu(cond) @ w + b).reshape(cond.shape[0], N_MOD, 1, w.shape[0])


def _grid_sincos(rows, d, dtype):
    f32 = jnp.float32
    nf = d // 4
    omega = 1.0 / (POS_BASE ** (jnp.arange(nf, dtype=f32) / nf))
    r = jnp.broadcast_to(jnp.arange(rows, dtype=f32)[:, None, None] * omega, (rows, GRID_W, nf))
    cl = jnp.broadcast_to(jnp.arange(GRID_W, dtype=f32)[None, :, None] * omega, (rows, GRID_W, nf))
    pe = jnp.concatenate([jnp.sin(r), jnp.cos(r), jnp.sin(cl), jnp.cos(cl)], axis=-1)
    return pe.reshape(rows * GRID_W, d).astype(dtype)


def _split_cols(z):
    offs = [int(o) for o in np.cumsum(IN_SIZES)[:-1]]
    return jnp.split(z, offs, axis=-1)


def _heads(a, n_heads):
    b, t, w = a.shape
    return a.reshape(b, t, n_heads, w // n_heads).transpose(0, 2, 1, 3)


def _merge_heads_norm(h, g, dtype):
    b, nh, t, d = h.shape
    hf = h.astype(jnp.float32)
    hf = hf * lax.rsqrt(jnp.mean(hf * hf, axis=-1, keepdims=True) + EPS)
    hf = hf.transpose(0, 2, 1, 3).reshape(b, t, nh * d)
    return (hf * g.astype(jnp.float32)).astype(dtype)


def _centred_conv(a, w, bias):
    k = w.shape[0]
    p = k // 2
    t = a.shape[1]
    ap = jnp.pad(a, ((0, 0), (p, p), (0, 0)))
    out = bias
    for i in range(k):
        out = out + ap[:, i:i + t] * w[i]
    return out


def _to_chunks(a, L):
    t = a.shape[2]
    return jnp.moveaxis(a.reshape(a.shape[:2] + (t // L, L) + a.shape[3:]), 2, 0)


def _from_chunks(hs):
    nc, b, h, L, d = hs.shape
    return jnp.moveaxis(hs, 0, 2).reshape(b, h, nc * L, d)


def _mlstm_scan(q, k, v, ig, lf, state):
    L = ML_CHUNK
    mask = jnp.tril(jnp.ones((L, L), dtype=bool))
    f32 = jnp.float32

    def step(carry, inp):
        C, n, m = carry
        qc, kc, vc, ic, fc = inp
        b = jnp.cumsum(fc, axis=-1)
        inter = b + m[..., None]
        dmat = b[..., :, None] - b[..., None, :] + ic[..., None, :]
        dmat = jnp.where(mask, dmat, -jnp.inf)
        m_t = jnp.maximum(inter, jnp.max(dmat, axis=-1))
        w_intra = jnp.exp(dmat - m_t[..., None])
        w_inter = jnp.exp(inter - m_t)
        s = jnp.einsum('bhtd,bhsd->bhts', qc, kc) * w_intra
        num = jnp.einsum('bhts,bhsv->bhtv', s, vc) + w_inter[..., None] * jnp.einsum('bhtd,bhdv->bhtv', qc, C)
        den = jnp.sum(s, axis=-1) + w_inter * jnp.einsum('bhtd,bhd->bht', qc, n)
        h = num / jnp.maximum(jnp.abs(den), jnp.exp(-m_t))[..., None]
        bL = b[..., -1]
        dec = bL[..., None] - b + ic
        m_new = jnp.maximum(bL + m, jnp.max(dec, axis=-1))
        wk = jnp.exp(dec - m_new[..., None])
        wc = jnp.exp(bL + m - m_new)
        C_new = wc[..., None, None] * C + jnp.einsum('bhs,bhsd,bhsv->bhdv', wk, kc, vc)
        n_new = wc[..., None] * n + jnp.einsum('bhs,bhsd->bhd', wk, kc)
        return (C_new, n_new, m_new), h

    xs = tuple(_to_chunks(a.astype(f32), L) for a in (q, k, v, ig, lf))
    carry, hs = lax.scan(step, state, xs)
    return _from_chunks(hs), carry


def _gla_scan(q, k, v, la, state):
    L = GLA_CHUNK
    mask = jnp.tril(jnp.ones((L, L), dtype=bool))[:, :, None]
    f32 = jnp.float32

    def step(S, inp):
        qc, kc, vc, lc = inp
        b = jnp.cumsum(lc, axis=2)
        rel = b[:, :, :, None, :] - b[:, :, None, :, :]
        rel = jnp.where(mask, rel, -jnp.inf)
        A = jnp.einsum('bhtd,bhsd,bhtsd->bhts', qc, kc, jnp.exp(rel))
        o = jnp.einsum('bhts,bhsv->bhtv', A, vc) + jnp.einsum('bhtd,bhdv->bhtv', qc * jnp.exp(b), S)
        bL = b[:, :, -1]
        S_new = jnp.exp(bL)[..., None] * S + jnp.einsum('bhsd,bhsv->bhdv', kc * jnp.exp(bL[:, :, None] - b), vc)
        return S_new, o

    xs = tuple(_to_chunks(a.astype(f32), L) for a in (q, k, v, la))
    S, hs = lax.scan(step, state, xs)
    return _from_chunks(hs), S


def _bidirectional(scan, ctx_f, ctx_b, lat_f, lat_b, state0):
    flip = lambda a: jnp.flip(a, axis=2)
    h_cf, s_f = scan(*ctx_f, state0)
    h_lf, _ = scan(*lat_f, s_f)
    h_cb, s_b = scan(*[flip(a) for a in ctx_b], state0)
    h_lb, _ = scan(*[flip(a) for a in lat_b], s_b)
    return h_cf + flip(h_cb), h_lf + flip(h_lb)


def _project(u, w_in, ml_conv_w, ml_conv_b, ml_gate_b, gla_gate_w2, gla_gate_b):
    f32 = jnp.float32
    b, t, _ = u.shape
    z = u @ w_in
    ml_q, ml_k, ml_v, ml_o, ml_g, g_q, g_k, g_v, g_g, g_lr = _split_cols(z)
    qk = jax.nn.silu(_centred_conv(jnp.concatenate([ml_q, ml_k], axis=-1), ml_conv_w, ml_conv_b))
    mq = _heads(qk[..., :ML_QK], ML_HEADS) * (ML_DK ** -0.5)
    mk = _heads(qk[..., ML_QK:], ML_HEADS)
    mv = _heads(ml_v, ML_HEADS)
    gates = (ml_g.astype(f32).reshape(b, t, 4, ML_HEADS) + ml_gate_b.astype(f32)).transpose(2, 0, 3, 1)
    ml_fwd = (mq, mk, mv, gates[0], jax.nn.log_sigmoid(gates[1]))
    ml_bwd = (mq, mk, mv, gates[2], jax.nn.log_sigmoid(gates[3]))
    gq = _heads(g_q, GLA_HEADS) * (GLA_DK ** -0.5)
    gk = _heads(g_k, GLA_HEADS)
    gv = _heads(g_v, GLA_HEADS)
    lr = g_lr.astype(f32).reshape(b, t, 2, GLA_RANK)
    pre = jnp.einsum('btzr,zrc->zbtc', lr, gla_gate_w2.astype(f32)) + gla_gate_b.astype(f32)[:, None, None, :]
    la = jax.nn.log_sigmoid(pre) / GLA_NORMALIZER
    la = la.reshape(2, b, t, GLA_HEADS, GLA_DK).transpose(0, 1, 3, 2, 4)
    return ml_fwd, ml_bwd, ml_o, (gq, gk, gv, la[0]), (gq, gk, gv, la[1]), g_g


def _hybrid_mixer(u_lat, u_ctx, w_in, ml_conv_w, ml_conv_b, ml_gate_b, ml_norm_g,
                  gla_gate_w2, gla_gate_b, gla_norm_g, w_out, ctx_out):
    p_lat = _project(u_lat, w_in, ml_conv_w, ml_conv_b, ml_gate_b, gla_gate_w2, gla_gate_b)
    p_ctx = _project(u_ctx, w_in, ml_conv_w, ml_conv_b, ml_gate_b, gla_gate_w2, gla_gate_b)
    b = u_lat.shape[0]
    f32 = jnp.float32
    ml_state0 = (jnp.zeros((b, ML_HEADS, ML_DK, ML_DV), f32), jnp.zeros((b, ML_HEADS, ML_DK), f32),
                 jnp.zeros((b, ML_HEADS), f32))
    gla_state0 = jnp.zeros((b, GLA_HEADS, GLA_DK, GLA_DV), f32)
    ml_c, ml_l = _bidirectional(_mlstm_scan, p_ctx[0], p_ctx[1], p_lat[0], p_lat[1], ml_state0)
    gla_c, gla_l = _bidirectional(_gla_scan, p_ctx[3], p_ctx[4], p_lat[3], p_lat[4], gla_state0)

    def merge(ml_h, ml_o, gla_h, g_g, dtype):
        a = _merge_heads_norm(ml_h, ml_norm_g, dtype) * jax.nn.sigmoid(ml_o)
        g = _merge_heads_norm(gla_h, gla_norm_g, dtype) * jax.nn.silu(g_g)
        return jnp.concatenate([a, g], axis=-1) @ w_out

    y_lat = merge(ml_l, p_lat[2], gla_l, p_lat[5], u_lat.dtype)
    y_ctx = merge(ml_c, p_ctx[2], gla_c, p_ctx[5], u_ctx.dtype) if ctx_out else None
    return y_lat, y_ctx


def _moe(u, router_w, router_b, w_gu, b_gu, w_down, b_down):
    bsz, t, d = u.shape
    tok = u.reshape(bsz * t, d)
    logits = (tok @ router_w + router_b).astype(jnp.float32)
    top_v, top_i = lax.top_k(logits, TOP_K)
    probs = jax.nn.softmax(top_v, axis=-1)
    gates = jnp.sum(jax.nn.one_hot(top_i, N_EXPERTS, dtype=jnp.float32) * probs[..., None], axis=1).astype(tok.dtype)
    out = jnp.zeros_like(tok)
    for e in range(N_EXPERTS):
        gu = tok @ w_gu[e] + b_gu[e]
        gate = jnp.minimum(gu[:, :D_EXPERT], SWIGLU_LIMIT)
        up = jnp.clip(gu[:, D_EXPERT:], -SWIGLU_LIMIT, SWIGLU_LIMIT)
        glu = gate * jax.nn.sigmoid(SWIGLU_ALPHA * gate)
        out = out + gates[:, e:e + 1] * (((up + 1) * glu) @ w_down[e] + b_down[e])
    return out.reshape(bsz, t, d)


def setup_inputs(seed: int = 0) -> dict:
    key = jax.random.key(seed)
    ks = jax.random.split(key, 24)
    f32 = jnp.float32
    nrm = lambda k, s, sc: jax.random.normal(k, s, f32) * sc
    D = D_MODEL
    f_bias = jnp.linspace(ML_F_BIAS_LO, ML_F_BIAS_HI, ML_HEADS, dtype=f32)
    zero_h = jnp.zeros((ML_HEADS,), f32)
    gate_base = jnp.stack([zero_h, f_bias, zero_h, f_bias])
    return {
        'x': nrm(ks[0], (BATCH, SEQ, D), 1.0),
        'c': nrm(ks[1], (BATCH, D), 1.0),
        'ctx': nrm(ks[2], (BATCH, CTX_LEN, D), 1.0),
        'c_ctx': nrm(ks[3], (D,), 1.0),
        'ada_w': nrm(ks[4], (DEPTH, D, N_MOD * D), 0.5 * D ** -0.5),
        'ada_b': nrm(ks[5], (DEPTH, N_MOD * D), 0.02),
        'norm1_g': 1.0 + nrm(ks[6], (DEPTH, D), 0.02),
        'w_in': nrm(ks[7], (DEPTH, D, IN_WIDTH), D ** -0.5),
        'ml_conv_w': nrm(ks[8], (DEPTH, ML_CONV, 2 * ML_QK), ML_CONV ** -0.5),
        'ml_conv_b': nrm(ks[9], (DEPTH, 2 * ML_QK), 0.02),
        'ml_gate_b': gate_base[None] + nrm(ks[10], (DEPTH, 4, ML_HEADS), 0.1),
        'ml_norm_g': 1.0 + nrm(ks[11], (DEPTH, ML_V), 0.02),
        'gla_gate_w2': nrm(ks[12], (DEPTH, 2, GLA_RANK, GLA_QK), GLA_RANK ** -0.5),
        'gla_gate_b': 1.0 + nrm(ks[13], (DEPTH, 2, GLA_QK), 0.1),
        'gla_norm_g': 1.0 + nrm(ks[14], (DEPTH, GLA_V), 0.02),
        'w_out': nrm(ks[15], (DEPTH, MIX_WIDTH, D), MIX_WIDTH ** -0.5),
        'norm2_g': 1.0 + nrm(ks[16], (DEPTH, D), 0.02),
        'router_w': nrm(ks[17], (DEPTH, D, N_EXPERTS), D ** -0.5),
        'router_b': nrm(ks[18], (DEPTH, N_EXPERTS), 0.01),
        'moe_w_gu': nrm(ks[19], (DEPTH, N_EXPERTS, D, 2 * D_EXPERT), D ** -0.5),
        'moe_b_gu': nrm(ks[20], (DEPTH, N_EXPERTS, 2 * D_EXPERT), 0.02),
        'moe_w_down': nrm(ks[21], (DEPTH, N_EXPERTS, D_EXPERT, D), D_EXPERT ** -0.5),
        'moe_b_down': nrm(ks[22], (DEPTH, N_EXPERTS, D), 0.02),
        'final_norm_g': 1.0 + nrm(ks[23], (D,), 0.02),
    }


def reference(x, c, ctx, c_ctx, ada_w, ada_b, norm1_g, w_in, ml_conv_w, ml_conv_b, ml_gate_b,
              ml_norm_g, gla_gate_w2, gla_gate_b, gla_norm_g, w_out, norm2_g, router_w, router_b,
              moe_w_gu, moe_b_gu, moe_w_down, moe_b_down, final_norm_g):
    T = x.shape[1]
    ROWS = T // GRID_W
    x = x + _grid_sincos(ROWS, x.shape[-1], x.dtype)[None]
    for l in range(DEPTH):
        last = l == DEPTH - 1
        m_lat = _adaln(c, ada_w[l], ada_b[l])
        m_ctx = _adaln(c_ctx[None, :], ada_w[l], ada_b[l])
        u_lat = _modulate(_rms(x, norm1_g[l]), m_lat[:, 0], m_lat[:, 1])
        u_ctx = _modulate(_rms(ctx, norm1_g[l]), m_ctx[:, 0], m_ctx[:, 1])
        y_lat, y_ctx = _hybrid_mixer(u_lat, u_ctx, w_in[l], ml_conv_w[l], ml_conv_b[l], ml_gate_b[l],
                                     ml_norm_g[l], gla_gate_w2[l], gla_gate_b[l], gla_norm_g[l], w_out[l],
                                     not last)
        x = x + m_lat[:, 2] * y_lat
        x = x + m_lat[:, 5] * _moe(_modulate(_rms(x, norm2_g[l]), m_lat[:, 3], m_lat[:, 4]), router_w[l],
                                   router_b[l], moe_w_gu[l], moe_b_gu[l], moe_w_down[l], moe_b_down[l])
        if not last:
            ctx = ctx + m_ctx[:, 2] * y_ctx
            ctx = ctx + m_ctx[:, 5] * _moe(_modulate(_rms(ctx, norm2_g[l]), m_ctx[:, 3], m_ctx[:, 4]),
                                           router_w[l], router_b[l], moe_w_gu[l], moe_b_gu[l],
                                           moe_w_down[l], moe_b_down[l])
    return _rms(x, final_norm_g)
```

```python
import math
import os
import types
import numpy as np
from contextlib import ExitStack
import concourse.bass as bass
import concourse.mybir as mybir
from concourse.bass_utils import run_bass_kernel_spmd

F32 = mybir.dt.float32
BF16 = mybir.dt.bfloat16
AF = mybir.ActivationFunctionType
ALU = mybir.AluOpType
AX = mybir.AxisListType

D = 1024
KC = 8
EPS = 1e-6
LIM = 7.0
ALPHA = 1.702


class Cfg:
    def __init__(self, NB=4, T=2048, TC=256, E=32, debug=False, stages=99):
        self.NB, self.T, self.TC, self.E = NB, T, TC, E
        self.NT = T + TC
        self.NTT = self.NT // 128
        self.NTC = TC // 128
        self.NLT = T // 128
        self.NCH = self.NT // 64
        self.NCC = TC // 64
        self.debug = debug
        self.stages = stages


def _snapshot(fn):
    if fn is None or fn.__closure__ is None:
        return fn
    cells = []
    for c in fn.__closure__:
        try:
            cells.append(types.CellType(c.cell_contents))
        except ValueError:
            cells.append(c)
    return types.FunctionType(fn.__code__, fn.__globals__, fn.__name__, fn.__defaults__, tuple(cells))


class Prog:
    ENGS = ('pe', 'act', 'dve', 'pool', 'sp')

    def __init__(self, nc, st):
        self.nc = nc
        self.sem = {e: st.enter_context(nc.semaphore('sem_' + e)) for e in self.ENGS}
        self.cnt = dict.fromkeys(self.ENGS, 0)
        self.seen = {e: {} for e in self.ENGS}
        self.streams = {e: [] for e in self.ENGS}
        self.res = {}
        self.pend = {e: [] for e in self.ENGS}
        self.dsem, self.dcnt, self.drr = {}, {}, {}
        for q, n in (('sp', 14), ('pool', 10), ('act', 4)):
            self.dsem[q] = [st.enter_context(nc.semaphore(f'dma_{q}{i}')) for i in range(n)]
            self.dcnt[q] = [0] * n
            self.drr[q] = 0
        self.all_dma_events = []

    def _need(self, eng, ev, waits, raw):
        key, sem, val = ev
        if key == eng and not raw and eng == 'pe':
            return
        if val is None:
            raise RuntimeError(f'pending event consumed: {key} by {eng}')
        if self.seen[eng].get(key, 0) >= val:
            return
        if key in waits and waits[key][2] >= val:
            return
        waits[key] = (key, sem, val)

    def _deps(self, eng, reads, writes):
        waits = {}
        for r in reads:
            s = self.res.get(r)
            if s and s[0] is not None:
                self._need(eng, s[0], waits, True)
        for w in writes:
            s = self.res.get(w)
            if s:
                if s[0] is not None:
                    self._need(eng, s[0], waits, False)
                for ev in s[1].values():
                    self._need(eng, ev, waits, False)
        wl = list(waits.values())
        for key, sem, val in wl:
            self.seen[eng][key] = max(self.seen[eng].get(key, 0), val)
        return wl

    def _register(self, ev, reads, writes):
        for r in reads:
            s = self.res.setdefault(r, [None, {}])
            s[1][ev[0]] = ev
        for w in writes:
            self.res[w] = [ev, {}]

    def op(self, eng, fn, reads=(), writes=(), inc=True):
        fn = _snapshot(fn)
        wl = self._deps(eng, reads, writes)
        if inc:
            self.cnt[eng] += 1
            ev = [eng, self.sem[eng], self.cnt[eng]]
            for p in self.pend[eng]:
                p[2] = self.cnt[eng]
            self.pend[eng] = []
        else:
            ev = [eng, self.sem[eng], None]
            self.pend[eng].append(ev)
        self._register(ev, reads, writes)
        self.streams[eng].append((wl, fn, 'inc' if inc else None))

    def dma(self, q, out, in_, reads=(), writes=(), **kw):
        k = self.drr[q]
        self.drr[q] = (k + 1) % len(self.dsem[q])
        sem = self.dsem[q][k]
        key = ('d', q, k)
        wl = self._deps(q, reads, writes)
        prev = self.dcnt[q][k]
        if prev > 0 and self.seen[q].get(key, 0) < 16 * prev:
            wl.append((key, sem, 16 * prev))
            self.seen[q][key] = 16 * prev
        self.dcnt[q][k] += 1
        ev = [key, sem, 16 * self.dcnt[q][k]]
        self._register(ev, reads, writes)
        self.all_dma_events.append(ev)

        def fn(e, out=out, in_=in_, kw=kw, sem=sem):
            e.dma_start(out=out, in_=in_, **kw).then_inc(sem, 16)
        self.streams[q].append((wl, fn, 'dma'))

    def barrier(self):
        for e in self.ENGS:
            wl = []
            for o in self.ENGS:
                if self.cnt[o] > self.seen[e].get(o, 0):
                    if self.pend[o]:
                        raise RuntimeError('barrier with pending non-inc ops on ' + o)
                    wl.append((o, self.sem[o], self.cnt[o]))
                    self.seen[e][o] = self.cnt[o]
            for q in self.dsem:
                for k, c in enumerate(self.dcnt[q]):
                    key = ('d', q, k)
                    if c > 0 and self.seen[e].get(key, 0) < 16 * c:
                        wl.append((key, self.dsem[q][k], 16 * c))
                        self.seen[e][key] = 16 * c
            if wl:
                self.streams[e].append((wl, None, None))
        self.res = {}

    def emit(self, block):
        decos = {'pe': block.tensor, 'act': block.scalar, 'dve': block.vector,
                 'pool': block.gpsimd, 'sp': block.sync}
        for name in self.ENGS:
            stream = self.streams[name]
            sem_e = self.sem[name]

            def body(e, stream=stream, sem_e=sem_e):
                for wl, fn, kind in stream:
                    for key, sem, val in wl:
                        e.wait_ge(sem, val)
                    if fn is None:
                        continue
                    ins = fn(e)
                    if kind == 'inc':
                        ins.then_inc(sem_e, 1)
            decos[name](body)


def build(cfg):
    NB, T, TC, E = cfg.NB, cfg.T, cfg.TC, cfg.E
    NT, NTT, NTC, NLT, NCH, NCC = cfg.NT, cfg.NTT, cfg.NTC, cfg.NLT, cfg.NCH, cfg.NCC
    NBC = NB + 1
    nc = bass.Bass("TRN2", target_bir_lowering=False)

    def din(name, shape):
        return nc.dram_tensor(name, list(shape), F32, kind="ExternalInput").ap()

    x_d = din("x", [NB, T, D])
    c_d = din("c", [NB, D])
    ctx_d = din("ctx", [NB, TC, D])
    cctx_d = din("c_ctx", [1, D])
    adaw_d = din("ada_w", [D, 6 * D])
    adab_d = din("ada_b", [1, 6 * D])
    n1g_d = din("norm1_g", [1, D])
    win_d = din("w_in", [D, 3120])
    cw_d = din("ml_conv_w", [3, 512])
    cb_d = din("ml_conv_b", [1, 512])
    mgb_d = din("ml_gate_b", [4, 4])
    mng_d = din("ml_norm_g", [1, 512])
    gw2_d = din("gla_gate_w2", [2, 16, 256])
    ggb_d = din("gla_gate_b", [2, 256])
    gng_d = din("gla_norm_g", [1, 512])
    wout_d = din("w_out", [D, D])
    n2g_d = din("norm2_g", [1, D])
    rw_d = din("router_w", [D, E])
    rb_d = din("router_b", [1, E])
    wgu_d = din("moe_w_gu", [E, D, 2 * D])
    bgu_d = din("moe_b_gu", [E, 2 * D])
    wdn_d = din("moe_w_down", [E, D, D])
    bdn_d = din("moe_b_down", [E, D])
    fng_d = din("final_norm_g", [1, D])
    out_d = nc.dram_tensor("out", [NB, T, D], F32, kind="ExternalOutput").ap()
    pe_d = nc.dram_tensor("pe_scratch", [T, D], F32, kind="Internal").ap()
    gsc_d = nc.dram_tensor("gate_scratch", [NBC, 2, D], F32, kind="Internal").ap()
    dbg = {}
    if cfg.debug:
        dbg['xmid'] = nc.dram_tensor("dbg_xmid", [NB, T, D], F32, kind="ExternalOutput").ap()
        dbg['mT'] = nc.dram_tensor("dbg_mT", [NB, 128, KC, T], BF16, kind="ExternalOutput").ap()
        dbg['uT'] = nc.dram_tensor("dbg_uT", [NB, 128, KC, NT], BF16, kind="ExternalOutput").ap()
        dbg['H'] = nc.dram_tensor("dbg_H", [NB, 128, NLT, 512], F32, kind="ExternalOutput").ap()
        dbg['H1'] = nc.dram_tensor("dbg_H1", [NB, 128, NLT, 512], F32, kind="ExternalOutput").ap()
        dbg['HM'] = nc.dram_tensor("dbg_HM", [NB, 128, NLT, 512], F32, kind="ExternalOutput").ap()
        dbg['edT'] = nc.dram_tensor("dbg_edT", [NB, 128, NTT, 8], F32, kind="ExternalOutput").ap()
        dbg['wcB'] = nc.dram_tensor("dbg_wcB", [NB, 128, 2, NCH], F32, kind="ExternalOutput").ap()
        dbg['qkT'] = nc.dram_tensor("dbg_qkT", [NB, 128, 4, NT], BF16, kind="ExternalOutput").ap()
        dbg['vtok'] = nc.dram_tensor("dbg_vtok", [NB, 128, NTT, 4, 129], BF16, kind="ExternalOutput").ap()
        dbg['kTok'] = nc.dram_tensor("dbg_kTok", [NB, 128, NTT, 256], BF16, kind="ExternalOutput").ap()
        dbg['qt'] = nc.dram_tensor("dbg_qt", [NB, 128, 2, NT], BF16, kind="ExternalOutput").ap()
        dbg['kt'] = nc.dram_tensor("dbg_kt", [NB, 128, 2, NT], BF16, kind="ExternalOutput").ap()
        dbg['gv'] = nc.dram_tensor("dbg_gv", [NB, 128, NTT, 512], BF16, kind="ExternalOutput").ap()
        dbg['khTok'] = nc.dram_tensor("dbg_khTok", [NB, 128, NTT, 256], BF16, kind="ExternalOutput").ap()
        dbg['ebL'] = nc.dram_tensor("dbg_ebL", [NB, 128, 2, NCH], F32, kind="ExternalOutput").ap()

    st = ExitStack()
    P = Prog(nc, st)

    def sb(name, shape, dt=F32):
        return st.enter_context(nc.sbuf_tensor(name, list(shape), dt))

    ps = [st.enter_context(nc.psum_tensor(f"ps{i}", [128, 512], F32)) for i in range(8)]
    psb = [p[:].bitcast(BF16) for p in ps]

    def PS(i):
        return ('ps', i)

    ident_f = sb("ident_f", [128, 128])
    ident_b = sb("ident_b", [128, 128], BF16)
    ones_f = sb("ones_f", [128, 128])
    ones_nt = sb("ones_nt", [128, NT], BF16)
    maskF = sb("maskF", [128, 64])
    maskB = sb("maskB", [128, 64])
    selh = sb("selh", [4, 2, 2, 64])
    eps_c = sb("eps_c", [128, 1])
    one_c = sb("one_c", [128, 1])
    P.op('pool', lambda e: e.memset(one_c[:], 1.0), writes=['one_c'])
    P.op('pool', lambda e: e.memset(eps_c[:], EPS), writes=['eps_c'])
    P.op('pool', lambda e: e.memset(ones_f[:], 1.0), writes=['ones_f'])
    P.op('pool', lambda e: e.memset(ones_nt[:], 1.0), writes=['ones_nt'])
    P.op('pool', lambda e: e.affine_select(out=ident_f[:], in_=ones_f[:], pattern=[[-1, 128]],
                                           compare_op=ALU.is_equal, fill=0.0, base=0, channel_multiplier=1),
         reads=['ones_f'], writes=['ident_f'])
    P.op('dve', lambda e: e.tensor_copy(out=ident_b[:], in_=ident_f[:]), reads=['ident_f'], writes=['ident_b'])
    for half in range(2):
        sl = slice(half * 64, half * 64 + 64)
        P.op('pool', lambda e, sl=sl: e.affine_select(out=maskF[sl, :], in_=ones_f[sl, 0:64], pattern=[[1, 64]],
                                                      compare_op=ALU.is_ge, fill=0.0, base=0, channel_multiplier=-1),
             reads=['ones_f'], writes=['maskF'])
        P.op('pool', lambda e, sl=sl: e.affine_select(out=maskB[sl, :], in_=ones_f[sl, 0:64], pattern=[[-1, 64]],
                                                      compare_op=ALU.is_ge, fill=0.0, base=0, channel_multiplier=1),
             reads=['ones_f'], writes=['maskB'])
    P.op('pool', lambda e: e.affine_select(out=selh[:].rearrange("p a b c -> p (a b c)"), in_=ones_nt[0:4, 0:256],
                                           pattern=[[-2, 2], [-1, 2], [0, 64]], compare_op=ALU.is_equal, fill=0.0, base=0,
                                           channel_multiplier=1),
         reads=['ones_nt'], writes=['selh'])

    modT = sb("modT", [128, 4, KC, NBC])
    rw_sb = sb("rw_sb", [128, KC, E])
    rb_bc = sb("rb_bc", [128, E])
    bguT = sb("bguT", [128, 16, E])
    cw_sb = sb("cw_sb", [128, 4, 3])
    cb_sb = sb("cb_sb", [128, 4])
    mgb_sb = sb("mgb_sb", [4, 4])
    wg_sb = sb("wg_sb", [128, KC, 16], BF16)
    wlr_sb = sb("wlr_sb", [128, KC, 32], BF16)
    gw2_sb = sb("gw2_sb", [16, 2, 256], BF16)
    ggbT = sb("ggbT", [128, 2, 2])
    nc_allow = nc.allow_non_contiguous_dma(reason="tiny param layouts")
    st.enter_context(nc_allow)

    P.dma('sp', rw_sb[:], rw_d.rearrange("(k p) e -> p k e", p=128), writes=['rw_sb'])
    P.dma('sp', rb_bc[:], rb_d.to_broadcast([128, E]), writes=['rb_bc'])
    for q in range(4):
        P.dma('sp', cw_sb[:, q, :], cw_d[:, q * 128:(q + 1) * 128].rearrange("i p -> p i"), writes=['cw_sb'])
    P.dma('sp', cb_sb[:], cb_d.rearrange("o (q p) -> p (o q)", p=128), writes=['cb_sb'])
    P.dma('sp', mgb_sb[:], mgb_d.rearrange("g h -> h g"), writes=['mgb_sb'])
    P.dma('sp', ggbT[:], ggb_d.rearrange("z (j p) -> p z j", p=128), writes=['ggbT'])
    P.dma('pool', wg_sb[:], win_d[:, 1536:1552].rearrange("(k p) c -> p k c", p=128), writes=['wg_sb'])
    P.dma('pool', wlr_sb[:], win_d[:, 3088:3120].rearrange("(k p) c -> p k c", p=128), writes=['wlr_sb'])
    P.dma('pool', gw2_sb[:], gw2_d.rearrange("z r c -> r z c"), writes=['gw2_sb'])
    with nc.sbuf_tensor("bgu_rows", [E, 2 * D], F32) as bgu_rows:
        P.dma('sp', bgu_rows[:], bgu_d, writes=['bgu_rows'])
        for j in range(16):
            P.op('pe', lambda e, j=j: e.transpose(out=ps[0][:, j * E:(j + 1) * E], in_=bgu_rows[:, j * 128:(j + 1) * 128],
                                                  identity=ident_f[0:E, 0:E]), reads=['bgu_rows', 'ident_f'], writes=[PS(0)],
                 inc=(j == 15))
        P.op('dve', lambda e: e.tensor_copy(out=bguT[:].rearrange("p j e -> p (j e)"), in_=ps[0][:, 0:16 * E]),
             reads=[PS(0)], writes=['bguT'])
        P.op('dve', lambda e: e.tensor_scalar(out=bguT[:, 8:16, :], in0=bguT[:, 8:16, :], scalar1=1.0, scalar2=None, op0=ALU.add),
             reads=['bguT'], writes=['bguT'])
        P.barrier()
    P.op('dve', lambda e: e.tensor_scalar(out=ggbT[:], in0=ggbT[:], scalar1=-1.0, scalar2=None, op0=ALU.mult),
         reads=['ggbT'], writes=['ggbT'])

    with nc.sbuf_tensor("om", [128, 256], F32) as om, nc.sbuf_tensor("jf", [128, 256], F32) as jf, \
            nc.sbuf_tensor("pidx", [128, 2], F32) as pidx, nc.sbuf_tensor("arg", [128, 256], F32) as arg, \
            nc.sbuf_tensor("petile", [128, 2, D], F32) as petile, nc.sbuf_tensor("omr0", [128, 256], F32) as omr0, \
            nc.sbuf_tensor("argc", [128, 256], F32) as argc, nc.sbuf_tensor("argr", [128, 256], F32) as argr, \
            nc.sbuf_tensor("arg2", [128, 256], F32) as arg2:
        P.op('pool', lambda e: e.iota(out=jf[:], pattern=[[1, 256]], base=0, channel_multiplier=0,
                                      allow_small_or_imprecise_dtypes=True), writes=['jf'])
        P.op('act', lambda e: e.activation(out=om[:], in_=jf[:], func=AF.Exp, scale=-math.log(10000.0) / 256.0),
             reads=['jf'], writes=['om'])
        for half in range(2):
            sl = slice(half * 64, half * 64 + 64)
            P.op('pool', lambda e, sl=sl: e.iota(out=pidx[sl, 0:1], pattern=[[0, 1]], base=0, channel_multiplier=1,
                                                 allow_small_or_imprecise_dtypes=True), writes=['pidx'])
            P.op('pool', lambda e, sl=sl, half=half: e.memset(pidx[sl, 1:2], float(half)), writes=['pidx'])
        PI = math.pi

        def sincos(dst_sin, dst_cos, argap, rd):
            MAGIC = 12582912.0
            for dst, off in ((dst_sin, 0.0), (dst_cos, 0.5 * PI)):
                P.op('dve', lambda e, off=off: e.tensor_scalar(out=arg2[:], in0=argap, scalar1=off, scalar2=None, op0=ALU.add),
                     reads=rd, writes=['arg2'])
                P.op('dve', lambda e: e.tensor_scalar(out=arg[:], in0=arg2[:], scalar1=1.0 / (2 * PI), scalar2=MAGIC,
                                                      op0=ALU.mult, op1=ALU.add), reads=['arg2'], writes=['arg'])
                P.op('dve', lambda e: e.tensor_scalar(out=arg[:], in0=arg[:], scalar1=-MAGIC, scalar2=None, op0=ALU.add),
                     reads=['arg'], writes=['arg'])
                P.op('dve', lambda e: e.scalar_tensor_tensor(out=arg[:], in0=arg[:], scalar=-2 * PI, in1=arg2[:],
                                                             op0=ALU.mult, op1=ALU.add), reads=['arg', 'arg2'], writes=['arg'])
                P.op('dve', lambda e: e.tensor_scalar(out=arg[:], in0=arg[:], scalar1=-PI, scalar2=PI, op0=ALU.max, op1=ALU.min),
                     reads=['arg'], writes=['arg'])
                P.op('act', lambda e, dst=dst: e.activation(out=dst, in_=arg[:], func=AF.Sin), reads=['arg'],
                     writes=['petile'])
        P.op('dve', lambda e: e.tensor_scalar(out=omr0[:], in0=om[:], scalar1=pidx[:, 1:2], scalar2=None, op0=ALU.mult),
             reads=['om', 'pidx'], writes=['omr0'])
        P.op('dve', lambda e: e.tensor_scalar(out=argc[:], in0=om[:], scalar1=pidx[:, 0:1], scalar2=None, op0=ALU.mult),
             reads=['om', 'pidx'], writes=['argc'])
        for k in range(NLT):
            buf = k % 2
            if k < 2:
                sincos(petile[:, buf, 512:768], petile[:, buf, 768:1024], argc[:], ['argc'])
            P.op('dve', lambda e, k=k: e.scalar_tensor_tensor(out=argr[:], in0=om[:], scalar=float(2 * k), in1=omr0[:],
                                                              op0=ALU.mult, op1=ALU.add), reads=['om', 'omr0'], writes=['argr'])
            sincos(petile[:, buf, 0:256], petile[:, buf, 256:512], argr[:], ['argr'])
            P.dma('sp', pe_d[k * 128:(k + 1) * 128, :], petile[:, buf, :], reads=['petile'], writes=['pe_d'])
        P.barrier()

    with nc.sbuf_tensor("cin", [NBC, D], F32) as cin, nc.sbuf_tensor("csig", [NBC, D], F32) as csig, \
            nc.sbuf_tensor("scT", [128, KC, NBC], F32) as scT, nc.sbuf_tensor("adab", [1, 6 * D], F32) as adab, \
            nc.sbuf_tensor("modrows", [NBC, 6 * D], F32) as modrows, \
            nc.sbuf_tensor("adaw", [128, 2, KC, 512], F32) as adaw, \
            nc.sbuf_tensor("grows", [NBC, 2, D], F32) as grows, \
            nc.sbuf_tensor("n1bc", [NBC, D], F32) as n1bc, nc.sbuf_tensor("n2bc", [NBC, D], F32) as n2bc:
        P.dma('sp', n1bc[:], n1g_d.to_broadcast([NBC, D]), writes=['n1bc'])
        P.dma('sp', n2bc[:], n2g_d.to_broadcast([NBC, D]), writes=['n2bc'])
        P.dma('sp', cin[0:NB, :], c_d, writes=['cin'])
        P.dma('sp', cin[NB:NBC, :], cctx_d, writes=['cin'])
        P.dma('sp', adab[:], adab_d, writes=['adab'])
        P.op('act', lambda e: e.activation(out=csig[:], in_=cin[:], func=AF.Sigmoid), reads=['cin'], writes=['csig'])
        P.op('dve', lambda e: e.tensor_tensor(out=csig[:], in0=csig[:], in1=cin[:], op=ALU.mult),
             reads=['csig', 'cin'], writes=['csig'])
        for k in range(KC):
            P.op('pe', lambda e, k=k: e.transpose(out=ps[0][:, k * NBC:(k + 1) * NBC], in_=csig[:, k * 128:(k + 1) * 128],
                                                  identity=ident_f[0:NBC, 0:NBC]),
                 reads=['csig', 'ident_f'], writes=[PS(0)], inc=(k == KC - 1))
        P.op('dve', lambda e: e.tensor_copy(out=scT[:].rearrange("p k b -> p (k b)"), in_=ps[0][:, 0:KC * NBC]),
             reads=[PS(0)], writes=['scT'])
        for cg in range(12):
            buf = cg % 2
            P.dma('sp', adaw[:, buf], adaw_d[:, cg * 512:(cg + 1) * 512].rearrange("(k p) c -> p k c", p=128),
                  writes=[('adaw', buf)])
            pb = 1 + (cg % 2)
            for k in range(KC):
                P.op('pe', lambda e, k=k, buf=buf, pb=pb: e.matmul(ps[pb][0:NBC, :], lhsT=scT[:, k, :], rhs=adaw[:, buf, k, :],
                                                                   start=(k == 0), stop=False),
                     reads=['scT', ('adaw', buf)], writes=[PS(pb)], inc=False)
            P.op('pe', lambda e, cg=cg, pb=pb: e.matmul(ps[pb][0:NBC, :], lhsT=ones_f[0:1, 0:NBC],
                                                        rhs=adab[0:1, cg * 512:(cg + 1) * 512], start=False, stop=True),
                 reads=['ones_f', 'adab'], writes=[PS(pb)])
            P.op('act', lambda e, cg=cg, pb=pb: e.copy(out=modrows[:, cg * 512:(cg + 1) * 512], in_=ps[pb][0:NBC, :]),
                 reads=[PS(pb)], writes=['modrows'])
        P.op('dve', lambda e: e.scalar_tensor_tensor(out=grows[:, 0, :], in0=modrows[:, D:2 * D], scalar=1.0, in1=n1bc[:],
                                                     op0=ALU.add, op1=ALU.mult), reads=['modrows', 'n1bc'], writes=['grows'])
        P.op('dve', lambda e: e.scalar_tensor_tensor(out=grows[:, 1, :], in0=modrows[:, 4 * D:5 * D], scalar=1.0, in1=n2bc[:],
                                                     op0=ALU.add, op1=ALU.mult), reads=['modrows', 'n2bc'], writes=['grows'])
        srcs = [grows[:, 0, :], modrows[:, 0:D], grows[:, 1, :], modrows[:, 3 * D:4 * D]]
        for m in range(4):
            for k in range(KC):
                o = (m * KC + k) * NBC
                P.op('pe', lambda e, m=m, k=k, o=o: e.transpose(out=ps[3][:, o:o + NBC], in_=srcs[m][:, k * 128:(k + 1) * 128],
                                                                identity=ident_f[0:NBC, 0:NBC]),
                     reads=['grows', 'modrows', 'ident_f'], writes=[PS(3)], inc=(m == 3 and k == KC - 1))
        P.op('dve', lambda e: e.tensor_copy(out=modT[:].rearrange("p m k b -> p (m k b)"), in_=ps[3][:, 0:4 * KC * NBC]),
             reads=[PS(3)], writes=['modT'])
        P.dma('sp', gsc_d[:, 0, :], modrows[:, 2 * D:3 * D], reads=['modrows'], writes=['gsc_d'])
        P.dma('sp', gsc_d[:, 1, :], modrows[:, 5 * D:6 * D], reads=['modrows'], writes=['gsc_d'])
        P.barrier()

    def rms_to_featmajor(xt_ap, xt_res, dstT, tok0, gsel, bsel, scratch, xn, ssq, pbanks, extra_f32=None):
        P.op('act', lambda e: e.activation(out=scratch, in_=xt_ap, func=AF.Square, accum_out=ssq),
             reads=[xt_res], writes=['rms_scratch', 'ssq'])
        P.op('act', lambda e: e.activation(out=ssq, in_=ssq, func=AF.Sqrt, scale=1.0 / D, bias=eps_c[:]),
             reads=['ssq'], writes=['ssq'])
        P.op('dve', lambda e: e.reciprocal(out=ssq, in_=ssq), reads=['ssq'], writes=['ssq'])
        P.op('dve', lambda e: e.tensor_scalar(out=xn, in0=xt_ap, scalar1=ssq, scalar2=None, op0=ALU.mult),
             reads=['ssq', xt_res], writes=['xn'])
        for k in range(KC):
            pb = pbanks[k // 4]
            P.op('pe', lambda e, k=k, pb=pb: e.transpose(out=ps[pb][:, (k % 4) * 128:(k % 4 + 1) * 128],
                                                         in_=xn[:, k * 128:(k + 1) * 128], identity=ident_f[:]),
                 reads=['xn', 'ident_f'], writes=[PS(pb)], inc=(k % 4 == 3))
        for k in range(KC):
            pb = pbanks[k // 4]
            P.op('act', lambda e, k=k, pb=pb: e.activation(out=dstT[:, k, tok0:tok0 + 128],
                                                           in_=ps[pb][:, (k % 4) * 128:(k % 4 + 1) * 128], func=AF.Identity,
                                                           scale=modT[:, gsel, k, bsel:bsel + 1],
                                                           bias=modT[:, gsel + 1, k, bsel:bsel + 1]),
                 reads=[PS(pb), 'modT'], writes=[('dstT', tok0)])
            if extra_f32 is not None:
                P.op('act', lambda e, k=k, pb=pb: e.activation(out=extra_f32[:, k, :],
                                                               in_=ps[pb][:, (k % 4) * 128:(k % 4 + 1) * 128], func=AF.Identity,
                                                               scale=modT[:, gsel, k, bsel:bsel + 1],
                                                               bias=modT[:, gsel + 1, k, bsel:bsel + 1]),
                     reads=[PS(pb), 'modT'], writes=['extra_f32'])

    ARENA = max(NLT * D, NLT * 512 + max(3 * NT, NLT * 256 + NTT * 128 + NT // 2 + 64))
    arena = sb("arena", [128, ARENA])
    x_res = arena[:, 0:NLT * D].rearrange("p (j c) -> p j c", c=D)
    gbc = sb("gbc", [128, D])
    rms_scratch = sb("rms_scratch", [128, D])
    xn = sb("xn", [128, D])
    ssq = sb("ssq", [128, 1])

    def make_gbc(b, which):
        P.dma('sp', gbc[:], gsc_d[b:b + 1, which, :].to_broadcast([128, D]), writes=['gbc'])

    for b in range(NB):
        if cfg.stages >= 2:
            build_mixer(nc, P, cfg, b, dict(
                ps=ps, psb=psb, PS=PS, ident_f=ident_f, ident_b=ident_b, ones_f=ones_f, ones_nt=ones_nt, maskF=maskF,
                maskB=maskB, selh=selh, modT=modT, x_d=x_d, ctx_d=ctx_d, pe_d=pe_d, win_d=win_d, wout_d=wout_d,
                cw_sb=cw_sb, cb_sb=cb_sb, mgb_sb=mgb_sb, wg_sb=wg_sb, wlr_sb=wlr_sb, gw2_sb=gw2_sb, ggbT=ggbT,
                mng_d=mng_d, gng_d=gng_d, x_res=x_res, arena=arena, eps_c=eps_c, one_c=one_c, gbc=gbc,
                rms_scratch=rms_scratch, xn=xn, ssq=ssq,
                make_gbc=make_gbc, rms_to_featmajor=rms_to_featmajor, dbg=dbg))
        else:
            with nc.sbuf_tensor(f"pt0_{b}", [128, 2, D], F32) as pt0:
                for j in range(NLT):
                    P.dma('sp', x_res[:, j, :], x_d[b, j * 128:(j + 1) * 128, :], writes=[('x_res', j)])
                    P.dma('sp', pt0[:, j % 2, :], pe_d[j * 128:(j + 1) * 128, :], reads=['pe_d'], writes=[('pt0', j % 2)])
                    P.op('dve', lambda e, j=j: e.tensor_tensor(out=x_res[:, j, :], in0=x_res[:, j, :], in1=pt0[:, j % 2, :],
                                                               op=ALU.add), reads=[('x_res', j), ('pt0', j % 2)],
                         writes=[('x_res', j)])
                P.barrier()
        if cfg.debug:
            for j in range(NLT):
                P.dma('sp', dbg['xmid'][b, j * 128:(j + 1) * 128, :], x_res[:, j, :], reads=[('x_res', j)],
                      writes=[('dbgx', j)])
        build_moe(nc, P, cfg, b, dict(
            ps=ps, PS=PS, ident_f=ident_f, modT=modT, x_res=x_res, gbc=gbc, rms_scratch=rms_scratch, xn=xn, ssq=ssq,
            make_gbc=make_gbc, rms_to_featmajor=rms_to_featmajor, rw_sb=rw_sb, rb_bc=rb_bc, bdn_d=bdn_d, bguT=bguT,
            wgu_d=wgu_d, wdn_d=wdn_d, fng_d=fng_d, out_d=out_d, eps_c=eps_c))

    P.barrier()
    with nc.Block() as block:
        P.emit(block)
    st.close()
    return nc


def build_moe(nc, P, cfg, b, g):
    NB, T, E, NLT = cfg.NB, cfg.T, cfg.E, cfg.NLT
    ps, PS, x_res, gbc = g['ps'], g['PS'], g['x_res'], g['gbc']
    ident_f, modT = g['ident_f'], g['modT']
    rw_sb, rb_bc, bguT = g['rw_sb'], g['rb_bc'], g['bguT']
    wgu_d, wdn_d, fng_d, out_d = g['wgu_d'], g['wdn_d'], g['fng_d'], g['out_d']
    TG = 512 if T >= 512 else T
    NG = T // TG
    TPG = TG // 128
    mst = ExitStack()
    u2T = mst.enter_context(nc.sbuf_tensor(f"u2T_{b}", [128, KC, T], BF16))
    gates = mst.enter_context(nc.sbuf_tensor(f"gates_{b}", [128, NLT, E], F32))
    lg = mst.enter_context(nc.sbuf_tensor(f"lg_{b}", [128, E], F32))
    top8 = mst.enter_context(nc.sbuf_tensor(f"top8_{b}", [128, 8], F32))
    rsum = mst.enter_context(nc.sbuf_tensor(f"rsum_{b}", [128, 2], F32))
    wgu = mst.enter_context(nc.sbuf_tensor(f"wgu_{b}", [128, 2, KC, 1024], BF16))
    wdn = mst.enter_context(nc.sbuf_tensor(f"wdn_{b}", [128, 2, 4, D], BF16))
    hT = mst.enter_context(nc.sbuf_tensor(f"hT_{b}", [128, 2, 4, TG], BF16))
    g1 = mst.enter_context(nc.sbuf_tensor(f"g1_{b}", [128, 2, TG], F32))
    t1 = mst.enter_context(nc.sbuf_tensor(f"t1_{b}", [128, 2, TG], F32))
    sl = mst.enter_context(nc.sbuf_tensor(f"sl_{b}", [128, 2, TG], F32))
    wdn32 = mst.enter_context(nc.sbuf_tensor(f"wdn32_{b}", [128, 4, D], F32))
    w32flat = wdn32[:].rearrange("p k c -> p (k c)")
    u2f = w32flat[:, 0:KC * 128].rearrange("p (k c) -> p k c", c=128)
    bdn_sb = w32flat[0:E, 1024:1024 + D]
    gatesT = w32flat[0:E, 2048:2048 + 128]
    if True:
        xn, ssq, rms_scratch = g['xn'], g['ssq'], g['rms_scratch']
        g['make_gbc'](b, 1)
        P.dma('sp', bdn_sb, g['bdn_d'], writes=['bdn_sb'])
        for j in range(NLT):
            g['rms_to_featmajor'](x_res[:, j, :], ('x_res', j), u2T, j * 128, 2, b, rms_scratch[:], xn[:], ssq[:], (0, 1),
                                  extra_f32=u2f)
            for k in range(KC):
                P.op('pe', lambda e, k=k: e.matmul(ps[2][:, 0:E], lhsT=u2f[:, k, :], rhs=rw_sb[:, k, :], start=(k == 0),
                                                   stop=(k == KC - 1)), reads=['extra_f32', 'rw_sb'], writes=[PS(2)],
                     inc=(k == KC - 1))
            P.op('dve', lambda e: e.tensor_tensor(out=lg[:], in0=ps[2][:, 0:E], in1=rb_bc[:], op=ALU.add),
                 reads=[PS(2), 'rb_bc'], writes=['lg'])
            P.op('dve', lambda e: e.max(out=top8[:], in_=lg[:]), reads=['lg'], writes=['top8'])
            P.op('dve', lambda e: e.tensor_scalar(out=rsum[:, 0:1], in0=top8[:, 0:1], scalar1=-1.0, scalar2=None, op0=ALU.mult),
                 reads=['top8'], writes=['rsum'])
            P.op('act', lambda e, j=j: e.activation(out=gates[:, j, :], in_=lg[:], func=AF.Exp, bias=rsum[:, 0:1], scale=1.0),
                 reads=['lg', 'rsum'], writes=[('gates', j)])
            P.op('dve', lambda e: e.tensor_scalar(out=lg[:], in0=lg[:], scalar1=top8[:, 3:4], scalar2=None, op0=ALU.is_ge),
                 reads=['lg', 'top8', ('gates', j)], writes=['lg'])
            P.op('dve', lambda e, j=j: e.tensor_tensor(out=gates[:, j, :], in0=gates[:, j, :], in1=lg[:], op=ALU.mult),
                 reads=[('gates', j), 'lg'], writes=[('gates', j)])
            P.op('dve', lambda e, j=j: e.tensor_reduce(out=rsum[:, 1:2], in_=gates[:, j, :], axis=AX.X, op=ALU.add),
                 reads=[('gates', j)], writes=['rsum'])
            P.op('dve', lambda e: e.reciprocal(out=rsum[:, 1:2], in_=rsum[:, 1:2]), reads=['rsum'], writes=['rsum'])
            P.op('dve', lambda e, j=j: e.tensor_scalar(out=gates[:, j, :], in0=gates[:, j, :], scalar1=rsum[:, 1:2],
                                                       scalar2=None, op0=ALU.mult), reads=[('gates', j), 'rsum'],
                 writes=[('gates', j)])
            P.op('pe', lambda e, j=j: e.transpose(out=ps[3][0:E, 0:128], in_=gates[:, j, :], identity=ident_f[:]),
                 reads=[('gates', j), 'ident_f'], writes=[PS(3)])
            P.op('act', lambda e: e.copy(out=gatesT, in_=ps[3][0:E, 0:128]), reads=[PS(3)], writes=['gatesT'])
            P.op('dve', lambda e, j=j: e.tensor_scalar(out=gates[:, j, :], in0=gates[:, j, :], scalar1=1.0 / ALPHA, scalar2=None,
                                                       op0=ALU.mult), reads=[('gates', j)], writes=[('gates', j)])
            for half in range(2):
                hs = slice(half * 512, (half + 1) * 512)
                P.op('pe', lambda e, hs=hs, half=half: e.matmul(ps[4 + half][:, :], lhsT=gatesT, rhs=bdn_sb[:, hs],
                                                                start=True, stop=True), reads=['gatesT', 'bdn_sb'],
                     writes=[PS(4 + half)])
                P.op('dve', lambda e, half=half, hs=hs: e.tensor_tensor(out=rms_scratch[:, hs], in0=ps[4 + half][:, :], in1=gbc[:, hs],
                                                                        op=ALU.mult), reads=[PS(4 + half), 'gbc'],
                     writes=['rms_scratch'])
                P.op('pool', lambda e, j=j, hs=hs, half=half: e.tensor_tensor(out=x_res[:, j, hs], in0=x_res[:, j, hs],
                                                                              in1=rms_scratch[:, hs], op=ALU.add),
                     reads=['rms_scratch', ('x_res', j)], writes=[('x_res', j)])
        P.barrier()
        acc_i = 0

        def load_weights(step):
            ex_, fh_ = step // 2, step % 2
            buf_ = step % 2
            for part in range(2):
                c0 = part * 1024 + fh_ * 512
                P.dma('pool', wgu[:, buf_, :, part * 512:(part + 1) * 512],
                      wgu_d[ex_, :, c0:c0 + 512].rearrange("(k p) c -> p k c", p=128), writes=[('wgu', buf_)])
            P.dma('sp', wdn32[:], wdn_d[ex_, fh_ * 512:(fh_ + 1) * 512, :].rearrange("(k p) c -> p k c", p=128),
                  writes=['wdn32'])
            for k4 in range(4):
                P.op('pool', lambda e, k4=k4: e.tensor_tensor(out=wdn[:, buf_, k4, :], in0=wdn32[:, k4, :], in1=gbc[:], op=ALU.mult),
                     reads=['wdn32', 'gbc'], writes=[('wdn', buf_)])

        acc_box = [0]

        def emit_G(step, tg, gi):
            ex, fh = step // 2, step % 2
            buf = step % 2
            hb = gi % 2
            toks = slice(tg * TG, (tg + 1) * TG)
            for fc in range(4):
                fidx = fh * 4 + fc
                pg, pu = (fc % 2) * 2, 1 + (fc % 2) * 2
                for k in range(KC):
                    P.op('pe', lambda e, k=k, fc=fc, pg=pg: e.matmul(
                        ps[pg][:, 0:TG], lhsT=wgu[:, buf, k, fc * 128:(fc + 1) * 128], rhs=u2T[:, k, toks],
                        start=(k == 0), stop=(k == KC - 1)), reads=[('wgu', buf)], writes=[PS(pg)], inc=(k == KC - 1))
                for k in range(KC):
                    P.op('pe', lambda e, k=k, fc=fc, pu=pu: e.matmul(
                        ps[pu][:, 0:TG], lhsT=wgu[:, buf, k, 512 + fc * 128:512 + (fc + 1) * 128], rhs=u2T[:, k, toks],
                        start=(k == 0), stop=(k == KC - 1)), reads=[('wgu', buf)], writes=[PS(pu)], inc=(k == KC - 1))
                eb = fc % 2
                P.op('dve', lambda e, pg=pg, eb=eb, fidx=fidx: e.tensor_scalar(
                    out=g1[:, eb, :], in0=ps[pg][:, 0:TG], scalar1=bguT[:, fidx, ex:ex + 1], scalar2=LIM,
                    op0=ALU.add, op1=ALU.min), reads=[PS(pg), 'bguT'], writes=[('g1', eb)])
                P.op('act', lambda e, eb=eb: e.activation(out=sl[:, eb, :], in_=g1[:, eb, :], func=AF.Silu, scale=ALPHA),
                     reads=[('g1', eb)], writes=[('sl', eb)])
                P.op('dve', lambda e, pu=pu, eb=eb, fidx=fidx: e.tensor_scalar(
                    out=t1[:, eb, :], in0=ps[pu][:, 0:TG], scalar1=bguT[:, 8 + fidx, ex:ex + 1], scalar2=1.0 - LIM,
                    op0=ALU.add, op1=ALU.max), reads=[PS(pu), 'bguT'], writes=[('t1', eb)])
                P.op('dve', lambda e, eb=eb, fc=fc: e.scalar_tensor_tensor(
                    out=hT[:, hb, fc, :], in0=t1[:, eb, :], scalar=1.0 + LIM, in1=sl[:, eb, :],
                    op0=ALU.min, op1=ALU.mult), reads=[('t1', eb), ('sl', eb)], writes=[('hT', hb, fc)])

        def emit_D(step, tg, gi):
            ex = step // 2
            buf = step % 2
            hb = gi % 2
            for tt in range(TPG):
                j = tg * TPG + tt
                for half in range(2):
                    pb = 4 + ((tt * 2 + half) % 4)
                    for fc in range(4):
                        P.op('pe', lambda e, fc=fc, pb=pb, tt=tt, half=half: e.matmul(
                            ps[pb][:, :], lhsT=hT[:, hb, fc, tt * 128:(tt + 1) * 128],
                            rhs=wdn[:, buf, fc, half * 512:(half + 1) * 512], start=(fc == 0), stop=(fc == 3)),
                            reads=[('hT', hb, fc), ('wdn', buf)], writes=[PS(pb)], inc=(fc == 3))
                    hs = slice(half * 512, (half + 1) * 512)
                    P.op('dve', lambda e, pb=pb, hs=hs, j=j: e.scalar_tensor_tensor(
                        out=x_res[:, j, hs], in0=ps[pb][:, :], scalar=gates[:, j, ex:ex + 1], in1=x_res[:, j, hs],
                        op0=ALU.mult, op1=ALU.add), reads=[PS(pb), ('x_res', j)], writes=[('x_res', j)])

        items = [(st_, tg) for st_ in range(2 * E) for tg in range(NG)]
        load_weights(0)
        load_weights(1)
        emit_G(items[0][0], items[0][1], 0)
        for i, (st_, tg) in enumerate(items):
            if i + 1 < len(items):
                emit_G(items[i + 1][0], items[i + 1][1], i + 1)
            emit_D(st_, tg, i)
            if tg == NG - 1 and st_ + 2 < 2 * E:
                load_weights(st_ + 2)
        P.barrier()
        P.dma('sp', gbc[:], fng_d.to_broadcast([128, D]), writes=['gbc'])
        for j in range(NLT):
            P.op('act', lambda e, j=j: e.activation(out=rms_scratch[:], in_=x_res[:, j, :], func=AF.Square, accum_out=ssq[:]),
                 reads=[('x_res', j)], writes=['rms_scratch', 'ssq'])
            P.op('act', lambda e: e.activation(out=ssq[:], in_=ssq[:], func=AF.Sqrt, scale=1.0 / D, bias=g['eps_c'][:]),
                 reads=['ssq'], writes=['ssq'])
            P.op('dve', lambda e: e.reciprocal(out=ssq[:], in_=ssq[:]), reads=['ssq'], writes=['ssq'])
            P.op('dve', lambda e, j=j: e.tensor_scalar(out=xn[:], in0=x_res[:, j, :], scalar1=ssq[:], scalar2=None,
                                                       op0=ALU.mult), reads=['ssq', ('x_res', j)], writes=['xn'])
            P.op('pool', lambda e, j=j: e.tensor_tensor(out=x_res[:, j, :], in0=xn[:], in1=gbc[:], op=ALU.mult),
                 reads=['xn', 'gbc'], writes=[('x_res', j)])
            P.dma('sp', out_d[b, j * 128:(j + 1) * 128, :], x_res[:, j, :], reads=[('x_res', j)], writes=[('out', b, j)])
        P.barrier()
    mst.close()


def build_mixer(nc, P, cfg, b, g):
    NB, T, TC = cfg.NB, cfg.T, cfg.TC
    NT, NTT, NTC, NLT, NCH, NCC = cfg.NT, cfg.NTT, cfg.NTC, cfg.NLT, cfg.NCH, cfg.NCC
    ps, psb, PS = g['ps'], g['psb'], g['PS']
    ident_f, ident_b, ones_f, ones_nt = g['ident_f'], g['ident_b'], g['ones_f'], g['ones_nt']
    masks = (g['maskF'], g['maskB'])
    selh, modT = g['selh'], g['modT']
    x_d, ctx_d, pe_d, win_d, wout_d = g['x_d'], g['ctx_d'], g['pe_d'], g['win_d'], g['wout_d']
    cw_sb, cb_sb, mgb_sb, wg_sb, wlr_sb, gw2_sb, ggbT = (g['cw_sb'], g['cb_sb'], g['mgb_sb'], g['wg_sb'], g['wlr_sb'],
                                                         g['gw2_sb'], g['ggbT'])
    arena, x_res, gbc = g['arena'], g['x_res'], g['gbc']
    rms_scratch, xn, ssq, eps_c, one_c = g['rms_scratch'], g['xn'], g['ssq'], g['eps_c'], g['one_c']
    dbg = g['dbg']
    groups = [(t0, min(512, NT - t0)) for t0 in range(0, NT, 512)]
    HOFF = NLT * 512
    Hacc = arena[:, 0:HOFF].rearrange("p (j c) -> p j c", c=512)
    gt_tiles = [arena[0:4, HOFF + i * NT:HOFF + (i + 1) * NT] for i in range(3)]

    def S(name):
        return f"{name}_{b}"

    def win_load(dst, c0, ncol, res):
        P.dma('pool', dst, win_d[:, c0:c0 + ncol].rearrange("(k p) c -> p k c", p=128), writes=[res])

    def proj_featmajor(w, col0, dst_fn, evac):
        for gi, (t0, n) in enumerate(groups):
            pb = 2 + gi % 2
            for k in range(KC):
                P.op('pe', lambda e, k=k, pb=pb, t0=t0, n=n: e.matmul(ps[pb][:, 0:n], lhsT=w[:, k, col0:col0 + 128],
                                                                      rhs=uT[:, k, t0:t0 + n], start=(k == 0), stop=(k == KC - 1)),
                     reads=['w_cur'], writes=[PS(pb)], inc=(k == KC - 1))
            evac(pb, t0, n)

    def per_head_norm_stats(j):
        P.op('dve', lambda e: e.tensor_tensor(out=rms_scratch[:, 0:512], in0=Hacc[:, j, :], in1=Hacc[:, j, :], op=ALU.mult),
             reads=[('Hacc', j)], writes=['rms_scratch'])
        P.op('dve', lambda e: e.tensor_reduce(out=hst[:, 0:4], in_=rms_scratch[:, 0:512].rearrange("p (h c) -> p h c", c=128),
                                              axis=AX.X, op=ALU.add), reads=['rms_scratch'], writes=['hst'])
        P.op('act', lambda e: e.activation(out=hst[:, 0:4], in_=hst[:, 0:4], func=AF.Sqrt, scale=1.0 / 128, bias=eps_c[:]),
             reads=['hst'], writes=['hst'])
        P.op('dve', lambda e: e.reciprocal(out=hst[:, 0:4], in_=hst[:, 0:4]), reads=['hst'], writes=['hst'])
        P.op('dve', lambda e: e.tensor_tensor(out=rms_scratch[:, 0:512].rearrange("p (h c) -> p h c", c=128),
                                              in0=Hacc[:, j, :].rearrange("p (h c) -> p h c", c=128),
                                              in1=hst[:, 0:4].unsqueeze(2).to_broadcast([128, 4, 128]), op=ALU.mult),
             reads=['hst', ('Hacc', j)], writes=['rms_scratch'])

    st = ExitStack()
    uT = st.enter_context(nc.sbuf_tensor(S("uT"), [128, KC, NT], BF16))
    mT = uT[:].rearrange("p k n -> p (k n)")[:, 0:KC * T].rearrange("p (k t) -> p k t", t=T)
    a_tok = arena[:, HOFF:HOFF + NLT * 256].bitcast(BF16).rearrange("p (j c) -> p j c", c=512)
    AO = HOFF + NLT * 256
    khTok = arena[:, AO:AO + NTT * 128].bitcast(BF16).rearrange("p (i c) -> p i c", c=256)
    khT = arena[:, AO + NTT * 128:AO + NTT * 128 + NT // 2].bitcast(BF16)

    def g_tok(j):
        return arena[:, j * 512:j * 512 + 256].bitcast(BF16)
    hst = st.enter_context(nc.sbuf_tensor(S("hst"), [128, 8], F32))
    gnbc = st.enter_context(nc.sbuf_tensor(S("gnbc"), [128, 512], F32))
    wbuf = st.enter_context(nc.sbuf_tensor(S("wbuf"), [128, KC, 512], BF16))
    Pt = st.enter_context(nc.sbuf_tensor(S("Pt"), [128, 2, 2, 64], BF16))
    dn = st.enter_context(nc.sbuf_tensor(S("dn"), [128, 8], F32))

    with nc.sbuf_tensor(S("xt"), [128, 2, D], F32) as xt, nc.sbuf_tensor(S("pt"), [128, 2, D], F32) as pt:
        for i in range(NTT):
            bf = i % 2
            if i < NTC:
                P.dma('sp', xt[:, bf, :], ctx_d[b, i * 128:(i + 1) * 128, :], writes=[('xt', bf)])
                bsel = NB
            else:
                j = i - NTC
                P.dma('sp', xt[:, bf, :], x_d[b, j * 128:(j + 1) * 128, :], writes=[('xt', bf)])
                P.dma('sp', pt[:, bf, :], pe_d[j * 128:(j + 1) * 128, :], writes=[('pt', bf)])
                P.op('pool', lambda e, bf=bf: e.tensor_tensor(out=xt[:, bf, :], in0=xt[:, bf, :], in1=pt[:, bf, :], op=ALU.add),
                     reads=[('xt', bf), ('pt', bf)], writes=[('xt', bf)])
                bsel = b
            g['rms_to_featmajor'](xt[:, bf, :], ('xt', bf), uT, i * 128, 0, bsel, rms_scratch[:], xn[:], ssq[:], (0, 1))
        P.barrier()

    mst = ExitStack()
    qkT = mst.enter_context(nc.sbuf_tensor(S("qkT"), [128, 4, NT], BF16))
    kTok = mst.enter_context(nc.sbuf_tensor(S("kTok"), [128, NTT, 256], BF16))
    if True:
        with nc.sbuf_tensor(S("zt"), [128, NT], F32) as zt, nc.sbuf_tensor(S("ot"), [128, NT], F32) as ot, \
                nc.sbuf_tensor(S("sg"), [128, NT], F32) as sg:
            win_load(wbuf[:], 0, 512, 'w_cur')
            for qc in range(4):
                proj_featmajor(wbuf, qc * 128, None,
                               lambda pb, t0, n: P.op('act', lambda e: e.copy(out=zt[:, t0:t0 + n], in_=ps[pb][:, 0:n]),
                                                      reads=[PS(pb)], writes=['zt']))
                P.op('dve', lambda e, qc=qc: e.tensor_scalar(out=ot[:], in0=zt[:], scalar1=cw_sb[:, qc, 1:2],
                                                             scalar2=cb_sb[:, qc:qc + 1], op0=ALU.mult, op1=ALU.add),
                     reads=['zt', 'cw_sb', 'cb_sb'], writes=['ot'])
                for lo, hi in ((0, TC), (TC, NT)):
                    P.op('dve', lambda e, qc=qc, lo=lo, hi=hi: e.scalar_tensor_tensor(
                        out=ot[:, lo + 1:hi], in0=zt[:, lo:hi - 1], scalar=cw_sb[:, qc, 0:1], in1=ot[:, lo + 1:hi],
                        op0=ALU.mult, op1=ALU.add), reads=['zt', 'ot'], writes=['ot'])
                    P.op('dve', lambda e, qc=qc, lo=lo, hi=hi: e.scalar_tensor_tensor(
                        out=ot[:, lo:hi - 1], in0=zt[:, lo + 1:hi], scalar=cw_sb[:, qc, 2:3], in1=ot[:, lo:hi - 1],
                        op0=ALU.mult, op1=ALU.add), reads=['zt', 'ot'], writes=['ot'])
                P.op('act', lambda e: e.activation(out=sg[:], in_=ot[:], func=AF.Sigmoid), reads=['ot'], writes=['sg'])
                P.op('dve', lambda e, qc=qc: e.scalar_tensor_tensor(out=qkT[:, qc, :], in0=ot[:], scalar=(0.125 if qc < 2 else 1.0),
                                                                    in1=sg[:], op0=ALU.mult, op1=ALU.mult),
                     reads=['ot', 'sg'], writes=['qkT'])
            P.barrier()
        vtok = mst.enter_context(nc.sbuf_tensor(S("vtok"), [128, NTT, 4, 129], BF16))
        kp = mst.enter_context(nc.sbuf_tensor(S("kp"), [128, NTT, 256], BF16))
        edT = mst.enter_context(nc.sbuf_tensor(S("edT"), [128, NTT, 8], F32))
        wcB = mst.enter_context(nc.sbuf_tensor(S("wcB"), [128, 2, NCH], F32))
        Chat = mst.enter_context(nc.sbuf_tensor(S("Chat"), [128, 2, 129], F32))
        CbfA = mst.enter_context(nc.sbuf_tensor(S("CbfA"), [128, 2, 2, 129], BF16))
        CbfB = mst.enter_context(nc.sbuf_tensor(S("CbfB"), [128, 2, 2, 129], BF16))
        Aend = mst.enter_context(nc.sbuf_tensor(S("Aend"), [4, 3, NCH], F32))
        for i in range(NTT):
            for kc in range(2):
                P.op('pe', lambda e, i=i, kc=kc: e.transpose(out=psb[1][:, kc * 128:(kc + 1) * 128],
                                                             in_=qkT[:, 2 + kc, i * 128:(i + 1) * 128], identity=ident_b[:]),
                     reads=['ident_b'], writes=[PS(1)], inc=(kc == 1))
            P.op('dve', lambda e, i=i: e.tensor_copy(out=kTok[:, i, :], in_=psb[1][:, 0:256]), reads=[PS(1)], writes=['kTok'])
        win_load(wbuf[:], 512, 512, 'w_cur')
        P.op('pool', lambda e: e.memset(vtok[:, :, :, 128:129], 1.0), writes=['vtok'])
        for i in range(NTT):
            pb = 2 + i % 2
            for k in range(KC):
                P.op('pe', lambda e, i=i, k=k, pb=pb: e.matmul(ps[pb][:, :], lhsT=uT[:, k, i * 128:(i + 1) * 128], rhs=wbuf[:, k, :],
                                                               start=(k == 0), stop=(k == KC - 1)),
                     reads=['w_cur'], writes=[PS(pb)], inc=(k == KC - 1))
            P.op('act', lambda e, i=i, pb=pb: e.copy(out=vtok[:, i, :, 0:128], in_=ps[pb][:, :].rearrange("p (h c) -> p h c", c=128)),
                 reads=[PS(pb)], writes=['vtok'])
        P.barrier()

        DIRS = [int(c) for c in os.environ.get('KDIRS', '01')]
        for d in DIRS:
            IGt, FPt, T3 = gt_tiles
            for gi, dst in ((2 * d, IGt), (2 * d + 1, FPt)):
                for gj, (t0, n) in enumerate(groups):
                    pb = 2 + gj % 2
                    for k in range(KC):
                        P.op('pe', lambda e, k=k, pb=pb, t0=t0, n=n, gi=gi: e.matmul(
                            ps[pb][0:4, 0:n], lhsT=wg_sb[:, k, gi * 4:(gi + 1) * 4], rhs=uT[:, k, t0:t0 + n],
                            start=(k == 0), stop=(k == KC - 1)), reads=['wg_sb'], writes=[PS(pb)], inc=(k == KC - 1))
                    P.op('act', lambda e, pb=pb, t0=t0, n=n, gi=gi, dst=dst: e.activation(
                        out=dst[:, t0:t0 + n], in_=ps[pb][0:4, 0:n], func=AF.Identity, bias=mgb_sb[:, gi:gi + 1], scale=1.0),
                        reads=[PS(pb), 'mgb_sb'], writes=[('gt', gi % 2)])
            P.op('act', lambda e: e.activation(out=FPt, in_=FPt, func=AF.Exp, scale=-1.0), reads=[('gt', 1)], writes=[('gt', 1)])
            P.op('act', lambda e: e.activation(out=FPt, in_=FPt, func=AF.Ln, bias=one_c[0:4, :], scale=1.0),
                 reads=[('gt', 1)], writes=[('gt', 1)])

            def scan(out_t, d0, d1, op0, op1, rd, wr):
                if d == 0:
                    P.op('dve', lambda e: e.tensor_tensor_scan(out=out_t[:, 0:NT], data0=d0[:, 0:NT], data1=d1[:, 0:NT], initial=0.0,
                                                               op0=op0, op1=op1), reads=rd, writes=wr)
                else:
                    P.op('dve', lambda e: e.tensor_tensor_scan(out=out_t[:, 0:TC][:, ::-1], data0=d0[:, 0:TC][:, ::-1],
                                                               data1=d1[:, 0:TC][:, ::-1], initial=0.0, op0=op0, op1=op1),
                         reads=rd, writes=wr)
                    P.op('dve', lambda e: e.tensor_tensor_scan(out=out_t[:, TC:NT][:, ::-1], data0=d0[:, TC:NT][:, ::-1],
                                                               data1=d1[:, TC:NT][:, ::-1], initial=out_t[:, 0:1], op0=op0, op1=op1),
                         reads=rd + wr, writes=wr)
            scan(T3, ones_nt[0:4, :], FPt, ALU.mult, ALU.add, [('gt', 1), 'ones_nt'], [('gt', 2)])
            P.op('dve', lambda e: e.tensor_tensor(out=IGt, in0=IGt, in1=T3, op=ALU.add), reads=[('gt', 0), ('gt', 2)],
                 writes=[('gt', 0)])
            scan(FPt, IGt, IGt, ALU.max, ALU.max, [('gt', 0)], [('gt', 1)])
            endcol = 63 if d == 0 else 0
            A3 = FPt.rearrange("p (c l) -> p c l", l=64)
            P.op('dve', lambda e: e.tensor_copy(out=Aend[:, 0, :], in_=A3[:, :, endcol]), reads=[('gt', 1)], writes=['Aend'])
            P.op('pool', lambda e: e.memset(Aend[:, 1, :], 0.0), writes=['Aprev'])
            if d == 0:
                P.op('dve', lambda e: e.tensor_copy(out=Aend[:, 1, 1:NCH], in_=Aend[:, 0, 0:NCH - 1]), reads=['Aend', 'Aprev'],
                     writes=['Aprev'])
            else:
                if NCC > 1:
                    P.op('dve', lambda e: e.tensor_copy(out=Aend[:, 1, 0:NCC - 1], in_=Aend[:, 0, 1:NCC]), reads=['Aend', 'Aprev'],
                         writes=['Aprev'])
                P.op('dve', lambda e: e.tensor_copy(out=Aend[:, 1, NCC:NCH - 1], in_=Aend[:, 0, NCC + 1:NCH]),
                     reads=['Aend', 'Aprev'], writes=['Aprev'])
                P.op('dve', lambda e: e.tensor_copy(out=Aend[:, 1, NCH - 1:NCH], in_=Aend[:, 0, 0:1]), reads=['Aend', 'Aprev'],
                     writes=['Aprev'])
            P.op('dve', lambda e: e.tensor_tensor(out=Aend[:, 2, :], in0=Aend[:, 1, :], in1=Aend[:, 0, :], op=ALU.subtract),
                 reads=['Aend', 'Aprev'], writes=['wc'])
            P.op('act', lambda e: e.activation(out=Aend[:, 2, :], in_=Aend[:, 2, :], func=AF.Exp), reads=['wc'], writes=['wc'])
            for ti_, nm in ((IGt, 0), (T3, 2)):
                P.op('dve', lambda e, ti_=ti_: e.tensor_tensor(out=ti_.rearrange("p (c l) -> p c l", l=64),
                                                               in0=ti_.rearrange("p (c l) -> p c l", l=64),
                                                               in1=Aend[:, 0, :].unsqueeze(2).to_broadcast([4, NCH, 64]),
                                                               op=ALU.subtract), reads=[('gt', nm), 'Aend'], writes=[('gt', nm)])
                P.op('act', lambda e, ti_=ti_: e.activation(out=ti_, in_=ti_, func=AF.Exp), reads=[('gt', nm)], writes=[('gt', nm)])
            for i in range(NTT):
                P.op('pe', lambda e, i=i: e.matmul(ps[2][:, i * 8:i * 8 + 4], lhsT=IGt[:, i * 128:(i + 1) * 128], rhs=ident_f[0:4, 0:4],
                                                   start=True, stop=True), reads=[('gt', 0), 'ident_f'], writes=[PS(2)], inc=False)
                P.op('pe', lambda e, i=i: e.matmul(ps[2][:, i * 8 + 4:i * 8 + 8], lhsT=T3[:, i * 128:(i + 1) * 128],
                                                   rhs=ident_f[0:4, 0:4], start=True, stop=True), reads=[('gt', 2), 'ident_f'],
                     writes=[PS(2)], inc=(i == NTT - 1))
            P.op('dve', lambda e: e.tensor_copy(out=edT[:].rearrange("p i c -> p (i c)"), in_=ps[2][:, 0:NTT * 8]), reads=[PS(2)],
                 writes=['edT'])
            for pr in range(2):
                P.op('pe', lambda e, pr=pr: e.matmul(ps[3][:, pr * NCH:(pr + 1) * NCH],
                                                     lhsT=selh[0:4, pr].rearrange("p a b -> p (a b)"), rhs=Aend[:, 2, :],
                                                     start=True, stop=True), reads=['selh', 'wc'], writes=[PS(3)], inc=(pr == 1))
            P.op('dve', lambda e: e.tensor_copy(out=wcB[:].rearrange("p a c -> p (a c)"), in_=ps[3][:, 0:2 * NCH]), reads=[PS(3)],
                 writes=['wcB'])
            P.op('dve', lambda e: e.tensor_tensor(out=kp[:].rearrange("p i (h c) -> p i h c", c=64),
                                                  in0=kTok[:].rearrange("p i (h c) -> p i h c", c=64),
                                                  in1=edT[:, :, 0:4].unsqueeze(3).to_broadcast([128, NTT, 4, 64]), op=ALU.mult),
                 reads=['kTok', 'edT'], writes=['kp'])
            P.barrier()
            P.op('pool', lambda e: e.memset(Chat[:], 0.0), writes=['Chat'])
            P.op('pool', lambda e: e.memset(CbfA[:], 0.0), writes=[('CbfA', 0), ('CbfA', 1)])
            P.op('pool', lambda e: e.memset(CbfB[:], 0.0), writes=[('CbfB', 0), ('CbfB', 1)])
            order = list(range(NCH)) if d == 0 else list(range(NCC - 1, -1, -1)) + list(range(NCH - 1, NCC - 1, -1))
            mask = masks[d]
            first_dir = (d == DIRS[0])

            def ml_geo(step):
                c = order[step]
                ti, p0 = c // 2, (c % 2) * 64
                return c, ti, slice(p0, p0 + 64), slice(c * 64, (c + 1) * 64), c >= NCC, 6 + step % 2, c % 2

            def ml_pre(step):
                c, ti, rows, toks, lat, pbD, hf = ml_geo(step)
                if lat:
                    for h in range(4):
                        hp = (h % 2) * 64
                        P.op('pe', lambda e, h=h, hp=hp: e.matmul(
                            ps[2 + h % 2][rows, (h // 2) * 64:(h // 2) * 64 + 64], lhsT=qkT[hp:hp + 64, 2 + h // 2, toks],
                            rhs=qkT[hp:hp + 64, h // 2, toks], start=True, stop=True), writes=[('psh', 2 + h % 2, hf)], inc=(h >= 2))
                for h in range(4):
                    P.op('pe', lambda e, h=h: e.matmul(
                        ps[pbD][(h % 2) * 64:(h % 2) * 64 + 64, (h // 2) * 129:(h // 2 + 1) * 129],
                        lhsT=kp[rows, ti, h * 64:(h + 1) * 64], rhs=vtok[rows, ti, h, :], start=True, stop=True),
                        reads=['kp'], writes=[PS(pbD)], inc=(h == 3))
                if lat:
                    for h in range(4):
                        par, hh = h % 2, h // 2
                        P.op('dve', lambda e, h=h, par=par, hh=hh: e.scalar_tensor_tensor(
                            out=Pt[rows, par, hh, :], in0=ps[2 + par][rows, hh * 64:(hh + 1) * 64], scalar=edT[rows, ti, h:h + 1],
                            in1=mask[rows, :], op0=ALU.mult, op1=ALU.mult), reads=[('psh', 2 + par, hf), 'edT'],
                            writes=[('Pt', hf, h)])

            def ml_main(step):
                c, ti, rows, toks, lat, pbD, hf = ml_geo(step)
                for pr in range(2):
                    P.op('dve', lambda e, pr=pr: e.scalar_tensor_tensor(
                        out=Chat[:, pr, :], in0=Chat[:, pr, :], scalar=wcB[:, pr, c:c + 1], in1=ps[pbD][:, pr * 129:(pr + 1) * 129],
                        op0=ALU.mult, op1=ALU.add), reads=['Chat', 'wcB', PS(pbD)], writes=['Chat'])
                if step + 1 < len(order):
                    cn = order[step + 1]
                    for pr in range(2):
                        P.op('act', lambda e, pr=pr: e.activation(out=CbfA[0:64, (step + 1) % 2, pr, :], in_=Chat[0:64, pr, :], func=AF.Identity,
                                                                  scale=wcB[0:64, pr, cn:cn + 1]),
                             reads=['Chat', 'wcB'], writes=[('CbfA', (step + 1) % 2)])
                        P.op('act', lambda e, pr=pr: e.activation(out=CbfB[64:128, (step + 1) % 2, pr, :], in_=Chat[64:128, pr, :], func=AF.Identity,
                                                                  scale=wcB[64:128, pr, cn:cn + 1]),
                             reads=['Chat', 'wcB'], writes=[('CbfB', (step + 1) % 2)])

                if lat:
                    j = ti - NTC
                    for h in range(4):
                        par, pr = h % 2, h // 2
                        P.op('pe', lambda e, h=h, par=par, pr=pr: e.matmul(
                            ps[4 + pr][rows, par * 129:(par + 1) * 129], lhsT=Pt[rows, par, pr, :], rhs=vtok[rows, ti, h, :],
                            start=True, stop=False), reads=[('Pt', hf, h)], writes=[('psh', 4 + pr, hf)], inc=False)
                        P.op('pe', lambda e, par=par, pr=pr: e.matmul(
                            ps[4 + pr][rows, par * 129:(par + 1) * 129], lhsT=qkT[:, pr, toks],
                            rhs=(CbfA if par == 0 else CbfB)[:, step % 2, pr, :], start=False, stop=True),
                            reads=[('CbfA', step % 2), ('CbfB', step % 2)], writes=[('psh', 4 + pr, hf)], inc=True)
                    for pr in range(2):
                        den = lambda pr=pr, rows=rows: ps[4 + pr][rows, 0:258].rearrange("p (a c) -> p a c", c=129)[:, :, 128]
                        P.op('dve', lambda e, pr=pr, den=den: e.tensor_tensor(
                            out=dn[rows, 2 * pr:2 * pr + 2], in0=den(), in1=edT[rows, ti, 4 + 2 * pr:6 + 2 * pr], op=ALU.max),
                            reads=[('psh', 4 + pr, hf), 'edT'], writes=[('dn', hf)])
                        P.op('dve', lambda e, pr=pr, den=den: e.scalar_tensor_tensor(
                            out=dn[rows, 2 * pr:2 * pr + 2], in0=den(), scalar=-1.0, in1=dn[rows, 2 * pr:2 * pr + 2],
                            op0=ALU.mult, op1=ALU.max), reads=[('psh', 4 + pr, hf), ('dn', hf)], writes=[('dn', hf)])
                    P.op('dve', lambda e: e.reciprocal(out=dn[rows, 4:8], in_=dn[rows, 0:4]), reads=[('dn', hf)],
                         writes=[('rd', hf)])
                    for h in range(4):
                        src = lambda h=h, rows=rows: ps[4 + h // 2][rows, (h % 2) * 129:(h % 2) * 129 + 128]
                        if first_dir:
                            P.op('dve', lambda e, h=h, src=src: e.tensor_scalar(
                                out=Hacc[rows, j, h * 128:(h + 1) * 128], in0=src(), scalar1=dn[rows, 4 + h:5 + h], scalar2=None,
                                op0=ALU.mult), reads=[('psh', 4 + h // 2, hf), ('rd', hf)], writes=[('Hacc', j)])
                        else:
                            P.op('dve', lambda e, h=h, src=src: e.scalar_tensor_tensor(
                                out=Hacc[rows, j, h * 128:(h + 1) * 128], in0=src(), scalar=dn[rows, 4 + h:5 + h],
                                in1=Hacc[rows, j, h * 128:(h + 1) * 128], op0=ALU.mult, op1=ALU.add),
                                reads=[('psh', 4 + h // 2, hf), ('rd', hf), ('Hacc', j)], writes=[('Hacc', j)])
            ml_pre(0)
            for step in range(len(order)):
                if step + 1 < len(order):
                    ml_pre(step + 1)
                ml_main(step)
            P.barrier()
        if dbg:
            P.dma('sp', dbg['HM'][b], Hacc, writes=['dbgHM'])
            P.dma('sp', dbg['edT'][b], edT[:], writes=['dbgedT'])
            P.dma('sp', dbg['wcB'][b], wcB[:], writes=['dbgwcB'])
            P.dma('sp', dbg['qkT'][b], qkT[:], writes=['dbgqkT'])
            P.dma('sp', dbg['vtok'][b], vtok[:], writes=['dbgvtok'])
            P.dma('sp', dbg['kTok'][b], kTok[:], writes=['dbgkTok'])
            P.barrier()
        win_load(wbuf[:], 1024, 512, 'w_cur')
        P.dma('sp', gnbc[:], g['mng_d'].to_broadcast([128, 512]), writes=['gnbc'])
        for j in range(NLT):
            tk = slice(TC + j * 128, TC + (j + 1) * 128)
            for k in range(KC):
                P.op('pe', lambda e, k=k, tk=tk: e.matmul(ps[2][:, :], lhsT=uT[:, k, tk], rhs=wbuf[:, k, :], start=(k == 0),
                                                          stop=(k == KC - 1)), reads=['w_cur'], writes=[PS(2)], inc=(k == KC - 1))
            P.op('act', lambda e: e.activation(out=xn[:, 0:512], in_=ps[2][:, :], func=AF.Sigmoid), reads=[PS(2)], writes=['xn'])
            per_head_norm_stats(j)
            P.op('dve', lambda e: e.tensor_tensor(out=rms_scratch[:, 0:512], in0=rms_scratch[:, 0:512], in1=gnbc[:], op=ALU.mult),
                 reads=['rms_scratch', 'gnbc'], writes=['rms_scratch'])
            P.op('dve', lambda e: e.tensor_tensor(out=a_tok[:, j, :], in0=rms_scratch[:, 0:512], in1=xn[:, 0:512], op=ALU.mult),
                 reads=['rms_scratch', 'xn'], writes=[('a_tok', j)])
        P.barrier()

    mst.close()
    KSTOP = os.environ.get('KSTOP', '')
    rm = ones_nt
    with nc.sbuf_tensor(S("gv"), [128, NTT, 512], BF16) as gv, \
            nc.sbuf_tensor(S("qt"), [128, 2, NT], BF16) as qt, nc.sbuf_tensor(S("kt"), [128, 2, NT], BF16) as kt, \
            nc.sbuf_tensor(S("kraw"), [128, NT], BF16) as kraw, \
            nc.sbuf_tensor(S("lrT"), [16, NT], BF16) as lrT, \
            nc.sbuf_tensor(S("tA"), [128, NT], F32) as tA, nc.sbuf_tensor(S("tB"), [128, NT], F32) as tB, \
            nc.sbuf_tensor(S("ebL"), [128, 2, NCH], F32) as ebL, \
            nc.sbuf_tensor(S("Sst"), [128, 2, 128], F32) as Sst, nc.sbuf_tensor(S("SbfA"), [128, 2, 2, 128], BF16) as SbfA, \
            nc.sbuf_tensor(S("SbfB"), [128, 2, 2, 128], BF16) as SbfB:
        win_load(wbuf[:], 2064, 512, 'w_cur')
        for i in range(NTT):
            pb = 2 + i % 2
            for k in range(KC):
                P.op('pe', lambda e, i=i, k=k, pb=pb: e.matmul(ps[pb][:, :], lhsT=uT[:, k, i * 128:(i + 1) * 128], rhs=wbuf[:, k, :],
                                                               start=(k == 0), stop=(k == KC - 1)),
                     reads=['w_cur'], writes=[PS(pb)], inc=(k == KC - 1))
            P.op('act', lambda e, i=i, pb=pb: e.copy(out=gv[:, i, :], in_=ps[pb][:, :]), reads=[PS(pb)], writes=['gv'])
        P.barrier()
        DIRS = [int(c) for c in os.environ.get('KDIRS', '01')]
        if KSTOP == 'mlstm':
            DIRS = []
        for d in DIRS:
            endcol = 63 if d == 0 else 0
            win_load(wbuf[:], 1552, 512, 'w_cur')
            P.op('pool', lambda e: e.memset(rm[:], 1.0), writes=['rm'])
            zc = 0 if d == 0 else 63
            P.op('pool', lambda e: e.memset(rm[:].rearrange("p (c l) -> p c l", l=64)[:, :, zc:zc + 1], 0.0), writes=['rm'])
            for gj, (t0, n) in enumerate(groups):
                pb = 2 + gj % 2
                for k in range(KC):
                    P.op('pe', lambda e, k=k, pb=pb, t0=t0, n=n: e.matmul(
                        ps[pb][0:16, 0:n], lhsT=wlr_sb[:, k, d * 16:(d + 1) * 16], rhs=uT[:, k, t0:t0 + n],
                        start=(k == 0), stop=(k == KC - 1)), reads=['wlr_sb'], writes=[PS(pb)], inc=(k == KC - 1))
                P.op('act', lambda e, pb=pb, t0=t0, n=n: e.copy(out=lrT[:, t0:t0 + n], in_=ps[pb][0:16, 0:n]),
                     reads=[PS(pb)], writes=['lrT'])
            for jc in range(2):
                for gj, (t0, n) in enumerate(groups):
                    pb = 2 + gj % 2
                    P.op('pe', lambda e, pb=pb, t0=t0, n=n: e.matmul(
                        ps[pb][:, 0:n], lhsT=gw2_sb[:, d, jc * 128:(jc + 1) * 128], rhs=lrT[:, t0:t0 + n], start=True, stop=True),
                        reads=['gw2_sb', 'lrT'], writes=[PS(pb)])
                    P.op('act', lambda e, pb=pb, t0=t0, n=n: e.activation(
                        out=tA[:, t0:t0 + n], in_=ps[pb][:, 0:n], func=AF.Exp, scale=-1.0, bias=ggbT[:, d, jc:jc + 1]),
                        reads=[PS(pb), 'ggbT'], writes=['tA'])
                P.op('act', lambda e: e.activation(out=tA[:], in_=tA[:], func=AF.Ln, bias=one_c[:], scale=1.0), reads=['tA'],
                     writes=['tA'])
                if d == 0:
                    P.op('dve', lambda e: e.tensor_tensor_scan(out=tB[:], data0=rm[:], data1=tA[:], initial=0.0,
                                                               op0=ALU.mult, op1=ALU.add), reads=['tA', 'rm'], writes=['tB'])
                else:
                    P.op('dve', lambda e: e.tensor_tensor_scan(out=tB[:, ::-1], data0=rm[:, ::-1], data1=tA[:, ::-1], initial=0.0,
                                                               op0=ALU.mult, op1=ALU.add), reads=['tA', 'rm'], writes=['tB'])
                bL = tB[:].rearrange("p (c l) -> p c l", l=64)[:, :, endcol]
                P.op('act', lambda e: e.activation(out=ebL[:, jc, :], in_=bL, func=AF.Exp, scale=-1.0 / 16),
                     reads=['tB'], writes=['ebL'])
                P.op('act', lambda e: e.activation(out=tA[:], in_=tB[:], func=AF.Exp, scale=-1.0 / 16), reads=['tB', 'tA'],
                     writes=['tA'])
                proj_featmajor(wbuf, jc * 128, None,
                               lambda pb, t0, n: P.op('dve', lambda e: e.scalar_tensor_tensor(
                                   out=qt[:, jc, t0:t0 + n], in0=ps[pb][:, 0:n], scalar=0.125, in1=tA[:, t0:t0 + n],
                                   op0=ALU.mult, op1=ALU.mult), reads=[PS(pb), 'tA'], writes=['qt']))
                P.op('act', lambda e: e.activation(out=tA[:], in_=tB[:], func=AF.Exp, scale=1.0 / 16), reads=['tB', 'qt', 'tA'],
                     writes=['tA'])

                def evac_k(pb, t0, n):
                    P.op('dve', lambda e: e.tensor_copy(out=kraw[:, t0:t0 + n], in_=ps[pb][:, 0:n]), reads=[PS(pb)], writes=['kraw'])
                    P.op('dve', lambda e: e.tensor_tensor(out=kt[:, jc, t0:t0 + n], in0=ps[pb][:, 0:n], in1=tA[:, t0:t0 + n],
                                                          op=ALU.mult), reads=[PS(pb), 'tA'], writes=['kt'])
                proj_featmajor(wbuf, 256 + jc * 128, None, evac_k)
                P.op('dve', lambda e: e.tensor_tensor(out=tA[:].rearrange("p (c l) -> p c l", l=64),
                                                      in0=tB[:].rearrange("p (c l) -> p c l", l=64),
                                                      in1=bL.unsqueeze(2).to_broadcast([128, NCH, 64]), op=ALU.subtract),
                     reads=['tB', 'kt', 'tA'], writes=['tA'])
                P.op('act', lambda e: e.activation(out=tA[:], in_=tA[:], func=AF.Exp, scale=1.0 / 16), reads=['tA'], writes=['tA'])
                P.op('dve', lambda e: e.tensor_tensor(out=khT, in0=kraw[:], in1=tA[:], op=ALU.mult),
                     reads=['tA', 'kraw'], writes=['khT'])
                for i in range(NTT):
                    P.op('pe', lambda e, i=i: e.transpose(out=psb[1][:, 0:128], in_=khT[:, i * 128:(i + 1) * 128], identity=ident_b[:]),
                         reads=['khT', 'ident_b'], writes=[PS(1)])
                    P.op('dve', lambda e, i=i: e.tensor_copy(out=khTok[:, i, jc * 128:(jc + 1) * 128], in_=psb[1][:, 0:128]),
                         reads=[PS(1)], writes=['khTok'])
            P.barrier()
            P.op('pool', lambda e: e.memset(Sst[:], 0.0), writes=['Sst'])
            P.op('pool', lambda e: e.memset(SbfA[:], 0.0), writes=[('SbfA', 0), ('SbfA', 1)])
            P.op('pool', lambda e: e.memset(SbfB[:], 0.0), writes=[('SbfB', 0), ('SbfB', 1)])
            order = list(range(NCH)) if d == 0 else list(range(NCC - 1, -1, -1)) + list(range(NCH - 1, NCC - 1, -1))
            mask = masks[d]
            first_dir = (d == DIRS[0])

            def gl_geo(step):
                c = order[step]
                ti, p0 = c // 2, (c % 2) * 64
                return c, ti, slice(p0, p0 + 64), slice(c * 64, (c + 1) * 64), c >= NCC, 6 + step % 2, c % 2

            def gl_pre(step):
                c, ti, rows, toks, lat, pbD, hf = gl_geo(step)
                if lat:
                    for h in range(4):
                        hp = (h % 2) * 64
                        P.op('pe', lambda e, h=h, hp=hp: e.matmul(
                            ps[2 + h % 2][rows, (h // 2) * 64:(h // 2) * 64 + 64], lhsT=kt[hp:hp + 64, h // 2, toks],
                            rhs=qt[hp:hp + 64, h // 2, toks], start=True, stop=True), writes=[('psh', 2 + h % 2, hf)], inc=(h >= 2))
                for h in range(4):
                    P.op('pe', lambda e, h=h: e.matmul(
                        ps[pbD][(h % 2) * 64:(h % 2) * 64 + 64, (h // 2) * 128:(h // 2 + 1) * 128],
                        lhsT=khTok[rows, ti, h * 64:(h + 1) * 64], rhs=gv[rows, ti, h * 128:(h + 1) * 128], start=True, stop=True),
                        writes=[PS(pbD)], inc=(h == 3))
                if lat:
                    for par in range(2):
                        P.op('dve', lambda e, par=par: e.tensor_tensor(
                            out=Pt[rows, par, :, :], in0=ps[2 + par][rows, 0:128].rearrange("p (a c) -> p a c", c=64),
                            in1=mask[rows, :].unsqueeze(1).to_broadcast([64, 2, 64]), op=ALU.mult),
                            reads=[('psh', 2 + par, hf)], writes=[('Pt', hf, par)])

            def gl_main(step):
                c, ti, rows, toks, lat, pbD, hf = gl_geo(step)
                for pr in range(2):
                    P.op('dve', lambda e, pr=pr: e.scalar_tensor_tensor(
                        out=Sst[:, pr, :], in0=Sst[:, pr, :], scalar=ebL[:, pr, c:c + 1], in1=ps[pbD][:, pr * 128:(pr + 1) * 128],
                        op0=ALU.mult, op1=ALU.add), reads=['Sst', 'ebL', PS(pbD)], writes=['Sst'])
                P.op('act', lambda e: e.copy(out=SbfA[0:64, (step + 1) % 2], in_=Sst[0:64]), reads=['Sst'], writes=[('SbfA', (step + 1) % 2)])
                P.op('act', lambda e: e.copy(out=SbfB[64:128, (step + 1) % 2], in_=Sst[64:128]), reads=['Sst'], writes=[('SbfB', (step + 1) % 2)])

                if lat:
                    j = ti - NTC
                    for h in range(4):
                        par, pr = h % 2, h // 2
                        P.op('pe', lambda e, h=h, par=par, pr=pr: e.matmul(
                            ps[4][rows, h * 128:(h + 1) * 128], lhsT=Pt[rows, par, pr, :], rhs=gv[rows, ti, h * 128:(h + 1) * 128],
                            start=True, stop=False), reads=[('Pt', hf, par)], writes=[('psh', 4, hf)], inc=False)
                        P.op('pe', lambda e, h=h, par=par, pr=pr: e.matmul(
                            ps[4][rows, h * 128:(h + 1) * 128], lhsT=qt[:, pr, toks], rhs=(SbfA if par == 0 else SbfB)[:, step % 2, pr, :],
                            start=False, stop=True), reads=[('SbfA', step % 2), ('SbfB', step % 2)], writes=[('psh', 4, hf)], inc=(h == 3))
                    if first_dir:
                        P.op('act', lambda e: e.copy(out=Hacc[rows, j, :], in_=ps[4][rows, :]), reads=[('psh', 4, hf)],
                             writes=[('Hacc', j)])
                    else:
                        P.op('act', lambda e: e.copy(out=rms_scratch[rows, 0:512], in_=ps[4][rows, :]), reads=[('psh', 4, hf)],
                             writes=[('otmp', hf)])
                        P.op('pool', lambda e: e.tensor_tensor(out=Hacc[rows, j, :], in0=Hacc[rows, j, :],
                                                               in1=rms_scratch[rows, 0:512], op=ALU.add),
                             reads=[('otmp', hf), ('Hacc', j)], writes=[('Hacc', j)])
            gl_pre(0)
            for step in range(len(order)):
                if step + 1 < len(order):
                    gl_pre(step + 1)
                gl_main(step)
            P.barrier()
            if dbg and d == DIRS[0]:
                P.dma('sp', dbg['H1'][b], Hacc, writes=['dbgH1'])
                P.barrier()
        if dbg:
            P.dma('sp', dbg['H'][b], Hacc, writes=['dbgH'])
            P.dma('sp', dbg['qt'][b], qt[:], writes=['dbgqt'])
            P.dma('sp', dbg['kt'][b], kt[:], writes=['dbgkt'])
            P.dma('sp', dbg['gv'][b], gv[:], writes=['dbggv'])
            P.dma('sp', dbg['khTok'][b], khTok, writes=['dbgkh'])
            P.dma('sp', dbg['ebL'][b], ebL[:], writes=['dbgebl'])
            P.barrier()
        P.op('pool', lambda e: e.memset(ones_nt[:], 1.0), writes=['rm'])
        win_load(wbuf[:], 2576, 512, 'w_cur')
        P.dma('sp', gnbc[:], g['gng_d'].to_broadcast([128, 512]), writes=['gnbc'])
        for j in range(NLT):
            tk = slice(TC + j * 128, TC + (j + 1) * 128)
            for k in range(KC):
                P.op('pe', lambda e, k=k, tk=tk: e.matmul(ps[2][:, :], lhsT=uT[:, k, tk], rhs=wbuf[:, k, :], start=(k == 0),
                                                          stop=(k == KC - 1)), reads=['w_cur'], writes=[PS(2)], inc=(k == KC - 1))
            P.op('act', lambda e: e.activation(out=xn[:, 0:512], in_=ps[2][:, :], func=AF.Sigmoid), reads=[PS(2)], writes=['xn'])
            P.op('dve', lambda e: e.tensor_tensor(out=xn[:, 0:512], in0=ps[2][:, :], in1=xn[:, 0:512], op=ALU.mult),
                 reads=[PS(2), 'xn'], writes=['xn'])
            per_head_norm_stats(j)
            P.op('dve', lambda e: e.tensor_tensor(out=rms_scratch[:, 0:512], in0=rms_scratch[:, 0:512], in1=gnbc[:], op=ALU.mult),
                 reads=['rms_scratch', 'gnbc'], writes=['rms_scratch'])
            P.op('dve', lambda e: e.tensor_tensor(out=g_tok(j), in0=rms_scratch[:, 0:512], in1=xn[:, 0:512], op=ALU.mult),
                 reads=['rms_scratch', 'xn', ('Hacc', j)], writes=[('g_tok', j)])
        P.barrier()

    if dbg:
        P.dma('sp', dbg['uT'][b], uT[:], writes=['dbg_uT'])
        P.barrier()
    for j in range(NLT if KSTOP == '' else 0):
        for src_i, src in enumerate((a_tok[:, j, :], g_tok(j))):
            pb = src_i
            for q in range(4):
                P.op('pe', lambda e, q=q, src=src, pb=pb: e.transpose(out=psb[pb][:, q * 128:(q + 1) * 128],
                                                                      in_=src[:, q * 128:(q + 1) * 128], identity=ident_b[:]),
                     reads=['ident_b'], writes=[PS(pb)], inc=(q == 3))
            P.op('act' if src_i == 0 else 'dve',
                 (lambda e, j=j, pb=pb, src_i=src_i: e.copy(out=mT[:, 4 * src_i:4 * src_i + 4, j * 128:(j + 1) * 128],
                                                            in_=psb[pb][:, 0:512].rearrange("p (q c) -> p q c", c=128)))
                 if src_i == 0 else
                 (lambda e, j=j, pb=pb, src_i=src_i: e.tensor_copy(out=mT[:, 4 * src_i:4 * src_i + 4, j * 128:(j + 1) * 128],
                                                                   in_=psb[pb][:, 0:512].rearrange("p (q c) -> p q c", c=128))),
                 reads=[PS(pb)], writes=[('mT', j, src_i)])
    P.barrier()
    if dbg:
        P.dma('sp', dbg['mT'][b], mT, writes=['dbg_mT'])
        P.barrier()
    with nc.sbuf_tensor(S("wout"), [128, KC, D], BF16) as wout, nc.sbuf_tensor(S("xt2"), [128, 2, D], F32) as xt2, \
            nc.sbuf_tensor(S("pt2"), [128, 2, D], F32) as pt2:
        for half in range(2):
            P.dma('pool', wout[:, :, half * 512:(half + 1) * 512],
                  wout_d[:, half * 512:(half + 1) * 512].rearrange("(k p) c -> p k c", p=128), writes=['wout'])
        g['make_gbc'](b, 0)
        for j in range(NLT):
            bf = j % 2
            P.dma('sp', x_res[:, j, :], x_d[b, j * 128:(j + 1) * 128, :], writes=[('x_res', j)])
            P.dma('sp', pt2[:, bf, :], pe_d[j * 128:(j + 1) * 128, :], writes=[('pt2', bf)])
            P.op('pool', lambda e, j=j, bf=bf: e.tensor_tensor(out=x_res[:, j, :], in0=x_res[:, j, :], in1=pt2[:, bf, :], op=ALU.add),
                 reads=[('x_res', j), ('pt2', bf)], writes=[('x_res', j)])
            for half in range(2):
                pb = 2 + half
                hs = slice(half * 512, (half + 1) * 512)
                for k in range(KC):
                    P.op('pe', lambda e, k=k, j=j, pb=pb, hs=hs: e.matmul(ps[pb][:, :], lhsT=mT[:, k, j * 128:(j + 1) * 128],
                                                                          rhs=wout[:, k, hs], start=(k == 0), stop=(k == KC - 1)),
                         reads=['wout'], writes=[PS(pb)], inc=(k == KC - 1))
                P.op('dve', lambda e, pb=pb, hs=hs, bf=bf: e.tensor_tensor(out=xt2[:, bf, hs], in0=ps[pb][:, :], in1=gbc[:, hs],
                                                                           op=ALU.mult), reads=[PS(pb), 'gbc'], writes=[('xt2', bf, half)])
                if dbg:
                    pass
                P.op('pool', lambda e, j=j, hs=hs, bf=bf: e.tensor_tensor(out=x_res[:, j, hs], in0=x_res[:, j, hs], in1=xt2[:, bf, hs],
                                                                          op=ALU.add), reads=[('xt2', bf, half), ('x_res', j)],
                     writes=[('x_res', j)])
        P.barrier()
    st.close()


_CACHE = {}


def _get_nc(cfg_key):
    if cfg_key not in _CACHE:
        _CACHE[cfg_key] = build(Cfg(*cfg_key))
    return _CACHE[cfg_key]


def make_in_maps(inputs, n_cores, NB):
    f = lambda a: np.ascontiguousarray(np.asarray(a, dtype=np.float32))
    shared = {
        "c_ctx": f(inputs["c_ctx"]).reshape(1, D),
        "ada_w": f(inputs["ada_w"])[0], "ada_b": f(inputs["ada_b"]).reshape(1, -1),
        "norm1_g": f(inputs["norm1_g"]).reshape(1, D), "w_in": f(inputs["w_in"])[0],
        "ml_conv_w": f(inputs["ml_conv_w"])[0], "ml_conv_b": f(inputs["ml_conv_b"]).reshape(1, -1),
        "ml_gate_b": f(inputs["ml_gate_b"])[0], "ml_norm_g": f(inputs["ml_norm_g"]).reshape(1, -1),
        "gla_gate_w2": f(inputs["gla_gate_w2"])[0], "gla_gate_b": f(inputs["gla_gate_b"])[0],
        "gla_norm_g": f(inputs["gla_norm_g"]).reshape(1, -1), "w_out": f(inputs["w_out"])[0],
        "norm2_g": f(inputs["norm2_g"]).reshape(1, D), "router_w": f(inputs["router_w"])[0],
        "router_b": f(inputs["router_b"]).reshape(1, -1), "moe_w_gu": f(inputs["moe_w_gu"])[0],
        "moe_b_gu": f(inputs["moe_b_gu"])[0], "moe_w_down": f(inputs["moe_w_down"])[0],
        "moe_b_down": f(inputs["moe_b_down"])[0], "final_norm_g": f(inputs["final_norm_g"]).reshape(1, D),
    }
    x, c, ctx = f(inputs["x"]), f(inputs["c"]), f(inputs["ctx"])
    maps = []
    for i in range(n_cores):
        m = dict(shared)
        m["x"] = x[i * NB:(i + 1) * NB]
        m["c"] = c[i * NB:(i + 1) * NB]
        m["ctx"] = ctx[i * NB:(i + 1) * NB]
        maps.append(m)
    return maps


def kernel(**inputs):
    n_cores = 8
    B = inputs["x"].shape[0]
    NB = B // n_cores
    T = inputs["x"].shape[1]
    TC = inputs["ctx"].shape[1]
    E = inputs["router_w"].shape[-1]
    nc = _get_nc((NB, T, TC, E, False, 99))
    maps = make_in_maps(inputs, n_cores, NB)
    res = run_bass_kernel_spmd(nc, maps, core_ids=list(range(n_cores)))
    return np.concatenate([r["out"] for r in res.results], axis=0)
```

```python
import math
import os
import types
import numpy as np
from contextlib import ExitStack
import concourse.bass as bass
import concourse.mybir as mybir
from concourse.bass_utils import run_bass_kernel_spmd

F32 = mybir.dt.float32
BF16 = mybir.dt.bfloat16
AF = mybir.ActivationFunctionType
ALU = mybir.AluOpType
AX = mybir.AxisListType

D = 1024
KC = 8
EPS = 1e-6
LIM = 7.0
ALPHA = 1.702


class Cfg:
    def __init__(self, NB=4, T=2048, TC=256, E=32, debug=False, stages=99):
        self.NB, self.T, self.TC, self.E = NB, T, TC, E
        self.NT = T + TC
        self.NTT = self.NT // 128
        self.NTC = TC // 128
        self.NLT = T // 128
        self.NCH = self.NT // 64
        self.NCC = TC // 64
        self.debug = debug
        self.stages = stages


def _snapshot(fn):
    if fn is None or fn.__closure__ is None:
        return fn
    cells = []
    for c in fn.__closure__:
        try:
            cells.append(types.CellType(c.cell_contents))
        except ValueError:
            cells.append(c)
    return types.FunctionType(fn.__code__, fn.__globals__, fn.__name__, fn.__defaults__, tuple(cells))


class Prog:
    ENGS = ('pe', 'act', 'dve', 'pool', 'sp')

    def __init__(self, nc, st):
        self.nc = nc
        self.sem = {e: st.enter_context(nc.semaphore('sem_' + e)) for e in self.ENGS}
        self.cnt = dict.fromkeys(self.ENGS, 0)
        self.seen = {e: {} for e in self.ENGS}
        self.streams = {e: [] for e in self.ENGS}
        self.res = {}
        self.pend = {e: [] for e in self.ENGS}
        self.dsem, self.dcnt, self.drr = {}, {}, {}
        for q, n in (('sp', 14), ('pool', 10), ('act', 4)):
            self.dsem[q] = [st.enter_context(nc.semaphore(f'dma_{q}{i}')) for i in range(n)]
            self.dcnt[q] = [0] * n
            self.drr[q] = 0
        self.all_dma_events = []

    def _need(self, eng, ev, waits, raw):
        key, sem, val = ev
        if key == eng and not raw and eng == 'pe':
            return
        if val is None:
            raise RuntimeError(f'pending event consumed: {key} by {eng}')
        if self.seen[eng].get(key, 0) >= val:
            return
        if key in waits and waits[key][2] >= val:
            return
        waits[key] = (key, sem, val)

    def _deps(self, eng, reads, writes):
        waits = {}
        for r in reads:
            s = self.res.get(r)
            if s and s[0] is not None:
                self._need(eng, s[0], waits, True)
        for w in writes:
            s = self.res.get(w)
            if s:
                if s[0] is not None:
                    self._need(eng, s[0], waits, False)
                for ev in s[1].values():
                    self._need(eng, ev, waits, False)
        wl = list(waits.values())
        for key, sem, val in wl:
            self.seen[eng][key] = max(self.seen[eng].get(key, 0), val)
        return wl

    def _register(self, ev, reads, writes):
        for r in reads:
            s = self.res.setdefault(r, [None, {}])
            s[1][ev[0]] = ev
        for w in writes:
            self.res[w] = [ev, {}]

    def op(self, eng, fn, reads=(), writes=(), inc=True):
        fn = _snapshot(fn)
        wl = self._deps(eng, reads, writes)
        if inc:
            self.cnt[eng] += 1
            ev = [eng, self.sem[eng], self.cnt[eng]]
            for p in self.pend[eng]:
                p[2] = self.cnt[eng]
            self.pend[eng] = []
        else:
            ev = [eng, self.sem[eng], None]
            self.pend[eng].append(ev)
        self._register(ev, reads, writes)
        self.streams[eng].append((wl, fn, 'inc' if inc else None))

    def dma(self, q, out, in_, reads=(), writes=(), **kw):
        k = self.drr[q]
        self.drr[q] = (k + 1) % len(self.dsem[q])
        sem = self.dsem[q][k]
        key = ('d', q, k)
        wl = self._deps(q, reads, writes)
        prev = self.dcnt[q][k]
        if prev > 0 and self.seen[q].get(key, 0) < 16 * prev:
            wl.append((key, sem, 16 * prev))
            self.seen[q][key] = 16 * prev
        self.dcnt[q][k] += 1
        ev = [key, sem, 16 * self.dcnt[q][k]]
        self._register(ev, reads, writes)
        self.all_dma_events.append(ev)

        def fn(e, out=out, in_=in_, kw=kw, sem=sem):
            e.dma_start(out=out, in_=in_, **kw).then_inc(sem, 16)
        self.streams[q].append((wl, fn, 'dma'))

    def barrier(self):
        for e in self.ENGS:
            wl = []
            for o in self.ENGS:
                if self.cnt[o] > self.seen[e].get(o, 0):
                    if self.pend[o]:
                        raise RuntimeError('barrier with pending non-inc ops on ' + o)
                    wl.append((o, self.sem[o], self.cnt[o]))
                    self.seen[e][o] = self.cnt[o]
            for q in self.dsem:
                for k, c in enumerate(self.dcnt[q]):
                    key = ('d', q, k)
                    if c > 0 and self.seen[e].get(key, 0) < 16 * c:
                        wl.append((key, self.dsem[q][k], 16 * c))
                        self.seen[e][key] = 16 * c
            if wl:
                self.streams[e].append((wl, None, None))
        self.res = {}

    def emit(self, block):
        decos = {'pe': block.tensor, 'act': block.scalar, 'dve': block.vector,
                 'pool': block.gpsimd, 'sp': block.sync}
        for name in self.ENGS:
            stream = self.streams[name]
            sem_e = self.sem[name]

            def body(e, stream=stream, sem_e=sem_e):
                for wl, fn, kind in stream:
                    for key, sem, val in wl:
                        e.wait_ge(sem, val)
                    if fn is None:
                        continue
                    ins = fn(e)
                    if kind == 'inc':
                        ins.then_inc(sem_e, 1)
            decos[name](body)


def build(cfg):
    NB, T, TC, E = cfg.NB, cfg.T, cfg.TC, cfg.E
    NT, NTT, NTC, NLT, NCH, NCC = cfg.NT, cfg.NTT, cfg.NTC, cfg.NLT, cfg.NCH, cfg.NCC
    NBC = NB + 1
    nc = bass.Bass("TRN2", target_bir_lowering=False)

    def din(name, shape):
        return nc.dram_tensor(name, list(shape), F32, kind="ExternalInput").ap()

    x_d = din("x", [NB, T, D])
    c_d = din("c", [NB, D])
    ctx_d = din("ctx", [NB, TC, D])
    cctx_d = din("c_ctx", [1, D])
    adaw_d = din("ada_w", [D, 6 * D])
    adab_d = din("ada_b", [1, 6 * D])
    n1g_d = din("norm1_g", [1, D])
    win_d = din("w_in", [D, 3120])
    cw_d = din("ml_conv_w", [3, 512])
    cb_d = din("ml_conv_b", [1, 512])
    mgb_d = din("ml_gate_b", [4, 4])
    mng_d = din("ml_norm_g", [1, 512])
    gw2_d = din("gla_gate_w2", [2, 16, 256])
    ggb_d = din("gla_gate_b", [2, 256])
    gng_d = din("gla_norm_g", [1, 512])
    wout_d = din("w_out", [D, D])
    n2g_d = din("norm2_g", [1, D])
    rw_d = din("router_w", [D, E])
    rb_d = din("router_b", [1, E])
    wgu_d = din("moe_w_gu", [E, D, 2 * D])
    bgu_d = din("moe_b_gu", [E, 2 * D])
    wdn_d = din("moe_w_down", [E, D, D])
    bdn_d = din("moe_b_down", [E, D])
    fng_d = din("final_norm_g", [1, D])
    out_d = nc.dram_tensor("out", [NB, T, D], F32, kind="ExternalOutput").ap()
    pe_d = nc.dram_tensor("pe_scratch", [T, D], F32, kind="Internal").ap()
    gsc_d = nc.dram_tensor("gate_scratch", [NBC, 2, D], F32, kind="Internal").ap()
    dbg = {}
    if cfg.debug:
        dbg['xmid'] = nc.dram_tensor("dbg_xmid", [NB, T, D], F32, kind="ExternalOutput").ap()
        dbg['mT'] = nc.dram_tensor("dbg_mT", [NB, 128, KC, T], BF16, kind="ExternalOutput").ap()
        dbg['uT'] = nc.dram_tensor("dbg_uT", [NB, 128, KC, NT], BF16, kind="ExternalOutput").ap()
        dbg['H'] = nc.dram_tensor("dbg_H", [NB, 128, NLT, 512], F32, kind="ExternalOutput").ap()
        dbg['H1'] = nc.dram_tensor("dbg_H1", [NB, 128, NLT, 512], F32, kind="ExternalOutput").ap()
        dbg['HM'] = nc.dram_tensor("dbg_HM", [NB, 128, NLT, 512], F32, kind="ExternalOutput").ap()
        dbg['edT'] = nc.dram_tensor("dbg_edT", [NB, 128, NTT, 8], F32, kind="ExternalOutput").ap()
        dbg['wcB'] = nc.dram_tensor("dbg_wcB", [NB, 128, 2, NCH], F32, kind="ExternalOutput").ap()
        dbg['qkT'] = nc.dram_tensor("dbg_qkT", [NB, 128, 4, NT], BF16, kind="ExternalOutput").ap()
        dbg['vtok'] = nc.dram_tensor("dbg_vtok", [NB, 128, NTT, 4, 129], BF16, kind="ExternalOutput").ap()
        dbg['kTok'] = nc.dram_tensor("dbg_kTok", [NB, 128, NTT, 256], BF16, kind="ExternalOutput").ap()
        dbg['qt'] = nc.dram_tensor("dbg_qt", [NB, 128, 2, NT], BF16, kind="ExternalOutput").ap()
        dbg['kt'] = nc.dram_tensor("dbg_kt", [NB, 128, 2, NT], BF16, kind="ExternalOutput").ap()
        dbg['gv'] = nc.dram_tensor("dbg_gv", [NB, 128, NTT, 512], BF16, kind="ExternalOutput").ap()
        dbg['khTok'] = nc.dram_tensor("dbg_khTok", [NB, 128, NTT, 256], BF16, kind="ExternalOutput").ap()
        dbg['ebL'] = nc.dram_tensor("dbg_ebL", [NB, 128, 2, NCH], F32, kind="ExternalOutput").ap()

    st = ExitStack()
    P = Prog(nc, st)

    def sb(name, shape, dt=F32):
        return st.enter_context(nc.sbuf_tensor(name, list(shape), dt))

    ps = [st.enter_context(nc.psum_tensor(f"ps{i}", [128, 512], F32)) for i in range(8)]
    psb = [p[:].bitcast(BF16) for p in ps]

    def PS(i):
        return ('ps', i)

    ident_f = sb("ident_f", [128, 128])
    ident_b = sb("ident_b", [128, 128], BF16)
    ones_f = sb("ones_f", [128, 128])
    ones_nt = sb("ones_nt", [128, NT], BF16)
    maskF = sb("maskF", [128, 64])
    maskB = sb("maskB", [128, 64])
    selh = sb("selh", [4, 2, 2, 64])
    eps_c = sb("eps_c", [128, 1])
    one_c = sb("one_c", [128, 1])
    P.op('pool', lambda e: e.memset(one_c[:], 1.0), writes=['one_c'])
    P.op('pool', lambda e: e.memset(eps_c[:], EPS), writes=['eps_c'])
    P.op('pool', lambda e: e.memset(ones_f[:], 1.0), writes=['ones_f'])
    P.op('pool', lambda e: e.memset(ones_nt[:], 1.0), writes=['ones_nt'])
    P.op('pool', lambda e: e.affine_select(out=ident_f[:], in_=ones_f[:], pattern=[[-1, 128]],
                                           compare_op=ALU.is_equal, fill=0.0, base=0, channel_multiplier=1),
         reads=['ones_f'], writes=['ident_f'])
    P.op('dve', lambda e: e.tensor_copy(out=ident_b[:], in_=ident_f[:]), reads=['ident_f'], writes=['ident_b'])
    for half in range(2):
        sl = slice(half * 64, half * 64 + 64)
        P.op('pool', lambda e, sl=sl: e.affine_select(out=maskF[sl, :], in_=ones_f[sl, 0:64], pattern=[[1, 64]],
                                                      compare_op=ALU.is_ge, fill=0.0, base=0, channel_multiplier=-1),
             reads=['ones_f'], writes=['maskF'])
        P.op('pool', lambda e, sl=sl: e.affine_select(out=maskB[sl, :], in_=ones_f[sl, 0:64], pattern=[[-1, 64]],
                                                      compare_op=ALU.is_ge, fill=0.0, base=0, channel_multiplier=1),
             reads=['ones_f'], writes=['maskB'])
    P.op('pool', lambda e: e.affine_select(out=selh[:].rearrange("p a b c -> p (a b c)"), in_=ones_nt[0:4, 0:256],
                                           pattern=[[-2, 2], [-1, 2], [0, 64]], compare_op=ALU.is_equal, fill=0.0, base=0,
                                           channel_multiplier=1),
         reads=['ones_nt'], writes=['selh'])

    modT = sb("modT", [128, 4, KC, NBC])
    rw_sb = sb("rw_sb", [128, KC, E])
    rb_bc = sb("rb_bc", [128, E])
    bguT = sb("bguT", [128, 16, E])
    cw_sb = sb("cw_sb", [128, 4, 3])
    cb_sb = sb("cb_sb", [128, 4])
    mgb_sb = sb("mgb_sb", [4, 4])
    wg_sb = sb("wg_sb", [128, KC, 16], BF16)
    wlr_sb = sb("wlr_sb", [128, KC, 32], BF16)
    gw2_sb = sb("gw2_sb", [16, 2, 256], BF16)
    ggbT = sb("ggbT", [128, 2, 2])
    nc_allow = nc.allow_non_contiguous_dma(reason="tiny param layouts")
    st.enter_context(nc_allow)

    P.dma('sp', rw_sb[:], rw_d.rearrange("(k p) e -> p k e", p=128), writes=['rw_sb'])
    P.dma('sp', rb_bc[:], rb_d.to_broadcast([128, E]), writes=['rb_bc'])
    for q in range(4):
        P.dma('sp', cw_sb[:, q, :], cw_d[:, q * 128:(q + 1) * 128].rearrange("i p -> p i"), writes=['cw_sb'])
    P.dma('sp', cb_sb[:], cb_d.rearrange("o (q p) -> p (o q)", p=128), writes=['cb_sb'])
    P.dma('sp', mgb_sb[:], mgb_d.rearrange("g h -> h g"), writes=['mgb_sb'])
    P.dma('sp', ggbT[:], ggb_d.rearrange("z (j p) -> p z j", p=128), writes=['ggbT'])
    P.dma('pool', wg_sb[:], win_d[:, 1536:1552].rearrange("(k p) c -> p k c", p=128), writes=['wg_sb'])
    P.dma('pool', wlr_sb[:], win_d[:, 3088:3120].rearrange("(k p) c -> p k c", p=128), writes=['wlr_sb'])
    P.dma('pool', gw2_sb[:], gw2_d.rearrange("z r c -> r z c"), writes=['gw2_sb'])
    with nc.sbuf_tensor("bgu_rows", [E, 2 * D], F32) as bgu_rows:
        P.dma('sp', bgu_rows[:], bgu_d, writes=['bgu_rows'])
        for j in range(16):
            P.op('pe', lambda e, j=j: e.transpose(out=ps[0][:, j * E:(j + 1) * E], in_=bgu_rows[:, j * 128:(j + 1) * 128],
                                                  identity=ident_f[0:E, 0:E]), reads=['bgu_rows', 'ident_f'], writes=[PS(0)],
                 inc=(j == 15))
        P.op('dve', lambda e: e.tensor_copy(out=bguT[:].rearrange("p j e -> p (j e)"), in_=ps[0][:, 0:16 * E]),
             reads=[PS(0)], writes=['bguT'])
        P.op('dve', lambda e: e.tensor_scalar(out=bguT[:, 8:16, :], in0=bguT[:, 8:16, :], scalar1=1.0, scalar2=None, op0=ALU.add),
             reads=['bguT'], writes=['bguT'])
        P.barrier()
    P.op('dve', lambda e: e.tensor_scalar(out=ggbT[:], in0=ggbT[:], scalar1=-1.0, scalar2=None, op0=ALU.mult),
         reads=['ggbT'], writes=['ggbT'])

    with nc.sbuf_tensor("om", [128, 256], F32) as om, nc.sbuf_tensor("jf", [128, 256], F32) as jf, \
            nc.sbuf_tensor("pidx", [128, 2], F32) as pidx, nc.sbuf_tensor("arg", [128, 256], F32) as arg, \
            nc.sbuf_tensor("petile", [128, 2, D], F32) as petile, nc.sbuf_tensor("omr0", [128, 256], F32) as omr0, \
            nc.sbuf_tensor("argc", [128, 256], F32) as argc, nc.sbuf_tensor("argr", [128, 256], F32) as argr, \
            nc.sbuf_tensor("arg2", [128, 256], F32) as arg2:
        P.op('pool', lambda e: e.iota(out=jf[:], pattern=[[1, 256]], base=0, channel_multiplier=0,
                                      allow_small_or_imprecise_dtypes=True), writes=['jf'])
        P.op('act', lambda e: e.activation(out=om[:], in_=jf[:], func=AF.Exp, scale=-math.log(10000.0) / 256.0),
             reads=['jf'], writes=['om'])
        for half in range(2):
            sl = slice(half * 64, half * 64 + 64)
            P.op('pool', lambda e, sl=sl: e.iota(out=pidx[sl, 0:1], pattern=[[0, 1]], base=0, channel_multiplier=1,
                                                 allow_small_or_imprecise_dtypes=True), writes=['pidx'])
            P.op('pool', lambda e, sl=sl, half=half: e.memset(pidx[sl, 1:2], float(half)), writes=['pidx'])
        PI = math.pi

        def sincos(dst_sin, dst_cos, argap, rd):
            MAGIC = 12582912.0
            for dst, off in ((dst_sin, 0.0), (dst_cos, 0.5 * PI)):
                P.op('dve', lambda e, off=off: e.tensor_scalar(out=arg2[:], in0=argap, scalar1=off, scalar2=None, op0=ALU.add),
                     reads=rd, writes=['arg2'])
                P.op('dve', lambda e: e.tensor_scalar(out=arg[:], in0=arg2[:], scalar1=1.0 / (2 * PI), scalar2=MAGIC,
                                                      op0=ALU.mult, op1=ALU.add), reads=['arg2'], writes=['arg'])
                P.op('dve', lambda e: e.tensor_scalar(out=arg[:], in0=arg[:], scalar1=-MAGIC, scalar2=None, op0=ALU.add),
                     reads=['arg'], writes=['arg'])
                P.op('dve', lambda e: e.scalar_tensor_tensor(out=arg[:], in0=arg[:], scalar=-2 * PI, in1=arg2[:],
                                                             op0=ALU.mult, op1=ALU.add), reads=['arg', 'arg2'], writes=['arg'])
                P.op('dve', lambda e: e.tensor_scalar(out=arg[:], in0=arg[:], scalar1=-PI, scalar2=PI, op0=ALU.max, op1=ALU.min),
                     reads=['arg'], writes=['arg'])
                P.op('act', lambda e, dst=dst: e.activation(out=dst, in_=arg[:], func=AF.Sin), reads=['arg'],
                     writes=['petile'])
        P.op('dve', lambda e: e.tensor_scalar(out=omr0[:], in0=om[:], scalar1=pidx[:, 1:2], scalar2=None, op0=ALU.mult),
             reads=['om', 'pidx'], writes=['omr0'])
        P.op('dve', lambda e: e.tensor_scalar(out=argc[:], in0=om[:], scalar1=pidx[:, 0:1], scalar2=None, op0=ALU.mult),
             reads=['om', 'pidx'], writes=['argc'])
        for k in range(NLT):
            buf = k % 2
            if k < 2:
                sincos(petile[:, buf, 512:768], petile[:, buf, 768:1024], argc[:], ['argc'])
            P.op('dve', lambda e, k=k: e.scalar_tensor_tensor(out=argr[:], in0=om[:], scalar=float(2 * k), in1=omr0[:],
                                                              op0=ALU.mult, op1=ALU.add), reads=['om', 'omr0'], writes=['argr'])
            sincos(petile[:, buf, 0:256], petile[:, buf, 256:512], argr[:], ['argr'])
            P.dma('sp', pe_d[k * 128:(k + 1) * 128, :], petile[:, buf, :], reads=['petile'], writes=['pe_d'])
        P.barrier()

    with nc.sbuf_tensor("cin", [NBC, D], F32) as cin, nc.sbuf_tensor("csig", [NBC, D], F32) as csig, \
            nc.sbuf_tensor("scT", [128, KC, NBC], F32) as scT, nc.sbuf_tensor("adab", [1, 6 * D], F32) as adab, \
            nc.sbuf_tensor("modrows", [NBC, 6 * D], F32) as modrows, \
            nc.sbuf_tensor("adaw", [128, 2, KC, 512], F32) as adaw, \
            nc.sbuf_tensor("grows", [NBC, 2, D], F32) as grows, \
            nc.sbuf_tensor("n1bc", [NBC, D], F32) as n1bc, nc.sbuf_tensor("n2bc", [NBC, D], F32) as n2bc:
        P.dma('sp', n1bc[:], n1g_d.to_broadcast([NBC, D]), writes=['n1bc'])
        P.dma('sp', n2bc[:], n2g_d.to_broadcast([NBC, D]), writes=['n2bc'])
        P.dma('sp', cin[0:NB, :], c_d, writes=['cin'])
        P.dma('sp', cin[NB:NBC, :], cctx_d, writes=['cin'])
        P.dma('sp', adab[:], adab_d, writes=['adab'])
        P.op('act', lambda e: e.activation(out=csig[:], in_=cin[:], func=AF.Sigmoid), reads=['cin'], writes=['csig'])
        P.op('dve', lambda e: e.tensor_tensor(out=csig[:], in0=csig[:], in1=cin[:], op=ALU.mult),
             reads=['csig', 'cin'], writes=['csig'])
        for k in range(KC):
            P.op('pe', lambda e, k=k: e.transpose(out=ps[0][:, k * NBC:(k + 1) * NBC], in_=csig[:, k * 128:(k + 1) * 128],
                                                  identity=ident_f[0:NBC, 0:NBC]),
                 reads=['csig', 'ident_f'], writes=[PS(0)], inc=(k == KC - 1))
        P.op('dve', lambda e: e.tensor_copy(out=scT[:].rearrange("p k b -> p (k b)"), in_=ps[0][:, 0:KC * NBC]),
             reads=[PS(0)], writes=['scT'])
        for cg in range(12):
            buf = cg % 2
            P.dma('sp', adaw[:, buf], adaw_d[:, cg * 512:(cg + 1) * 512].rearrange("(k p) c -> p k c", p=128),
                  writes=[('adaw', buf)])
            pb = 1 + (cg % 2)
            for k in range(KC):
                P.op('pe', lambda e, k=k, buf=buf, pb=pb: e.matmul(ps[pb][0:NBC, :], lhsT=scT[:, k, :], rhs=adaw[:, buf, k, :],
                                                                   start=(k == 0), stop=False),
                     reads=['scT', ('adaw', buf)], writes=[PS(pb)], inc=False)
            P.op('pe', lambda e, cg=cg, pb=pb: e.matmul(ps[pb][0:NBC, :], lhsT=ones_f[0:1, 0:NBC],
                                                        rhs=adab[0:1, cg * 512:(cg + 1) * 512], start=False, stop=True),
                 reads=['ones_f', 'adab'], writes=[PS(pb)])
            P.op('act', lambda e, cg=cg, pb=pb: e.copy(out=modrows[:, cg * 512:(cg + 1) * 512], in_=ps[pb][0:NBC, :]),
                 reads=[PS(pb)], writes=['modrows'])
        P.op('dve', lambda e: e.scalar_tensor_tensor(out=grows[:, 0, :], in0=modrows[:, D:2 * D], scalar=1.0, in1=n1bc[:],
                                                     op0=ALU.add, op1=ALU.mult), reads=['modrows', 'n1bc'], writes=['grows'])
        P.op('dve', lambda e: e.scalar_tensor_tensor(out=grows[:, 1, :], in0=modrows[:, 4 * D:5 * D], scalar=1.0, in1=n2bc[:],
                                                     op0=ALU.add, op1=ALU.mult), reads=['modrows', 'n2bc'], writes=['grows'])
        srcs = [grows[:, 0, :], modrows[:, 0:D], grows[:, 1, :], modrows[:, 3 * D:4 * D]]
        for m in range(4):
            for k in range(KC):
                o = (m * KC + k) * NBC
                P.op('pe', lambda e, m=m, k=k, o=o: e.transpose(out=ps[3][:, o:o + NBC], in_=srcs[m][:, k * 128:(k + 1) * 128],
                                                                identity=ident_f[0:NBC, 0:NBC]),
                     reads=['grows', 'modrows', 'ident_f'], writes=[PS(3)], inc=(m == 3 and k == KC - 1))
        P.op('dve', lambda e: e.tensor_copy(out=modT[:].rearrange("p m k b -> p (m k b)"), in_=ps[3][:, 0:4 * KC * NBC]),
             reads=[PS(3)], writes=['modT'])
        P.dma('sp', gsc_d[:, 0, :], modrows[:, 2 * D:3 * D], reads=['modrows'], writes=['gsc_d'])
        P.dma('sp', gsc_d[:, 1, :], modrows[:, 5 * D:6 * D], reads=['modrows'], writes=['gsc_d'])
        P.barrier()

    def rms_to_featmajor(xt_ap, xt_res, dstT, tok0, gsel, bsel, scratch, xn, ssq, pbanks, extra_f32=None, extra_res='extra_f32'):
        P.op('act', lambda e: e.activation(out=scratch, in_=xt_ap, func=AF.Square, accum_out=ssq),
             reads=[xt_res], writes=['rms_scratch', 'ssq'])
        P.op('act', lambda e: e.activation(out=ssq, in_=ssq, func=AF.Sqrt, scale=1.0 / D, bias=eps_c[:]),
             reads=['ssq'], writes=['ssq'])
        P.op('dve', lambda e: e.reciprocal(out=ssq, in_=ssq), reads=['ssq'], writes=['ssq'])
        P.op('dve', lambda e: e.tensor_scalar(out=xn, in0=xt_ap, scalar1=ssq, scalar2=None, op0=ALU.mult),
             reads=['ssq', xt_res], writes=['xn'])
        for k in range(KC):
            pb = pbanks[k // 4]
            P.op('pe', lambda e, k=k, pb=pb: e.transpose(out=ps[pb][:, (k % 4) * 128:(k % 4 + 1) * 128],
                                                         in_=xn[:, k * 128:(k + 1) * 128], identity=ident_f[:]),
                 reads=['xn', 'ident_f'], writes=[PS(pb)], inc=(k % 4 == 3))
        for k in range(KC):
            pb = pbanks[k // 4]
            P.op('act', lambda e, k=k, pb=pb: e.activation(out=dstT[:, k, tok0:tok0 + 128],
                                                           in_=ps[pb][:, (k % 4) * 128:(k % 4 + 1) * 128], func=AF.Identity,
                                                           scale=modT[:, gsel, k, bsel:bsel + 1],
                                                           bias=modT[:, gsel + 1, k, bsel:bsel + 1]),
                 reads=[PS(pb), 'modT'], writes=[('dstT', tok0)])
            if extra_f32 is not None:
                P.op('act', lambda e, k=k, pb=pb: e.activation(out=extra_f32[:, k, :],
                                                               in_=ps[pb][:, (k % 4) * 128:(k % 4 + 1) * 128], func=AF.Identity,
                                                               scale=modT[:, gsel, k, bsel:bsel + 1],
                                                               bias=modT[:, gsel + 1, k, bsel:bsel + 1]),
                     reads=[PS(pb), 'modT'], writes=[extra_res])

    ARENA = max(NLT * D, NLT * 512 + max(3 * NT, NLT * 256 + NTT * 128 + NT // 2 + 64))
    arena = sb("arena", [128, ARENA])
    x_res = arena[:, 0:NLT * D].rearrange("p (j c) -> p j c", c=D)
    gbc = sb("gbc", [128, D])
    rms_scratch = sb("rms_scratch", [128, D])
    xn = sb("xn", [128, D])
    ssq = sb("ssq", [128, 1])

    def make_gbc(b, which):
        P.dma('sp', gbc[:], gsc_d[b:b + 1, which, :].to_broadcast([128, D]), writes=['gbc'])

    for b in range(NB):
        if cfg.stages >= 2:
            build_mixer(nc, P, cfg, b, dict(
                ps=ps, psb=psb, PS=PS, ident_f=ident_f, ident_b=ident_b, ones_f=ones_f, ones_nt=ones_nt, maskF=maskF,
                maskB=maskB, selh=selh, modT=modT, x_d=x_d, ctx_d=ctx_d, pe_d=pe_d, win_d=win_d, wout_d=wout_d,
                cw_sb=cw_sb, cb_sb=cb_sb, mgb_sb=mgb_sb, wg_sb=wg_sb, wlr_sb=wlr_sb, gw2_sb=gw2_sb, ggbT=ggbT,
                mng_d=mng_d, gng_d=gng_d, x_res=x_res, arena=arena, eps_c=eps_c, one_c=one_c, gbc=gbc,
                rms_scratch=rms_scratch, xn=xn, ssq=ssq,
                make_gbc=make_gbc, rms_to_featmajor=rms_to_featmajor, dbg=dbg))
        else:
            with nc.sbuf_tensor(f"pt0_{b}", [128, 2, D], F32) as pt0:
                for j in range(NLT):
                    P.dma('sp', x_res[:, j, :], x_d[b, j * 128:(j + 1) * 128, :], writes=[('x_res', j)])
                    P.dma('sp', pt0[:, j % 2, :], pe_d[j * 128:(j + 1) * 128, :], reads=['pe_d'], writes=[('pt0', j % 2)])
                    P.op('dve', lambda e, j=j: e.tensor_tensor(out=x_res[:, j, :], in0=x_res[:, j, :], in1=pt0[:, j % 2, :],
                                                               op=ALU.add), reads=[('x_res', j), ('pt0', j % 2)],
                         writes=[('x_res', j)])
                P.barrier()
        if cfg.debug:
            for j in range(NLT):
                P.dma('sp', dbg['xmid'][b, j * 128:(j + 1) * 128, :], x_res[:, j, :], reads=[('x_res', j)],
                      writes=[('dbgx', j)])
        build_moe(nc, P, cfg, b, dict(
            ps=ps, PS=PS, ident_f=ident_f, modT=modT, x_res=x_res, gbc=gbc, rms_scratch=rms_scratch, xn=xn, ssq=ssq,
            make_gbc=make_gbc, rms_to_featmajor=rms_to_featmajor, rw_sb=rw_sb, rb_bc=rb_bc, bdn_d=bdn_d, bguT=bguT,
            wgu_d=wgu_d, wdn_d=wdn_d, fng_d=fng_d, out_d=out_d, eps_c=eps_c))

    P.barrier()
    with nc.Block() as block:
        P.emit(block)
    st.close()
    return nc


def build_moe(nc, P, cfg, b, g):
    NB, T, E, NLT = cfg.NB, cfg.T, cfg.E, cfg.NLT
    ps, PS, x_res, gbc = g['ps'], g['PS'], g['x_res'], g['gbc']
    ident_f, modT = g['ident_f'], g['modT']
    rw_sb, rb_bc, bguT = g['rw_sb'], g['rb_bc'], g['bguT']
    wgu_d, wdn_d, fng_d, out_d = g['wgu_d'], g['wdn_d'], g['fng_d'], g['out_d']
    TG = 512 if T >= 512 else T
    NG = T // TG
    TPG = TG // 128
    mst = ExitStack()
    u2T = mst.enter_context(nc.sbuf_tensor(f"u2T_{b}", [128, KC, T], BF16))
    gates = mst.enter_context(nc.sbuf_tensor(f"gates_{b}", [128, NLT, E], F32))
    lg = mst.enter_context(nc.sbuf_tensor(f"lg_{b}", [128, E], F32))
    top8 = mst.enter_context(nc.sbuf_tensor(f"top8_{b}", [128, 8], F32))
    rsum = mst.enter_context(nc.sbuf_tensor(f"rsum_{b}", [128, 2], F32))
    wgu = mst.enter_context(nc.sbuf_tensor(f"wgu_{b}", [128, 2, KC, 1024], BF16))
    wdn = mst.enter_context(nc.sbuf_tensor(f"wdn_{b}", [128, 2, 4, D], BF16))
    hT = mst.enter_context(nc.sbuf_tensor(f"hT_{b}", [128, 2, 4, TG], BF16))
    g1 = mst.enter_context(nc.sbuf_tensor(f"g1_{b}", [128, 2, TG], F32))
    t1 = mst.enter_context(nc.sbuf_tensor(f"t1_{b}", [128, 2, TG], F32))
    sl = mst.enter_context(nc.sbuf_tensor(f"sl_{b}", [128, 2, TG], F32))
    wdn32 = mst.enter_context(nc.sbuf_tensor(f"wdn32_{b}", [128, 4, D], F32))
    w32flat = wdn32[:].rearrange("p k c -> p (k c)")
    u2f = w32flat[:, 0:KC * 128].rearrange("p (k c) -> p k c", c=128)
    bdn_sb = w32flat[0:E, 1024:1024 + D]
    gatesT = w32flat[0:E, 2048:2048 + 128]
    if True:
        xn, ssq, rms_scratch = g['xn'], g['ssq'], g['rms_scratch']
        g['make_gbc'](b, 1)
        P.dma('sp', bdn_sb, g['bdn_d'], writes=['bdn_sb'])
        u2fs = [u2f, w32flat[:, 3072:3072 + KC * 128].rearrange("p (k c) -> p k c", c=128)]

        def pro_front(j):
            g['rms_to_featmajor'](x_res[:, j, :], ('x_res', j), u2T, j * 128, 2, b, rms_scratch[:], xn[:], ssq[:], (0, 1),
                                  extra_f32=u2fs[j % 2], extra_res=('u2f', j % 2))

        def pro_back(j):
            u2f = u2fs[j % 2]
            for k in range(KC):
                P.op('pe', lambda e, k=k: e.matmul(ps[2][:, 0:E], lhsT=u2f[:, k, :], rhs=rw_sb[:, k, :], start=(k == 0),
                                                   stop=(k == KC - 1)), reads=[('u2f', j % 2), 'rw_sb'], writes=[PS(2)],
                     inc=(k == KC - 1))
            P.op('dve', lambda e: e.tensor_tensor(out=lg[:], in0=ps[2][:, 0:E], in1=rb_bc[:], op=ALU.add),
                 reads=[PS(2), 'rb_bc'], writes=['lg'])
            P.op('dve', lambda e: e.max(out=top8[:], in_=lg[:]), reads=['lg'], writes=['top8'])
            P.op('dve', lambda e: e.tensor_scalar(out=rsum[:, 0:1], in0=top8[:, 0:1], scalar1=-1.0, scalar2=None, op0=ALU.mult),
                 reads=['top8'], writes=['rsum'])
            P.op('act', lambda e, j=j: e.activation(out=gates[:, j, :], in_=lg[:], func=AF.Exp, bias=rsum[:, 0:1], scale=1.0),
                 reads=['lg', 'rsum'], writes=[('gates', j)])
            P.op('dve', lambda e: e.tensor_scalar(out=lg[:], in0=lg[:], scalar1=top8[:, 3:4], scalar2=None, op0=ALU.is_ge),
                 reads=['lg', 'top8', ('gates', j)], writes=['lg'])
            P.op('dve', lambda e, j=j: e.tensor_tensor(out=gates[:, j, :], in0=gates[:, j, :], in1=lg[:], op=ALU.mult),
                 reads=[('gates', j), 'lg'], writes=[('gates', j)])
            P.op('dve', lambda e, j=j: e.tensor_reduce(out=rsum[:, 1:2], in_=gates[:, j, :], axis=AX.X, op=ALU.add),
                 reads=[('gates', j)], writes=['rsum'])
            P.op('dve', lambda e: e.reciprocal(out=rsum[:, 1:2], in_=rsum[:, 1:2]), reads=['rsum'], writes=['rsum'])
            P.op('dve', lambda e, j=j: e.tensor_scalar(out=gates[:, j, :], in0=gates[:, j, :], scalar1=rsum[:, 1:2],
                                                       scalar2=None, op0=ALU.mult), reads=[('gates', j), 'rsum'],
                 writes=[('gates', j)])
            P.op('pe', lambda e, j=j: e.transpose(out=ps[3][0:E, 0:128], in_=gates[:, j, :], identity=ident_f[:]),
                 reads=[('gates', j), 'ident_f'], writes=[PS(3)])
            P.op('act', lambda e: e.copy(out=gatesT, in_=ps[3][0:E, 0:128]), reads=[PS(3)], writes=['gatesT'])
            P.op('dve', lambda e, j=j: e.tensor_scalar(out=gates[:, j, :], in0=gates[:, j, :], scalar1=1.0 / ALPHA, scalar2=None,
                                                       op0=ALU.mult), reads=[('gates', j)], writes=[('gates', j)])
            for half in range(2):
                hs = slice(half * 512, (half + 1) * 512)
                P.op('pe', lambda e, hs=hs, half=half: e.matmul(ps[4 + half][:, :], lhsT=gatesT, rhs=bdn_sb[:, hs],
                                                                start=True, stop=True), reads=['gatesT', 'bdn_sb'],
                     writes=[PS(4 + half)])
                tmpb = g1[:, half, :] if TG == 512 else rms_scratch[:, hs]
                tres = ('g1', half) if TG == 512 else 'rms_scratch'
                P.op('dve', lambda e, half=half, hs=hs, tmpb=tmpb: e.tensor_tensor(out=tmpb, in0=ps[4 + half][:, :], in1=gbc[:, hs],
                                                                                   op=ALU.mult), reads=[PS(4 + half), 'gbc'],
                     writes=[tres])
                P.op('pool', lambda e, j=j, hs=hs, tmpb=tmpb: e.tensor_tensor(out=x_res[:, j, hs], in0=x_res[:, j, hs],
                                                                              in1=tmpb, op=ALU.add),
                     reads=[tres, ('x_res', j)], writes=[('x_res', j)])
        pro_front(0)
        for j in range(NLT):
            if j + 1 < NLT:
                pro_front(j + 1)
            pro_back(j)
        P.barrier()
        acc_i = 0

        def load_weights(step):
            ex_, fh_ = step // 2, step % 2
            buf_ = step % 2
            for part in range(2):
                c0 = part * 1024 + fh_ * 512
                P.dma('pool', wgu[:, buf_, :, part * 512:(part + 1) * 512],
                      wgu_d[ex_, :, c0:c0 + 512].rearrange("(k p) c -> p k c", p=128), writes=[('wgu', buf_)])
            P.dma('sp', wdn32[:], wdn_d[ex_, fh_ * 512:(fh_ + 1) * 512, :].rearrange("(k p) c -> p k c", p=128),
                  writes=['wdn32'])
            for k4 in range(4):
                P.op('pool', lambda e, k4=k4: e.tensor_tensor(out=wdn[:, buf_, k4, :], in0=wdn32[:, k4, :], in1=gbc[:], op=ALU.mult),
                     reads=['wdn32', 'gbc'], writes=[('wdn', buf_)])

        acc_box = [0]

        def emit_G(step, tg, gi):
            ex, fh = step // 2, step % 2
            buf = step % 2
            hb = gi % 2
            toks = slice(tg * TG, (tg + 1) * TG)
            for fc in range(4):
                fidx = fh * 4 + fc
                pg, pu = (fc % 2) * 2, 1 + (fc % 2) * 2
                for k in range(KC):
                    P.op('pe', lambda e, k=k, fc=fc, pg=pg: e.matmul(
                        ps[pg][:, 0:TG], lhsT=wgu[:, buf, k, fc * 128:(fc + 1) * 128], rhs=u2T[:, k, toks],
                        start=(k == 0), stop=(k == KC - 1)), reads=[('wgu', buf)], writes=[PS(pg)], inc=(k == KC - 1))
                for k in range(KC):
                    P.op('pe', lambda e, k=k, fc=fc, pu=pu: e.matmul(
                        ps[pu][:, 0:TG], lhsT=wgu[:, buf, k, 512 + fc * 128:512 + (fc + 1) * 128], rhs=u2T[:, k, toks],
                        start=(k == 0), stop=(k == KC - 1)), reads=[('wgu', buf)], writes=[PS(pu)], inc=(k == KC - 1))
                eb = fc % 2
                P.op('dve', lambda e, pg=pg, eb=eb, fidx=fidx: e.tensor_scalar(
                    out=g1[:, eb, :], in0=ps[pg][:, 0:TG], scalar1=bguT[:, fidx, ex:ex + 1], scalar2=LIM,
                    op0=ALU.add, op1=ALU.min), reads=[PS(pg), 'bguT'], writes=[('g1', eb)])
                P.op('act', lambda e, eb=eb: e.activation(out=sl[:, eb, :], in_=g1[:, eb, :], func=AF.Silu, scale=ALPHA),
                     reads=[('g1', eb)], writes=[('sl', eb)])
                P.op('dve', lambda e, pu=pu, eb=eb, fidx=fidx: e.tensor_scalar(
                    out=t1[:, eb, :], in0=ps[pu][:, 0:TG], scalar1=bguT[:, 8 + fidx, ex:ex + 1], scalar2=1.0 - LIM,
                    op0=ALU.add, op1=ALU.max), reads=[PS(pu), 'bguT'], writes=[('t1', eb)])
                P.op('dve', lambda e, eb=eb, fc=fc: e.scalar_tensor_tensor(
                    out=hT[:, hb, fc, :], in0=t1[:, eb, :], scalar=1.0 + LIM, in1=sl[:, eb, :],
                    op0=ALU.min, op1=ALU.mult), reads=[('t1', eb), ('sl', eb)], writes=[('hT', hb, fc)])

        def emit_D(step, tg, gi):
            ex = step // 2
            buf = step % 2
            hb = gi % 2
            for tt in range(TPG):
                j = tg * TPG + tt
                for half in range(2):
                    pb = 4 + ((tt * 2 + half) % 4)
                    for fc in range(4):
                        P.op('pe', lambda e, fc=fc, pb=pb, tt=tt, half=half: e.matmul(
                            ps[pb][:, :], lhsT=hT[:, hb, fc, tt * 128:(tt + 1) * 128],
                            rhs=wdn[:, buf, fc, half * 512:(half + 1) * 512], start=(fc == 0), stop=(fc == 3)),
                            reads=[('hT', hb, fc), ('wdn', buf)], writes=[PS(pb)], inc=(fc == 3))
                    hs = slice(half * 512, (half + 1) * 512)
                    P.op('dve', lambda e, pb=pb, hs=hs, j=j: e.scalar_tensor_tensor(
                        out=x_res[:, j, hs], in0=ps[pb][:, :], scalar=gates[:, j, ex:ex + 1], in1=x_res[:, j, hs],
                        op0=ALU.mult, op1=ALU.add), reads=[PS(pb), ('x_res', j)], writes=[('x_res', j)])

        items = [(st_, tg) for st_ in range(2 * E) for tg in range(NG)]
        load_weights(0)
        load_weights(1)
        emit_G(items[0][0], items[0][1], 0)
        for i, (st_, tg) in enumerate(items):
            if i + 1 < len(items):
                emit_G(items[i + 1][0], items[i + 1][1], i + 1)
            emit_D(st_, tg, i)
            if tg == NG - 1 and st_ + 2 < 2 * E:
                load_weights(st_ + 2)
        P.barrier()
        P.dma('sp', gbc[:], fng_d.to_broadcast([128, D]), writes=['gbc'])
        for j in range(NLT):
            P.op('act', lambda e, j=j: e.activation(out=rms_scratch[:], in_=x_res[:, j, :], func=AF.Square, accum_out=ssq[:]),
                 reads=[('x_res', j)], writes=['rms_scratch', 'ssq'])
            P.op('act', lambda e: e.activation(out=ssq[:], in_=ssq[:], func=AF.Sqrt, scale=1.0 / D, bias=g['eps_c'][:]),
                 reads=['ssq'], writes=['ssq'])
            P.op('dve', lambda e: e.reciprocal(out=ssq[:], in_=ssq[:]), reads=['ssq'], writes=['ssq'])
            P.op('dve', lambda e, j=j: e.tensor_scalar(out=xn[:], in0=x_res[:, j, :], scalar1=ssq[:], scalar2=None,
                                                       op0=ALU.mult), reads=['ssq', ('x_res', j)], writes=['xn'])
            P.op('pool', lambda e, j=j: e.tensor_tensor(out=x_res[:, j, :], in0=xn[:], in1=gbc[:], op=ALU.mult),
                 reads=['xn', 'gbc'], writes=[('x_res', j)])
            P.dma('sp', out_d[b, j * 128:(j + 1) * 128, :], x_res[:, j, :], reads=[('x_res', j)], writes=[('out', b, j)])
        P.barrier()
    mst.close()


def build_mixer(nc, P, cfg, b, g):
    NB, T, TC = cfg.NB, cfg.T, cfg.TC
    NT, NTT, NTC, NLT, NCH, NCC = cfg.NT, cfg.NTT, cfg.NTC, cfg.NLT, cfg.NCH, cfg.NCC
    ps, psb, PS = g['ps'], g['psb'], g['PS']
    ident_f, ident_b, ones_f, ones_nt = g['ident_f'], g['ident_b'], g['ones_f'], g['ones_nt']
    masks = (g['maskF'], g['maskB'])
    selh, modT = g['selh'], g['modT']
    x_d, ctx_d, pe_d, win_d, wout_d = g['x_d'], g['ctx_d'], g['pe_d'], g['win_d'], g['wout_d']
    cw_sb, cb_sb, mgb_sb, wg_sb, wlr_sb, gw2_sb, ggbT = (g['cw_sb'], g['cb_sb'], g['mgb_sb'], g['wg_sb'], g['wlr_sb'],
                                                         g['gw2_sb'], g['ggbT'])
    arena, x_res, gbc = g['arena'], g['x_res'], g['gbc']
    rms_scratch, xn, ssq, eps_c, one_c = g['rms_scratch'], g['xn'], g['ssq'], g['eps_c'], g['one_c']
    dbg = g['dbg']
    groups = [(t0, min(512, NT - t0)) for t0 in range(0, NT, 512)]
    HOFF = NLT * 512
    Hacc = arena[:, 0:HOFF].rearrange("p (j c) -> p j c", c=512)
    gt_tiles = [arena[0:4, HOFF + i * NT:HOFF + (i + 1) * NT] for i in range(3)]

    def S(name):
        return f"{name}_{b}"

    def win_load(dst, c0, ncol, res):
        P.dma('pool', dst, win_d[:, c0:c0 + ncol].rearrange("(k p) c -> p k c", p=128), writes=[res])

    def proj_featmajor(w, col0, dst_fn, evac):
        for gi, (t0, n) in enumerate(groups):
            pb = 2 + gi % 2
            for k in range(KC):
                P.op('pe', lambda e, k=k, pb=pb, t0=t0, n=n: e.matmul(ps[pb][:, 0:n], lhsT=w[:, k, col0:col0 + 128],
                                                                      rhs=uT[:, k, t0:t0 + n], start=(k == 0), stop=(k == KC - 1)),
                     reads=['w_cur'], writes=[PS(pb)], inc=(k == KC - 1))
            evac(pb, t0, n)

    def per_head_norm_stats(j):
        P.op('dve', lambda e: e.tensor_tensor(out=rms_scratch[:, 0:512], in0=Hacc[:, j, :], in1=Hacc[:, j, :], op=ALU.mult),
             reads=[('Hacc', j)], writes=['rms_scratch'])
        P.op('dve', lambda e: e.tensor_reduce(out=hst[:, 0:4], in_=rms_scratch[:, 0:512].rearrange("p (h c) -> p h c", c=128),
                                              axis=AX.X, op=ALU.add), reads=['rms_scratch'], writes=['hst'])
        P.op('act', lambda e: e.activation(out=hst[:, 0:4], in_=hst[:, 0:4], func=AF.Sqrt, scale=1.0 / 128, bias=eps_c[:]),
             reads=['hst'], writes=['hst'])
        P.op('dve', lambda e: e.reciprocal(out=hst[:, 0:4], in_=hst[:, 0:4]), reads=['hst'], writes=['hst'])
        P.op('dve', lambda e: e.tensor_tensor(out=rms_scratch[:, 0:512].rearrange("p (h c) -> p h c", c=128),
                                              in0=Hacc[:, j, :].rearrange("p (h c) -> p h c", c=128),
                                              in1=hst[:, 0:4].unsqueeze(2).to_broadcast([128, 4, 128]), op=ALU.mult),
             reads=['hst', ('Hacc', j)], writes=['rms_scratch'])

    st = ExitStack()
    uT = st.enter_context(nc.sbuf_tensor(S("uT"), [128, KC, NT], BF16))
    mT = uT[:].rearrange("p k n -> p (k n)")[:, 0:KC * T].rearrange("p (k t) -> p k t", t=T)
    a_tok = arena[:, HOFF:HOFF + NLT * 256].bitcast(BF16).rearrange("p (j c) -> p j c", c=512)
    AO = HOFF + NLT * 256
    khTok = arena[:, AO:AO + NTT * 128].bitcast(BF16).rearrange("p (i c) -> p i c", c=256)
    khT = arena[:, AO + NTT * 128:AO + NTT * 128 + NT // 2].bitcast(BF16)

    def g_tok(j):
        return arena[:, j * 512:j * 512 + 256].bitcast(BF16)
    hst = st.enter_context(nc.sbuf_tensor(S("hst"), [128, 8], F32))
    gnbc = st.enter_context(nc.sbuf_tensor(S("gnbc"), [128, 512], F32))
    wbuf = st.enter_context(nc.sbuf_tensor(S("wbuf"), [128, KC, 512], BF16))
    Pt = st.enter_context(nc.sbuf_tensor(S("Pt"), [128, 2, 2, 64], BF16))
    dn = st.enter_context(nc.sbuf_tensor(S("dn"), [128, 8], F32))

    with nc.sbuf_tensor(S("xt"), [128, 2, D], F32) as xt, nc.sbuf_tensor(S("pt"), [128, 2, D], F32) as pt:
        for i in range(NTT):
            bf = i % 2
            if i < NTC:
                P.dma('sp', xt[:, bf, :], ctx_d[b, i * 128:(i + 1) * 128, :], writes=[('xt', bf)])
                bsel = NB
            else:
                j = i - NTC
                P.dma('sp', xt[:, bf, :], x_d[b, j * 128:(j + 1) * 128, :], writes=[('xt', bf)])
                P.dma('sp', pt[:, bf, :], pe_d[j * 128:(j + 1) * 128, :], writes=[('pt', bf)])
                P.op('pool', lambda e, bf=bf: e.tensor_tensor(out=xt[:, bf, :], in0=xt[:, bf, :], in1=pt[:, bf, :], op=ALU.add),
                     reads=[('xt', bf), ('pt', bf)], writes=[('xt', bf)])
                bsel = b
            g['rms_to_featmajor'](xt[:, bf, :], ('xt', bf), uT, i * 128, 0, bsel, rms_scratch[:], xn[:], ssq[:], (0, 1))
        P.barrier()

    mst = ExitStack()
    qkT = mst.enter_context(nc.sbuf_tensor(S("qkT"), [128, 4, NT], BF16))
    kTok = mst.enter_context(nc.sbuf_tensor(S("kTok"), [128, NTT, 256], BF16))
    if True:
        with nc.sbuf_tensor(S("zt"), [128, NT], F32) as zt, nc.sbuf_tensor(S("ot"), [128, NT], F32) as ot, \
                nc.sbuf_tensor(S("sg"), [128, NT], F32) as sg:
            win_load(wbuf[:], 0, 512, 'w_cur')
            for qc in range(4):
                proj_featmajor(wbuf, qc * 128, None,
                               lambda pb, t0, n: P.op('act', lambda e: e.copy(out=zt[:, t0:t0 + n], in_=ps[pb][:, 0:n]),
                                                      reads=[PS(pb)], writes=['zt']))
                P.op('dve', lambda e, qc=qc: e.tensor_scalar(out=ot[:], in0=zt[:], scalar1=cw_sb[:, qc, 1:2],
                                                             scalar2=cb_sb[:, qc:qc + 1], op0=ALU.mult, op1=ALU.add),
                     reads=['zt', 'cw_sb', 'cb_sb'], writes=['ot'])
                for lo, hi in ((0, TC), (TC, NT)):
                    P.op('dve', lambda e, qc=qc, lo=lo, hi=hi: e.scalar_tensor_tensor(
                        out=ot[:, lo + 1:hi], in0=zt[:, lo:hi - 1], scalar=cw_sb[:, qc, 0:1], in1=ot[:, lo + 1:hi],
                        op0=ALU.mult, op1=ALU.add), reads=['zt', 'ot'], writes=['ot'])
                    P.op('dve', lambda e, qc=qc, lo=lo, hi=hi: e.scalar_tensor_tensor(
                        out=ot[:, lo:hi - 1], in0=zt[:, lo + 1:hi], scalar=cw_sb[:, qc, 2:3], in1=ot[:, lo:hi - 1],
                        op0=ALU.mult, op1=ALU.add), reads=['zt', 'ot'], writes=['ot'])
                P.op('act', lambda e: e.activation(out=sg[:], in_=ot[:], func=AF.Sigmoid), reads=['ot'], writes=['sg'])
                P.op('dve', lambda e, qc=qc: e.scalar_tensor_tensor(out=qkT[:, qc, :], in0=ot[:], scalar=(0.125 if qc < 2 else 1.0),
                                                                    in1=sg[:], op0=ALU.mult, op1=ALU.mult),
                     reads=['ot', 'sg'], writes=['qkT'])
            P.barrier()
        vtok = mst.enter_context(nc.sbuf_tensor(S("vtok"), [128, NTT, 4, 129], BF16))
        kp = mst.enter_context(nc.sbuf_tensor(S("kp"), [128, NTT, 256], BF16))
        edT = mst.enter_context(nc.sbuf_tensor(S("edT"), [128, NTT, 8], F32))
        wcB = mst.enter_context(nc.sbuf_tensor(S("wcB"), [128, 2, NCH], F32))
        Chat = mst.enter_context(nc.sbuf_tensor(S("Chat"), [128, 2, 129], F32))
        CbfA = mst.enter_context(nc.sbuf_tensor(S("CbfA"), [128, 2, 2, 129], BF16))
        CbfB = mst.enter_context(nc.sbuf_tensor(S("CbfB"), [128, 2, 2, 129], BF16))
        Aend = mst.enter_context(nc.sbuf_tensor(S("Aend"), [4, 3, NCH], F32))
        for i in range(NTT):
            for kc in range(2):
                P.op('pe', lambda e, i=i, kc=kc: e.transpose(out=psb[1][:, kc * 128:(kc + 1) * 128],
                                                             in_=qkT[:, 2 + kc, i * 128:(i + 1) * 128], identity=ident_b[:]),
                     reads=['ident_b'], writes=[PS(1)], inc=(kc == 1))
            P.op('dve', lambda e, i=i: e.tensor_copy(out=kTok[:, i, :], in_=psb[1][:, 0:256]), reads=[PS(1)], writes=['kTok'])
        win_load(wbuf[:], 512, 512, 'w_cur')
        P.op('pool', lambda e: e.memset(vtok[:, :, :, 128:129], 1.0), writes=['vtok'])
        for i in range(NTT):
            pb = 2 + i % 2
            for k in range(KC):
                P.op('pe', lambda e, i=i, k=k, pb=pb: e.matmul(ps[pb][:, :], lhsT=uT[:, k, i * 128:(i + 1) * 128], rhs=wbuf[:, k, :],
                                                               start=(k == 0), stop=(k == KC - 1)),
                     reads=['w_cur'], writes=[PS(pb)], inc=(k == KC - 1))
            P.op('act', lambda e, i=i, pb=pb: e.copy(out=vtok[:, i, :, 0:128], in_=ps[pb][:, :].rearrange("p (h c) -> p h c", c=128)),
                 reads=[PS(pb)], writes=['vtok'])
        P.barrier()

        DIRS = [int(c) for c in os.environ.get('KDIRS', '01')]
        for d in DIRS:
            IGt, FPt, T3 = gt_tiles
            for gi, dst in ((2 * d, IGt), (2 * d + 1, FPt)):
                for gj, (t0, n) in enumerate(groups):
                    pb = 2 + gj % 2
                    for k in range(KC):
                        P.op('pe', lambda e, k=k, pb=pb, t0=t0, n=n, gi=gi: e.matmul(
                            ps[pb][0:4, 0:n], lhsT=wg_sb[:, k, gi * 4:(gi + 1) * 4], rhs=uT[:, k, t0:t0 + n],
                            start=(k == 0), stop=(k == KC - 1)), reads=['wg_sb'], writes=[PS(pb)], inc=(k == KC - 1))
                    P.op('act', lambda e, pb=pb, t0=t0, n=n, gi=gi, dst=dst: e.activation(
                        out=dst[:, t0:t0 + n], in_=ps[pb][0:4, 0:n], func=AF.Identity, bias=mgb_sb[:, gi:gi + 1], scale=1.0),
                        reads=[PS(pb), 'mgb_sb'], writes=[('gt', gi % 2)])
            P.op('act', lambda e: e.activation(out=FPt, in_=FPt, func=AF.Exp, scale=-1.0), reads=[('gt', 1)], writes=[('gt', 1)])
            P.op('act', lambda e: e.activation(out=FPt, in_=FPt, func=AF.Ln, bias=one_c[0:4, :], scale=1.0),
                 reads=[('gt', 1)], writes=[('gt', 1)])

            def scan(out_t, d0, d1, op0, op1, rd, wr):
                if d == 0:
                    P.op('dve', lambda e: e.tensor_tensor_scan(out=out_t[:, 0:NT], data0=d0[:, 0:NT], data1=d1[:, 0:NT], initial=0.0,
                                                               op0=op0, op1=op1), reads=rd, writes=wr)
                else:
                    P.op('dve', lambda e: e.tensor_tensor_scan(out=out_t[:, 0:TC][:, ::-1], data0=d0[:, 0:TC][:, ::-1],
                                                               data1=d1[:, 0:TC][:, ::-1], initial=0.0, op0=op0, op1=op1),
                         reads=rd, writes=wr)
                    P.op('dve', lambda e: e.tensor_tensor_scan(out=out_t[:, TC:NT][:, ::-1], data0=d0[:, TC:NT][:, ::-1],
                                                               data1=d1[:, TC:NT][:, ::-1], initial=out_t[:, 0:1], op0=op0, op1=op1),
                         reads=rd + wr, writes=wr)
            scan(T3, ones_nt[0:4, :], FPt, ALU.mult, ALU.add, [('gt', 1), 'ones_nt'], [('gt', 2)])
            P.op('dve', lambda e: e.tensor_tensor(out=IGt, in0=IGt, in1=T3, op=ALU.add), reads=[('gt', 0), ('gt', 2)],
                 writes=[('gt', 0)])
            scan(FPt, IGt, IGt, ALU.max, ALU.max, [('gt', 0)], [('gt', 1)])
            endcol = 63 if d == 0 else 0
            A3 = FPt.rearrange("p (c l) -> p c l", l=64)
            P.op('dve', lambda e: e.tensor_copy(out=Aend[:, 0, :], in_=A3[:, :, endcol]), reads=[('gt', 1)], writes=['Aend'])
            P.op('pool', lambda e: e.memset(Aend[:, 1, :], 0.0), writes=['Aprev'])
            if d == 0:
                P.op('dve', lambda e: e.tensor_copy(out=Aend[:, 1, 1:NCH], in_=Aend[:, 0, 0:NCH - 1]), reads=['Aend', 'Aprev'],
                     writes=['Aprev'])
            else:
                if NCC > 1:
                    P.op('dve', lambda e: e.tensor_copy(out=Aend[:, 1, 0:NCC - 1], in_=Aend[:, 0, 1:NCC]), reads=['Aend', 'Aprev'],
                         writes=['Aprev'])
                P.op('dve', lambda e: e.tensor_copy(out=Aend[:, 1, NCC:NCH - 1], in_=Aend[:, 0, NCC + 1:NCH]),
                     reads=['Aend', 'Aprev'], writes=['Aprev'])
                P.op('dve', lambda e: e.tensor_copy(out=Aend[:, 1, NCH - 1:NCH], in_=Aend[:, 0, 0:1]), reads=['Aend', 'Aprev'],
                     writes=['Aprev'])
            P.op('dve', lambda e: e.tensor_tensor(out=Aend[:, 2, :], in0=Aend[:, 1, :], in1=Aend[:, 0, :], op=ALU.subtract),
                 reads=['Aend', 'Aprev'], writes=['wc'])
            P.op('act', lambda e: e.activation(out=Aend[:, 2, :], in_=Aend[:, 2, :], func=AF.Exp), reads=['wc'], writes=['wc'])
            for ti_, nm in ((IGt, 0), (T3, 2)):
                P.op('dve', lambda e, ti_=ti_: e.tensor_tensor(out=ti_.rearrange("p (c l) -> p c l", l=64),
                                                               in0=ti_.rearrange("p (c l) -> p c l", l=64),
                                                               in1=Aend[:, 0, :].unsqueeze(2).to_broadcast([4, NCH, 64]),
                                                               op=ALU.subtract), reads=[('gt', nm), 'Aend'], writes=[('gt', nm)])
                P.op('act', lambda e, ti_=ti_: e.activation(out=ti_, in_=ti_, func=AF.Exp), reads=[('gt', nm)], writes=[('gt', nm)])
            for i in range(NTT):
                P.op('pe', lambda e, i=i: e.matmul(ps[2][:, i * 8:i * 8 + 4], lhsT=IGt[:, i * 128:(i + 1) * 128], rhs=ident_f[0:4, 0:4],
                                                   start=True, stop=True), reads=[('gt', 0), 'ident_f'], writes=[PS(2)], inc=False)
                P.op('pe', lambda e, i=i: e.matmul(ps[2][:, i * 8 + 4:i * 8 + 8], lhsT=T3[:, i * 128:(i + 1) * 128],
                                                   rhs=ident_f[0:4, 0:4], start=True, stop=True), reads=[('gt', 2), 'ident_f'],
                     writes=[PS(2)], inc=(i == NTT - 1))
            P.op('dve', lambda e: e.tensor_copy(out=edT[:].rearrange("p i c -> p (i c)"), in_=ps[2][:, 0:NTT * 8]), reads=[PS(2)],
                 writes=['edT'])
            for pr in range(2):
                P.op('pe', lambda e, pr=pr: e.matmul(ps[3][:, pr * NCH:(pr + 1) * NCH],
                                                     lhsT=selh[0:4, pr].rearrange("p a b -> p (a b)"), rhs=Aend[:, 2, :],
                                                     start=True, stop=True), reads=['selh', 'wc'], writes=[PS(3)], inc=(pr == 1))
            P.op('dve', lambda e: e.tensor_copy(out=wcB[:].rearrange("p a c -> p (a c)"), in_=ps[3][:, 0:2 * NCH]), reads=[PS(3)],
                 writes=['wcB'])
            P.op('dve', lambda e: e.tensor_tensor(out=kp[:].rearrange("p i (h c) -> p i h c", c=64),
                                                  in0=kTok[:].rearrange("p i (h c) -> p i h c", c=64),
                                                  in1=edT[:, :, 0:4].unsqueeze(3).to_broadcast([128, NTT, 4, 64]), op=ALU.mult),
                 reads=['kTok', 'edT'], writes=['kp'])
            P.barrier()
            P.op('pool', lambda e: e.memset(Chat[:], 0.0), writes=['Chat'])
            P.op('pool', lambda e: e.memset(CbfA[:], 0.0), writes=[('CbfA', 0), ('CbfA', 1)])
            P.op('pool', lambda e: e.memset(CbfB[:], 0.0), writes=[('CbfB', 0), ('CbfB', 1)])
            order = list(range(NCH)) if d == 0 else list(range(NCC - 1, -1, -1)) + list(range(NCH - 1, NCC - 1, -1))
            mask = masks[d]
            first_dir = (d == DIRS[0])

            def ml_geo(step):
                c = order[step]
                ti, p0 = c // 2, (c % 2) * 64
                return c, ti, slice(p0, p0 + 64), slice(c * 64, (c + 1) * 64), c >= NCC, 6 + step % 2, c % 2

            def ml_pre(step):
                c, ti, rows, toks, lat, pbD, hf = ml_geo(step)
                if lat:
                    for h in range(4):
                        hp = (h % 2) * 64
                        P.op('pe', lambda e, h=h, hp=hp: e.matmul(
                            ps[2 + h % 2][rows, (h // 2) * 64:(h // 2) * 64 + 64], lhsT=qkT[hp:hp + 64, 2 + h // 2, toks],
                            rhs=qkT[hp:hp + 64, h // 2, toks], start=True, stop=True), writes=[('psh', 2 + h % 2, hf)], inc=(h >= 2))
                for h in range(4):
                    P.op('pe', lambda e, h=h: e.matmul(
                        ps[pbD][(h % 2) * 64:(h % 2) * 64 + 64, (h // 2) * 129:(h // 2 + 1) * 129],
                        lhsT=kp[rows, ti, h * 64:(h + 1) * 64], rhs=vtok[rows, ti, h, :], start=True, stop=True),
                        reads=['kp'], writes=[PS(pbD)], inc=(h == 3))
                if lat:
                    for h in range(4):
                        par, hh = h % 2, h // 2
                        P.op('dve', lambda e, h=h, par=par, hh=hh: e.scalar_tensor_tensor(
                            out=Pt[rows, par, hh, :], in0=ps[2 + par][rows, hh * 64:(hh + 1) * 64], scalar=edT[rows, ti, h:h + 1],
                            in1=mask[rows, :], op0=ALU.mult, op1=ALU.mult), reads=[('psh', 2 + par, hf), 'edT'],
                            writes=[('Pt', hf, h)])

            def ml_main(step):
                c, ti, rows, toks, lat, pbD, hf = ml_geo(step)
                for pr in range(2):
                    P.op('dve', lambda e, pr=pr: e.scalar_tensor_tensor(
                        out=Chat[:, pr, :], in0=Chat[:, pr, :], scalar=wcB[:, pr, c:c + 1], in1=ps[pbD][:, pr * 129:(pr + 1) * 129],
                        op0=ALU.mult, op1=ALU.add), reads=['Chat', 'wcB', PS(pbD)], writes=['Chat'])
                if step + 1 < len(order):
                    cn = order[step + 1]
                    for pr in range(2):
                        P.op('act', lambda e, pr=pr: e.activation(out=CbfA[0:64, (step + 1) % 2, pr, :], in_=Chat[0:64, pr, :], func=AF.Identity,
                                                                  scale=wcB[0:64, pr, cn:cn + 1]),
                             reads=['Chat', 'wcB'], writes=[('CbfA', (step + 1) % 2)])
                        P.op('act', lambda e, pr=pr: e.activation(out=CbfB[64:128, (step + 1) % 2, pr, :], in_=Chat[64:128, pr, :], func=AF.Identity,
                                                                  scale=wcB[64:128, pr, cn:cn + 1]),
                             reads=['Chat', 'wcB'], writes=[('CbfB', (step + 1) % 2)])

                if lat:
                    j = ti - NTC
                    for h in range(4):
                        par, pr = h % 2, h // 2
                        P.op('pe', lambda e, h=h, par=par, pr=pr: e.matmul(
                            ps[4 + pr][rows, par * 129:(par + 1) * 129], lhsT=Pt[rows, par, pr, :], rhs=vtok[rows, ti, h, :],
                            start=True, stop=False), reads=[('Pt', hf, h)], writes=[('psh', 4 + pr, hf)], inc=False)
                        P.op('pe', lambda e, par=par, pr=pr: e.matmul(
                            ps[4 + pr][rows, par * 129:(par + 1) * 129], lhsT=qkT[:, pr, toks],
                            rhs=(CbfA if par == 0 else CbfB)[:, step % 2, pr, :], start=False, stop=True),
                            reads=[('CbfA', step % 2), ('CbfB', step % 2)], writes=[('psh', 4 + pr, hf)], inc=True)
                    for pr in range(2):
                        den = lambda pr=pr, rows=rows: ps[4 + pr][rows, 0:258].rearrange("p (a c) -> p a c", c=129)[:, :, 128]
                        P.op('dve', lambda e, pr=pr, den=den: e.tensor_tensor(
                            out=dn[rows, 2 * pr:2 * pr + 2], in0=den(), in1=edT[rows, ti, 4 + 2 * pr:6 + 2 * pr], op=ALU.max),
                            reads=[('psh', 4 + pr, hf), 'edT'], writes=[('dn', hf)])
                        P.op('dve', lambda e, pr=pr, den=den: e.scalar_tensor_tensor(
                            out=dn[rows, 2 * pr:2 * pr + 2], in0=den(), scalar=-1.0, in1=dn[rows, 2 * pr:2 * pr + 2],
                            op0=ALU.mult, op1=ALU.max), reads=[('psh', 4 + pr, hf), ('dn', hf)], writes=[('dn', hf)])
                    P.op('dve', lambda e: e.reciprocal(out=dn[rows, 4:8], in_=dn[rows, 0:4]), reads=[('dn', hf)],
                         writes=[('rd', hf)])
                    for h in range(4):
                        src = lambda h=h, rows=rows: ps[4 + h // 2][rows, (h % 2) * 129:(h % 2) * 129 + 128]
                        if first_dir:
                            P.op('dve', lambda e, h=h, src=src: e.tensor_scalar(
                                out=Hacc[rows, j, h * 128:(h + 1) * 128], in0=src(), scalar1=dn[rows, 4 + h:5 + h], scalar2=None,
                                op0=ALU.mult), reads=[('psh', 4 + h // 2, hf), ('rd', hf)], writes=[('Hacc', j)])
                        else:
                            P.op('dve', lambda e, h=h, src=src: e.scalar_tensor_tensor(
                                out=Hacc[rows, j, h * 128:(h + 1) * 128], in0=src(), scalar=dn[rows, 4 + h:5 + h],
                                in1=Hacc[rows, j, h * 128:(h + 1) * 128], op0=ALU.mult, op1=ALU.add),
                                reads=[('psh', 4 + h // 2, hf), ('rd', hf), ('Hacc', j)], writes=[('Hacc', j)])
            ml_pre(0)
            for step in range(len(order)):
                if step + 1 < len(order):
                    ml_pre(step + 1)
                ml_main(step)
            P.barrier()
        if dbg:
            P.dma('sp', dbg['HM'][b], Hacc, writes=['dbgHM'])
            P.dma('sp', dbg['edT'][b], edT[:], writes=['dbgedT'])
            P.dma('sp', dbg['wcB'][b], wcB[:], writes=['dbgwcB'])
            P.dma('sp', dbg['qkT'][b], qkT[:], writes=['dbgqkT'])
            P.dma('sp', dbg['vtok'][b], vtok[:], writes=['dbgvtok'])
            P.dma('sp', dbg['kTok'][b], kTok[:], writes=['dbgkTok'])
            P.barrier()
        win_load(wbuf[:], 1024, 512, 'w_cur')
        P.dma('sp', gnbc[:], g['mng_d'].to_broadcast([128, 512]), writes=['gnbc'])
        def mm_front(j):
            tk = slice(TC + j * 128, TC + (j + 1) * 128)
            xh = xn[:, (j % 2) * 512:(j % 2) * 512 + 512]
            pb = 2 + j % 2
            for k in range(KC):
                P.op('pe', lambda e, k=k: e.matmul(ps[pb][:, :], lhsT=uT[:, k, tk], rhs=wbuf[:, k, :], start=(k == 0),
                                                   stop=(k == KC - 1)), reads=['w_cur'], writes=[PS(pb)], inc=(k == KC - 1))
            P.op('act', lambda e: e.activation(out=xh, in_=ps[pb][:, :], func=AF.Sigmoid), reads=[PS(pb)], writes=[('xnh', j % 2)])

        def mm_back(j):
            xh = xn[:, (j % 2) * 512:(j % 2) * 512 + 512]
            per_head_norm_stats(j)
            P.op('dve', lambda e: e.tensor_tensor(out=rms_scratch[:, 0:512], in0=rms_scratch[:, 0:512], in1=gnbc[:], op=ALU.mult),
                 reads=['rms_scratch', 'gnbc'], writes=['rms_scratch'])
            P.op('dve', lambda e: e.tensor_tensor(out=a_tok[:, j, :], in0=rms_scratch[:, 0:512], in1=xh, op=ALU.mult),
                 reads=['rms_scratch', ('xnh', j % 2)], writes=[('a_tok', j)])

        mm_front(0)
        for j in range(NLT):
            if j + 1 < NLT:
                mm_front(j + 1)
            mm_back(j)
        P.barrier()

    mst.close()
    KSTOP = os.environ.get('KSTOP', '')
    rm = ones_nt
    with nc.sbuf_tensor(S("gv"), [128, NTT, 512], BF16) as gv, \
            nc.sbuf_tensor(S("qt"), [128, 2, NT], BF16) as qt, nc.sbuf_tensor(S("kt"), [128, 2, NT], BF16) as kt, \
            nc.sbuf_tensor(S("kraw"), [128, NT], BF16) as kraw, \
            nc.sbuf_tensor(S("lrT"), [16, NT], BF16) as lrT, \
            nc.sbuf_tensor(S("tA"), [128, NT], F32) as tA, nc.sbuf_tensor(S("tB"), [128, NT], F32) as tB, \
            nc.sbuf_tensor(S("ebL"), [128, 2, NCH], F32) as ebL, \
            nc.sbuf_tensor(S("Sst"), [128, 2, 128], F32) as Sst, nc.sbuf_tensor(S("SbfA"), [128, 2, 2, 128], BF16) as SbfA, \
            nc.sbuf_tensor(S("SbfB"), [128, 2, 2, 128], BF16) as SbfB:
        win_load(wbuf[:], 2064, 512, 'w_cur')
        for i in range(NTT):
            pb = 2 + i % 2
            for k in range(KC):
                P.op('pe', lambda e, i=i, k=k, pb=pb: e.matmul(ps[pb][:, :], lhsT=uT[:, k, i * 128:(i + 1) * 128], rhs=wbuf[:, k, :],
                                                               start=(k == 0), stop=(k == KC - 1)),
                     reads=['w_cur'], writes=[PS(pb)], inc=(k == KC - 1))
            P.op('act', lambda e, i=i, pb=pb: e.copy(out=gv[:, i, :], in_=ps[pb][:, :]), reads=[PS(pb)], writes=['gv'])
        P.barrier()
        DIRS = [int(c) for c in os.environ.get('KDIRS', '01')]
        if KSTOP == 'mlstm':
            DIRS = []
        for d in DIRS:
            endcol = 63 if d == 0 else 0
            win_load(wbuf[:], 1552, 512, 'w_cur')
            P.op('pool', lambda e: e.memset(rm[:], 1.0), writes=['rm'])
            zc = 0 if d == 0 else 63
            P.op('pool', lambda e: e.memset(rm[:].rearrange("p (c l) -> p c l", l=64)[:, :, zc:zc + 1], 0.0), writes=['rm'])
            for gj, (t0, n) in enumerate(groups):
                pb = 2 + gj % 2
                for k in range(KC):
                    P.op('pe', lambda e, k=k, pb=pb, t0=t0, n=n: e.matmul(
                        ps[pb][0:16, 0:n], lhsT=wlr_sb[:, k, d * 16:(d + 1) * 16], rhs=uT[:, k, t0:t0 + n],
                        start=(k == 0), stop=(k == KC - 1)), reads=['wlr_sb'], writes=[PS(pb)], inc=(k == KC - 1))
                P.op('act', lambda e, pb=pb, t0=t0, n=n: e.copy(out=lrT[:, t0:t0 + n], in_=ps[pb][0:16, 0:n]),
                     reads=[PS(pb)], writes=['lrT'])
            for jc in range(2):
                for gj, (t0, n) in enumerate(groups):
                    pb = 2 + gj % 2
                    P.op('pe', lambda e, pb=pb, t0=t0, n=n: e.matmul(
                        ps[pb][:, 0:n], lhsT=gw2_sb[:, d, jc * 128:(jc + 1) * 128], rhs=lrT[:, t0:t0 + n], start=True, stop=True),
                        reads=['gw2_sb', 'lrT'], writes=[PS(pb)])
                    P.op('act', lambda e, pb=pb, t0=t0, n=n: e.activation(
                        out=tA[:, t0:t0 + n], in_=ps[pb][:, 0:n], func=AF.Exp, scale=-1.0, bias=ggbT[:, d, jc:jc + 1]),
                        reads=[PS(pb), 'ggbT'], writes=['tA'])
                P.op('act', lambda e: e.activation(out=tA[:], in_=tA[:], func=AF.Ln, bias=one_c[:], scale=1.0), reads=['tA'],
                     writes=['tA'])
                if d == 0:
                    P.op('dve', lambda e: e.tensor_tensor_scan(out=tB[:], data0=rm[:], data1=tA[:], initial=0.0,
                                                               op0=ALU.mult, op1=ALU.add), reads=['tA', 'rm'], writes=['tB'])
                else:
                    P.op('dve', lambda e: e.tensor_tensor_scan(out=tB[:, ::-1], data0=rm[:, ::-1], data1=tA[:, ::-1], initial=0.0,
                                                               op0=ALU.mult, op1=ALU.add), reads=['tA', 'rm'], writes=['tB'])
                bL = tB[:].rearrange("p (c l) -> p c l", l=64)[:, :, endcol]
                P.op('act', lambda e: e.activation(out=ebL[:, jc, :], in_=bL, func=AF.Exp, scale=-1.0 / 16),
                     reads=['tB'], writes=['ebL'])
                P.op('act', lambda e: e.activation(out=tA[:], in_=tB[:], func=AF.Exp, scale=-1.0 / 16), reads=['tB', 'tA'],
                     writes=['tA'])
                proj_featmajor(wbuf, jc * 128, None,
                               lambda pb, t0, n: P.op('dve', lambda e: e.scalar_tensor_tensor(
                                   out=qt[:, jc, t0:t0 + n], in0=ps[pb][:, 0:n], scalar=0.125, in1=tA[:, t0:t0 + n],
                                   op0=ALU.mult, op1=ALU.mult), reads=[PS(pb), 'tA'], writes=['qt']))
                P.op('act', lambda e: e.activation(out=tA[:], in_=tB[:], func=AF.Exp, scale=1.0 / 16), reads=['tB', 'qt', 'tA'],
                     writes=['tA'])

                def evac_k(pb, t0, n):
                    P.op('dve', lambda e: e.tensor_copy(out=kraw[:, t0:t0 + n], in_=ps[pb][:, 0:n]), reads=[PS(pb)], writes=['kraw'])
                    P.op('dve', lambda e: e.tensor_tensor(out=kt[:, jc, t0:t0 + n], in0=ps[pb][:, 0:n], in1=tA[:, t0:t0 + n],
                                                          op=ALU.mult), reads=[PS(pb), 'tA'], writes=['kt'])
                proj_featmajor(wbuf, 256 + jc * 128, None, evac_k)
                P.op('dve', lambda e: e.tensor_tensor(out=tA[:].rearrange("p (c l) -> p c l", l=64),
                                                      in0=tB[:].rearrange("p (c l) -> p c l", l=64),
                                                      in1=bL.unsqueeze(2).to_broadcast([128, NCH, 64]), op=ALU.subtract),
                     reads=['tB', 'kt', 'tA'], writes=['tA'])
                P.op('act', lambda e: e.activation(out=tA[:], in_=tA[:], func=AF.Exp, scale=1.0 / 16), reads=['tA'], writes=['tA'])
                P.op('dve', lambda e: e.tensor_tensor(out=khT, in0=kraw[:], in1=tA[:], op=ALU.mult),
                     reads=['tA', 'kraw'], writes=['khT'])
                for i in range(NTT):
                    P.op('pe', lambda e, i=i: e.transpose(out=psb[1][:, 0:128], in_=khT[:, i * 128:(i + 1) * 128], identity=ident_b[:]),
                         reads=['khT', 'ident_b'], writes=[PS(1)])
                    P.op('dve', lambda e, i=i: e.tensor_copy(out=khTok[:, i, jc * 128:(jc + 1) * 128], in_=psb[1][:, 0:128]),
                         reads=[PS(1)], writes=['khTok'])
            P.barrier()
            P.op('pool', lambda e: e.memset(Sst[:], 0.0), writes=['Sst'])
            P.op('pool', lambda e: e.memset(SbfA[:], 0.0), writes=[('SbfA', 0), ('SbfA', 1)])
            P.op('pool', lambda e: e.memset(SbfB[:], 0.0), writes=[('SbfB', 0), ('SbfB', 1)])
            order = list(range(NCH)) if d == 0 else list(range(NCC - 1, -1, -1)) + list(range(NCH - 1, NCC - 1, -1))
            mask = masks[d]
            first_dir = (d == DIRS[0])

            def gl_geo(step):
                c = order[step]
                ti, p0 = c // 2, (c % 2) * 64
                return c, ti, slice(p0, p0 + 64), slice(c * 64, (c + 1) * 64), c >= NCC, 6 + step % 2, c % 2

            def gl_pre(step):
                c, ti, rows, toks, lat, pbD, hf = gl_geo(step)
                if lat:
                    for h in range(4):
                        hp = (h % 2) * 64
                        P.op('pe', lambda e, h=h, hp=hp: e.matmul(
                            ps[2 + h % 2][rows, (h // 2) * 64:(h // 2) * 64 + 64], lhsT=kt[hp:hp + 64, h // 2, toks],
                            rhs=qt[hp:hp + 64, h // 2, toks], start=True, stop=True), writes=[('psh', 2 + h % 2, hf)], inc=(h >= 2))
                for h in range(4):
                    P.op('pe', lambda e, h=h: e.matmul(
                        ps[pbD][(h % 2) * 64:(h % 2) * 64 + 64, (h // 2) * 128:(h // 2 + 1) * 128],
                        lhsT=khTok[rows, ti, h * 64:(h + 1) * 64], rhs=gv[rows, ti, h * 128:(h + 1) * 128], start=True, stop=True),
                        writes=[PS(pbD)], inc=(h == 3))
                if lat:
                    for par in range(2):
                        P.op('dve', lambda e, par=par: e.tensor_tensor(
                            out=Pt[rows, par, :, :], in0=ps[2 + par][rows, 0:128].rearrange("p (a c) -> p a c", c=64),
                            in1=mask[rows, :].unsqueeze(1).to_broadcast([64, 2, 64]), op=ALU.mult),
                            reads=[('psh', 2 + par, hf)], writes=[('Pt', hf, par)])

            def gl_main(step):
                c, ti, rows, toks, lat, pbD, hf = gl_geo(step)
                for pr in range(2):
                    P.op('dve', lambda e, pr=pr: e.scalar_tensor_tensor(
                        out=Sst[:, pr, :], in0=Sst[:, pr, :], scalar=ebL[:, pr, c:c + 1], in1=ps[pbD][:, pr * 128:(pr + 1) * 128],
                        op0=ALU.mult, op1=ALU.add), reads=['Sst', 'ebL', PS(pbD)], writes=['Sst'])
                P.op('act', lambda e: e.copy(out=SbfA[0:64, (step + 1) % 2], in_=Sst[0:64]), reads=['Sst'], writes=[('SbfA', (step + 1) % 2)])
                P.op('act', lambda e: e.copy(out=SbfB[64:128, (step + 1) % 2], in_=Sst[64:128]), reads=['Sst'], writes=[('SbfB', (step + 1) % 2)])

                if lat:
                    j = ti - NTC
                    for h in range(4):
                        par, pr = h % 2, h // 2
                        P.op('pe', lambda e, h=h, par=par, pr=pr: e.matmul(
                            ps[4][rows, h * 128:(h + 1) * 128], lhsT=Pt[rows, par, pr, :], rhs=gv[rows, ti, h * 128:(h + 1) * 128],
                            start=True, stop=False), reads=[('Pt', hf, par)], writes=[('psh', 4, hf)], inc=False)
                        P.op('pe', lambda e, h=h, par=par, pr=pr: e.matmul(
                            ps[4][rows, h * 128:(h + 1) * 128], lhsT=qt[:, pr, toks], rhs=(SbfA if par == 0 else SbfB)[:, step % 2, pr, :],
                            start=False, stop=True), reads=[('SbfA', step % 2), ('SbfB', step % 2)], writes=[('psh', 4, hf)], inc=(h == 3))
                    if first_dir:
                        P.op('act', lambda e: e.copy(out=Hacc[rows, j, :], in_=ps[4][rows, :]), reads=[('psh', 4, hf)],
                             writes=[('Hacc', j)])
                    else:
                        P.op('act', lambda e: e.copy(out=rms_scratch[rows, 0:512], in_=ps[4][rows, :]), reads=[('psh', 4, hf)],
                             writes=[('otmp', hf)])
                        P.op('pool', lambda e: e.tensor_tensor(out=Hacc[rows, j, :], in0=Hacc[rows, j, :],
                                                               in1=rms_scratch[rows, 0:512], op=ALU.add),
                             reads=[('otmp', hf), ('Hacc', j)], writes=[('Hacc', j)])
            gl_pre(0)
            for step in range(len(order)):
                if step + 1 < len(order):
                    gl_pre(step + 1)
                gl_main(step)
            P.barrier()
            if dbg and d == DIRS[0]:
                P.dma('sp', dbg['H1'][b], Hacc, writes=['dbgH1'])
                P.barrier()
        if dbg:
            P.dma('sp', dbg['H'][b], Hacc, writes=['dbgH'])
            P.dma('sp', dbg['qt'][b], qt[:], writes=['dbgqt'])
            P.dma('sp', dbg['kt'][b], kt[:], writes=['dbgkt'])
            P.dma('sp', dbg['gv'][b], gv[:], writes=['dbggv'])
            P.dma('sp', dbg['khTok'][b], khTok, writes=['dbgkh'])
            P.dma('sp', dbg['ebL'][b], ebL[:], writes=['dbgebl'])
            P.barrier()
        P.op('pool', lambda e: e.memset(ones_nt[:], 1.0), writes=['rm'])
        win_load(wbuf[:], 2576, 512, 'w_cur')
        P.dma('sp', gnbc[:], g['gng_d'].to_broadcast([128, 512]), writes=['gnbc'])

        def gm_front(j):
            tk = slice(TC + j * 128, TC + (j + 1) * 128)
            xh = xn[:, (j % 2) * 512:(j % 2) * 512 + 512]
            pb = 2 + j % 2
            for k in range(KC):
                P.op('pe', lambda e, k=k: e.matmul(ps[pb][:, :], lhsT=uT[:, k, tk], rhs=wbuf[:, k, :], start=(k == 0),
                                                   stop=(k == KC - 1)), reads=['w_cur'], writes=[PS(pb)], inc=(k == KC - 1))
            P.op('act', lambda e: e.activation(out=xh, in_=ps[pb][:, :], func=AF.Sigmoid), reads=[PS(pb)], writes=[('xnh', j % 2)])
            P.op('dve', lambda e: e.tensor_tensor(out=xh, in0=ps[pb][:, :], in1=xh, op=ALU.mult),
                 reads=[PS(pb), ('xnh', j % 2)], writes=[('xnh', j % 2)])

        def gm_back(j):
            xh = xn[:, (j % 2) * 512:(j % 2) * 512 + 512]
            per_head_norm_stats(j)
            P.op('dve', lambda e: e.tensor_tensor(out=rms_scratch[:, 0:512], in0=rms_scratch[:, 0:512], in1=gnbc[:], op=ALU.mult),
                 reads=['rms_scratch', 'gnbc'], writes=['rms_scratch'])
            P.op('dve', lambda e: e.tensor_tensor(out=g_tok(j), in0=rms_scratch[:, 0:512], in1=xh, op=ALU.mult),
                 reads=['rms_scratch', ('xnh', j % 2), ('Hacc', j)], writes=[('g_tok', j)])

        gm_front(0)
        for j in range(NLT):
            if j + 1 < NLT:
                gm_front(j + 1)
            gm_back(j)
        P.barrier()

    if dbg:
        P.dma('sp', dbg['uT'][b], uT[:], writes=['dbg_uT'])
        P.barrier()
    for j in range(NLT if KSTOP == '' else 0):
        for src_i, src in enumerate((a_tok[:, j, :], g_tok(j))):
            pb = src_i
            for q in range(4):
                P.op('pe', lambda e, q=q, src=src, pb=pb: e.transpose(out=psb[pb][:, q * 128:(q + 1) * 128],
                                                                      in_=src[:, q * 128:(q + 1) * 128], identity=ident_b[:]),
                     reads=['ident_b'], writes=[PS(pb)], inc=(q == 3))
            P.op('act' if src_i == 0 else 'dve',
                 (lambda e, j=j, pb=pb, src_i=src_i: e.copy(out=mT[:, 4 * src_i:4 * src_i + 4, j * 128:(j + 1) * 128],
                                                            in_=psb[pb][:, 0:512].rearrange("p (q c) -> p q c", c=128)))
                 if src_i == 0 else
                 (lambda e, j=j, pb=pb, src_i=src_i: e.tensor_copy(out=mT[:, 4 * src_i:4 * src_i + 4, j * 128:(j + 1) * 128],
                                                                   in_=psb[pb][:, 0:512].rearrange("p (q c) -> p q c", c=128))),
                 reads=[PS(pb)], writes=[('mT', j, src_i)])
    P.barrier()
    if dbg:
        P.dma('sp', dbg['mT'][b], mT, writes=['dbg_mT'])
        P.barrier()
    with nc.sbuf_tensor(S("wout"), [128, KC, D], BF16) as wout, nc.sbuf_tensor(S("xt2"), [128, 2, D], F32) as xt2, \
            nc.sbuf_tensor(S("pt2"), [128, 2, D], F32) as pt2:
        for half in range(2):
            P.dma('pool', wout[:, :, half * 512:(half + 1) * 512],
                  wout_d[:, half * 512:(half + 1) * 512].rearrange("(k p) c -> p k c", p=128), writes=['wout'])
        g['make_gbc'](b, 0)
        for j in range(NLT):
            bf = j % 2
            P.dma('sp', x_res[:, j, :], x_d[b, j * 128:(j + 1) * 128, :], writes=[('x_res', j)])
            P.dma('sp', pt2[:, bf, :], pe_d[j * 128:(j + 1) * 128, :], writes=[('pt2', bf)])
            P.op('pool', lambda e, j=j, bf=bf: e.tensor_tensor(out=x_res[:, j, :], in0=x_res[:, j, :], in1=pt2[:, bf, :], op=ALU.add),
                 reads=[('x_res', j), ('pt2', bf)], writes=[('x_res', j)])
            for half in range(2):
                pb = 2 + half
                hs = slice(half * 512, (half + 1) * 512)
                for k in range(KC):
                    P.op('pe', lambda e, k=k, j=j, pb=pb, hs=hs: e.matmul(ps[pb][:, :], lhsT=mT[:, k, j * 128:(j + 1) * 128],
                                                                          rhs=wout[:, k, hs], start=(k == 0), stop=(k == KC - 1)),
                         reads=['wout'], writes=[PS(pb)], inc=(k == KC - 1))
                P.op('dve', lambda e, pb=pb, hs=hs, bf=bf: e.tensor_tensor(out=xt2[:, bf, hs], in0=ps[pb][:, :], in1=gbc[:, hs],
                                                                           op=ALU.mult), reads=[PS(pb), 'gbc'], writes=[('xt2', bf, half)])
                if dbg:
                    pass
                P.op('pool', lambda e, j=j, hs=hs, bf=bf: e.tensor_tensor(out=x_res[:, j, hs], in0=x_res[:, j, hs], in1=xt2[:, bf, hs],
                                                                          op=ALU.add), reads=[('xt2', bf, half), ('x_res', j)],
                     writes=[('x_res', j)])
        P.barrier()
    st.close()


_CACHE = {}


def _get_nc(cfg_key):
    if cfg_key not in _CACHE:
        _CACHE[cfg_key] = build(Cfg(*cfg_key))
    return _CACHE[cfg_key]


def make_in_maps(inputs, n_cores, NB):
    f = lambda a: np.ascontiguousarray(np.asarray(a, dtype=np.float32))
    shared = {
        "c_ctx": f(inputs["c_ctx"]).reshape(1, D),
        "ada_w": f(inputs["ada_w"])[0], "ada_b": f(inputs["ada_b"]).reshape(1, -1),
        "norm1_g": f(inputs["norm1_g"]).reshape(1, D), "w_in": f(inputs["w_in"])[0],
        "ml_conv_w": f(inputs["ml_conv_w"])[0], "ml_conv_b": f(inputs["ml_conv_b"]).reshape(1, -1),
        "ml_gate_b": f(inputs["ml_gate_b"])[0], "ml_norm_g": f(inputs["ml_norm_g"]).reshape(1, -1),
        "gla_gate_w2": f(inputs["gla_gate_w2"])[0], "gla_gate_b": f(inputs["gla_gate_b"])[0],
        "gla_norm_g": f(inputs["gla_norm_g"]).reshape(1, -1), "w_out": f(inputs["w_out"])[0],
        "norm2_g": f(inputs["norm2_g"]).reshape(1, D), "router_w": f(inputs["router_w"])[0],
        "router_b": f(inputs["router_b"]).reshape(1, -1), "moe_w_gu": f(inputs["moe_w_gu"])[0],
        "moe_b_gu": f(inputs["moe_b_gu"])[0], "moe_w_down": f(inputs["moe_w_down"])[0],
        "moe_b_down": f(inputs["moe_b_down"])[0], "final_norm_g": f(inputs["final_norm_g"]).reshape(1, D),
    }
    x, c, ctx = f(inputs["x"]), f(inputs["c"]), f(inputs["ctx"])
    maps = []
    for i in range(n_cores):
        m = dict(shared)
        m["x"] = x[i * NB:(i + 1) * NB]
        m["c"] = c[i * NB:(i + 1) * NB]
        m["ctx"] = ctx[i * NB:(i + 1) * NB]
        maps.append(m)
    return maps


def kernel(**inputs):
    n_cores = 8
    B = inputs["x"].shape[0]
    NB = B // n_cores
    T = inputs["x"].shape[1]
    TC = inputs["ctx"].shape[1]
    E = inputs["router_w"].shape[-1]
    nc = _get_nc((NB, T, TC, E, False, 99))
    maps = make_in_maps(inputs, n_cores, NB)
    res = run_bass_kernel_spmd(nc, maps, core_ids=list(range(n_cores)))
    return np.concatenate([r["out"] for r in res.results], axis=0)
```

```python
import math
import os
import types
import numpy as np
from contextlib import ExitStack
import concourse.bass as bass
import concourse.mybir as mybir
from concourse.bass_utils import run_bass_kernel_spmd

F32 = mybir.dt.float32
BF16 = mybir.dt.bfloat16
AF = mybir.ActivationFunctionType
ALU = mybir.AluOpType
AX = mybir.AxisListType

D = 1024
KC = 8
EPS = 1e-6
LIM = 7.0
ALPHA = 1.702


class Cfg:
    def __init__(self, NB=4, T=2048, TC=256, E=32, debug=False, stages=99):
        self.NB, self.T, self.TC, self.E = NB, T, TC, E
        self.NT = T + TC
        self.NTT = self.NT // 128
        self.NTC = TC // 128
        self.NLT = T // 128
        self.NCH = self.NT // 64
        self.NCC = TC // 64
        self.debug = debug
        self.stages = stages


def _snapshot(fn):
    if fn is None or fn.__closure__ is None:
        return fn
    cells = []
    for c in fn.__closure__:
        try:
            cells.append(types.CellType(c.cell_contents))
        except ValueError:
            cells.append(c)
    return types.FunctionType(fn.__code__, fn.__globals__, fn.__name__, fn.__defaults__, tuple(cells))


class Prog:
    ENGS = ('pe', 'act', 'dve', 'pool', 'sp')

    def __init__(self, nc, st):
        self.nc = nc
        self.sem = {e: st.enter_context(nc.semaphore('sem_' + e)) for e in self.ENGS}
        self.cnt = dict.fromkeys(self.ENGS, 0)
        self.seen = {e: {} for e in self.ENGS}
        self.streams = {e: [] for e in self.ENGS}
        self.res = {}
        self.pend = {e: [] for e in self.ENGS}
        self.dsem, self.dcnt, self.drr = {}, {}, {}
        for q, n in (('sp', 14), ('pool', 10), ('act', 4)):
            self.dsem[q] = [st.enter_context(nc.semaphore(f'dma_{q}{i}')) for i in range(n)]
            self.dcnt[q] = [0] * n
            self.drr[q] = 0
        self.all_dma_events = []

    def _need(self, eng, ev, waits, raw):
        key, sem, val = ev
        if key == eng and not raw and eng == 'pe':
            return
        if val is None:
            raise RuntimeError(f'pending event consumed: {key} by {eng}')
        if self.seen[eng].get(key, 0) >= val:
            return
        if key in waits and waits[key][2] >= val:
            return
        waits[key] = (key, sem, val)

    def _deps(self, eng, reads, writes):
        waits = {}
        for r in reads:
            s = self.res.get(r)
            if s and s[0] is not None:
                self._need(eng, s[0], waits, True)
        for w in writes:
            s = self.res.get(w)
            if s:
                if s[0] is not None:
                    self._need(eng, s[0], waits, False)
                for ev in s[1].values():
                    self._need(eng, ev, waits, False)
        wl = list(waits.values())
        for key, sem, val in wl:
            self.seen[eng][key] = max(self.seen[eng].get(key, 0), val)
        return wl

    def _register(self, ev, reads, writes):
        for r in reads:
            s = self.res.setdefault(r, [None, {}])
            s[1][ev[0]] = ev
        for w in writes:
            self.res[w] = [ev, {}]

    def op(self, eng, fn, reads=(), writes=(), inc=True):
        fn = _snapshot(fn)
        wl = self._deps(eng, reads, writes)
        if inc:
            self.cnt[eng] += 1
            ev = [eng, self.sem[eng], self.cnt[eng]]
            for p in self.pend[eng]:
                p[2] = self.cnt[eng]
            self.pend[eng] = []
        else:
            ev = [eng, self.sem[eng], None]
            self.pend[eng].append(ev)
        self._register(ev, reads, writes)
        self.streams[eng].append((wl, fn, 'inc' if inc else None))

    def dma(self, q, out, in_, reads=(), writes=(), **kw):
        k = self.drr[q]
        self.drr[q] = (k + 1) % len(self.dsem[q])
        sem = self.dsem[q][k]
        key = ('d', q, k)
        wl = self._deps(q, reads, writes)
        prev = self.dcnt[q][k]
        if prev > 0 and self.seen[q].get(key, 0) < 16 * prev:
            wl.append((key, sem, 16 * prev))
            self.seen[q][key] = 16 * prev
        self.dcnt[q][k] += 1
        ev = [key, sem, 16 * self.dcnt[q][k]]
        self._register(ev, reads, writes)
        self.all_dma_events.append(ev)

        def fn(e, out=out, in_=in_, kw=kw, sem=sem):
            e.dma_start(out=out, in_=in_, **kw).then_inc(sem, 16)
        self.streams[q].append((wl, fn, 'dma'))

    def barrier(self):
        for e in self.ENGS:
            wl = []
            for o in self.ENGS:
                if self.cnt[o] > self.seen[e].get(o, 0):
                    if self.pend[o]:
                        raise RuntimeError('barrier with pending non-inc ops on ' + o)
                    wl.append((o, self.sem[o], self.cnt[o]))
                    self.seen[e][o] = self.cnt[o]
            for q in self.dsem:
                for k, c in enumerate(self.dcnt[q]):
                    key = ('d', q, k)
                    if c > 0 and self.seen[e].get(key, 0) < 16 * c:
                        wl.append((key, self.dsem[q][k], 16 * c))
                        self.seen[e][key] = 16 * c
            if wl:
                self.streams[e].append((wl, None, None))
        self.res = {}

    def emit(self, block):
        decos = {'pe': block.tensor, 'act': block.scalar, 'dve': block.vector,
                 'pool': block.gpsimd, 'sp': block.sync}
        for name in self.ENGS:
            stream = self.streams[name]
            sem_e = self.sem[name]

            def body(e, stream=stream, sem_e=sem_e):
                for wl, fn, kind in stream:
                    for key, sem, val in wl:
                        e.wait_ge(sem, val)
                    if fn is None:
                        continue
                    ins = fn(e)
                    if kind == 'inc':
                        ins.then_inc(sem_e, 1)
            decos[name](body)


def build(cfg):
    NB, T, TC, E = cfg.NB, cfg.T, cfg.TC, cfg.E
    NT, NTT, NTC, NLT, NCH, NCC = cfg.NT, cfg.NTT, cfg.NTC, cfg.NLT, cfg.NCH, cfg.NCC
    NBC = NB + 1
    nc = bass.Bass("TRN2", target_bir_lowering=False)

    def din(name, shape):
        return nc.dram_tensor(name, list(shape), F32, kind="ExternalInput").ap()

    x_d = din("x", [NB, T, D])
    c_d = din("c", [NB, D])
    ctx_d = din("ctx", [NB, TC, D])
    cctx_d = din("c_ctx", [1, D])
    adaw_d = din("ada_w", [D, 6 * D])
    adab_d = din("ada_b", [1, 6 * D])
    n1g_d = din("norm1_g", [1, D])
    win_d = din("w_in", [D, 3120])
    cw_d = din("ml_conv_w", [3, 512])
    cb_d = din("ml_conv_b", [1, 512])
    mgb_d = din("ml_gate_b", [4, 4])
    mng_d = din("ml_norm_g", [1, 512])
    gw2_d = din("gla_gate_w2", [2, 16, 256])
    ggb_d = din("gla_gate_b", [2, 256])
    gng_d = din("gla_norm_g", [1, 512])
    wout_d = din("w_out", [D, D])
    n2g_d = din("norm2_g", [1, D])
    rw_d = din("router_w", [D, E])
    rb_d = din("router_b", [1, E])
    wgu_d = din("moe_w_gu", [E, D, 2 * D])
    bgu_d = din("moe_b_gu", [E, 2 * D])
    wdn_d = din("moe_w_down", [E, D, D])
    bdn_d = din("moe_b_down", [E, D])
    fng_d = din("final_norm_g", [1, D])
    out_d = nc.dram_tensor("out", [NB, T, D], F32, kind="ExternalOutput").ap()
    pe_d = nc.dram_tensor("pe_scratch", [T, D], F32, kind="Internal").ap()
    gsc_d = nc.dram_tensor("gate_scratch", [NBC, 2, D], F32, kind="Internal").ap()
    dbg = {}
    if cfg.debug:
        dbg['xmid'] = nc.dram_tensor("dbg_xmid", [NB, T, D], F32, kind="ExternalOutput").ap()
        dbg['mT'] = nc.dram_tensor("dbg_mT", [NB, 128, KC, T], BF16, kind="ExternalOutput").ap()
        dbg['uT'] = nc.dram_tensor("dbg_uT", [NB, 128, KC, NT], BF16, kind="ExternalOutput").ap()
        dbg['H'] = nc.dram_tensor("dbg_H", [NB, 128, NLT, 512], F32, kind="ExternalOutput").ap()
        dbg['H1'] = nc.dram_tensor("dbg_H1", [NB, 128, NLT, 512], F32, kind="ExternalOutput").ap()
        dbg['HM'] = nc.dram_tensor("dbg_HM", [NB, 128, NLT, 512], F32, kind="ExternalOutput").ap()
        dbg['edT'] = nc.dram_tensor("dbg_edT", [NB, 128, NTT, 8], F32, kind="ExternalOutput").ap()
        dbg['wcB'] = nc.dram_tensor("dbg_wcB", [NB, 128, 2, NCH], F32, kind="ExternalOutput").ap()
        dbg['qkT'] = nc.dram_tensor("dbg_qkT", [NB, 128, 4, NT], BF16, kind="ExternalOutput").ap()
        dbg['vtok'] = nc.dram_tensor("dbg_vtok", [NB, 128, NTT, 4, 129], BF16, kind="ExternalOutput").ap()
        dbg['kTok'] = nc.dram_tensor("dbg_kTok", [NB, 128, NTT, 256], BF16, kind="ExternalOutput").ap()
        dbg['qt'] = nc.dram_tensor("dbg_qt", [NB, 128, 2, NT], BF16, kind="ExternalOutput").ap()
        dbg['kt'] = nc.dram_tensor("dbg_kt", [NB, 128, 2, NT], BF16, kind="ExternalOutput").ap()
        dbg['gv'] = nc.dram_tensor("dbg_gv", [NB, 128, NTT, 512], BF16, kind="ExternalOutput").ap()
        dbg['khTok'] = nc.dram_tensor("dbg_khTok", [NB, 128, NTT, 256], BF16, kind="ExternalOutput").ap()
        dbg['ebL'] = nc.dram_tensor("dbg_ebL", [NB, 128, 2, NCH], F32, kind="ExternalOutput").ap()

    st = ExitStack()
    P = Prog(nc, st)

    def sb(name, shape, dt=F32):
        return st.enter_context(nc.sbuf_tensor(name, list(shape), dt))

    ps = [st.enter_context(nc.psum_tensor(f"ps{i}", [128, 512], F32)) for i in range(8)]
    psb = [p[:].bitcast(BF16) for p in ps]

    def PS(i):
        return ('ps', i)

    ident_f = sb("ident_f", [128, 128])
    ident_b = sb("ident_b", [128, 128], BF16)
    ones_f = sb("ones_f", [128, 128])
    ones_nt = sb("ones_nt", [128, NT], BF16)
    maskF = sb("maskF", [128, 64])
    maskB = sb("maskB", [128, 64])
    selh = sb("selh", [4, 2, 2, 64])
    eps_c = sb("eps_c", [128, 1])
    one_c = sb("one_c", [128, 1])
    P.op('pool', lambda e: e.memset(one_c[:], 1.0), writes=['one_c'])
    P.op('pool', lambda e: e.memset(eps_c[:], EPS), writes=['eps_c'])
    P.op('pool', lambda e: e.memset(ones_f[:], 1.0), writes=['ones_f'])
    P.op('pool', lambda e: e.memset(ones_nt[:], 1.0), writes=['ones_nt'])
    P.op('pool', lambda e: e.affine_select(out=ident_f[:], in_=ones_f[:], pattern=[[-1, 128]],
                                           compare_op=ALU.is_equal, fill=0.0, base=0, channel_multiplier=1),
         reads=['ones_f'], writes=['ident_f'])
    P.op('dve', lambda e: e.tensor_copy(out=ident_b[:], in_=ident_f[:]), reads=['ident_f'], writes=['ident_b'])
    for half in range(2):
        sl = slice(half * 64, half * 64 + 64)
        P.op('pool', lambda e, sl=sl: e.affine_select(out=maskF[sl, :], in_=ones_f[sl, 0:64], pattern=[[1, 64]],
                                                      compare_op=ALU.is_ge, fill=0.0, base=0, channel_multiplier=-1),
             reads=['ones_f'], writes=['maskF'])
        P.op('pool', lambda e, sl=sl: e.affine_select(out=maskB[sl, :], in_=ones_f[sl, 0:64], pattern=[[-1, 64]],
                                                      compare_op=ALU.is_ge, fill=0.0, base=0, channel_multiplier=1),
             reads=['ones_f'], writes=['maskB'])
    P.op('pool', lambda e: e.affine_select(out=selh[:].rearrange("p a b c -> p (a b c)"), in_=ones_nt[0:4, 0:256],
                                           pattern=[[-2, 2], [-1, 2], [0, 64]], compare_op=ALU.is_equal, fill=0.0, base=0,
                                           channel_multiplier=1),
         reads=['ones_nt'], writes=['selh'])

    modT = sb("modT", [128, 4, KC, NBC])
    rw_sb = sb("rw_sb", [128, KC, E])
    rb_bc = sb("rb_bc", [128, E])
    bguT = sb("bguT", [128, 16, E])
    cw_sb = sb("cw_sb", [128, 4, 3])
    cb_sb = sb("cb_sb", [128, 4])
    mgb_sb = sb("mgb_sb", [4, 4])
    wg_sb = sb("wg_sb", [128, KC, 16], BF16)
    wlr_sb = sb("wlr_sb", [128, KC, 32], BF16)
    gw2_sb = sb("gw2_sb", [16, 2, 256], BF16)
    ggbT = sb("ggbT", [128, 2, 2])
    nc_allow = nc.allow_non_contiguous_dma(reason="tiny param layouts")
    st.enter_context(nc_allow)

    P.dma('sp', rw_sb[:], rw_d.rearrange("(k p) e -> p k e", p=128), writes=['rw_sb'])
    P.dma('sp', rb_bc[:], rb_d.to_broadcast([128, E]), writes=['rb_bc'])
    for q in range(4):
        P.dma('sp', cw_sb[:, q, :], cw_d[:, q * 128:(q + 1) * 128].rearrange("i p -> p i"), writes=['cw_sb'])
    P.dma('sp', cb_sb[:], cb_d.rearrange("o (q p) -> p (o q)", p=128), writes=['cb_sb'])
    P.dma('sp', mgb_sb[:], mgb_d.rearrange("g h -> h g"), writes=['mgb_sb'])
    P.dma('sp', ggbT[:], ggb_d.rearrange("z (j p) -> p z j", p=128), writes=['ggbT'])
    P.dma('pool', wg_sb[:], win_d[:, 1536:1552].rearrange("(k p) c -> p k c", p=128), writes=['wg_sb'])
    P.dma('pool', wlr_sb[:], win_d[:, 3088:3120].rearrange("(k p) c -> p k c", p=128), writes=['wlr_sb'])
    P.dma('pool', gw2_sb[:], gw2_d.rearrange("z r c -> r z c"), writes=['gw2_sb'])
    with nc.sbuf_tensor("bgu_rows", [E, 2 * D], F32) as bgu_rows:
        P.dma('sp', bgu_rows[:], bgu_d, writes=['bgu_rows'])
        for j in range(16):
            P.op('pe', lambda e, j=j: e.transpose(out=ps[0][:, j * E:(j + 1) * E], in_=bgu_rows[:, j * 128:(j + 1) * 128],
                                                  identity=ident_f[0:E, 0:E]), reads=['bgu_rows', 'ident_f'], writes=[PS(0)],
                 inc=(j == 15))
        P.op('dve', lambda e: e.tensor_copy(out=bguT[:].rearrange("p j e -> p (j e)"), in_=ps[0][:, 0:16 * E]),
             reads=[PS(0)], writes=['bguT'])
        P.op('dve', lambda e: e.tensor_scalar(out=bguT[:, 8:16, :], in0=bguT[:, 8:16, :], scalar1=1.0, scalar2=None, op0=ALU.add),
             reads=['bguT'], writes=['bguT'])
        P.barrier()
    P.op('dve', lambda e: e.tensor_scalar(out=ggbT[:], in0=ggbT[:], scalar1=-1.0, scalar2=None, op0=ALU.mult),
         reads=['ggbT'], writes=['ggbT'])

    with nc.sbuf_tensor("om", [128, 256], F32) as om, nc.sbuf_tensor("jf", [128, 256], F32) as jf, \
            nc.sbuf_tensor("pidx", [128, 2], F32) as pidx, nc.sbuf_tensor("arg", [128, 256], F32) as arg, \
            nc.sbuf_tensor("petile", [128, 2, D], F32) as petile, nc.sbuf_tensor("omr0", [128, 256], F32) as omr0, \
            nc.sbuf_tensor("argc", [128, 256], F32) as argc, nc.sbuf_tensor("argr", [128, 256], F32) as argr, \
            nc.sbuf_tensor("arg2", [128, 256], F32) as arg2:
        P.op('pool', lambda e: e.iota(out=jf[:], pattern=[[1, 256]], base=0, channel_multiplier=0,
                                      allow_small_or_imprecise_dtypes=True), writes=['jf'])
        P.op('act', lambda e: e.activation(out=om[:], in_=jf[:], func=AF.Exp, scale=-math.log(10000.0) / 256.0),
             reads=['jf'], writes=['om'])
        for half in range(2):
            sl = slice(half * 64, half * 64 + 64)
            P.op('pool', lambda e, sl=sl: e.iota(out=pidx[sl, 0:1], pattern=[[0, 1]], base=0, channel_multiplier=1,
                                                 allow_small_or_imprecise_dtypes=True), writes=['pidx'])
            P.op('pool', lambda e, sl=sl, half=half: e.memset(pidx[sl, 1:2], float(half)), writes=['pidx'])
        PI = math.pi

        def sincos(dst_sin, dst_cos, argap, rd):
            MAGIC = 12582912.0
            for dst, off in ((dst_sin, 0.0), (dst_cos, 0.5 * PI)):
                P.op('dve', lambda e, off=off: e.tensor_scalar(out=arg2[:], in0=argap, scalar1=off, scalar2=None, op0=ALU.add),
                     reads=rd, writes=['arg2'])
                P.op('dve', lambda e: e.tensor_scalar(out=arg[:], in0=arg2[:], scalar1=1.0 / (2 * PI), scalar2=MAGIC,
                                                      op0=ALU.mult, op1=ALU.add), reads=['arg2'], writes=['arg'])
                P.op('dve', lambda e: e.tensor_scalar(out=arg[:], in0=arg[:], scalar1=-MAGIC, scalar2=None, op0=ALU.add),
                     reads=['arg'], writes=['arg'])
                P.op('dve', lambda e: e.scalar_tensor_tensor(out=arg[:], in0=arg[:], scalar=-2 * PI, in1=arg2[:],
                                                             op0=ALU.mult, op1=ALU.add), reads=['arg', 'arg2'], writes=['arg'])
                P.op('dve', lambda e: e.tensor_scalar(out=arg[:], in0=arg[:], scalar1=-PI, scalar2=PI, op0=ALU.max, op1=ALU.min),
                     reads=['arg'], writes=['arg'])
                P.op('act', lambda e, dst=dst: e.activation(out=dst, in_=arg[:], func=AF.Sin), reads=['arg'],
                     writes=['petile'])
        P.op('dve', lambda e: e.tensor_scalar(out=omr0[:], in0=om[:], scalar1=pidx[:, 1:2], scalar2=None, op0=ALU.mult),
             reads=['om', 'pidx'], writes=['omr0'])
        P.op('dve', lambda e: e.tensor_scalar(out=argc[:], in0=om[:], scalar1=pidx[:, 0:1], scalar2=None, op0=ALU.mult),
             reads=['om', 'pidx'], writes=['argc'])
        for k in range(NLT):
            buf = k % 2
            if k < 2:
                sincos(petile[:, buf, 512:768], petile[:, buf, 768:1024], argc[:], ['argc'])
            P.op('dve', lambda e, k=k: e.scalar_tensor_tensor(out=argr[:], in0=om[:], scalar=float(2 * k), in1=omr0[:],
                                                              op0=ALU.mult, op1=ALU.add), reads=['om', 'omr0'], writes=['argr'])
            sincos(petile[:, buf, 0:256], petile[:, buf, 256:512], argr[:], ['argr'])
            P.dma('sp', pe_d[k * 128:(k + 1) * 128, :], petile[:, buf, :], reads=['petile'], writes=['pe_d'])
        P.barrier()

    with nc.sbuf_tensor("cin", [NBC, D], F32) as cin, nc.sbuf_tensor("csig", [NBC, D], F32) as csig, \
            nc.sbuf_tensor("scT", [128, KC, NBC], F32) as scT, nc.sbuf_tensor("adab", [1, 6 * D], F32) as adab, \
            nc.sbuf_tensor("modrows", [NBC, 6 * D], F32) as modrows, \
            nc.sbuf_tensor("adaw", [128, 2, KC, 512], F32) as adaw, \
            nc.sbuf_tensor("grows", [NBC, 2, D], F32) as grows, \
            nc.sbuf_tensor("n1bc", [NBC, D], F32) as n1bc, nc.sbuf_tensor("n2bc", [NBC, D], F32) as n2bc:
        P.dma('sp', n1bc[:], n1g_d.to_broadcast([NBC, D]), writes=['n1bc'])
        P.dma('sp', n2bc[:], n2g_d.to_broadcast([NBC, D]), writes=['n2bc'])
        P.dma('sp', cin[0:NB, :], c_d, writes=['cin'])
        P.dma('sp', cin[NB:NBC, :], cctx_d, writes=['cin'])
        P.dma('sp', adab[:], adab_d, writes=['adab'])
        P.op('act', lambda e: e.activation(out=csig[:], in_=cin[:], func=AF.Sigmoid), reads=['cin'], writes=['csig'])
        P.op('dve', lambda e: e.tensor_tensor(out=csig[:], in0=csig[:], in1=cin[:], op=ALU.mult),
             reads=['csig', 'cin'], writes=['csig'])
        for k in range(KC):
            P.op('pe', lambda e, k=k: e.transpose(out=ps[0][:, k * NBC:(k + 1) * NBC], in_=csig[:, k * 128:(k + 1) * 128],
                                                  identity=ident_f[0:NBC, 0:NBC]),
                 reads=['csig', 'ident_f'], writes=[PS(0)], inc=(k == KC - 1))
        P.op('dve', lambda e: e.tensor_copy(out=scT[:].rearrange("p k b -> p (k b)"), in_=ps[0][:, 0:KC * NBC]),
             reads=[PS(0)], writes=['scT'])
        for cg in range(12):
            buf = cg % 2
            P.dma('sp', adaw[:, buf], adaw_d[:, cg * 512:(cg + 1) * 512].rearrange("(k p) c -> p k c", p=128),
                  writes=[('adaw', buf)])
            pb = 1 + (cg % 2)
            for k in range(KC):
                P.op('pe', lambda e, k=k, buf=buf, pb=pb: e.matmul(ps[pb][0:NBC, :], lhsT=scT[:, k, :], rhs=adaw[:, buf, k, :],
                                                                   start=(k == 0), stop=False),
                     reads=['scT', ('adaw', buf)], writes=[PS(pb)], inc=False)
            P.op('pe', lambda e, cg=cg, pb=pb: e.matmul(ps[pb][0:NBC, :], lhsT=ones_f[0:1, 0:NBC],
                                                        rhs=adab[0:1, cg * 512:(cg + 1) * 512], start=False, stop=True),
                 reads=['ones_f', 'adab'], writes=[PS(pb)])
            P.op('act', lambda e, cg=cg, pb=pb: e.copy(out=modrows[:, cg * 512:(cg + 1) * 512], in_=ps[pb][0:NBC, :]),
                 reads=[PS(pb)], writes=['modrows'])
        P.op('dve', lambda e: e.scalar_tensor_tensor(out=grows[:, 0, :], in0=modrows[:, D:2 * D], scalar=1.0, in1=n1bc[:],
                                                     op0=ALU.add, op1=ALU.mult), reads=['modrows', 'n1bc'], writes=['grows'])
        P.op('dve', lambda e: e.scalar_tensor_tensor(out=grows[:, 1, :], in0=modrows[:, 4 * D:5 * D], scalar=1.0, in1=n2bc[:],
                                                     op0=ALU.add, op1=ALU.mult), reads=['modrows', 'n2bc'], writes=['grows'])
        srcs = [grows[:, 0, :], modrows[:, 0:D], grows[:, 1, :], modrows[:, 3 * D:4 * D]]
        for m in range(4):
            for k in range(KC):
                o = (m * KC + k) * NBC
                P.op('pe', lambda e, m=m, k=k, o=o: e.transpose(out=ps[3][:, o:o + NBC], in_=srcs[m][:, k * 128:(k + 1) * 128],
                                                                identity=ident_f[0:NBC, 0:NBC]),
                     reads=['grows', 'modrows', 'ident_f'], writes=[PS(3)], inc=(m == 3 and k == KC - 1))
        P.op('dve', lambda e: e.tensor_copy(out=modT[:].rearrange("p m k b -> p (m k b)"), in_=ps[3][:, 0:4 * KC * NBC]),
             reads=[PS(3)], writes=['modT'])
        P.dma('sp', gsc_d[:, 0, :], modrows[:, 2 * D:3 * D], reads=['modrows'], writes=['gsc_d'])
        P.dma('sp', gsc_d[:, 1, :], modrows[:, 5 * D:6 * D], reads=['modrows'], writes=['gsc_d'])
        P.barrier()

    def rms_to_featmajor(xt_ap, xt_res, dstT, tok0, gsel, bsel, scratch, xn, ssq, pbanks, extra_f32=None, extra_res='extra_f32'):
        P.op('act', lambda e: e.activation(out=scratch, in_=xt_ap, func=AF.Square, accum_out=ssq),
             reads=[xt_res], writes=['rms_scratch', 'ssq'])
        P.op('act', lambda e: e.activation(out=ssq, in_=ssq, func=AF.Sqrt, scale=1.0 / D, bias=eps_c[:]),
             reads=['ssq'], writes=['ssq'])
        P.op('dve', lambda e: e.reciprocal(out=ssq, in_=ssq), reads=['ssq'], writes=['ssq'])
        P.op('dve', lambda e: e.tensor_scalar(out=xn, in0=xt_ap, scalar1=ssq, scalar2=None, op0=ALU.mult),
             reads=['ssq', xt_res], writes=['xn'])
        for k in range(KC):
            pb = pbanks[k // 4]
            P.op('pe', lambda e, k=k, pb=pb: e.transpose(out=ps[pb][:, (k % 4) * 128:(k % 4 + 1) * 128],
                                                         in_=xn[:, k * 128:(k + 1) * 128], identity=ident_f[:]),
                 reads=['xn', 'ident_f'], writes=[PS(pb)], inc=(k % 4 == 3))
        for k in range(KC):
            pb = pbanks[k // 4]
            P.op('act', lambda e, k=k, pb=pb: e.activation(out=dstT[:, k, tok0:tok0 + 128],
                                                           in_=ps[pb][:, (k % 4) * 128:(k % 4 + 1) * 128], func=AF.Identity,
                                                           scale=modT[:, gsel, k, bsel:bsel + 1],
                                                           bias=modT[:, gsel + 1, k, bsel:bsel + 1]),
                 reads=[PS(pb), 'modT'], writes=[('dstT', tok0)])
            if extra_f32 is not None:
                P.op('act', lambda e, k=k, pb=pb: e.activation(out=extra_f32[:, k, :],
                                                               in_=ps[pb][:, (k % 4) * 128:(k % 4 + 1) * 128], func=AF.Identity,
                                                               scale=modT[:, gsel, k, bsel:bsel + 1],
                                                               bias=modT[:, gsel + 1, k, bsel:bsel + 1]),
                     reads=[PS(pb), 'modT'], writes=[extra_res])

    ARENA = max(NLT * D, NLT * 512 + max(3 * NT, NLT * 256 + NTT * 128 + NT // 2 + 64))
    arena = sb("arena", [128, ARENA])
    x_res = arena[:, 0:NLT * D].rearrange("p (j c) -> p j c", c=D)
    gbc = sb("gbc", [128, D])
    rms_scratch = sb("rms_scratch", [128, D])
    xn = sb("xn", [128, D])
    ssq = sb("ssq", [128, 1])

    def make_gbc(b, which):
        P.dma('sp', gbc[:], gsc_d[b:b + 1, which, :].to_broadcast([128, D]), writes=['gbc'])

    for b in range(NB):
        if cfg.stages >= 2:
            build_mixer(nc, P, cfg, b, dict(
                ps=ps, psb=psb, PS=PS, ident_f=ident_f, ident_b=ident_b, ones_f=ones_f, ones_nt=ones_nt, maskF=maskF,
                maskB=maskB, selh=selh, modT=modT, x_d=x_d, ctx_d=ctx_d, pe_d=pe_d, win_d=win_d, wout_d=wout_d,
                cw_sb=cw_sb, cb_sb=cb_sb, mgb_sb=mgb_sb, wg_sb=wg_sb, wlr_sb=wlr_sb, gw2_sb=gw2_sb, ggbT=ggbT,
                mng_d=mng_d, gng_d=gng_d, x_res=x_res, arena=arena, eps_c=eps_c, one_c=one_c, gbc=gbc,
                rms_scratch=rms_scratch, xn=xn, ssq=ssq,
                make_gbc=make_gbc, rms_to_featmajor=rms_to_featmajor, dbg=dbg))
        else:
            with nc.sbuf_tensor(f"pt0_{b}", [128, 2, D], F32) as pt0:
                for j in range(NLT):
                    P.dma('sp', x_res[:, j, :], x_d[b, j * 128:(j + 1) * 128, :], writes=[('x_res', j)])
                    P.dma('sp', pt0[:, j % 2, :], pe_d[j * 128:(j + 1) * 128, :], reads=['pe_d'], writes=[('pt0', j % 2)])
                    P.op('dve', lambda e, j=j: e.tensor_tensor(out=x_res[:, j, :], in0=x_res[:, j, :], in1=pt0[:, j % 2, :],
                                                               op=ALU.add), reads=[('x_res', j), ('pt0', j % 2)],
                         writes=[('x_res', j)])
                P.barrier()
        if cfg.debug:
            for j in range(NLT):
                P.dma('sp', dbg['xmid'][b, j * 128:(j + 1) * 128, :], x_res[:, j, :], reads=[('x_res', j)],
                      writes=[('dbgx', j)])
        build_moe(nc, P, cfg, b, dict(
            ps=ps, PS=PS, ident_f=ident_f, modT=modT, x_res=x_res, gbc=gbc, rms_scratch=rms_scratch, xn=xn, ssq=ssq,
            make_gbc=make_gbc, rms_to_featmajor=rms_to_featmajor, rw_sb=rw_sb, rb_bc=rb_bc, bdn_d=bdn_d, bguT=bguT,
            wgu_d=wgu_d, wdn_d=wdn_d, fng_d=fng_d, out_d=out_d, eps_c=eps_c))

    P.barrier()
    with nc.Block() as block:
        P.emit(block)
    st.close()
    return nc


def build_moe(nc, P, cfg, b, g):
    NB, T, E, NLT = cfg.NB, cfg.T, cfg.E, cfg.NLT
    ps, PS, x_res, gbc = g['ps'], g['PS'], g['x_res'], g['gbc']
    ident_f, modT = g['ident_f'], g['modT']
    rw_sb, rb_bc, bguT = g['rw_sb'], g['rb_bc'], g['bguT']
    wgu_d, wdn_d, fng_d, out_d = g['wgu_d'], g['wdn_d'], g['fng_d'], g['out_d']
    TG = 512 if T >= 512 else T
    NG = T // TG
    TPG = TG // 128
    mst = ExitStack()
    u2T = mst.enter_context(nc.sbuf_tensor(f"u2T_{b}", [128, KC, T], BF16))
    gates = mst.enter_context(nc.sbuf_tensor(f"gates_{b}", [128, NLT, E], F32))
    lg2 = mst.enter_context(nc.sbuf_tensor(f"lg_{b}", [128, 2, E], F32))
    top82 = mst.enter_context(nc.sbuf_tensor(f"top8_{b}", [128, 2, 8], F32))
    rsum2 = mst.enter_context(nc.sbuf_tensor(f"rsum_{b}", [128, 2, 2], F32))
    wgu = mst.enter_context(nc.sbuf_tensor(f"wgu_{b}", [128, 2, KC, 1024], BF16))
    wdn = mst.enter_context(nc.sbuf_tensor(f"wdn_{b}", [128, 2, 4, D], BF16))
    hT = mst.enter_context(nc.sbuf_tensor(f"hT_{b}", [128, 2, 4, TG], BF16))
    g1 = mst.enter_context(nc.sbuf_tensor(f"g1_{b}", [128, 2, TG], F32))
    t1 = mst.enter_context(nc.sbuf_tensor(f"t1_{b}", [128, 2, TG], F32))
    sl = mst.enter_context(nc.sbuf_tensor(f"sl_{b}", [128, 2, TG], F32))
    wdn32 = mst.enter_context(nc.sbuf_tensor(f"wdn32_{b}", [128, 4, D], F32))
    w32flat = wdn32[:].rearrange("p k c -> p (k c)")
    u2f = w32flat[:, 0:KC * 128].rearrange("p (k c) -> p k c", c=128)
    bdn_sb = w32flat[0:E, 1024:1024 + D]
    gatesT2 = [w32flat[0:E, 2048:2048 + 128], w32flat[0:E, 2304:2304 + 128]]
    if True:
        xn, ssq, rms_scratch = g['xn'], g['ssq'], g['rms_scratch']
        g['make_gbc'](b, 1)
        P.dma('sp', bdn_sb, g['bdn_d'], writes=['bdn_sb'])
        u2fs = [u2f, w32flat[:, 3072:3072 + KC * 128].rearrange("p (k c) -> p k c", c=128)]

        def pro_front(j):
            g['rms_to_featmajor'](x_res[:, j, :], ('x_res', j), u2T, j * 128, 2, b, rms_scratch[:], xn[:], ssq[:], (0, 1),
                                  extra_f32=u2fs[j % 2], extra_res=('u2f', j % 2))

        def pro_back(j):
            u2f = u2fs[j % 2]
            si = j % 2
            br, b0_, b1_ = (2, 3, 4) if si == 0 else (5, 6, 7)
            bb = (b0_, b1_)
            lg = lg2[:, si, :]
            top8 = top82[:, si, :]
            rsum = rsum2[:, si, :]
            gatesT = gatesT2[si]
            LG, T8, RS, GT = ('lg', si), ('top8', si), ('rsum', si), ('gatesT', si)
            for k in range(KC):
                P.op('pe', lambda e, k=k: e.matmul(ps[br][:, 0:E], lhsT=u2f[:, k, :], rhs=rw_sb[:, k, :], start=(k == 0),
                                                   stop=(k == KC - 1)), reads=[('u2f', j % 2), 'rw_sb'], writes=[PS(br)],
                     inc=(k == KC - 1))
            yield
            P.op('dve', lambda e: e.tensor_tensor(out=lg, in0=ps[br][:, 0:E], in1=rb_bc[:], op=ALU.add),
                 reads=[PS(br), 'rb_bc'], writes=[LG])
            yield
            P.op('dve', lambda e: e.max(out=top8, in_=lg), reads=[LG], writes=[T8])
            yield
            P.op('dve', lambda e: e.tensor_scalar(out=rsum[:, 0:1], in0=top8[:, 0:1], scalar1=-1.0, scalar2=None, op0=ALU.mult),
                 reads=[T8], writes=[RS])
            yield
            P.op('act', lambda e, j=j: e.activation(out=gates[:, j, :], in_=lg, func=AF.Exp, bias=rsum[:, 0:1], scale=1.0),
                 reads=[LG, RS], writes=[('gates', j)])
            yield
            P.op('dve', lambda e: e.tensor_scalar(out=lg, in0=lg, scalar1=top8[:, 3:4], scalar2=None, op0=ALU.is_ge),
                 reads=[LG, T8, ('gates', j)], writes=[LG])
            yield
            P.op('dve', lambda e, j=j: e.tensor_tensor(out=gates[:, j, :], in0=gates[:, j, :], in1=lg, op=ALU.mult),
                 reads=[('gates', j), LG], writes=[('gates', j)])
            yield
            P.op('dve', lambda e, j=j: e.tensor_reduce(out=rsum[:, 1:2], in_=gates[:, j, :], axis=AX.X, op=ALU.add),
                 reads=[('gates', j)], writes=[RS])
            yield
            P.op('dve', lambda e: e.reciprocal(out=rsum[:, 1:2], in_=rsum[:, 1:2]), reads=[RS], writes=[RS])
            yield
            P.op('dve', lambda e, j=j: e.tensor_scalar(out=gates[:, j, :], in0=gates[:, j, :], scalar1=rsum[:, 1:2],
                                                       scalar2=None, op0=ALU.mult), reads=[('gates', j), RS],
                 writes=[('gates', j)])
            P.op('pe', lambda e, j=j: e.transpose(out=ps[br][0:E, 128:256], in_=gates[:, j, :], identity=ident_f[:]),
                 reads=[('gates', j), 'ident_f'], writes=[PS(br)])
            yield
            P.op('act', lambda e: e.copy(out=gatesT, in_=ps[br][0:E, 128:256]), reads=[PS(br)], writes=[GT])
            yield
            P.op('dve', lambda e, j=j: e.tensor_scalar(out=gates[:, j, :], in0=gates[:, j, :], scalar1=1.0 / ALPHA, scalar2=None,
                                                       op0=ALU.mult), reads=[('gates', j)], writes=[('gates', j)])
            for half in range(2):
                hs = slice(half * 512, (half + 1) * 512)
                P.op('pe', lambda e, hs=hs, half=half: e.matmul(ps[bb[half]][:, :], lhsT=gatesT, rhs=bdn_sb[:, hs],
                                                                start=True, stop=True), reads=[GT, 'bdn_sb'],
                     writes=[PS(bb[half])])
                tmpb = (g1 if si == 0 else t1)[:, half, :] if TG == 512 else rms_scratch[:, hs]
                tres = ('btmp', si, half) if TG == 512 else 'rms_scratch'
                P.op('dve', lambda e, half=half, hs=hs, tmpb=tmpb: e.tensor_tensor(out=tmpb, in0=ps[bb[half]][:, :], in1=gbc[:, hs],
                                                                                   op=ALU.mult), reads=[PS(bb[half]), 'gbc'],
                     writes=[tres])
                P.op('pool', lambda e, j=j, hs=hs, tmpb=tmpb: e.tensor_tensor(out=x_res[:, j, hs], in0=x_res[:, j, hs],
                                                                              in1=tmpb, op=ALU.add),
                     reads=[tres, ('x_res', j)], writes=[('x_res', j)])
            yield
        def fronts(js):
            for jj in js:
                if jj < NLT:
                    pro_front(jj)
                yield

        pro_front(0)
        if NLT > 1:
            pro_front(1)
        for j0 in range(0, NLT, 2):
            gens = [pro_back(j0)] + ([pro_back(j0 + 1)] if j0 + 1 < NLT else []) + [fronts([j0 + 2, j0 + 3])]
            while gens:
                for gnr in list(gens):
                    try:
                        next(gnr)
                    except StopIteration:
                        gens.remove(gnr)
        P.barrier()
        acc_i = 0

        def load_weights(step):
            ex_, fh_ = step // 2, step % 2
            buf_ = step % 2
            for part in range(2):
                c0 = part * 1024 + fh_ * 512
                P.dma('pool', wgu[:, buf_, :, part * 512:(part + 1) * 512],
                      wgu_d[ex_, :, c0:c0 + 512].rearrange("(k p) c -> p k c", p=128), writes=[('wgu', buf_)])
            P.dma('sp', wdn32[:], wdn_d[ex_, fh_ * 512:(fh_ + 1) * 512, :].rearrange("(k p) c -> p k c", p=128),
                  writes=['wdn32'])
            for k4 in range(4):
                P.op('pool', lambda e, k4=k4: e.tensor_tensor(out=wdn[:, buf_, k4, :], in0=wdn32[:, k4, :], in1=gbc[:], op=ALU.mult),
                     reads=['wdn32', 'gbc'], writes=[('wdn', buf_)])

        acc_box = [0]

        def emit_G(step, tg, gi):
            ex, fh = step // 2, step % 2
            buf = step % 2
            hb = gi % 2
            toks = slice(tg * TG, (tg + 1) * TG)
            for fc in range(4):
                fidx = fh * 4 + fc
                pg, pu = (fc % 2) * 2, 1 + (fc % 2) * 2
                for k in range(KC):
                    P.op('pe', lambda e, k=k, fc=fc, pg=pg: e.matmul(
                        ps[pg][:, 0:TG], lhsT=wgu[:, buf, k, fc * 128:(fc + 1) * 128], rhs=u2T[:, k, toks],
                        start=(k == 0), stop=(k == KC - 1)), reads=[('wgu', buf)], writes=[PS(pg)], inc=(k == KC - 1))
                for k in range(KC):
                    P.op('pe', lambda e, k=k, fc=fc, pu=pu: e.matmul(
                        ps[pu][:, 0:TG], lhsT=wgu[:, buf, k, 512 + fc * 128:512 + (fc + 1) * 128], rhs=u2T[:, k, toks],
                        start=(k == 0), stop=(k == KC - 1)), reads=[('wgu', buf)], writes=[PS(pu)], inc=(k == KC - 1))
                eb = fc % 2
                P.op('dve', lambda e, pg=pg, eb=eb, fidx=fidx: e.tensor_scalar(
                    out=g1[:, eb, :], in0=ps[pg][:, 0:TG], scalar1=bguT[:, fidx, ex:ex + 1], scalar2=LIM,
                    op0=ALU.add, op1=ALU.min), reads=[PS(pg), 'bguT'], writes=[('g1', eb)])
                P.op('act', lambda e, eb=eb: e.activation(out=sl[:, eb, :], in_=g1[:, eb, :], func=AF.Silu, scale=ALPHA),
                     reads=[('g1', eb)], writes=[('sl', eb)])
                P.op('dve', lambda e, pu=pu, eb=eb, fidx=fidx: e.tensor_scalar(
                    out=t1[:, eb, :], in0=ps[pu][:, 0:TG], scalar1=bguT[:, 8 + fidx, ex:ex + 1], scalar2=1.0 - LIM,
                    op0=ALU.add, op1=ALU.max), reads=[PS(pu), 'bguT'], writes=[('t1', eb)])
                P.op('dve', lambda e, eb=eb, fc=fc: e.scalar_tensor_tensor(
                    out=hT[:, hb, fc, :], in0=t1[:, eb, :], scalar=1.0 + LIM, in1=sl[:, eb, :],
                    op0=ALU.min, op1=ALU.mult), reads=[('t1', eb), ('sl', eb)], writes=[('hT', hb, fc)])

        def emit_D(step, tg, gi):
            ex = step // 2
            buf = step % 2
            hb = gi % 2
            for tt in range(TPG):
                j = tg * TPG + tt
                for half in range(2):
                    pb = 4 + ((tt * 2 + half) % 4)
                    for fc in range(4):
                        P.op('pe', lambda e, fc=fc, pb=pb, tt=tt, half=half: e.matmul(
                            ps[pb][:, :], lhsT=hT[:, hb, fc, tt * 128:(tt + 1) * 128],
                            rhs=wdn[:, buf, fc, half * 512:(half + 1) * 512], start=(fc == 0), stop=(fc == 3)),
                            reads=[('hT', hb, fc), ('wdn', buf)], writes=[PS(pb)], inc=(fc == 3))
                    hs = slice(half * 512, (half + 1) * 512)
                    P.op('dve', lambda e, pb=pb, hs=hs, j=j: e.scalar_tensor_tensor(
                        out=x_res[:, j, hs], in0=ps[pb][:, :], scalar=gates[:, j, ex:ex + 1], in1=x_res[:, j, hs],
                        op0=ALU.mult, op1=ALU.add), reads=[PS(pb), ('x_res', j)], writes=[('x_res', j)])

        items = [(st_, tg) for st_ in range(2 * E) for tg in range(NG)]
        load_weights(0)
        load_weights(1)
        emit_G(items[0][0], items[0][1], 0)
        for i, (st_, tg) in enumerate(items):
            if i + 1 < len(items):
                emit_G(items[i + 1][0], items[i + 1][1], i + 1)
            emit_D(st_, tg, i)
            if tg == NG - 1 and st_ + 2 < 2 * E:
                load_weights(st_ + 2)
        P.barrier()
        P.dma('sp', gbc[:], fng_d.to_broadcast([128, D]), writes=['gbc'])
        for j in range(NLT):
            P.op('act', lambda e, j=j: e.activation(out=rms_scratch[:], in_=x_res[:, j, :], func=AF.Square, accum_out=ssq[:]),
                 reads=[('x_res', j)], writes=['rms_scratch', 'ssq'])
            P.op('act', lambda e: e.activation(out=ssq[:], in_=ssq[:], func=AF.Sqrt, scale=1.0 / D, bias=g['eps_c'][:]),
                 reads=['ssq'], writes=['ssq'])
            P.op('dve', lambda e: e.reciprocal(out=ssq[:], in_=ssq[:]), reads=['ssq'], writes=['ssq'])
            P.op('dve', lambda e, j=j: e.tensor_scalar(out=xn[:], in0=x_res[:, j, :], scalar1=ssq[:], scalar2=None,
                                                       op0=ALU.mult), reads=['ssq', ('x_res', j)], writes=['xn'])
            P.op('pool', lambda e, j=j: e.tensor_tensor(out=x_res[:, j, :], in0=xn[:], in1=gbc[:], op=ALU.mult),
                 reads=['xn', 'gbc'], writes=[('x_res', j)])
            P.dma('sp', out_d[b, j * 128:(j + 1) * 128, :], x_res[:, j, :], reads=[('x_res', j)], writes=[('out', b, j)])
        P.barrier()
    mst.close()


def build_mixer(nc, P, cfg, b, g):
    NB, T, TC = cfg.NB, cfg.T, cfg.TC
    NT, NTT, NTC, NLT, NCH, NCC = cfg.NT, cfg.NTT, cfg.NTC, cfg.NLT, cfg.NCH, cfg.NCC
    ps, psb, PS = g['ps'], g['psb'], g['PS']
    ident_f, ident_b, ones_f, ones_nt = g['ident_f'], g['ident_b'], g['ones_f'], g['ones_nt']
    masks = (g['maskF'], g['maskB'])
    selh, modT = g['selh'], g['modT']
    x_d, ctx_d, pe_d, win_d, wout_d = g['x_d'], g['ctx_d'], g['pe_d'], g['win_d'], g['wout_d']
    cw_sb, cb_sb, mgb_sb, wg_sb, wlr_sb, gw2_sb, ggbT = (g['cw_sb'], g['cb_sb'], g['mgb_sb'], g['wg_sb'], g['wlr_sb'],
                                                         g['gw2_sb'], g['ggbT'])
    arena, x_res, gbc = g['arena'], g['x_res'], g['gbc']
    rms_scratch, xn, ssq, eps_c, one_c = g['rms_scratch'], g['xn'], g['ssq'], g['eps_c'], g['one_c']
    dbg = g['dbg']
    groups = [(t0, min(512, NT - t0)) for t0 in range(0, NT, 512)]
    HOFF = NLT * 512
    Hacc = arena[:, 0:HOFF].rearrange("p (j c) -> p j c", c=512)
    gt_tiles = [arena[0:4, HOFF + i * NT:HOFF + (i + 1) * NT] for i in range(3)]

    def S(name):
        return f"{name}_{b}"

    def win_load(dst, c0, ncol, res):
        P.dma('pool', dst, win_d[:, c0:c0 + ncol].rearrange("(k p) c -> p k c", p=128), writes=[res])

    def proj_featmajor(w, col0, dst_fn, evac):
        for gi, (t0, n) in enumerate(groups):
            pb = 2 + gi % 2
            for k in range(KC):
                P.op('pe', lambda e, k=k, pb=pb, t0=t0, n=n: e.matmul(ps[pb][:, 0:n], lhsT=w[:, k, col0:col0 + 128],
                                                                      rhs=uT[:, k, t0:t0 + n], start=(k == 0), stop=(k == KC - 1)),
                     reads=['w_cur'], writes=[PS(pb)], inc=(k == KC - 1))
            evac(pb, t0, n)

    def per_head_norm_stats(j):
        P.op('dve', lambda e: e.tensor_tensor(out=rms_scratch[:, 0:512], in0=Hacc[:, j, :], in1=Hacc[:, j, :], op=ALU.mult),
             reads=[('Hacc', j)], writes=['rms_scratch'])
        P.op('dve', lambda e: e.tensor_reduce(out=hst[:, 0:4], in_=rms_scratch[:, 0:512].rearrange("p (h c) -> p h c", c=128),
                                              axis=AX.X, op=ALU.add), reads=['rms_scratch'], writes=['hst'])
        P.op('act', lambda e: e.activation(out=hst[:, 0:4], in_=hst[:, 0:4], func=AF.Sqrt, scale=1.0 / 128, bias=eps_c[:]),
             reads=['hst'], writes=['hst'])
        P.op('dve', lambda e: e.reciprocal(out=hst[:, 0:4], in_=hst[:, 0:4]), reads=['hst'], writes=['hst'])
        P.op('dve', lambda e: e.tensor_tensor(out=rms_scratch[:, 0:512].rearrange("p (h c) -> p h c", c=128),
                                              in0=Hacc[:, j, :].rearrange("p (h c) -> p h c", c=128),
                                              in1=hst[:, 0:4].unsqueeze(2).to_broadcast([128, 4, 128]), op=ALU.mult),
             reads=['hst', ('Hacc', j)], writes=['rms_scratch'])

    st = ExitStack()
    uT = st.enter_context(nc.sbuf_tensor(S("uT"), [128, KC, NT], BF16))
    mT = uT[:].rearrange("p k n -> p (k n)")[:, 0:KC * T].rearrange("p (k t) -> p k t", t=T)
    a_tok = arena[:, HOFF:HOFF + NLT * 256].bitcast(BF16).rearrange("p (j c) -> p j c", c=512)
    AO = HOFF + NLT * 256
    khTok = arena[:, AO:AO + NTT * 128].bitcast(BF16).rearrange("p (i c) -> p i c", c=256)
    khT = arena[:, AO + NTT * 128:AO + NTT * 128 + NT // 2].bitcast(BF16)

    def g_tok(j):
        return arena[:, j * 512:j * 512 + 256].bitcast(BF16)
    hst = st.enter_context(nc.sbuf_tensor(S("hst"), [128, 8], F32))
    gnbc = st.enter_context(nc.sbuf_tensor(S("gnbc"), [128, 512], F32))
    wbuf = st.enter_context(nc.sbuf_tensor(S("wbuf"), [128, KC, 512], BF16))
    Pt = st.enter_context(nc.sbuf_tensor(S("Pt"), [128, 2, 2, 64], BF16))
    dn = st.enter_context(nc.sbuf_tensor(S("dn"), [128, 8], F32))

    with nc.sbuf_tensor(S("xt"), [128, 2, D], F32) as xt, nc.sbuf_tensor(S("pt"), [128, 2, D], F32) as pt:
        for i in range(NTT):
            bf = i % 2
            if i < NTC:
                P.dma('sp', xt[:, bf, :], ctx_d[b, i * 128:(i + 1) * 128, :], writes=[('xt', bf)])
                bsel = NB
            else:
                j = i - NTC
                P.dma('sp', xt[:, bf, :], x_d[b, j * 128:(j + 1) * 128, :], writes=[('xt', bf)])
                P.dma('sp', pt[:, bf, :], pe_d[j * 128:(j + 1) * 128, :], writes=[('pt', bf)])
                P.op('pool', lambda e, bf=bf: e.tensor_tensor(out=xt[:, bf, :], in0=xt[:, bf, :], in1=pt[:, bf, :], op=ALU.add),
                     reads=[('xt', bf), ('pt', bf)], writes=[('xt', bf)])
                bsel = b
            g['rms_to_featmajor'](xt[:, bf, :], ('xt', bf), uT, i * 128, 0, bsel, rms_scratch[:], xn[:], ssq[:], (0, 1))
        P.barrier()

    mst = ExitStack()
    qkT = mst.enter_context(nc.sbuf_tensor(S("qkT"), [128, 4, NT], BF16))
    kTok = mst.enter_context(nc.sbuf_tensor(S("kTok"), [128, NTT, 256], BF16))
    if True:
        with nc.sbuf_tensor(S("zt"), [128, NT], F32) as zt, nc.sbuf_tensor(S("ot"), [128, NT], F32) as ot, \
                nc.sbuf_tensor(S("sg"), [128, NT], F32) as sg:
            win_load(wbuf[:], 0, 512, 'w_cur')
            for qc in range(4):
                proj_featmajor(wbuf, qc * 128, None,
                               lambda pb, t0, n: P.op('act', lambda e: e.copy(out=zt[:, t0:t0 + n], in_=ps[pb][:, 0:n]),
                                                      reads=[PS(pb)], writes=['zt']))
                P.op('dve', lambda e, qc=qc: e.tensor_scalar(out=ot[:], in0=zt[:], scalar1=cw_sb[:, qc, 1:2],
                                                             scalar2=cb_sb[:, qc:qc + 1], op0=ALU.mult, op1=ALU.add),
                     reads=['zt', 'cw_sb', 'cb_sb'], writes=['ot'])
                for lo, hi in ((0, TC), (TC, NT)):
                    P.op('dve', lambda e, qc=qc, lo=lo, hi=hi: e.scalar_tensor_tensor(
                        out=ot[:, lo + 1:hi], in0=zt[:, lo:hi - 1], scalar=cw_sb[:, qc, 0:1], in1=ot[:, lo + 1:hi],
                        op0=ALU.mult, op1=ALU.add), reads=['zt', 'ot'], writes=['ot'])
                    P.op('dve', lambda e, qc=qc, lo=lo, hi=hi: e.scalar_tensor_tensor(
                        out=ot[:, lo:hi - 1], in0=zt[:, lo + 1:hi], scalar=cw_sb[:, qc, 2:3], in1=ot[:, lo:hi - 1],
                        op0=ALU.mult, op1=ALU.add), reads=['zt', 'ot'], writes=['ot'])
                P.op('act', lambda e: e.activation(out=sg[:], in_=ot[:], func=AF.Sigmoid), reads=['ot'], writes=['sg'])
                P.op('dve', lambda e, qc=qc: e.scalar_tensor_tensor(out=qkT[:, qc, :], in0=ot[:], scalar=(0.125 if qc < 2 else 1.0),
                                                                    in1=sg[:], op0=ALU.mult, op1=ALU.mult),
                     reads=['ot', 'sg'], writes=['qkT'])
            P.barrier()
        vtok = mst.enter_context(nc.sbuf_tensor(S("vtok"), [128, NTT, 4, 129], BF16))
        kp = mst.enter_context(nc.sbuf_tensor(S("kp"), [128, NTT, 256], BF16))
        edT = mst.enter_context(nc.sbuf_tensor(S("edT"), [128, NTT, 8], F32))
        wcB = mst.enter_context(nc.sbuf_tensor(S("wcB"), [128, 2, NCH], F32))
        Chat = mst.enter_context(nc.sbuf_tensor(S("Chat"), [128, 2, 129], F32))
        CbfA = mst.enter_context(nc.sbuf_tensor(S("CbfA"), [128, 2, 2, 129], BF16))
        CbfB = mst.enter_context(nc.sbuf_tensor(S("CbfB"), [128, 2, 2, 129], BF16))
        Aend = mst.enter_context(nc.sbuf_tensor(S("Aend"), [4, 3, NCH], F32))
        for i in range(NTT):
            for kc in range(2):
                P.op('pe', lambda e, i=i, kc=kc: e.transpose(out=psb[1][:, kc * 128:(kc + 1) * 128],
                                                             in_=qkT[:, 2 + kc, i * 128:(i + 1) * 128], identity=ident_b[:]),
                     reads=['ident_b'], writes=[PS(1)], inc=(kc == 1))
            P.op('dve', lambda e, i=i: e.tensor_copy(out=kTok[:, i, :], in_=psb[1][:, 0:256]), reads=[PS(1)], writes=['kTok'])
        win_load(wbuf[:], 512, 512, 'w_cur')
        P.op('pool', lambda e: e.memset(vtok[:, :, :, 128:129], 1.0), writes=['vtok'])
        for i in range(NTT):
            pb = 2 + i % 2
            for k in range(KC):
                P.op('pe', lambda e, i=i, k=k, pb=pb: e.matmul(ps[pb][:, :], lhsT=uT[:, k, i * 128:(i + 1) * 128], rhs=wbuf[:, k, :],
                                                               start=(k == 0), stop=(k == KC - 1)),
                     reads=['w_cur'], writes=[PS(pb)], inc=(k == KC - 1))
            P.op('act', lambda e, i=i, pb=pb: e.copy(out=vtok[:, i, :, 0:128], in_=ps[pb][:, :].rearrange("p (h c) -> p h c", c=128)),
                 reads=[PS(pb)], writes=['vtok'])
        P.barrier()

        DIRS = [int(c) for c in os.environ.get('KDIRS', '01')]
        for d in DIRS:
            IGt, FPt, T3 = gt_tiles
            for gi, dst in ((2 * d, IGt), (2 * d + 1, FPt)):
                for gj, (t0, n) in enumerate(groups):
                    pb = 2 + gj % 2
                    for k in range(KC):
                        P.op('pe', lambda e, k=k, pb=pb, t0=t0, n=n, gi=gi: e.matmul(
                            ps[pb][0:4, 0:n], lhsT=wg_sb[:, k, gi * 4:(gi + 1) * 4], rhs=uT[:, k, t0:t0 + n],
                            start=(k == 0), stop=(k == KC - 1)), reads=['wg_sb'], writes=[PS(pb)], inc=(k == KC - 1))
                    P.op('act', lambda e, pb=pb, t0=t0, n=n, gi=gi, dst=dst: e.activation(
                        out=dst[:, t0:t0 + n], in_=ps[pb][0:4, 0:n], func=AF.Identity, bias=mgb_sb[:, gi:gi + 1], scale=1.0),
                        reads=[PS(pb), 'mgb_sb'], writes=[('gt', gi % 2)])
            P.op('act', lambda e: e.activation(out=FPt, in_=FPt, func=AF.Exp, scale=-1.0), reads=[('gt', 1)], writes=[('gt', 1)])
            P.op('act', lambda e: e.activation(out=FPt, in_=FPt, func=AF.Ln, bias=one_c[0:4, :], scale=1.0),
                 reads=[('gt', 1)], writes=[('gt', 1)])

            def scan(out_t, d0, d1, op0, op1, rd, wr):
                if d == 0:
                    P.op('dve', lambda e: e.tensor_tensor_scan(out=out_t[:, 0:NT], data0=d0[:, 0:NT], data1=d1[:, 0:NT], initial=0.0,
                                                               op0=op0, op1=op1), reads=rd, writes=wr)
                else:
                    P.op('dve', lambda e: e.tensor_tensor_scan(out=out_t[:, 0:TC][:, ::-1], data0=d0[:, 0:TC][:, ::-1],
                                                               data1=d1[:, 0:TC][:, ::-1], initial=0.0, op0=op0, op1=op1),
                         reads=rd, writes=wr)
                    P.op('dve', lambda e: e.tensor_tensor_scan(out=out_t[:, TC:NT][:, ::-1], data0=d0[:, TC:NT][:, ::-1],
                                                               data1=d1[:, TC:NT][:, ::-1], initial=out_t[:, 0:1], op0=op0, op1=op1),
                         reads=rd + wr, writes=wr)
            scan(T3, ones_nt[0:4, :], FPt, ALU.mult, ALU.add, [('gt', 1), 'ones_nt'], [('gt', 2)])
            P.op('dve', lambda e: e.tensor_tensor(out=IGt, in0=IGt, in1=T3, op=ALU.add), reads=[('gt', 0), ('gt', 2)],
                 writes=[('gt', 0)])
            scan(FPt, IGt, IGt, ALU.max, ALU.max, [('gt', 0)], [('gt', 1)])
            endcol = 63 if d == 0 else 0
            A3 = FPt.rearrange("p (c l) -> p c l", l=64)
            P.op('dve', lambda e: e.tensor_copy(out=Aend[:, 0, :], in_=A3[:, :, endcol]), reads=[('gt', 1)], writes=['Aend'])
            P.op('pool', lambda e: e.memset(Aend[:, 1, :], 0.0), writes=['Aprev'])
            if d == 0:
                P.op('dve', lambda e: e.tensor_copy(out=Aend[:, 1, 1:NCH], in_=Aend[:, 0, 0:NCH - 1]), reads=['Aend', 'Aprev'],
                     writes=['Aprev'])
            else:
                if NCC > 1:
                    P.op('dve', lambda e: e.tensor_copy(out=Aend[:, 1, 0:NCC - 1], in_=Aend[:, 0, 1:NCC]), reads=['Aend', 'Aprev'],
                         writes=['Aprev'])
                P.op('dve', lambda e: e.tensor_copy(out=Aend[:, 1, NCC:NCH - 1], in_=Aend[:, 0, NCC + 1:NCH]),
                     reads=['Aend', 'Aprev'], writes=['Aprev'])
                P.op('dve', lambda e: e.tensor_copy(out=Aend[:, 1, NCH - 1:NCH], in_=Aend[:, 0, 0:1]), reads=['Aend', 'Aprev'],
                     writes=['Aprev'])
            P.op('dve', lambda e: e.tensor_tensor(out=Aend[:, 2, :], in0=Aend[:, 1, :], in1=Aend[:, 0, :], op=ALU.subtract),
                 reads=['Aend', 'Aprev'], writes=['wc'])
            P.op('act', lambda e: e.activation(out=Aend[:, 2, :], in_=Aend[:, 2, :], func=AF.Exp), reads=['wc'], writes=['wc'])
            for ti_, nm in ((IGt, 0), (T3, 2)):
                P.op('dve', lambda e, ti_=ti_: e.tensor_tensor(out=ti_.rearrange("p (c l) -> p c l", l=64),
                                                               in0=ti_.rearrange("p (c l) -> p c l", l=64),
                                                               in1=Aend[:, 0, :].unsqueeze(2).to_broadcast([4, NCH, 64]),
                                                               op=ALU.subtract), reads=[('gt', nm), 'Aend'], writes=[('gt', nm)])
                P.op('act', lambda e, ti_=ti_: e.activation(out=ti_, in_=ti_, func=AF.Exp), reads=[('gt', nm)], writes=[('gt', nm)])
            for i in range(NTT):
                P.op('pe', lambda e, i=i: e.matmul(ps[2][:, i * 8:i * 8 + 4], lhsT=IGt[:, i * 128:(i + 1) * 128], rhs=ident_f[0:4, 0:4],
                                                   start=True, stop=True), reads=[('gt', 0), 'ident_f'], writes=[PS(2)], inc=False)
                P.op('pe', lambda e, i=i: e.matmul(ps[2][:, i * 8 + 4:i * 8 + 8], lhsT=T3[:, i * 128:(i + 1) * 128],
                                                   rhs=ident_f[0:4, 0:4], start=True, stop=True), reads=[('gt', 2), 'ident_f'],
                     writes=[PS(2)], inc=(i == NTT - 1))
            P.op('dve', lambda e: e.tensor_copy(out=edT[:].rearrange("p i c -> p (i c)"), in_=ps[2][:, 0:NTT * 8]), reads=[PS(2)],
                 writes=['edT'])
            for pr in range(2):
                P.op('pe', lambda e, pr=pr: e.matmul(ps[3][:, pr * NCH:(pr + 1) * NCH],
                                                     lhsT=selh[0:4, pr].rearrange("p a b -> p (a b)"), rhs=Aend[:, 2, :],
                                                     start=True, stop=True), reads=['selh', 'wc'], writes=[PS(3)], inc=(pr == 1))
            P.op('dve', lambda e: e.tensor_copy(out=wcB[:].rearrange("p a c -> p (a c)"), in_=ps[3][:, 0:2 * NCH]), reads=[PS(3)],
                 writes=['wcB'])
            P.op('dve', lambda e: e.tensor_tensor(out=kp[:].rearrange("p i (h c) -> p i h c", c=64),
                                                  in0=kTok[:].rearrange("p i (h c) -> p i h c", c=64),
                                                  in1=edT[:, :, 0:4].unsqueeze(3).to_broadcast([128, NTT, 4, 64]), op=ALU.mult),
                 reads=['kTok', 'edT'], writes=['kp'])
            P.barrier()
            P.op('pool', lambda e: e.memset(Chat[:], 0.0), writes=['Chat'])
            P.op('pool', lambda e: e.memset(CbfA[:], 0.0), writes=[('CbfA', 0), ('CbfA', 1)])
            P.op('pool', lambda e: e.memset(CbfB[:], 0.0), writes=[('CbfB', 0), ('CbfB', 1)])
            order = list(range(NCH)) if d == 0 else list(range(NCC - 1, -1, -1)) + list(range(NCH - 1, NCC - 1, -1))
            mask = masks[d]
            first_dir = (d == DIRS[0])

            def ml_geo(step):
                c = order[step]
                ti, p0 = c // 2, (c % 2) * 64
                return c, ti, slice(p0, p0 + 64), slice(c * 64, (c + 1) * 64), c >= NCC, 6 + step % 2, c % 2

            def ml_pre(step):
                c, ti, rows, toks, lat, pbD, hf = ml_geo(step)
                if lat:
                    for h in range(4):
                        hp = (h % 2) * 64
                        P.op('pe', lambda e, h=h, hp=hp: e.matmul(
                            ps[2 + h % 2][rows, (h // 2) * 64:(h // 2) * 64 + 64], lhsT=qkT[hp:hp + 64, 2 + h // 2, toks],
                            rhs=qkT[hp:hp + 64, h // 2, toks], start=True, stop=True), writes=[('psh', 2 + h % 2, hf)], inc=(h >= 2))
                for h in range(4):
                    P.op('pe', lambda e, h=h: e.matmul(
                        ps[pbD][(h % 2) * 64:(h % 2) * 64 + 64, (h // 2) * 129:(h // 2 + 1) * 129],
                        lhsT=kp[rows, ti, h * 64:(h + 1) * 64], rhs=vtok[rows, ti, h, :], start=True, stop=True),
                        reads=['kp'], writes=[PS(pbD)], inc=(h == 3))
                if lat:
                    for h in range(4):
                        par, hh = h % 2, h // 2
                        P.op('dve', lambda e, h=h, par=par, hh=hh: e.scalar_tensor_tensor(
                            out=Pt[rows, par, hh, :], in0=ps[2 + par][rows, hh * 64:(hh + 1) * 64], scalar=edT[rows, ti, h:h + 1],
                            in1=mask[rows, :], op0=ALU.mult, op1=ALU.mult), reads=[('psh', 2 + par, hf), 'edT'],
                            writes=[('Pt', hf, h)])

            def ml_main(step):
                c, ti, rows, toks, lat, pbD, hf = ml_geo(step)
                for pr in range(2):
                    P.op('dve', lambda e, pr=pr: e.scalar_tensor_tensor(
                        out=Chat[:, pr, :], in0=Chat[:, pr, :], scalar=wcB[:, pr, c:c + 1], in1=ps[pbD][:, pr * 129:(pr + 1) * 129],
                        op0=ALU.mult, op1=ALU.add), reads=['Chat', 'wcB', PS(pbD)], writes=['Chat'])
                if step + 1 < len(order):
                    cn = order[step + 1]
                    for pr in range(2):
                        P.op('act', lambda e, pr=pr: e.activation(out=CbfA[0:64, (step + 1) % 2, pr, :], in_=Chat[0:64, pr, :], func=AF.Identity,
                                                                  scale=wcB[0:64, pr, cn:cn + 1]),
                             reads=['Chat', 'wcB'], writes=[('CbfA', (step + 1) % 2)])
                        P.op('act', lambda e, pr=pr: e.activation(out=CbfB[64:128, (step + 1) % 2, pr, :], in_=Chat[64:128, pr, :], func=AF.Identity,
                                                                  scale=wcB[64:128, pr, cn:cn + 1]),
                             reads=['Chat', 'wcB'], writes=[('CbfB', (step + 1) % 2)])

                if lat:
                    j = ti - NTC
                    for h in range(4):
                        par, pr = h % 2, h // 2
                        P.op('pe', lambda e, h=h, par=par, pr=pr: e.matmul(
                            ps[4 + pr][rows, par * 129:(par + 1) * 129], lhsT=Pt[rows, par, pr, :], rhs=vtok[rows, ti, h, :],
                            start=True, stop=False), reads=[('Pt', hf, h)], writes=[('psh', 4 + pr, hf)], inc=False)
                        P.op('pe', lambda e, par=par, pr=pr: e.matmul(
                            ps[4 + pr][rows, par * 129:(par + 1) * 129], lhsT=qkT[:, pr, toks],
                            rhs=(CbfA if par == 0 else CbfB)[:, step % 2, pr, :], start=False, stop=True),
                            reads=[('CbfA', step % 2), ('CbfB', step % 2)], writes=[('psh', 4 + pr, hf)], inc=True)
                    for pr in range(2):
                        den = lambda pr=pr, rows=rows: ps[4 + pr][rows, 0:258].rearrange("p (a c) -> p a c", c=129)[:, :, 128]
                        P.op('dve', lambda e, pr=pr, den=den: e.tensor_tensor(
                            out=dn[rows, 2 * pr:2 * pr + 2], in0=den(), in1=edT[rows, ti, 4 + 2 * pr:6 + 2 * pr], op=ALU.max),
                            reads=[('psh', 4 + pr, hf), 'edT'], writes=[('dn', hf)])
                        P.op('dve', lambda e, pr=pr, den=den: e.scalar_tensor_tensor(
                            out=dn[rows, 2 * pr:2 * pr + 2], in0=den(), scalar=-1.0, in1=dn[rows, 2 * pr:2 * pr + 2],
                            op0=ALU.mult, op1=ALU.max), reads=[('psh', 4 + pr, hf), ('dn', hf)], writes=[('dn', hf)])
                    P.op('dve', lambda e: e.reciprocal(out=dn[rows, 4:8], in_=dn[rows, 0:4]), reads=[('dn', hf)],
                         writes=[('rd', hf)])
                    for h in range(4):
                        src = lambda h=h, rows=rows: ps[4 + h // 2][rows, (h % 2) * 129:(h % 2) * 129 + 128]
                        if first_dir:
                            P.op('dve', lambda e, h=h, src=src: e.tensor_scalar(
                                out=Hacc[rows, j, h * 128:(h + 1) * 128], in0=src(), scalar1=dn[rows, 4 + h:5 + h], scalar2=None,
                                op0=ALU.mult), reads=[('psh', 4 + h // 2, hf), ('rd', hf)], writes=[('Hacc', j)])
                        else:
                            P.op('dve', lambda e, h=h, src=src: e.scalar_tensor_tensor(
                                out=Hacc[rows, j, h * 128:(h + 1) * 128], in0=src(), scalar=dn[rows, 4 + h:5 + h],
                                in1=Hacc[rows, j, h * 128:(h + 1) * 128], op0=ALU.mult, op1=ALU.add),
                                reads=[('psh', 4 + h // 2, hf), ('rd', hf), ('Hacc', j)], writes=[('Hacc', j)])
            ml_pre(0)
            for step in range(len(order)):
                if step + 1 < len(order):
                    ml_pre(step + 1)
                ml_main(step)
            P.barrier()
        if dbg:
            P.dma('sp', dbg['HM'][b], Hacc, writes=['dbgHM'])
            P.dma('sp', dbg['edT'][b], edT[:], writes=['dbgedT'])
            P.dma('sp', dbg['wcB'][b], wcB[:], writes=['dbgwcB'])
            P.dma('sp', dbg['qkT'][b], qkT[:], writes=['dbgqkT'])
            P.dma('sp', dbg['vtok'][b], vtok[:], writes=['dbgvtok'])
            P.dma('sp', dbg['kTok'][b], kTok[:], writes=['dbgkTok'])
            P.barrier()
        win_load(wbuf[:], 1024, 512, 'w_cur')
        P.dma('sp', gnbc[:], g['mng_d'].to_broadcast([128, 512]), writes=['gnbc'])
        def mm_front(j):
            tk = slice(TC + j * 128, TC + (j + 1) * 128)
            xh = xn[:, (j % 2) * 512:(j % 2) * 512 + 512]
            pb = 2 + j % 2
            for k in range(KC):
                P.op('pe', lambda e, k=k: e.matmul(ps[pb][:, :], lhsT=uT[:, k, tk], rhs=wbuf[:, k, :], start=(k == 0),
                                                   stop=(k == KC - 1)), reads=['w_cur'], writes=[PS(pb)], inc=(k == KC - 1))
            P.op('act', lambda e: e.activation(out=xh, in_=ps[pb][:, :], func=AF.Sigmoid), reads=[PS(pb)], writes=[('xnh', j % 2)])

        def mm_back(j):
            xh = xn[:, (j % 2) * 512:(j % 2) * 512 + 512]
            per_head_norm_stats(j)
            P.op('dve', lambda e: e.tensor_tensor(out=rms_scratch[:, 0:512], in0=rms_scratch[:, 0:512], in1=gnbc[:], op=ALU.mult),
                 reads=['rms_scratch', 'gnbc'], writes=['rms_scratch'])
            P.op('dve', lambda e: e.tensor_tensor(out=a_tok[:, j, :], in0=rms_scratch[:, 0:512], in1=xh, op=ALU.mult),
                 reads=['rms_scratch', ('xnh', j % 2)], writes=[('a_tok', j)])

        mm_front(0)
        for j in range(NLT):
            if j + 1 < NLT:
                mm_front(j + 1)
            mm_back(j)
        P.barrier()

    mst.close()
    KSTOP = os.environ.get('KSTOP', '')
    rm = ones_nt
    with nc.sbuf_tensor(S("gv"), [128, NTT, 512], BF16) as gv, \
            nc.sbuf_tensor(S("qt"), [128, 2, NT], BF16) as qt, nc.sbuf_tensor(S("kt"), [128, 2, NT], BF16) as kt, \
            nc.sbuf_tensor(S("kraw"), [128, NT], BF16) as kraw, \
            nc.sbuf_tensor(S("lrT"), [16, NT], BF16) as lrT, \
            nc.sbuf_tensor(S("tA"), [128, NT], F32) as tA, nc.sbuf_tensor(S("tB"), [128, NT], F32) as tB, \
            nc.sbuf_tensor(S("ebL"), [128, 2, NCH], F32) as ebL, \
            nc.sbuf_tensor(S("Sst"), [128, 2, 128], F32) as Sst, nc.sbuf_tensor(S("SbfA"), [128, 2, 2, 128], BF16) as SbfA, \
            nc.sbuf_tensor(S("SbfB"), [128, 2, 2, 128], BF16) as SbfB:
        win_load(wbuf[:], 2064, 512, 'w_cur')
        for i in range(NTT):
            pb = 2 + i % 2
            for k in range(KC):
                P.op('pe', lambda e, i=i, k=k, pb=pb: e.matmul(ps[pb][:, :], lhsT=uT[:, k, i * 128:(i + 1) * 128], rhs=wbuf[:, k, :],
                                                               start=(k == 0), stop=(k == KC - 1)),
                     reads=['w_cur'], writes=[PS(pb)], inc=(k == KC - 1))
            P.op('act', lambda e, i=i, pb=pb: e.copy(out=gv[:, i, :], in_=ps[pb][:, :]), reads=[PS(pb)], writes=['gv'])
        P.barrier()
        DIRS = [int(c) for c in os.environ.get('KDIRS', '01')]
        if KSTOP == 'mlstm':
            DIRS = []
        for d in DIRS:
            endcol = 63 if d == 0 else 0
            win_load(wbuf[:], 1552, 512, 'w_cur')
            P.op('pool', lambda e: e.memset(rm[:], 1.0), writes=['rm'])
            zc = 0 if d == 0 else 63
            P.op('pool', lambda e: e.memset(rm[:].rearrange("p (c l) -> p c l", l=64)[:, :, zc:zc + 1], 0.0), writes=['rm'])
            for gj, (t0, n) in enumerate(groups):
                pb = 2 + gj % 2
                for k in range(KC):
                    P.op('pe', lambda e, k=k, pb=pb, t0=t0, n=n: e.matmul(
                        ps[pb][0:16, 0:n], lhsT=wlr_sb[:, k, d * 16:(d + 1) * 16], rhs=uT[:, k, t0:t0 + n],
                        start=(k == 0), stop=(k == KC - 1)), reads=['wlr_sb'], writes=[PS(pb)], inc=(k == KC - 1))
                P.op('act', lambda e, pb=pb, t0=t0, n=n: e.copy(out=lrT[:, t0:t0 + n], in_=ps[pb][0:16, 0:n]),
                     reads=[PS(pb)], writes=['lrT'])
            for jc in range(2):
                for gj, (t0, n) in enumerate(groups):
                    pb = 2 + gj % 2
                    P.op('pe', lambda e, pb=pb, t0=t0, n=n: e.matmul(
                        ps[pb][:, 0:n], lhsT=gw2_sb[:, d, jc * 128:(jc + 1) * 128], rhs=lrT[:, t0:t0 + n], start=True, stop=True),
                        reads=['gw2_sb', 'lrT'], writes=[PS(pb)])
                    P.op('act', lambda e, pb=pb, t0=t0, n=n: e.activation(
                        out=tA[:, t0:t0 + n], in_=ps[pb][:, 0:n], func=AF.Exp, scale=-1.0, bias=ggbT[:, d, jc:jc + 1]),
                        reads=[PS(pb), 'ggbT'], writes=['tA'])
                P.op('act', lambda e: e.activation(out=tA[:], in_=tA[:], func=AF.Ln, bias=one_c[:], scale=1.0), reads=['tA'],
                     writes=['tA'])
                if d == 0:
                    P.op('dve', lambda e: e.tensor_tensor_scan(out=tB[:], data0=rm[:], data1=tA[:], initial=0.0,
                                                               op0=ALU.mult, op1=ALU.add), reads=['tA', 'rm'], writes=['tB'])
                else:
                    P.op('dve', lambda e: e.tensor_tensor_scan(out=tB[:, ::-1], data0=rm[:, ::-1], data1=tA[:, ::-1], initial=0.0,
                                                               op0=ALU.mult, op1=ALU.add), reads=['tA', 'rm'], writes=['tB'])
                bL = tB[:].rearrange("p (c l) -> p c l", l=64)[:, :, endcol]
                P.op('act', lambda e: e.activation(out=ebL[:, jc, :], in_=bL, func=AF.Exp, scale=-1.0 / 16),
                     reads=['tB'], writes=['ebL'])
                P.op('act', lambda e: e.activation(out=tA[:], in_=tB[:], func=AF.Exp, scale=-1.0 / 16), reads=['tB', 'tA'],
                     writes=['tA'])
                proj_featmajor(wbuf, jc * 128, None,
                               lambda pb, t0, n: P.op('dve', lambda e: e.scalar_tensor_tensor(
                                   out=qt[:, jc, t0:t0 + n], in0=ps[pb][:, 0:n], scalar=0.125, in1=tA[:, t0:t0 + n],
                                   op0=ALU.mult, op1=ALU.mult), reads=[PS(pb), 'tA'], writes=['qt']))
                P.op('act', lambda e: e.activation(out=tA[:], in_=tB[:], func=AF.Exp, scale=1.0 / 16), reads=['tB', 'qt', 'tA'],
                     writes=['tA'])

                def evac_k(pb, t0, n):
                    P.op('dve', lambda e: e.tensor_copy(out=kraw[:, t0:t0 + n], in_=ps[pb][:, 0:n]), reads=[PS(pb)], writes=['kraw'])
                    P.op('dve', lambda e: e.tensor_tensor(out=kt[:, jc, t0:t0 + n], in0=ps[pb][:, 0:n], in1=tA[:, t0:t0 + n],
                                                          op=ALU.mult), reads=[PS(pb), 'tA'], writes=['kt'])
                proj_featmajor(wbuf, 256 + jc * 128, None, evac_k)
                P.op('dve', lambda e: e.tensor_tensor(out=tA[:].rearrange("p (c l) -> p c l", l=64),
                                                      in0=tB[:].rearrange("p (c l) -> p c l", l=64),
                                                      in1=bL.unsqueeze(2).to_broadcast([128, NCH, 64]), op=ALU.subtract),
                     reads=['tB', 'kt', 'tA'], writes=['tA'])
                P.op('act', lambda e: e.activation(out=tA[:], in_=tA[:], func=AF.Exp, scale=1.0 / 16), reads=['tA'], writes=['tA'])
                P.op('dve', lambda e: e.tensor_tensor(out=khT, in0=kraw[:], in1=tA[:], op=ALU.mult),
                     reads=['tA', 'kraw'], writes=['khT'])
                for i in range(NTT):
                    P.op('pe', lambda e, i=i: e.transpose(out=psb[1][:, 0:128], in_=khT[:, i * 128:(i + 1) * 128], identity=ident_b[:]),
                         reads=['khT', 'ident_b'], writes=[PS(1)])
                    P.op('dve', lambda e, i=i: e.tensor_copy(out=khTok[:, i, jc * 128:(jc + 1) * 128], in_=psb[1][:, 0:128]),
                         reads=[PS(1)], writes=['khTok'])
            P.barrier()
            P.op('pool', lambda e: e.memset(Sst[:], 0.0), writes=['Sst'])
            P.op('pool', lambda e: e.memset(SbfA[:], 0.0), writes=[('SbfA', 0), ('SbfA', 1)])
            P.op('pool', lambda e: e.memset(SbfB[:], 0.0), writes=[('SbfB', 0), ('SbfB', 1)])
            order = list(range(NCH)) if d == 0 else list(range(NCC - 1, -1, -1)) + list(range(NCH - 1, NCC - 1, -1))
            mask = masks[d]
            first_dir = (d == DIRS[0])

            def gl_geo(step):
                c = order[step]
                ti, p0 = c // 2, (c % 2) * 64
                return c, ti, slice(p0, p0 + 64), slice(c * 64, (c + 1) * 64), c >= NCC, 6 + step % 2, c % 2

            def gl_pre(step):
                c, ti, rows, toks, lat, pbD, hf = gl_geo(step)
                if lat:
                    for h in range(4):
                        hp = (h % 2) * 64
                        P.op('pe', lambda e, h=h, hp=hp: e.matmul(
                            ps[2 + h % 2][rows, (h // 2) * 64:(h // 2) * 64 + 64], lhsT=kt[hp:hp + 64, h // 2, toks],
                            rhs=qt[hp:hp + 64, h // 2, toks], start=True, stop=True), writes=[('psh', 2 + h % 2, hf)], inc=(h >= 2))
                for h in range(4):
                    P.op('pe', lambda e, h=h: e.matmul(
                        ps[pbD][(h % 2) * 64:(h % 2) * 64 + 64, (h // 2) * 128:(h // 2 + 1) * 128],
                        lhsT=khTok[rows, ti, h * 64:(h + 1) * 64], rhs=gv[rows, ti, h * 128:(h + 1) * 128], start=True, stop=True),
                        writes=[PS(pbD)], inc=(h == 3))
                if lat:
                    for par in range(2):
                        P.op('dve', lambda e, par=par: e.tensor_tensor(
                            out=Pt[rows, par, :, :], in0=ps[2 + par][rows, 0:128].rearrange("p (a c) -> p a c", c=64),
                            in1=mask[rows, :].unsqueeze(1).to_broadcast([64, 2, 64]), op=ALU.mult),
                            reads=[('psh', 2 + par, hf)], writes=[('Pt', hf, par)])

            def gl_main(step):
                c, ti, rows, toks, lat, pbD, hf = gl_geo(step)
                for pr in range(2):
                    P.op('dve', lambda e, pr=pr: e.scalar_tensor_tensor(
                        out=Sst[:, pr, :], in0=Sst[:, pr, :], scalar=ebL[:, pr, c:c + 1], in1=ps[pbD][:, pr * 128:(pr + 1) * 128],
                        op0=ALU.mult, op1=ALU.add), reads=['Sst', 'ebL', PS(pbD)], writes=['Sst'])
                P.op('act', lambda e: e.copy(out=SbfA[0:64, (step + 1) % 2], in_=Sst[0:64]), reads=['Sst'], writes=[('SbfA', (step + 1) % 2)])
                P.op('act', lambda e: e.copy(out=SbfB[64:128, (step + 1) % 2], in_=Sst[64:128]), reads=['Sst'], writes=[('SbfB', (step + 1) % 2)])

                if lat:
                    j = ti - NTC
                    for h in range(4):
                        par, pr = h % 2, h // 2
                        P.op('pe', lambda e, h=h, par=par, pr=pr: e.matmul(
                            ps[4][rows, h * 128:(h + 1) * 128], lhsT=Pt[rows, par, pr, :], rhs=gv[rows, ti, h * 128:(h + 1) * 128],
                            start=True, stop=False), reads=[('Pt', hf, par)], writes=[('psh', 4, hf)], inc=False)
                        P.op('pe', lambda e, h=h, par=par, pr=pr: e.matmul(
                            ps[4][rows, h * 128:(h + 1) * 128], lhsT=qt[:, pr, toks], rhs=(SbfA if par == 0 else SbfB)[:, step % 2, pr, :],
                            start=False, stop=True), reads=[('SbfA', step % 2), ('SbfB', step % 2)], writes=[('psh', 4, hf)], inc=(h == 3))
                    if first_dir:
                        P.op('act', lambda e: e.copy(out=Hacc[rows, j, :], in_=ps[4][rows, :]), reads=[('psh', 4, hf)],
                             writes=[('Hacc', j)])
                    else:
                        P.op('act', lambda e: e.copy(out=rms_scratch[rows, 0:512], in_=ps[4][rows, :]), reads=[('psh', 4, hf)],
                             writes=[('otmp', hf)])
                        P.op('pool', lambda e: e.tensor_tensor(out=Hacc[rows, j, :], in0=Hacc[rows, j, :],
                                                               in1=rms_scratch[rows, 0:512], op=ALU.add),
                             reads=[('otmp', hf), ('Hacc', j)], writes=[('Hacc', j)])
            gl_pre(0)
            for step in range(len(order)):
                if step + 1 < len(order):
                    gl_pre(step + 1)
                gl_main(step)
            P.barrier()
            if dbg and d == DIRS[0]:
                P.dma('sp', dbg['H1'][b], Hacc, writes=['dbgH1'])
                P.barrier()
        if dbg:
            P.dma('sp', dbg['H'][b], Hacc, writes=['dbgH'])
            P.dma('sp', dbg['qt'][b], qt[:], writes=['dbgqt'])
            P.dma('sp', dbg['kt'][b], kt[:], writes=['dbgkt'])
            P.dma('sp', dbg['gv'][b], gv[:], writes=['dbggv'])
            P.dma('sp', dbg['khTok'][b], khTok, writes=['dbgkh'])
            P.dma('sp', dbg['ebL'][b], ebL[:], writes=['dbgebl'])
            P.barrier()
        P.op('pool', lambda e: e.memset(ones_nt[:], 1.0), writes=['rm'])
        win_load(wbuf[:], 2576, 512, 'w_cur')
        P.dma('sp', gnbc[:], g['gng_d'].to_broadcast([128, 512]), writes=['gnbc'])

        def gm_front(j):
            tk = slice(TC + j * 128, TC + (j + 1) * 128)
            xh = xn[:, (j % 2) * 512:(j % 2) * 512 + 512]
            pb = 2 + j % 2
            for k in range(KC):
                P.op('pe', lambda e, k=k: e.matmul(ps[pb][:, :], lhsT=uT[:, k, tk], rhs=wbuf[:, k, :], start=(k == 0),
                                                   stop=(k == KC - 1)), reads=['w_cur'], writes=[PS(pb)], inc=(k == KC - 1))
            P.op('act', lambda e: e.activation(out=xh, in_=ps[pb][:, :], func=AF.Sigmoid), reads=[PS(pb)], writes=[('xnh', j % 2)])
            P.op('dve', lambda e: e.tensor_tensor(out=xh, in0=ps[pb][:, :], in1=xh, op=ALU.mult),
                 reads=[PS(pb), ('xnh', j % 2)], writes=[('xnh', j % 2)])

        def gm_back(j):
            xh = xn[:, (j % 2) * 512:(j % 2) * 512 + 512]
            per_head_norm_stats(j)
            P.op('dve', lambda e: e.tensor_tensor(out=rms_scratch[:, 0:512], in0=rms_scratch[:, 0:512], in1=gnbc[:], op=ALU.mult),
                 reads=['rms_scratch', 'gnbc'], writes=['rms_scratch'])
            P.op('dve', lambda e: e.tensor_tensor(out=g_tok(j), in0=rms_scratch[:, 0:512], in1=xh, op=ALU.mult),
                 reads=['rms_scratch', ('xnh', j % 2), ('Hacc', j)], writes=[('g_tok', j)])

        gm_front(0)
        for j in range(NLT):
            if j + 1 < NLT:
                gm_front(j + 1)
            gm_back(j)
        P.barrier()

    if dbg:
        P.dma('sp', dbg['uT'][b], uT[:], writes=['dbg_uT'])
        P.barrier()
    for j in range(NLT if KSTOP == '' else 0):
        for src_i, src in enumerate((a_tok[:, j, :], g_tok(j))):
            pb = src_i
            for q in range(4):
                P.op('pe', lambda e, q=q, src=src, pb=pb: e.transpose(out=psb[pb][:, q * 128:(q + 1) * 128],
                                                                      in_=src[:, q * 128:(q + 1) * 128], identity=ident_b[:]),
                     reads=['ident_b'], writes=[PS(pb)], inc=(q == 3))
            P.op('act' if src_i == 0 else 'dve',
                 (lambda e, j=j, pb=pb, src_i=src_i: e.copy(out=mT[:, 4 * src_i:4 * src_i + 4, j * 128:(j + 1) * 128],
                                                            in_=psb[pb][:, 0:512].rearrange("p (q c) -> p q c", c=128)))
                 if src_i == 0 else
                 (lambda e, j=j, pb=pb, src_i=src_i: e.tensor_copy(out=mT[:, 4 * src_i:4 * src_i + 4, j * 128:(j + 1) * 128],
                                                                   in_=psb[pb][:, 0:512].rearrange("p (q c) -> p q c", c=128))),
                 reads=[PS(pb)], writes=[('mT', j, src_i)])
    P.barrier()
    if dbg:
        P.dma('sp', dbg['mT'][b], mT, writes=['dbg_mT'])
        P.barrier()
    with nc.sbuf_tensor(S("wout"), [128, KC, D], BF16) as wout, nc.sbuf_tensor(S("xt2"), [128, 2, D], F32) as xt2, \
            nc.sbuf_tensor(S("pt2"), [128, 2, D], F32) as pt2:
        for half in range(2):
            P.dma('pool', wout[:, :, half * 512:(half + 1) * 512],
                  wout_d[:, half * 512:(half + 1) * 512].rearrange("(k p) c -> p k c", p=128), writes=['wout'])
        g['make_gbc'](b, 0)
        for j in range(NLT):
            bf = j % 2
            P.dma('sp', x_res[:, j, :], x_d[b, j * 128:(j + 1) * 128, :], writes=[('x_res', j)])
            P.dma('sp', pt2[:, bf, :], pe_d[j * 128:(j + 1) * 128, :], writes=[('pt2', bf)])
            P.op('pool', lambda e, j=j, bf=bf: e.tensor_tensor(out=x_res[:, j, :], in0=x_res[:, j, :], in1=pt2[:, bf, :], op=ALU.add),
                 reads=[('x_res', j), ('pt2', bf)], writes=[('x_res', j)])
            for half in range(2):
                pb = 2 + half
                hs = slice(half * 512, (half + 1) * 512)
                for k in range(KC):
                    P.op('pe', lambda e, k=k, j=j, pb=pb, hs=hs: e.matmul(ps[pb][:, :], lhsT=mT[:, k, j * 128:(j + 1) * 128],
                                                                          rhs=wout[:, k, hs], start=(k == 0), stop=(k == KC - 1)),
                         reads=['wout'], writes=[PS(pb)], inc=(k == KC - 1))
                P.op('dve', lambda e, pb=pb, hs=hs, bf=bf: e.tensor_tensor(out=xt2[:, bf, hs], in0=ps[pb][:, :], in1=gbc[:, hs],
                                                                           op=ALU.mult), reads=[PS(pb), 'gbc'], writes=[('xt2', bf, half)])
                if dbg:
                    pass
                P.op('pool', lambda e, j=j, hs=hs, bf=bf: e.tensor_tensor(out=x_res[:, j, hs], in0=x_res[:, j, hs], in1=xt2[:, bf, hs],
                                                                          op=ALU.add), reads=[('xt2', bf, half), ('x_res', j)],
                     writes=[('x_res', j)])
        P.barrier()
    st.close()


_CACHE = {}


def _get_nc(cfg_key):
    if cfg_key not in _CACHE:
        _CACHE[cfg_key] = build(Cfg(*cfg_key))
    return _CACHE[cfg_key]


def make_in_maps(inputs, n_cores, NB):
    f = lambda a: np.ascontiguousarray(np.asarray(a, dtype=np.float32))
    shared = {
        "c_ctx": f(inputs["c_ctx"]).reshape(1, D),
        "ada_w": f(inputs["ada_w"])[0], "ada_b": f(inputs["ada_b"]).reshape(1, -1),
        "norm1_g": f(inputs["norm1_g"]).reshape(1, D), "w_in": f(inputs["w_in"])[0],
        "ml_conv_w": f(inputs["ml_conv_w"])[0], "ml_conv_b": f(inputs["ml_conv_b"]).reshape(1, -1),
        "ml_gate_b": f(inputs["ml_gate_b"])[0], "ml_norm_g": f(inputs["ml_norm_g"]).reshape(1, -1),
        "gla_gate_w2": f(inputs["gla_gate_w2"])[0], "gla_gate_b": f(inputs["gla_gate_b"])[0],
        "gla_norm_g": f(inputs["gla_norm_g"]).reshape(1, -1), "w_out": f(inputs["w_out"])[0],
        "norm2_g": f(inputs["norm2_g"]).reshape(1, D), "router_w": f(inputs["router_w"])[0],
        "router_b": f(inputs["router_b"]).reshape(1, -1), "moe_w_gu": f(inputs["moe_w_gu"])[0],
        "moe_b_gu": f(inputs["moe_b_gu"])[0], "moe_w_down": f(inputs["moe_w_down"])[0],
        "moe_b_down": f(inputs["moe_b_down"])[0], "final_norm_g": f(inputs["final_norm_g"]).reshape(1, D),
    }
    x, c, ctx = f(inputs["x"]), f(inputs["c"]), f(inputs["ctx"])
    maps = []
    for i in range(n_cores):
        m = dict(shared)
        m["x"] = x[i * NB:(i + 1) * NB]
        m["c"] = c[i * NB:(i + 1) * NB]
        m["ctx"] = ctx[i * NB:(i + 1) * NB]
        maps.append(m)
    return maps


def kernel(**inputs):
    n_cores = 8
    B = inputs["x"].shape[0]
    NB = B // n_cores
    T = inputs["x"].shape[1]
    TC = inputs["ctx"].shape[1]
    E = inputs["router_w"].shape[-1]
    nc = _get_nc((NB, T, TC, E, False, 99))
    maps = make_in_maps(inputs, n_cores, NB)
    res = run_bass_kernel_spmd(nc, maps, core_ids=list(range(n_cores)))
    return np.concatenate([r["out"] for r in res.results], axis=0)
```

```python
import math
import os
import types
import numpy as np
from contextlib import ExitStack
import concourse.bass as bass
import concourse.mybir as mybir
from concourse.bass_utils import run_bass_kernel_spmd

F32 = mybir.dt.float32
BF16 = mybir.dt.bfloat16
AF = mybir.ActivationFunctionType
ALU = mybir.AluOpType
AX = mybir.AxisListType

D = 1024
KC = 8
EPS = 1e-6
LIM = 7.0
ALPHA = 1.702


class Cfg:
    def __init__(self, NB=4, T=2048, TC=256, E=32, debug=False, stages=99):
        self.NB, self.T, self.TC, self.E = NB, T, TC, E
        self.NT = T + TC
        self.NTT = self.NT // 128
        self.NTC = TC // 128
        self.NLT = T // 128
        self.NCH = self.NT // 64
        self.NCC = TC // 64
        self.debug = debug
        self.stages = stages


def _snapshot(fn):
    if fn is None or fn.__closure__ is None:
        return fn
    cells = []
    for c in fn.__closure__:
        try:
            cells.append(types.CellType(c.cell_contents))
        except ValueError:
            cells.append(c)
    return types.FunctionType(fn.__code__, fn.__globals__, fn.__name__, fn.__defaults__, tuple(cells))


class Prog:
    ENGS = ('pe', 'act', 'dve', 'pool', 'sp')

    def __init__(self, nc, st):
        self.nc = nc
        self.sem = {e: st.enter_context(nc.semaphore('sem_' + e)) for e in self.ENGS}
        self.cnt = dict.fromkeys(self.ENGS, 0)
        self.seen = {e: {} for e in self.ENGS}
        self.streams = {e: [] for e in self.ENGS}
        self.res = {}
        self.pend = {e: [] for e in self.ENGS}
        self.dsem, self.dcnt, self.drr = {}, {}, {}
        for q, n in (('sp', 14), ('pool', 10), ('act', 4)):
            self.dsem[q] = [st.enter_context(nc.semaphore(f'dma_{q}{i}')) for i in range(n)]
            self.dcnt[q] = [0] * n
            self.drr[q] = 0
        self.all_dma_events = []

    def _need(self, eng, ev, waits, raw):
        key, sem, val = ev
        if key == eng and not raw and eng == 'pe':
            return
        if val is None:
            raise RuntimeError(f'pending event consumed: {key} by {eng}')
        if self.seen[eng].get(key, 0) >= val:
            return
        if key in waits and waits[key][2] >= val:
            return
        waits[key] = (key, sem, val)

    def _deps(self, eng, reads, writes):
        waits = {}
        for r in reads:
            s = self.res.get(r)
            if s and s[0] is not None:
                self._need(eng, s[0], waits, True)
        for w in writes:
            s = self.res.get(w)
            if s:
                if s[0] is not None:
                    self._need(eng, s[0], waits, False)
                for ev in s[1].values():
                    self._need(eng, ev, waits, False)
        wl = list(waits.values())
        for key, sem, val in wl:
            self.seen[eng][key] = max(self.seen[eng].get(key, 0), val)
        return wl

    def _register(self, ev, reads, writes):
        for r in reads:
            s = self.res.setdefault(r, [None, {}])
            s[1][ev[0]] = ev
        for w in writes:
            self.res[w] = [ev, {}]

    def op(self, eng, fn, reads=(), writes=(), inc=True):
        fn = _snapshot(fn)
        wl = self._deps(eng, reads, writes)
        if inc:
            self.cnt[eng] += 1
            ev = [eng, self.sem[eng], self.cnt[eng]]
            for p in self.pend[eng]:
                p[2] = self.cnt[eng]
            self.pend[eng] = []
        else:
            ev = [eng, self.sem[eng], None]
            self.pend[eng].append(ev)
        self._register(ev, reads, writes)
        self.streams[eng].append((wl, fn, 'inc' if inc else None))

    def dma(self, q, out, in_, reads=(), writes=(), **kw):
        k = self.drr[q]
        self.drr[q] = (k + 1) % len(self.dsem[q])
        sem = self.dsem[q][k]
        key = ('d', q, k)
        wl = self._deps(q, reads, writes)
        prev = self.dcnt[q][k]
        if prev > 0 and self.seen[q].get(key, 0) < 16 * prev:
            wl.append((key, sem, 16 * prev))
            self.seen[q][key] = 16 * prev
        self.dcnt[q][k] += 1
        ev = [key, sem, 16 * self.dcnt[q][k]]
        self._register(ev, reads, writes)
        self.all_dma_events.append(ev)

        def fn(e, out=out, in_=in_, kw=kw, sem=sem):
            e.dma_start(out=out, in_=in_, **kw).then_inc(sem, 16)
        self.streams[q].append((wl, fn, 'dma'))

    def barrier(self):
        for e in self.ENGS:
            wl = []
            for o in self.ENGS:
                if self.cnt[o] > self.seen[e].get(o, 0):
                    if self.pend[o]:
                        raise RuntimeError('barrier with pending non-inc ops on ' + o)
                    wl.append((o, self.sem[o], self.cnt[o]))
                    self.seen[e][o] = self.cnt[o]
            for q in self.dsem:
                for k, c in enumerate(self.dcnt[q]):
                    key = ('d', q, k)
                    if c > 0 and self.seen[e].get(key, 0) < 16 * c:
                        wl.append((key, self.dsem[q][k], 16 * c))
                        self.seen[e][key] = 16 * c
            if wl:
                self.streams[e].append((wl, None, None))
        self.res = {}

    def emit(self, block):
        decos = {'pe': block.tensor, 'act': block.scalar, 'dve': block.vector,
                 'pool': block.gpsimd, 'sp': block.sync}
        for name in self.ENGS:
            stream = self.streams[name]
            sem_e = self.sem[name]

            def body(e, stream=stream, sem_e=sem_e):
                for wl, fn, kind in stream:
                    for key, sem, val in wl:
                        e.wait_ge(sem, val)
                    if fn is None:
                        continue
                    ins = fn(e)
                    if kind == 'inc':
                        ins.then_inc(sem_e, 1)
            decos[name](body)


def build(cfg):
    NB, T, TC, E = cfg.NB, cfg.T, cfg.TC, cfg.E
    NT, NTT, NTC, NLT, NCH, NCC = cfg.NT, cfg.NTT, cfg.NTC, cfg.NLT, cfg.NCH, cfg.NCC
    NBC = NB + 1
    nc = bass.Bass("TRN2", target_bir_lowering=False)

    def din(name, shape):
        return nc.dram_tensor(name, list(shape), F32, kind="ExternalInput").ap()

    x_d = din("x", [NB, T, D])
    c_d = din("c", [NB, D])
    ctx_d = din("ctx", [NB, TC, D])
    cctx_d = din("c_ctx", [1, D])
    adaw_d = din("ada_w", [D, 6 * D])
    adab_d = din("ada_b", [1, 6 * D])
    n1g_d = din("norm1_g", [1, D])
    win_d = din("w_in", [D, 3120])
    cw_d = din("ml_conv_w", [3, 512])
    cb_d = din("ml_conv_b", [1, 512])
    mgb_d = din("ml_gate_b", [4, 4])
    mng_d = din("ml_norm_g", [1, 512])
    gw2_d = din("gla_gate_w2", [2, 16, 256])
    ggb_d = din("gla_gate_b", [2, 256])
    gng_d = din("gla_norm_g", [1, 512])
    wout_d = din("w_out", [D, D])
    n2g_d = din("norm2_g", [1, D])
    rw_d = din("router_w", [D, E])
    rb_d = din("router_b", [1, E])
    wgu_d = din("moe_w_gu", [E, D, 2 * D])
    bgu_d = din("moe_b_gu", [E, 2 * D])
    wdn_d = din("moe_w_down", [E, D, D])
    bdn_d = din("moe_b_down", [E, D])
    fng_d = din("final_norm_g", [1, D])
    out_d = nc.dram_tensor("out", [NB, T, D], F32, kind="ExternalOutput").ap()
    pe_d = nc.dram_tensor("pe_scratch", [T, D], F32, kind="Internal").ap()
    gsc_d = nc.dram_tensor("gate_scratch", [NBC, 2, D], F32, kind="Internal").ap()
    dbg = {}
    if cfg.debug:
        dbg['xmid'] = nc.dram_tensor("dbg_xmid", [NB, T, D], F32, kind="ExternalOutput").ap()
        dbg['mT'] = nc.dram_tensor("dbg_mT", [NB, 128, KC, T], BF16, kind="ExternalOutput").ap()
        dbg['uT'] = nc.dram_tensor("dbg_uT", [NB, 128, KC, NT], BF16, kind="ExternalOutput").ap()
        dbg['H'] = nc.dram_tensor("dbg_H", [NB, 128, NLT, 512], F32, kind="ExternalOutput").ap()
        dbg['H1'] = nc.dram_tensor("dbg_H1", [NB, 128, NLT, 512], F32, kind="ExternalOutput").ap()
        dbg['HM'] = nc.dram_tensor("dbg_HM", [NB, 128, NLT, 512], F32, kind="ExternalOutput").ap()
        dbg['edT'] = nc.dram_tensor("dbg_edT", [NB, 128, NTT, 8], F32, kind="ExternalOutput").ap()
        dbg['wcB'] = nc.dram_tensor("dbg_wcB", [NB, 128, 2, NCH], F32, kind="ExternalOutput").ap()
        dbg['qkT'] = nc.dram_tensor("dbg_qkT", [NB, 128, 4, NT], BF16, kind="ExternalOutput").ap()
        dbg['vtok'] = nc.dram_tensor("dbg_vtok", [NB, 128, NTT, 4, 129], BF16, kind="ExternalOutput").ap()
        dbg['kTok'] = nc.dram_tensor("dbg_kTok", [NB, 128, NTT, 256], BF16, kind="ExternalOutput").ap()
        dbg['qt'] = nc.dram_tensor("dbg_qt", [NB, 128, 2, NT], BF16, kind="ExternalOutput").ap()
        dbg['kt'] = nc.dram_tensor("dbg_kt", [NB, 128, 2, NT], BF16, kind="ExternalOutput").ap()
        dbg['gv'] = nc.dram_tensor("dbg_gv", [NB, 128, NTT, 512], BF16, kind="ExternalOutput").ap()
        dbg['khTok'] = nc.dram_tensor("dbg_khTok", [NB, 128, NTT, 256], BF16, kind="ExternalOutput").ap()
        dbg['ebL'] = nc.dram_tensor("dbg_ebL", [NB, 128, 2, NCH], F32, kind="ExternalOutput").ap()

    st = ExitStack()
    P = Prog(nc, st)

    def sb(name, shape, dt=F32):
        return st.enter_context(nc.sbuf_tensor(name, list(shape), dt))

    ps = [st.enter_context(nc.psum_tensor(f"ps{i}", [128, 512], F32)) for i in range(8)]
    psb = [p[:].bitcast(BF16) for p in ps]

    def PS(i):
        return ('ps', i)

    ident_f = sb("ident_f", [128, 128])
    ident_b = sb("ident_b", [128, 128], BF16)
    ones_f = sb("ones_f", [128, 128])
    ones_nt = sb("ones_nt", [128, NT], BF16)
    maskF = sb("maskF", [128, 64])
    maskB = sb("maskB", [128, 64])
    selh = sb("selh", [4, 2, 2, 64])
    eps_c = sb("eps_c", [128, 1])
    one_c = sb("one_c", [128, 1])
    P.op('pool', lambda e: e.memset(one_c[:], 1.0), writes=['one_c'])
    P.op('pool', lambda e: e.memset(eps_c[:], EPS), writes=['eps_c'])
    P.op('pool', lambda e: e.memset(ones_f[:], 1.0), writes=['ones_f'])
    P.op('pool', lambda e: e.memset(ones_nt[:], 1.0), writes=['ones_nt'])
    P.op('pool', lambda e: e.affine_select(out=ident_f[:], in_=ones_f[:], pattern=[[-1, 128]],
                                           compare_op=ALU.is_equal, fill=0.0, base=0, channel_multiplier=1),
         reads=['ones_f'], writes=['ident_f'])
    P.op('dve', lambda e: e.tensor_copy(out=ident_b[:], in_=ident_f[:]), reads=['ident_f'], writes=['ident_b'])
    for half in range(2):
        sl = slice(half * 64, half * 64 + 64)
        P.op('pool', lambda e, sl=sl: e.affine_select(out=maskF[sl, :], in_=ones_f[sl, 0:64], pattern=[[1, 64]],
                                                      compare_op=ALU.is_ge, fill=0.0, base=0, channel_multiplier=-1),
             reads=['ones_f'], writes=['maskF'])
        P.op('pool', lambda e, sl=sl: e.affine_select(out=maskB[sl, :], in_=ones_f[sl, 0:64], pattern=[[-1, 64]],
                                                      compare_op=ALU.is_ge, fill=0.0, base=0, channel_multiplier=1),
             reads=['ones_f'], writes=['maskB'])
    P.op('pool', lambda e: e.affine_select(out=selh[:].rearrange("p a b c -> p (a b c)"), in_=ones_nt[0:4, 0:256],
                                           pattern=[[-2, 2], [-1, 2], [0, 64]], compare_op=ALU.is_equal, fill=0.0, base=0,
                                           channel_multiplier=1),
         reads=['ones_nt'], writes=['selh'])

    modT = sb("modT", [128, 4, KC, NBC])
    rw_sb = sb("rw_sb", [128, KC, E])
    rb_bc = sb("rb_bc", [128, E])
    bguT = sb("bguT", [128, 16, E])
    cw_sb = sb("cw_sb", [128, 4, 3])
    cb_sb = sb("cb_sb", [128, 4])
    mgb_sb = sb("mgb_sb", [4, 4])
    wg_sb = sb("wg_sb", [128, KC, 16], BF16)
    wlr_sb = sb("wlr_sb", [128, KC, 32], BF16)
    gw2_sb = sb("gw2_sb", [16, 2, 256], BF16)
    ggbT = sb("ggbT", [128, 2, 2])
    nc_allow = nc.allow_non_contiguous_dma(reason="tiny param layouts")
    st.enter_context(nc_allow)

    P.dma('sp', rw_sb[:], rw_d.rearrange("(k p) e -> p k e", p=128), writes=['rw_sb'])
    P.dma('sp', rb_bc[:], rb_d.to_broadcast([128, E]), writes=['rb_bc'])
    for q in range(4):
        P.dma('sp', cw_sb[:, q, :], cw_d[:, q * 128:(q + 1) * 128].rearrange("i p -> p i"), writes=['cw_sb'])
    P.dma('sp', cb_sb[:], cb_d.rearrange("o (q p) -> p (o q)", p=128), writes=['cb_sb'])
    P.dma('sp', mgb_sb[:], mgb_d.rearrange("g h -> h g"), writes=['mgb_sb'])
    P.dma('sp', ggbT[:], ggb_d.rearrange("z (j p) -> p z j", p=128), writes=['ggbT'])
    P.dma('pool', wg_sb[:], win_d[:, 1536:1552].rearrange("(k p) c -> p k c", p=128), writes=['wg_sb'])
    P.dma('pool', wlr_sb[:], win_d[:, 3088:3120].rearrange("(k p) c -> p k c", p=128), writes=['wlr_sb'])
    P.dma('pool', gw2_sb[:], gw2_d.rearrange("z r c -> r z c"), writes=['gw2_sb'])
    with nc.sbuf_tensor("bgu_rows", [E, 2 * D], F32) as bgu_rows:
        P.dma('sp', bgu_rows[:], bgu_d, writes=['bgu_rows'])
        for j in range(16):
            P.op('pe', lambda e, j=j: e.transpose(out=ps[0][:, j * E:(j + 1) * E], in_=bgu_rows[:, j * 128:(j + 1) * 128],
                                                  identity=ident_f[0:E, 0:E]), reads=['bgu_rows', 'ident_f'], writes=[PS(0)],
                 inc=(j == 15))
        P.op('dve', lambda e: e.tensor_copy(out=bguT[:].rearrange("p j e -> p (j e)"), in_=ps[0][:, 0:16 * E]),
             reads=[PS(0)], writes=['bguT'])
        P.op('dve', lambda e: e.tensor_scalar(out=bguT[:, 8:16, :], in0=bguT[:, 8:16, :], scalar1=1.0, scalar2=None, op0=ALU.add),
             reads=['bguT'], writes=['bguT'])
        P.barrier()
    P.op('dve', lambda e: e.tensor_scalar(out=ggbT[:], in0=ggbT[:], scalar1=-1.0, scalar2=None, op0=ALU.mult),
         reads=['ggbT'], writes=['ggbT'])

    with nc.sbuf_tensor("om", [128, 256], F32) as om, nc.sbuf_tensor("jf", [128, 256], F32) as jf, \
            nc.sbuf_tensor("pidx", [128, 2], F32) as pidx, nc.sbuf_tensor("arg", [128, 256], F32) as arg, \
            nc.sbuf_tensor("petile", [128, 2, D], F32) as petile, nc.sbuf_tensor("omr0", [128, 256], F32) as omr0, \
            nc.sbuf_tensor("argc", [128, 256], F32) as argc, nc.sbuf_tensor("argr", [128, 256], F32) as argr, \
            nc.sbuf_tensor("arg2", [128, 256], F32) as arg2:
        P.op('pool', lambda e: e.iota(out=jf[:], pattern=[[1, 256]], base=0, channel_multiplier=0,
                                      allow_small_or_imprecise_dtypes=True), writes=['jf'])
        P.op('act', lambda e: e.activation(out=om[:], in_=jf[:], func=AF.Exp, scale=-math.log(10000.0) / 256.0),
             reads=['jf'], writes=['om'])
        for half in range(2):
            sl = slice(half * 64, half * 64 + 64)
            P.op('pool', lambda e, sl=sl: e.iota(out=pidx[sl, 0:1], pattern=[[0, 1]], base=0, channel_multiplier=1,
                                                 allow_small_or_imprecise_dtypes=True), writes=['pidx'])
            P.op('pool', lambda e, sl=sl, half=half: e.memset(pidx[sl, 1:2], float(half)), writes=['pidx'])
        PI = math.pi

        def sincos(dst_sin, dst_cos, argap, rd):
            MAGIC = 12582912.0
            for dst, off in ((dst_sin, 0.0), (dst_cos, 0.5 * PI)):
                P.op('dve', lambda e, off=off: e.tensor_scalar(out=arg2[:], in0=argap, scalar1=off, scalar2=None, op0=ALU.add),
                     reads=rd, writes=['arg2'])
                P.op('dve', lambda e: e.tensor_scalar(out=arg[:], in0=arg2[:], scalar1=1.0 / (2 * PI), scalar2=MAGIC,
                                                      op0=ALU.mult, op1=ALU.add), reads=['arg2'], writes=['arg'])
                P.op('dve', lambda e: e.tensor_scalar(out=arg[:], in0=arg[:], scalar1=-MAGIC, scalar2=None, op0=ALU.add),
                     reads=['arg'], writes=['arg'])
                P.op('dve', lambda e: e.scalar_tensor_tensor(out=arg[:], in0=arg[:], scalar=-2 * PI, in1=arg2[:],
                                                             op0=ALU.mult, op1=ALU.add), reads=['arg', 'arg2'], writes=['arg'])
                P.op('dve', lambda e: e.tensor_scalar(out=arg[:], in0=arg[:], scalar1=-PI, scalar2=PI, op0=ALU.max, op1=ALU.min),
                     reads=['arg'], writes=['arg'])
                P.op('act', lambda e, dst=dst: e.activation(out=dst, in_=arg[:], func=AF.Sin), reads=['arg'],
                     writes=['petile'])
        P.op('dve', lambda e: e.tensor_scalar(out=omr0[:], in0=om[:], scalar1=pidx[:, 1:2], scalar2=None, op0=ALU.mult),
             reads=['om', 'pidx'], writes=['omr0'])
        P.op('dve', lambda e: e.tensor_scalar(out=argc[:], in0=om[:], scalar1=pidx[:, 0:1], scalar2=None, op0=ALU.mult),
             reads=['om', 'pidx'], writes=['argc'])
        for k in range(NLT):
            buf = k % 2
            if k < 2:
                sincos(petile[:, buf, 512:768], petile[:, buf, 768:1024], argc[:], ['argc'])
            P.op('dve', lambda e, k=k: e.scalar_tensor_tensor(out=argr[:], in0=om[:], scalar=float(2 * k), in1=omr0[:],
                                                              op0=ALU.mult, op1=ALU.add), reads=['om', 'omr0'], writes=['argr'])
            sincos(petile[:, buf, 0:256], petile[:, buf, 256:512], argr[:], ['argr'])
            P.dma('sp', pe_d[k * 128:(k + 1) * 128, :], petile[:, buf, :], reads=['petile'], writes=['pe_d'])
        P.barrier()

    with nc.sbuf_tensor("cin", [NBC, D], F32) as cin, nc.sbuf_tensor("csig", [NBC, D], F32) as csig, \
            nc.sbuf_tensor("scT", [128, KC, NBC], F32) as scT, nc.sbuf_tensor("adab", [1, 6 * D], F32) as adab, \
            nc.sbuf_tensor("modrows", [NBC, 6 * D], F32) as modrows, \
            nc.sbuf_tensor("adaw", [128, 2, KC, 512], F32) as adaw, \
            nc.sbuf_tensor("grows", [NBC, 2, D], F32) as grows, \
            nc.sbuf_tensor("n1bc", [NBC, D], F32) as n1bc, nc.sbuf_tensor("n2bc", [NBC, D], F32) as n2bc:
        P.dma('sp', n1bc[:], n1g_d.to_broadcast([NBC, D]), writes=['n1bc'])
        P.dma('sp', n2bc[:], n2g_d.to_broadcast([NBC, D]), writes=['n2bc'])
        P.dma('sp', cin[0:NB, :], c_d, writes=['cin'])
        P.dma('sp', cin[NB:NBC, :], cctx_d, writes=['cin'])
        P.dma('sp', adab[:], adab_d, writes=['adab'])
        P.op('act', lambda e: e.activation(out=csig[:], in_=cin[:], func=AF.Sigmoid), reads=['cin'], writes=['csig'])
        P.op('dve', lambda e: e.tensor_tensor(out=csig[:], in0=csig[:], in1=cin[:], op=ALU.mult),
             reads=['csig', 'cin'], writes=['csig'])
        for k in range(KC):
            P.op('pe', lambda e, k=k: e.transpose(out=ps[0][:, k * NBC:(k + 1) * NBC], in_=csig[:, k * 128:(k + 1) * 128],
                                                  identity=ident_f[0:NBC, 0:NBC]),
                 reads=['csig', 'ident_f'], writes=[PS(0)], inc=(k == KC - 1))
        P.op('dve', lambda e: e.tensor_copy(out=scT[:].rearrange("p k b -> p (k b)"), in_=ps[0][:, 0:KC * NBC]),
             reads=[PS(0)], writes=['scT'])
        for cg in range(12):
            buf = cg % 2
            P.dma('sp', adaw[:, buf], adaw_d[:, cg * 512:(cg + 1) * 512].rearrange("(k p) c -> p k c", p=128),
                  writes=[('adaw', buf)])
            pb = 1 + (cg % 2)
            for k in range(KC):
                P.op('pe', lambda e, k=k, buf=buf, pb=pb: e.matmul(ps[pb][0:NBC, :], lhsT=scT[:, k, :], rhs=adaw[:, buf, k, :],
                                                                   start=(k == 0), stop=False),
                     reads=['scT', ('adaw', buf)], writes=[PS(pb)], inc=False)
            P.op('pe', lambda e, cg=cg, pb=pb: e.matmul(ps[pb][0:NBC, :], lhsT=ones_f[0:1, 0:NBC],
                                                        rhs=adab[0:1, cg * 512:(cg + 1) * 512], start=False, stop=True),
                 reads=['ones_f', 'adab'], writes=[PS(pb)])
            P.op('act', lambda e, cg=cg, pb=pb: e.copy(out=modrows[:, cg * 512:(cg + 1) * 512], in_=ps[pb][0:NBC, :]),
                 reads=[PS(pb)], writes=['modrows'])
        P.op('dve', lambda e: e.scalar_tensor_tensor(out=grows[:, 0, :], in0=modrows[:, D:2 * D], scalar=1.0, in1=n1bc[:],
                                                     op0=ALU.add, op1=ALU.mult), reads=['modrows', 'n1bc'], writes=['grows'])
        P.op('dve', lambda e: e.scalar_tensor_tensor(out=grows[:, 1, :], in0=modrows[:, 4 * D:5 * D], scalar=1.0, in1=n2bc[:],
                                                     op0=ALU.add, op1=ALU.mult), reads=['modrows', 'n2bc'], writes=['grows'])
        srcs = [grows[:, 0, :], modrows[:, 0:D], grows[:, 1, :], modrows[:, 3 * D:4 * D]]
        for m in range(4):
            for k in range(KC):
                o = (m * KC + k) * NBC
                P.op('pe', lambda e, m=m, k=k, o=o: e.transpose(out=ps[3][:, o:o + NBC], in_=srcs[m][:, k * 128:(k + 1) * 128],
                                                                identity=ident_f[0:NBC, 0:NBC]),
                     reads=['grows', 'modrows', 'ident_f'], writes=[PS(3)], inc=(m == 3 and k == KC - 1))
        P.op('dve', lambda e: e.tensor_copy(out=modT[:].rearrange("p m k b -> p (m k b)"), in_=ps[3][:, 0:4 * KC * NBC]),
             reads=[PS(3)], writes=['modT'])
        P.dma('sp', gsc_d[:, 0, :], modrows[:, 2 * D:3 * D], reads=['modrows'], writes=['gsc_d'])
        P.dma('sp', gsc_d[:, 1, :], modrows[:, 5 * D:6 * D], reads=['modrows'], writes=['gsc_d'])
        P.barrier()

    def rms_to_featmajor(xt_ap, xt_res, dstT, tok0, gsel, bsel, scratch, xn, ssq, pbanks, extra_f32=None, extra_res='extra_f32'):
        P.op('act', lambda e: e.activation(out=scratch, in_=xt_ap, func=AF.Square, accum_out=ssq),
             reads=[xt_res], writes=['rms_scratch', 'ssq'])
        P.op('act', lambda e: e.activation(out=ssq, in_=ssq, func=AF.Sqrt, scale=1.0 / D, bias=eps_c[:]),
             reads=['ssq'], writes=['ssq'])
        P.op('dve', lambda e: e.reciprocal(out=ssq, in_=ssq), reads=['ssq'], writes=['ssq'])
        P.op('dve', lambda e: e.tensor_scalar(out=xn, in0=xt_ap, scalar1=ssq, scalar2=None, op0=ALU.mult),
             reads=['ssq', xt_res], writes=['xn'])
        for k in range(KC):
            pb = pbanks[k // 4]
            P.op('pe', lambda e, k=k, pb=pb: e.transpose(out=ps[pb][:, (k % 4) * 128:(k % 4 + 1) * 128],
                                                         in_=xn[:, k * 128:(k + 1) * 128], identity=ident_f[:]),
                 reads=['xn', 'ident_f'], writes=[PS(pb)], inc=(k % 4 == 3))
        for k in range(KC):
            pb = pbanks[k // 4]
            P.op('act', lambda e, k=k, pb=pb: e.activation(out=dstT[:, k, tok0:tok0 + 128],
                                                           in_=ps[pb][:, (k % 4) * 128:(k % 4 + 1) * 128], func=AF.Identity,
                                                           scale=modT[:, gsel, k, bsel:bsel + 1],
                                                           bias=modT[:, gsel + 1, k, bsel:bsel + 1]),
                 reads=[PS(pb), 'modT'], writes=[('dstT', tok0)])
            if extra_f32 is not None:
                P.op('act', lambda e, k=k, pb=pb: e.activation(out=extra_f32[:, k, :],
                                                               in_=ps[pb][:, (k % 4) * 128:(k % 4 + 1) * 128], func=AF.Identity,
                                                               scale=modT[:, gsel, k, bsel:bsel + 1],
                                                               bias=modT[:, gsel + 1, k, bsel:bsel + 1]),
                     reads=[PS(pb), 'modT'], writes=[extra_res])

    ARENA = max(NLT * D, NLT * 512 + max(3 * NT, NLT * 256 + NTT * 128 + NT // 2 + 64))
    arena = sb("arena", [128, ARENA])
    x_res = arena[:, 0:NLT * D].rearrange("p (j c) -> p j c", c=D)
    gbc = sb("gbc", [128, D])
    rms_scratch = sb("rms_scratch", [128, D])
    xn = sb("xn", [128, D])
    ssq = sb("ssq", [128, 1])

    def make_gbc(b, which):
        P.dma('sp', gbc[:], gsc_d[b:b + 1, which, :].to_broadcast([128, D]), writes=['gbc'])

    for b in range(NB):
        if cfg.stages >= 2:
            build_mixer(nc, P, cfg, b, dict(
                ps=ps, psb=psb, PS=PS, ident_f=ident_f, ident_b=ident_b, ones_f=ones_f, ones_nt=ones_nt, maskF=maskF,
                maskB=maskB, selh=selh, modT=modT, x_d=x_d, ctx_d=ctx_d, pe_d=pe_d, win_d=win_d, wout_d=wout_d,
                cw_sb=cw_sb, cb_sb=cb_sb, mgb_sb=mgb_sb, wg_sb=wg_sb, wlr_sb=wlr_sb, gw2_sb=gw2_sb, ggbT=ggbT,
                mng_d=mng_d, gng_d=gng_d, x_res=x_res, arena=arena, eps_c=eps_c, one_c=one_c, gbc=gbc,
                rms_scratch=rms_scratch, xn=xn, ssq=ssq,
                make_gbc=make_gbc, rms_to_featmajor=rms_to_featmajor, dbg=dbg))
        else:
            with nc.sbuf_tensor(f"pt0_{b}", [128, 2, D], F32) as pt0:
                for j in range(NLT):
                    P.dma('sp', x_res[:, j, :], x_d[b, j * 128:(j + 1) * 128, :], writes=[('x_res', j)])
                    P.dma('sp', pt0[:, j % 2, :], pe_d[j * 128:(j + 1) * 128, :], reads=['pe_d'], writes=[('pt0', j % 2)])
                    P.op('dve', lambda e, j=j: e.tensor_tensor(out=x_res[:, j, :], in0=x_res[:, j, :], in1=pt0[:, j % 2, :],
                                                               op=ALU.add), reads=[('x_res', j), ('pt0', j % 2)],
                         writes=[('x_res', j)])
                P.barrier()
        if cfg.debug:
            for j in range(NLT):
                P.dma('sp', dbg['xmid'][b, j * 128:(j + 1) * 128, :], x_res[:, j, :], reads=[('x_res', j)],
                      writes=[('dbgx', j)])
        build_moe(nc, P, cfg, b, dict(
            ps=ps, PS=PS, ident_f=ident_f, modT=modT, x_res=x_res, gbc=gbc, rms_scratch=rms_scratch, xn=xn, ssq=ssq,
            make_gbc=make_gbc, rms_to_featmajor=rms_to_featmajor, rw_sb=rw_sb, rb_bc=rb_bc, bdn_d=bdn_d, bguT=bguT,
            wgu_d=wgu_d, wdn_d=wdn_d, fng_d=fng_d, out_d=out_d, eps_c=eps_c))

    P.barrier()
    with nc.Block() as block:
        P.emit(block)
    st.close()
    return nc


def build_moe(nc, P, cfg, b, g):
    NB, T, E, NLT = cfg.NB, cfg.T, cfg.E, cfg.NLT
    ps, PS, x_res, gbc = g['ps'], g['PS'], g['x_res'], g['gbc']
    ident_f, modT = g['ident_f'], g['modT']
    rw_sb, rb_bc, bguT = g['rw_sb'], g['rb_bc'], g['bguT']
    wgu_d, wdn_d, fng_d, out_d = g['wgu_d'], g['wdn_d'], g['fng_d'], g['out_d']
    TG = 512 if T >= 512 else T
    NG = T // TG
    TPG = TG // 128
    mst = ExitStack()
    u2T = mst.enter_context(nc.sbuf_tensor(f"u2T_{b}", [128, KC, T], BF16))
    gates = mst.enter_context(nc.sbuf_tensor(f"gates_{b}", [128, NLT, E], F32))
    lg2 = mst.enter_context(nc.sbuf_tensor(f"lg_{b}", [128, 2, E], F32))
    top82 = mst.enter_context(nc.sbuf_tensor(f"top8_{b}", [128, 2, 8], F32))
    rsum2 = mst.enter_context(nc.sbuf_tensor(f"rsum_{b}", [128, 2, 2], F32))
    wgu = mst.enter_context(nc.sbuf_tensor(f"wgu_{b}", [128, 2, KC, 1024], BF16))
    wdn = mst.enter_context(nc.sbuf_tensor(f"wdn_{b}", [128, 2, 4, D], BF16))
    hT = mst.enter_context(nc.sbuf_tensor(f"hT_{b}", [128, 2, 4, TG], BF16))
    g1 = mst.enter_context(nc.sbuf_tensor(f"g1_{b}", [128, 2, TG], F32))
    t1 = mst.enter_context(nc.sbuf_tensor(f"t1_{b}", [128, 2, TG], F32))
    sl = mst.enter_context(nc.sbuf_tensor(f"sl_{b}", [128, 2, TG], F32))
    wdn32 = mst.enter_context(nc.sbuf_tensor(f"wdn32_{b}", [128, 4, D], F32))
    w32flat = wdn32[:].rearrange("p k c -> p (k c)")
    u2f = w32flat[:, 0:KC * 128].rearrange("p (k c) -> p k c", c=128)
    bdn_sb = w32flat[0:E, 1024:1024 + D]
    gatesT2 = [w32flat[0:E, 2048:2048 + 128], w32flat[0:E, 2304:2304 + 128]]
    if True:
        xn, ssq, rms_scratch = g['xn'], g['ssq'], g['rms_scratch']
        g['make_gbc'](b, 1)
        P.dma('sp', bdn_sb, g['bdn_d'], writes=['bdn_sb'])
        u2fs = [u2f, w32flat[:, 3072:3072 + KC * 128].rearrange("p (k c) -> p k c", c=128)]

        def pro_front(j):
            g['rms_to_featmajor'](x_res[:, j, :], ('x_res', j), u2T, j * 128, 2, b, rms_scratch[:], xn[:], ssq[:], (0, 1),
                                  extra_f32=u2fs[j % 2], extra_res=('u2f', j % 2))

        def pro_back(j):
            u2f = u2fs[j % 2]
            si = j % 2
            br, b0_, b1_ = (2, 3, 4) if si == 0 else (5, 6, 7)
            bb = (b0_, b1_)
            lg = lg2[:, si, :]
            top8 = top82[:, si, :]
            rsum = rsum2[:, si, :]
            gatesT = gatesT2[si]
            LG, T8, RS, GT = ('lg', si), ('top8', si), ('rsum', si), ('gatesT', si)
            for k in range(KC):
                P.op('pe', lambda e, k=k: e.matmul(ps[br][:, 0:E], lhsT=u2f[:, k, :], rhs=rw_sb[:, k, :], start=(k == 0),
                                                   stop=(k == KC - 1)), reads=[('u2f', j % 2), 'rw_sb'], writes=[PS(br)],
                     inc=(k == KC - 1))
            yield
            P.op('dve', lambda e: e.tensor_tensor(out=lg, in0=ps[br][:, 0:E], in1=rb_bc[:], op=ALU.add),
                 reads=[PS(br), 'rb_bc'], writes=[LG])
            yield
            P.op('dve', lambda e: e.max(out=top8, in_=lg), reads=[LG], writes=[T8])
            yield
            P.op('dve', lambda e: e.tensor_scalar(out=rsum[:, 0:1], in0=top8[:, 0:1], scalar1=-1.0, scalar2=None, op0=ALU.mult),
                 reads=[T8], writes=[RS])
            yield
            P.op('act', lambda e, j=j: e.activation(out=gates[:, j, :], in_=lg, func=AF.Exp, bias=rsum[:, 0:1], scale=1.0),
                 reads=[LG, RS], writes=[('gates', j)])
            yield
            P.op('dve', lambda e: e.tensor_scalar(out=lg, in0=lg, scalar1=top8[:, 3:4], scalar2=None, op0=ALU.is_ge),
                 reads=[LG, T8, ('gates', j)], writes=[LG])
            yield
            P.op('dve', lambda e, j=j: e.tensor_tensor(out=gates[:, j, :], in0=gates[:, j, :], in1=lg, op=ALU.mult),
                 reads=[('gates', j), LG], writes=[('gates', j)])
            yield
            P.op('dve', lambda e, j=j: e.tensor_reduce(out=rsum[:, 1:2], in_=gates[:, j, :], axis=AX.X, op=ALU.add),
                 reads=[('gates', j)], writes=[RS])
            yield
            P.op('dve', lambda e: e.reciprocal(out=rsum[:, 1:2], in_=rsum[:, 1:2]), reads=[RS], writes=[RS])
            yield
            P.op('dve', lambda e, j=j: e.tensor_scalar(out=gates[:, j, :], in0=gates[:, j, :], scalar1=rsum[:, 1:2],
                                                       scalar2=None, op0=ALU.mult), reads=[('gates', j), RS],
                 writes=[('gates', j)])
            P.op('pe', lambda e, j=j: e.transpose(out=ps[br][0:E, 128:256], in_=gates[:, j, :], identity=ident_f[:]),
                 reads=[('gates', j), 'ident_f'], writes=[PS(br)])
            yield
            P.op('act', lambda e: e.copy(out=gatesT, in_=ps[br][0:E, 128:256]), reads=[PS(br)], writes=[GT])
            yield
            P.op('dve', lambda e, j=j: e.tensor_scalar(out=gates[:, j, :], in0=gates[:, j, :], scalar1=1.0 / ALPHA, scalar2=None,
                                                       op0=ALU.mult), reads=[('gates', j)], writes=[('gates', j)])
            for half in range(2):
                hs = slice(half * 512, (half + 1) * 512)
                P.op('pe', lambda e, hs=hs, half=half: e.matmul(ps[bb[half]][:, :], lhsT=gatesT, rhs=bdn_sb[:, hs],
                                                                start=True, stop=True), reads=[GT, 'bdn_sb'],
                     writes=[PS(bb[half])])
                tmpb = (g1 if si == 0 else t1)[:, half, :] if TG == 512 else rms_scratch[:, hs]
                tres = ('btmp', si, half) if TG == 512 else 'rms_scratch'
                P.op('dve', lambda e, half=half, hs=hs, tmpb=tmpb: e.tensor_tensor(out=tmpb, in0=ps[bb[half]][:, :], in1=gbc[:, hs],
                                                                                   op=ALU.mult), reads=[PS(bb[half]), 'gbc'],
                     writes=[tres])
                P.op('pool', lambda e, j=j, hs=hs, tmpb=tmpb: e.tensor_tensor(out=x_res[:, j, hs], in0=x_res[:, j, hs],
                                                                              in1=tmpb, op=ALU.add),
                     reads=[tres, ('x_res', j)], writes=[('x_res', j)])
            yield
        def fronts(js):
            for jj in js:
                if jj < NLT:
                    pro_front(jj)
                yield

        pro_front(0)
        if NLT > 1:
            pro_front(1)
        for j0 in range(0, NLT, 2):
            gens = [pro_back(j0)] + ([pro_back(j0 + 1)] if j0 + 1 < NLT else []) + [fronts([j0 + 2, j0 + 3])]
            while gens:
                for gnr in list(gens):
                    try:
                        next(gnr)
                    except StopIteration:
                        gens.remove(gnr)
        P.barrier()
        acc_i = 0

        def load_weights(step):
            ex_, fh_ = step // 2, step % 2
            buf_ = step % 2
            for part in range(2):
                c0 = part * 1024 + fh_ * 512
                P.dma('pool', wgu[:, buf_, :, part * 512:(part + 1) * 512],
                      wgu_d[ex_, :, c0:c0 + 512].rearrange("(k p) c -> p k c", p=128), writes=[('wgu', buf_)])
            P.dma('sp', wdn32[:], wdn_d[ex_, fh_ * 512:(fh_ + 1) * 512, :].rearrange("(k p) c -> p k c", p=128),
                  writes=['wdn32'])
            for k4 in range(4):
                P.op('pool', lambda e, k4=k4: e.tensor_tensor(out=wdn[:, buf_, k4, :], in0=wdn32[:, k4, :], in1=gbc[:], op=ALU.mult),
                     reads=['wdn32', 'gbc'], writes=[('wdn', buf_)])

        acc_box = [0]

        def emit_G(step, tg, gi):
            ex, fh = step // 2, step % 2
            buf = step % 2
            hb = gi % 2
            toks = slice(tg * TG, (tg + 1) * TG)
            for fc in range(4):
                fidx = fh * 4 + fc
                pg, pu = (fc % 2) * 2, 1 + (fc % 2) * 2
                for k in range(KC):
                    P.op('pe', lambda e, k=k, fc=fc, pg=pg: e.matmul(
                        ps[pg][:, 0:TG], lhsT=wgu[:, buf, k, fc * 128:(fc + 1) * 128], rhs=u2T[:, k, toks],
                        start=(k == 0), stop=(k == KC - 1)), reads=[('wgu', buf)], writes=[PS(pg)], inc=(k == KC - 1))
                for k in range(KC):
                    P.op('pe', lambda e, k=k, fc=fc, pu=pu: e.matmul(
                        ps[pu][:, 0:TG], lhsT=wgu[:, buf, k, 512 + fc * 128:512 + (fc + 1) * 128], rhs=u2T[:, k, toks],
                        start=(k == 0), stop=(k == KC - 1)), reads=[('wgu', buf)], writes=[PS(pu)], inc=(k == KC - 1))
                eb = fc % 2
                P.op('dve', lambda e, pg=pg, eb=eb, fidx=fidx: e.tensor_scalar(
                    out=g1[:, eb, :], in0=ps[pg][:, 0:TG], scalar1=bguT[:, fidx, ex:ex + 1], scalar2=LIM,
                    op0=ALU.add, op1=ALU.min), reads=[PS(pg), 'bguT'], writes=[('g1', eb)])
                P.op('act', lambda e, eb=eb: e.activation(out=sl[:, eb, :], in_=g1[:, eb, :], func=AF.Silu, scale=ALPHA),
                     reads=[('g1', eb)], writes=[('sl', eb)])
                P.op('dve', lambda e, pu=pu, eb=eb, fidx=fidx: e.tensor_scalar(
                    out=t1[:, eb, :], in0=ps[pu][:, 0:TG], scalar1=bguT[:, 8 + fidx, ex:ex + 1], scalar2=1.0 - LIM,
                    op0=ALU.add, op1=ALU.max), reads=[PS(pu), 'bguT'], writes=[('t1', eb)])
                P.op('dve', lambda e, eb=eb, fc=fc: e.scalar_tensor_tensor(
                    out=hT[:, hb, fc, :], in0=t1[:, eb, :], scalar=1.0 + LIM, in1=sl[:, eb, :],
                    op0=ALU.min, op1=ALU.mult), reads=[('t1', eb), ('sl', eb)], writes=[('hT', hb, fc)])

        def emit_D(step, tg, gi):
            ex = step // 2
            buf = step % 2
            hb = gi % 2
            for tt in range(TPG):
                j = tg * TPG + tt
                for half in range(2):
                    pb = 4 + ((tt * 2 + half) % 4)
                    for fc in range(4):
                        P.op('pe', lambda e, fc=fc, pb=pb, tt=tt, half=half: e.matmul(
                            ps[pb][:, :], lhsT=hT[:, hb, fc, tt * 128:(tt + 1) * 128],
                            rhs=wdn[:, buf, fc, half * 512:(half + 1) * 512], start=(fc == 0), stop=(fc == 3)),
                            reads=[('hT', hb, fc), ('wdn', buf)], writes=[PS(pb)], inc=(fc == 3))
                    hs = slice(half * 512, (half + 1) * 512)
                    P.op('dve', lambda e, pb=pb, hs=hs, j=j: e.scalar_tensor_tensor(
                        out=x_res[:, j, hs], in0=ps[pb][:, :], scalar=gates[:, j, ex:ex + 1], in1=x_res[:, j, hs],
                        op0=ALU.mult, op1=ALU.add), reads=[PS(pb), ('x_res', j)], writes=[('x_res', j)])

        items = [(st_, tg) for st_ in range(2 * E) for tg in range(NG)]
        load_weights(0)
        load_weights(1)
        emit_G(items[0][0], items[0][1], 0)
        for i, (st_, tg) in enumerate(items):
            if i + 1 < len(items):
                emit_G(items[i + 1][0], items[i + 1][1], i + 1)
            emit_D(st_, tg, i)
            if tg == NG - 1 and st_ + 2 < 2 * E:
                load_weights(st_ + 2)
        P.barrier()
        P.dma('sp', gbc[:], fng_d.to_broadcast([128, D]), writes=['gbc'])
        dbl = (TG == 512)
        xnF = [xn[:], g1[:].rearrange("p a c -> p (a c)") if dbl else xn[:]]
        scF = [rms_scratch[:], t1[:].rearrange("p a c -> p (a c)") if dbl else rms_scratch[:]]
        ssF = [ssq[:], sl[:, 0, 0:1] if dbl else ssq[:]]

        def fin_tile(j):
            sp = (j % 2) if dbl else 0
            xnb, scr, sq = xnF[sp], scF[sp], ssF[sp]
            XN, SQ, SC = ('xnF', sp), ('ssF', sp), ('scF', sp)
            P.op('act', lambda e: e.activation(out=scr, in_=x_res[:, j, :], func=AF.Square, accum_out=sq),
                 reads=[('x_res', j)], writes=[SC, SQ])
            yield
            P.op('act', lambda e: e.activation(out=sq, in_=sq, func=AF.Sqrt, scale=1.0 / D, bias=g['eps_c'][:]),
                 reads=[SQ], writes=[SQ])
            yield
            P.op('dve', lambda e: e.reciprocal(out=sq, in_=sq), reads=[SQ], writes=[SQ])
            yield
            P.op('dve', lambda e: e.tensor_scalar(out=xnb, in0=x_res[:, j, :], scalar1=sq, scalar2=None, op0=ALU.mult),
                 reads=[SQ, ('x_res', j)], writes=[XN])
            yield
            P.op('pool', lambda e: e.tensor_tensor(out=x_res[:, j, :], in0=xnb, in1=gbc[:], op=ALU.mult),
                 reads=[XN, 'gbc'], writes=[('x_res', j)])
            P.dma('sp', out_d[b, j * 128:(j + 1) * 128, :], x_res[:, j, :], reads=[('x_res', j)], writes=[('out', b, j)])
            yield

        nxt = 0
        active = []
        while nxt < NLT or active:
            while len(active) < (2 if dbl else 1) and nxt < NLT:
                active.append(fin_tile(nxt))
                nxt += 1
            for gnr in list(active):
                try:
                    next(gnr)
                except StopIteration:
                    active.remove(gnr)
        P.barrier()
    mst.close()


def build_mixer(nc, P, cfg, b, g):
    NB, T, TC = cfg.NB, cfg.T, cfg.TC
    NT, NTT, NTC, NLT, NCH, NCC = cfg.NT, cfg.NTT, cfg.NTC, cfg.NLT, cfg.NCH, cfg.NCC
    ps, psb, PS = g['ps'], g['psb'], g['PS']
    ident_f, ident_b, ones_f, ones_nt = g['ident_f'], g['ident_b'], g['ones_f'], g['ones_nt']
    masks = (g['maskF'], g['maskB'])
    selh, modT = g['selh'], g['modT']
    x_d, ctx_d, pe_d, win_d, wout_d = g['x_d'], g['ctx_d'], g['pe_d'], g['win_d'], g['wout_d']
    cw_sb, cb_sb, mgb_sb, wg_sb, wlr_sb, gw2_sb, ggbT = (g['cw_sb'], g['cb_sb'], g['mgb_sb'], g['wg_sb'], g['wlr_sb'],
                                                         g['gw2_sb'], g['ggbT'])
    arena, x_res, gbc = g['arena'], g['x_res'], g['gbc']
    rms_scratch, xn, ssq, eps_c, one_c = g['rms_scratch'], g['xn'], g['ssq'], g['eps_c'], g['one_c']
    dbg = g['dbg']
    groups = [(t0, min(512, NT - t0)) for t0 in range(0, NT, 512)]
    HOFF = NLT * 512
    Hacc = arena[:, 0:HOFF].rearrange("p (j c) -> p j c", c=512)
    gt_tiles = [arena[0:4, HOFF + i * NT:HOFF + (i + 1) * NT] for i in range(3)]

    def S(name):
        return f"{name}_{b}"

    def win_load(dst, c0, ncol, res):
        P.dma('pool', dst, win_d[:, c0:c0 + ncol].rearrange("(k p) c -> p k c", p=128), writes=[res])

    def proj_featmajor(w, col0, dst_fn, evac):
        for gi, (t0, n) in enumerate(groups):
            pb = 2 + gi % 2
            for k in range(KC):
                P.op('pe', lambda e, k=k, pb=pb, t0=t0, n=n: e.matmul(ps[pb][:, 0:n], lhsT=w[:, k, col0:col0 + 128],
                                                                      rhs=uT[:, k, t0:t0 + n], start=(k == 0), stop=(k == KC - 1)),
                     reads=['w_cur'], writes=[PS(pb)], inc=(k == KC - 1))
            evac(pb, t0, n)

    def per_head_norm_stats(j):
        P.op('dve', lambda e: e.tensor_tensor(out=rms_scratch[:, 0:512], in0=Hacc[:, j, :], in1=Hacc[:, j, :], op=ALU.mult),
             reads=[('Hacc', j)], writes=['rms_scratch'])
        P.op('dve', lambda e: e.tensor_reduce(out=hst[:, 0:4], in_=rms_scratch[:, 0:512].rearrange("p (h c) -> p h c", c=128),
                                              axis=AX.X, op=ALU.add), reads=['rms_scratch'], writes=['hst'])
        P.op('act', lambda e: e.activation(out=hst[:, 0:4], in_=hst[:, 0:4], func=AF.Sqrt, scale=1.0 / 128, bias=eps_c[:]),
             reads=['hst'], writes=['hst'])
        P.op('dve', lambda e: e.reciprocal(out=hst[:, 0:4], in_=hst[:, 0:4]), reads=['hst'], writes=['hst'])
        P.op('dve', lambda e: e.tensor_tensor(out=rms_scratch[:, 0:512].rearrange("p (h c) -> p h c", c=128),
                                              in0=Hacc[:, j, :].rearrange("p (h c) -> p h c", c=128),
                                              in1=hst[:, 0:4].unsqueeze(2).to_broadcast([128, 4, 128]), op=ALU.mult),
             reads=['hst', ('Hacc', j)], writes=['rms_scratch'])

    st = ExitStack()
    uT = st.enter_context(nc.sbuf_tensor(S("uT"), [128, KC, NT], BF16))
    mT = uT[:].rearrange("p k n -> p (k n)")[:, 0:KC * T].rearrange("p (k t) -> p k t", t=T)
    a_tok = arena[:, HOFF:HOFF + NLT * 256].bitcast(BF16).rearrange("p (j c) -> p j c", c=512)
    AO = HOFF + NLT * 256
    khTok = arena[:, AO:AO + NTT * 128].bitcast(BF16).rearrange("p (i c) -> p i c", c=256)
    khT = arena[:, AO + NTT * 128:AO + NTT * 128 + NT // 2].bitcast(BF16)

    def g_tok(j):
        return arena[:, j * 512:j * 512 + 256].bitcast(BF16)
    hst = st.enter_context(nc.sbuf_tensor(S("hst"), [128, 8], F32))
    gnbc = st.enter_context(nc.sbuf_tensor(S("gnbc"), [128, 512], F32))
    wbuf = st.enter_context(nc.sbuf_tensor(S("wbuf"), [128, KC, 512], BF16))
    Pt = st.enter_context(nc.sbuf_tensor(S("Pt"), [128, 2, 2, 64], BF16))
    dn = st.enter_context(nc.sbuf_tensor(S("dn"), [128, 8], F32))

    with nc.sbuf_tensor(S("xt"), [128, 2, D], F32) as xt, nc.sbuf_tensor(S("pt"), [128, 2, D], F32) as pt, \
            nc.sbuf_tensor(S("xnA"), [128, D], F32) as xnA, nc.sbuf_tensor(S("scrA"), [128, D], F32) as scrA, \
            nc.sbuf_tensor(S("ssqA"), [128, 2], F32) as ssqA:
        def stageA_tile(i):
            sp = i % 2
            xnb = xn[:] if sp == 0 else xnA[:]
            scr = rms_scratch[:] if sp == 0 else scrA[:]
            sq = ssqA[:, sp:sp + 1]
            pbanks = (0, 1) if sp == 0 else (2, 3)
            XT, XN, SQ, SC = ('xt', sp), ('xnA', sp), ('ssqA', sp), ('scrA', sp)
            if i < NTC:
                P.dma('sp', xt[:, sp, :], ctx_d[b, i * 128:(i + 1) * 128, :], writes=[XT])
                bsel = NB
            else:
                j = i - NTC
                P.dma('sp', xt[:, sp, :], x_d[b, j * 128:(j + 1) * 128, :], writes=[XT])
                P.dma('sp', pt[:, sp, :], pe_d[j * 128:(j + 1) * 128, :], writes=[('pt', sp)])
                P.op('pool', lambda e: e.tensor_tensor(out=xt[:, sp, :], in0=xt[:, sp, :], in1=pt[:, sp, :], op=ALU.add),
                     reads=[XT, ('pt', sp)], writes=[XT])
                bsel = b
            yield
            P.op('act', lambda e: e.activation(out=scr, in_=xt[:, sp, :], func=AF.Square, accum_out=sq), reads=[XT], writes=[SC, SQ])
            yield
            P.op('act', lambda e: e.activation(out=sq, in_=sq, func=AF.Sqrt, scale=1.0 / D, bias=eps_c[:]), reads=[SQ], writes=[SQ])
            yield
            P.op('dve', lambda e: e.reciprocal(out=sq, in_=sq), reads=[SQ], writes=[SQ])
            yield
            P.op('dve', lambda e: e.tensor_scalar(out=xnb, in0=xt[:, sp, :], scalar1=sq, scalar2=None, op0=ALU.mult),
                 reads=[SQ, XT], writes=[XN])
            yield
            for k in range(KC):
                pb = pbanks[k // 4]
                P.op('pe', lambda e, k=k, pb=pb: e.transpose(out=ps[pb][:, (k % 4) * 128:(k % 4 + 1) * 128],
                                                             in_=xnb[:, k * 128:(k + 1) * 128], identity=ident_f[:]),
                     reads=[XN, 'ident_f'], writes=[PS(pb)], inc=(k % 4 == 3))
            yield
            for k in range(KC):
                pb = pbanks[k // 4]
                P.op('act', lambda e, k=k, pb=pb: e.activation(out=uT[:, k, i * 128:(i + 1) * 128],
                                                               in_=ps[pb][:, (k % 4) * 128:(k % 4 + 1) * 128], func=AF.Identity,
                                                               scale=modT[:, 0, k, bsel:bsel + 1], bias=modT[:, 1, k, bsel:bsel + 1]),
                     reads=[PS(pb), 'modT'], writes=[('uT', i)])
                if k == 3:
                    yield
            yield

        nxt = 0
        active = []
        while nxt < NTT or active:
            while len(active) < 2 and nxt < NTT:
                active.append(stageA_tile(nxt))
                nxt += 1
            for gnr in list(active):
                try:
                    next(gnr)
                except StopIteration:
                    active.remove(gnr)
        P.barrier()

    mst = ExitStack()
    qkT = mst.enter_context(nc.sbuf_tensor(S("qkT"), [128, 4, NT], BF16))
    kTok = mst.enter_context(nc.sbuf_tensor(S("kTok"), [128, NTT, 256], BF16))
    if True:
        with nc.sbuf_tensor(S("zt"), [128, NT], F32) as zt, nc.sbuf_tensor(S("ot"), [128, NT], F32) as ot, \
                nc.sbuf_tensor(S("sg"), [128, NT], F32) as sg:
            win_load(wbuf[:], 0, 512, 'w_cur')
            for qc in range(4):
                proj_featmajor(wbuf, qc * 128, None,
                               lambda pb, t0, n: P.op('act', lambda e: e.copy(out=zt[:, t0:t0 + n], in_=ps[pb][:, 0:n]),
                                                      reads=[PS(pb)], writes=['zt']))
                P.op('dve', lambda e, qc=qc: e.tensor_scalar(out=ot[:], in0=zt[:], scalar1=cw_sb[:, qc, 1:2],
                                                             scalar2=cb_sb[:, qc:qc + 1], op0=ALU.mult, op1=ALU.add),
                     reads=['zt', 'cw_sb', 'cb_sb'], writes=['ot'])
                for lo, hi in ((0, TC), (TC, NT)):
                    P.op('dve', lambda e, qc=qc, lo=lo, hi=hi: e.scalar_tensor_tensor(
                        out=ot[:, lo + 1:hi], in0=zt[:, lo:hi - 1], scalar=cw_sb[:, qc, 0:1], in1=ot[:, lo + 1:hi],
                        op0=ALU.mult, op1=ALU.add), reads=['zt', 'ot'], writes=['ot'])
                    P.op('dve', lambda e, qc=qc, lo=lo, hi=hi: e.scalar_tensor_tensor(
                        out=ot[:, lo:hi - 1], in0=zt[:, lo + 1:hi], scalar=cw_sb[:, qc, 2:3], in1=ot[:, lo:hi - 1],
                        op0=ALU.mult, op1=ALU.add), reads=['zt', 'ot'], writes=['ot'])
                P.op('act', lambda e: e.activation(out=sg[:], in_=ot[:], func=AF.Sigmoid), reads=['ot'], writes=['sg'])
                P.op('dve', lambda e, qc=qc: e.scalar_tensor_tensor(out=qkT[:, qc, :], in0=ot[:], scalar=(0.125 if qc < 2 else 1.0),
                                                                    in1=sg[:], op0=ALU.mult, op1=ALU.mult),
                     reads=['ot', 'sg'], writes=['qkT'])
            P.barrier()
        vtok = mst.enter_context(nc.sbuf_tensor(S("vtok"), [128, NTT, 4, 129], BF16))
        kp = mst.enter_context(nc.sbuf_tensor(S("kp"), [128, NTT, 256], BF16))
        edT = mst.enter_context(nc.sbuf_tensor(S("edT"), [128, NTT, 8], F32))
        wcB = mst.enter_context(nc.sbuf_tensor(S("wcB"), [128, 2, NCH], F32))
        Chat = mst.enter_context(nc.sbuf_tensor(S("Chat"), [128, 2, 129], F32))
        CbfA = mst.enter_context(nc.sbuf_tensor(S("CbfA"), [128, 2, 2, 129], BF16))
        CbfB = mst.enter_context(nc.sbuf_tensor(S("CbfB"), [128, 2, 2, 129], BF16))
        Aend = mst.enter_context(nc.sbuf_tensor(S("Aend"), [4, 3, NCH], F32))
        for i in range(NTT):
            for kc in range(2):
                P.op('pe', lambda e, i=i, kc=kc: e.transpose(out=psb[1][:, kc * 128:(kc + 1) * 128],
                                                             in_=qkT[:, 2 + kc, i * 128:(i + 1) * 128], identity=ident_b[:]),
                     reads=['ident_b'], writes=[PS(1)], inc=(kc == 1))
            P.op('dve', lambda e, i=i: e.tensor_copy(out=kTok[:, i, :], in_=psb[1][:, 0:256]), reads=[PS(1)], writes=['kTok'])
        win_load(wbuf[:], 512, 512, 'w_cur')
        P.op('pool', lambda e: e.memset(vtok[:, :, :, 128:129], 1.0), writes=['vtok'])
        for i in range(NTT):
            pb = 2 + i % 2
            for k in range(KC):
                P.op('pe', lambda e, i=i, k=k, pb=pb: e.matmul(ps[pb][:, :], lhsT=uT[:, k, i * 128:(i + 1) * 128], rhs=wbuf[:, k, :],
                                                               start=(k == 0), stop=(k == KC - 1)),
                     reads=['w_cur'], writes=[PS(pb)], inc=(k == KC - 1))
            P.op('act', lambda e, i=i, pb=pb: e.copy(out=vtok[:, i, :, 0:128], in_=ps[pb][:, :].rearrange("p (h c) -> p h c", c=128)),
                 reads=[PS(pb)], writes=['vtok'])
        P.barrier()

        DIRS = [int(c) for c in os.environ.get('KDIRS', '01')]
        for d in DIRS:
            IGt, FPt, T3 = gt_tiles
            for gi, dst in ((2 * d, IGt), (2 * d + 1, FPt)):
                for gj, (t0, n) in enumerate(groups):
                    pb = 2 + gj % 2
                    for k in range(KC):
                        P.op('pe', lambda e, k=k, pb=pb, t0=t0, n=n, gi=gi: e.matmul(
                            ps[pb][0:4, 0:n], lhsT=wg_sb[:, k, gi * 4:(gi + 1) * 4], rhs=uT[:, k, t0:t0 + n],
                            start=(k == 0), stop=(k == KC - 1)), reads=['wg_sb'], writes=[PS(pb)], inc=(k == KC - 1))
                    P.op('act', lambda e, pb=pb, t0=t0, n=n, gi=gi, dst=dst: e.activation(
                        out=dst[:, t0:t0 + n], in_=ps[pb][0:4, 0:n], func=AF.Identity, bias=mgb_sb[:, gi:gi + 1], scale=1.0),
                        reads=[PS(pb), 'mgb_sb'], writes=[('gt', gi % 2)])
            P.op('act', lambda e: e.activation(out=FPt, in_=FPt, func=AF.Exp, scale=-1.0), reads=[('gt', 1)], writes=[('gt', 1)])
            P.op('act', lambda e: e.activation(out=FPt, in_=FPt, func=AF.Ln, bias=one_c[0:4, :], scale=1.0),
                 reads=[('gt', 1)], writes=[('gt', 1)])

            def scan(out_t, d0, d1, op0, op1, rd, wr):
                if d == 0:
                    P.op('dve', lambda e: e.tensor_tensor_scan(out=out_t[:, 0:NT], data0=d0[:, 0:NT], data1=d1[:, 0:NT], initial=0.0,
                                                               op0=op0, op1=op1), reads=rd, writes=wr)
                else:
                    P.op('dve', lambda e: e.tensor_tensor_scan(out=out_t[:, 0:TC][:, ::-1], data0=d0[:, 0:TC][:, ::-1],
                                                               data1=d1[:, 0:TC][:, ::-1], initial=0.0, op0=op0, op1=op1),
                         reads=rd, writes=wr)
                    P.op('dve', lambda e: e.tensor_tensor_scan(out=out_t[:, TC:NT][:, ::-1], data0=d0[:, TC:NT][:, ::-1],
                                                               data1=d1[:, TC:NT][:, ::-1], initial=out_t[:, 0:1], op0=op0, op1=op1),
                         reads=rd + wr, writes=wr)
            scan(T3, ones_nt[0:4, :], FPt, ALU.mult, ALU.add, [('gt', 1), 'ones_nt'], [('gt', 2)])
            P.op('dve', lambda e: e.tensor_tensor(out=IGt, in0=IGt, in1=T3, op=ALU.add), reads=[('gt', 0), ('gt', 2)],
                 writes=[('gt', 0)])
            scan(FPt, IGt, IGt, ALU.max, ALU.max, [('gt', 0)], [('gt', 1)])
            endcol = 63 if d == 0 else 0
            A3 = FPt.rearrange("p (c l) -> p c l", l=64)
            P.op('dve', lambda e: e.tensor_copy(out=Aend[:, 0, :], in_=A3[:, :, endcol]), reads=[('gt', 1)], writes=['Aend'])
            P.op('pool', lambda e: e.memset(Aend[:, 1, :], 0.0), writes=['Aprev'])
            if d == 0:
                P.op('dve', lambda e: e.tensor_copy(out=Aend[:, 1, 1:NCH], in_=Aend[:, 0, 0:NCH - 1]), reads=['Aend', 'Aprev'],
                     writes=['Aprev'])
            else:
                if NCC > 1:
                    P.op('dve', lambda e: e.tensor_copy(out=Aend[:, 1, 0:NCC - 1], in_=Aend[:, 0, 1:NCC]), reads=['Aend', 'Aprev'],
                         writes=['Aprev'])
                P.op('dve', lambda e: e.tensor_copy(out=Aend[:, 1, NCC:NCH - 1], in_=Aend[:, 0, NCC + 1:NCH]),
                     reads=['Aend', 'Aprev'], writes=['Aprev'])
                P.op('dve', lambda e: e.tensor_copy(out=Aend[:, 1, NCH - 1:NCH], in_=Aend[:, 0, 0:1]), reads=['Aend', 'Aprev'],
                     writes=['Aprev'])
            P.op('dve', lambda e: e.tensor_tensor(out=Aend[:, 2, :], in0=Aend[:, 1, :], in1=Aend[:, 0, :], op=ALU.subtract),
                 reads=['Aend', 'Aprev'], writes=['wc'])
            P.op('act', lambda e: e.activation(out=Aend[:, 2, :], in_=Aend[:, 2, :], func=AF.Exp), reads=['wc'], writes=['wc'])
            for ti_, nm in ((IGt, 0), (T3, 2)):
                P.op('dve', lambda e, ti_=ti_: e.tensor_tensor(out=ti_.rearrange("p (c l) -> p c l", l=64),
                                                               in0=ti_.rearrange("p (c l) -> p c l", l=64),
                                                               in1=Aend[:, 0, :].unsqueeze(2).to_broadcast([4, NCH, 64]),
                                                               op=ALU.subtract), reads=[('gt', nm), 'Aend'], writes=[('gt', nm)])
                P.op('act', lambda e, ti_=ti_: e.activation(out=ti_, in_=ti_, func=AF.Exp), reads=[('gt', nm)], writes=[('gt', nm)])
            for i in range(NTT):
                P.op('pe', lambda e, i=i: e.matmul(ps[2][:, i * 8:i * 8 + 4], lhsT=IGt[:, i * 128:(i + 1) * 128], rhs=ident_f[0:4, 0:4],
                                                   start=True, stop=True), reads=[('gt', 0), 'ident_f'], writes=[PS(2)], inc=False)
                P.op('pe', lambda e, i=i: e.matmul(ps[2][:, i * 8 + 4:i * 8 + 8], lhsT=T3[:, i * 128:(i + 1) * 128],
                                                   rhs=ident_f[0:4, 0:4], start=True, stop=True), reads=[('gt', 2), 'ident_f'],
                     writes=[PS(2)], inc=(i == NTT - 1))
            P.op('dve', lambda e: e.tensor_copy(out=edT[:].rearrange("p i c -> p (i c)"), in_=ps[2][:, 0:NTT * 8]), reads=[PS(2)],
                 writes=['edT'])
            for pr in range(2):
                P.op('pe', lambda e, pr=pr: e.matmul(ps[3][:, pr * NCH:(pr + 1) * NCH],
                                                     lhsT=selh[0:4, pr].rearrange("p a b -> p (a b)"), rhs=Aend[:, 2, :],
                                                     start=True, stop=True), reads=['selh', 'wc'], writes=[PS(3)], inc=(pr == 1))
            P.op('dve', lambda e: e.tensor_copy(out=wcB[:].rearrange("p a c -> p (a c)"), in_=ps[3][:, 0:2 * NCH]), reads=[PS(3)],
                 writes=['wcB'])
            P.op('dve', lambda e: e.tensor_tensor(out=kp[:].rearrange("p i (h c) -> p i h c", c=64),
                                                  in0=kTok[:].rearrange("p i (h c) -> p i h c", c=64),
                                                  in1=edT[:, :, 0:4].unsqueeze(3).to_broadcast([128, NTT, 4, 64]), op=ALU.mult),
                 reads=['kTok', 'edT'], writes=['kp'])
            P.barrier()
            P.op('pool', lambda e: e.memset(Chat[:], 0.0), writes=['Chat'])
            P.op('pool', lambda e: e.memset(CbfA[:], 0.0), writes=[('CbfA', 0), ('CbfA', 1)])
            P.op('pool', lambda e: e.memset(CbfB[:], 0.0), writes=[('CbfB', 0), ('CbfB', 1)])
            order = list(range(NCH)) if d == 0 else list(range(NCC - 1, -1, -1)) + list(range(NCH - 1, NCC - 1, -1))
            mask = masks[d]
            first_dir = (d == DIRS[0])

            def ml_geo(step):
                c = order[step]
                ti, p0 = c // 2, (c % 2) * 64
                return c, ti, slice(p0, p0 + 64), slice(c * 64, (c + 1) * 64), c >= NCC, 6 + step % 2, c % 2

            def ml_pre(step):
                c, ti, rows, toks, lat, pbD, hf = ml_geo(step)
                if lat:
                    for h in range(4):
                        hp = (h % 2) * 64
                        P.op('pe', lambda e, h=h, hp=hp: e.matmul(
                            ps[2 + h % 2][rows, (h // 2) * 64:(h // 2) * 64 + 64], lhsT=qkT[hp:hp + 64, 2 + h // 2, toks],
                            rhs=qkT[hp:hp + 64, h // 2, toks], start=True, stop=True), writes=[('psh', 2 + h % 2, hf)], inc=(h >= 2))
                for h in range(4):
                    P.op('pe', lambda e, h=h: e.matmul(
                        ps[pbD][(h % 2) * 64:(h % 2) * 64 + 64, (h // 2) * 129:(h // 2 + 1) * 129],
                        lhsT=kp[rows, ti, h * 64:(h + 1) * 64], rhs=vtok[rows, ti, h, :], start=True, stop=True),
                        reads=['kp'], writes=[PS(pbD)], inc=(h == 3))
                if lat:
                    for h in range(4):
                        par, hh = h % 2, h // 2
                        P.op('dve', lambda e, h=h, par=par, hh=hh: e.scalar_tensor_tensor(
                            out=Pt[rows, par, hh, :], in0=ps[2 + par][rows, hh * 64:(hh + 1) * 64], scalar=edT[rows, ti, h:h + 1],
                            in1=mask[rows, :], op0=ALU.mult, op1=ALU.mult), reads=[('psh', 2 + par, hf), 'edT'],
                            writes=[('Pt', hf, h)])

            def ml_main(step):
                c, ti, rows, toks, lat, pbD, hf = ml_geo(step)
                for pr in range(2):
                    P.op('dve', lambda e, pr=pr: e.scalar_tensor_tensor(
                        out=Chat[:, pr, :], in0=Chat[:, pr, :], scalar=wcB[:, pr, c:c + 1], in1=ps[pbD][:, pr * 129:(pr + 1) * 129],
                        op0=ALU.mult, op1=ALU.add), reads=['Chat', 'wcB', PS(pbD)], writes=['Chat'])
                if step + 1 < len(order):
                    cn = order[step + 1]
                    for pr in range(2):
                        P.op('act', lambda e, pr=pr: e.activation(out=CbfA[0:64, (step + 1) % 2, pr, :], in_=Chat[0:64, pr, :], func=AF.Identity,
                                                                  scale=wcB[0:64, pr, cn:cn + 1]),
                             reads=['Chat', 'wcB'], writes=[('CbfA', (step + 1) % 2)])
                        P.op('act', lambda e, pr=pr: e.activation(out=CbfB[64:128, (step + 1) % 2, pr, :], in_=Chat[64:128, pr, :], func=AF.Identity,
                                                                  scale=wcB[64:128, pr, cn:cn + 1]),
                             reads=['Chat', 'wcB'], writes=[('CbfB', (step + 1) % 2)])

                if lat:
                    j = ti - NTC
                    for h in range(4):
                        par, pr = h % 2, h // 2
                        P.op('pe', lambda e, h=h, par=par, pr=pr: e.matmul(
                            ps[4 + pr][rows, par * 129:(par + 1) * 129], lhsT=Pt[rows, par, pr, :], rhs=vtok[rows, ti, h, :],
                            start=True, stop=False), reads=[('Pt', hf, h)], writes=[('psh', 4 + pr, hf)], inc=False)
                        P.op('pe', lambda e, par=par, pr=pr: e.matmul(
                            ps[4 + pr][rows, par * 129:(par + 1) * 129], lhsT=qkT[:, pr, toks],
                            rhs=(CbfA if par == 0 else CbfB)[:, step % 2, pr, :], start=False, stop=True),
                            reads=[('CbfA', step % 2), ('CbfB', step % 2)], writes=[('psh', 4 + pr, hf)], inc=True)
                    for pr in range(2):
                        den = lambda pr=pr, rows=rows: ps[4 + pr][rows, 0:258].rearrange("p (a c) -> p a c", c=129)[:, :, 128]
                        P.op('dve', lambda e, pr=pr, den=den: e.tensor_tensor(
                            out=dn[rows, 2 * pr:2 * pr + 2], in0=den(), in1=edT[rows, ti, 4 + 2 * pr:6 + 2 * pr], op=ALU.max),
                            reads=[('psh', 4 + pr, hf), 'edT'], writes=[('dn', hf)])
                        P.op('dve', lambda e, pr=pr, den=den: e.scalar_tensor_tensor(
                            out=dn[rows, 2 * pr:2 * pr + 2], in0=den(), scalar=-1.0, in1=dn[rows, 2 * pr:2 * pr + 2],
                            op0=ALU.mult, op1=ALU.max), reads=[('psh', 4 + pr, hf), ('dn', hf)], writes=[('dn', hf)])
                    P.op('dve', lambda e: e.reciprocal(out=dn[rows, 4:8], in_=dn[rows, 0:4]), reads=[('dn', hf)],
                         writes=[('rd', hf)])
                    for h in range(4):
                        src = lambda h=h, rows=rows: ps[4 + h // 2][rows, (h % 2) * 129:(h % 2) * 129 + 128]
                        if first_dir:
                            P.op('dve', lambda e, h=h, src=src: e.tensor_scalar(
                                out=Hacc[rows, j, h * 128:(h + 1) * 128], in0=src(), scalar1=dn[rows, 4 + h:5 + h], scalar2=None,
                                op0=ALU.mult), reads=[('psh', 4 + h // 2, hf), ('rd', hf)], writes=[('Hacc', j)])
                        else:
                            P.op('dve', lambda e, h=h, src=src: e.scalar_tensor_tensor(
                                out=Hacc[rows, j, h * 128:(h + 1) * 128], in0=src(), scalar=dn[rows, 4 + h:5 + h],
                                in1=Hacc[rows, j, h * 128:(h + 1) * 128], op0=ALU.mult, op1=ALU.add),
                                reads=[('psh', 4 + h // 2, hf), ('rd', hf), ('Hacc', j)], writes=[('Hacc', j)])
            ml_pre(0)
            for step in range(len(order)):
                if step + 1 < len(order):
                    ml_pre(step + 1)
                ml_main(step)
            P.barrier()
        if dbg:
            P.dma('sp', dbg['HM'][b], Hacc, writes=['dbgHM'])
            P.dma('sp', dbg['edT'][b], edT[:], writes=['dbgedT'])
            P.dma('sp', dbg['wcB'][b], wcB[:], writes=['dbgwcB'])
            P.dma('sp', dbg['qkT'][b], qkT[:], writes=['dbgqkT'])
            P.dma('sp', dbg['vtok'][b], vtok[:], writes=['dbgvtok'])
            P.dma('sp', dbg['kTok'][b], kTok[:], writes=['dbgkTok'])
            P.barrier()
        win_load(wbuf[:], 1024, 512, 'w_cur')
        P.dma('sp', gnbc[:], g['mng_d'].to_broadcast([128, 512]), writes=['gnbc'])
        def mm_front(j):
            tk = slice(TC + j * 128, TC + (j + 1) * 128)
            xh = xn[:, (j % 2) * 512:(j % 2) * 512 + 512]
            pb = 2 + j % 2
            for k in range(KC):
                P.op('pe', lambda e, k=k: e.matmul(ps[pb][:, :], lhsT=uT[:, k, tk], rhs=wbuf[:, k, :], start=(k == 0),
                                                   stop=(k == KC - 1)), reads=['w_cur'], writes=[PS(pb)], inc=(k == KC - 1))
            P.op('act', lambda e: e.activation(out=xh, in_=ps[pb][:, :], func=AF.Sigmoid), reads=[PS(pb)], writes=[('xnh', j % 2)])

        def mm_back(j):
            xh = xn[:, (j % 2) * 512:(j % 2) * 512 + 512]
            per_head_norm_stats(j)
            P.op('dve', lambda e: e.tensor_tensor(out=rms_scratch[:, 0:512], in0=rms_scratch[:, 0:512], in1=gnbc[:], op=ALU.mult),
                 reads=['rms_scratch', 'gnbc'], writes=['rms_scratch'])
            P.op('dve', lambda e: e.tensor_tensor(out=a_tok[:, j, :], in0=rms_scratch[:, 0:512], in1=xh, op=ALU.mult),
                 reads=['rms_scratch', ('xnh', j % 2)], writes=[('a_tok', j)])

        mm_front(0)
        for j in range(NLT):
            if j + 1 < NLT:
                mm_front(j + 1)
            mm_back(j)
        P.barrier()

    mst.close()
    KSTOP = os.environ.get('KSTOP', '')
    rm = ones_nt
    with nc.sbuf_tensor(S("gv"), [128, NTT, 512], BF16) as gv, \
            nc.sbuf_tensor(S("qt"), [128, 2, NT], BF16) as qt, nc.sbuf_tensor(S("kt"), [128, 2, NT], BF16) as kt, \
            nc.sbuf_tensor(S("kraw"), [128, NT], BF16) as kraw, \
            nc.sbuf_tensor(S("lrT"), [16, NT], BF16) as lrT, \
            nc.sbuf_tensor(S("tA"), [128, NT], F32) as tA, nc.sbuf_tensor(S("tB"), [128, NT], F32) as tB, \
            nc.sbuf_tensor(S("ebL"), [128, 2, NCH], F32) as ebL, \
            nc.sbuf_tensor(S("Sst"), [128, 2, 128], F32) as Sst, nc.sbuf_tensor(S("SbfA"), [128, 2, 2, 128], BF16) as SbfA, \
            nc.sbuf_tensor(S("SbfB"), [128, 2, 2, 128], BF16) as SbfB:
        win_load(wbuf[:], 2064, 512, 'w_cur')
        for i in range(NTT):
            pb = 2 + i % 2
            for k in range(KC):
                P.op('pe', lambda e, i=i, k=k, pb=pb: e.matmul(ps[pb][:, :], lhsT=uT[:, k, i * 128:(i + 1) * 128], rhs=wbuf[:, k, :],
                                                               start=(k == 0), stop=(k == KC - 1)),
                     reads=['w_cur'], writes=[PS(pb)], inc=(k == KC - 1))
            P.op('act', lambda e, i=i, pb=pb: e.copy(out=gv[:, i, :], in_=ps[pb][:, :]), reads=[PS(pb)], writes=['gv'])
        P.barrier()
        DIRS = [int(c) for c in os.environ.get('KDIRS', '01')]
        if KSTOP == 'mlstm':
            DIRS = []
        for d in DIRS:
            endcol = 63 if d == 0 else 0
            win_load(wbuf[:], 1552, 512, 'w_cur')
            P.op('pool', lambda e: e.memset(rm[:], 1.0), writes=['rm'])
            zc = 0 if d == 0 else 63
            P.op('pool', lambda e: e.memset(rm[:].rearrange("p (c l) -> p c l", l=64)[:, :, zc:zc + 1], 0.0), writes=['rm'])
            for gj, (t0, n) in enumerate(groups):
                pb = 2 + gj % 2
                for k in range(KC):
                    P.op('pe', lambda e, k=k, pb=pb, t0=t0, n=n: e.matmul(
                        ps[pb][0:16, 0:n], lhsT=wlr_sb[:, k, d * 16:(d + 1) * 16], rhs=uT[:, k, t0:t0 + n],
                        start=(k == 0), stop=(k == KC - 1)), reads=['wlr_sb'], writes=[PS(pb)], inc=(k == KC - 1))
                P.op('act', lambda e, pb=pb, t0=t0, n=n: e.copy(out=lrT[:, t0:t0 + n], in_=ps[pb][0:16, 0:n]),
                     reads=[PS(pb)], writes=['lrT'])
            for jc in range(2):
                for gj, (t0, n) in enumerate(groups):
                    pb = 2 + gj % 2
                    P.op('pe', lambda e, pb=pb, t0=t0, n=n: e.matmul(
                        ps[pb][:, 0:n], lhsT=gw2_sb[:, d, jc * 128:(jc + 1) * 128], rhs=lrT[:, t0:t0 + n], start=True, stop=True),
                        reads=['gw2_sb', 'lrT'], writes=[PS(pb)])
                    P.op('act', lambda e, pb=pb, t0=t0, n=n: e.activation(
                        out=tA[:, t0:t0 + n], in_=ps[pb][:, 0:n], func=AF.Exp, scale=-1.0, bias=ggbT[:, d, jc:jc + 1]),
                        reads=[PS(pb), 'ggbT'], writes=['tA'])
                P.op('act', lambda e: e.activation(out=tA[:], in_=tA[:], func=AF.Ln, bias=one_c[:], scale=1.0), reads=['tA'],
                     writes=['tA'])
                if d == 0:
                    P.op('dve', lambda e: e.tensor_tensor_scan(out=tB[:], data0=rm[:], data1=tA[:], initial=0.0,
                                                               op0=ALU.mult, op1=ALU.add), reads=['tA', 'rm'], writes=['tB'])
                else:
                    P.op('dve', lambda e: e.tensor_tensor_scan(out=tB[:, ::-1], data0=rm[:, ::-1], data1=tA[:, ::-1], initial=0.0,
                                                               op0=ALU.mult, op1=ALU.add), reads=['tA', 'rm'], writes=['tB'])
                bL = tB[:].rearrange("p (c l) -> p c l", l=64)[:, :, endcol]
                P.op('act', lambda e: e.activation(out=ebL[:, jc, :], in_=bL, func=AF.Exp, scale=-1.0 / 16),
                     reads=['tB'], writes=['ebL'])
                P.op('act', lambda e: e.activation(out=tA[:], in_=tB[:], func=AF.Exp, scale=-1.0 / 16), reads=['tB', 'tA'],
                     writes=['tA'])
                proj_featmajor(wbuf, jc * 128, None,
                               lambda pb, t0, n: P.op('dve', lambda e: e.scalar_tensor_tensor(
                                   out=qt[:, jc, t0:t0 + n], in0=ps[pb][:, 0:n], scalar=0.125, in1=tA[:, t0:t0 + n],
                                   op0=ALU.mult, op1=ALU.mult), reads=[PS(pb), 'tA'], writes=['qt']))
                P.op('act', lambda e: e.activation(out=tA[:], in_=tB[:], func=AF.Exp, scale=1.0 / 16), reads=['tB', 'qt', 'tA'],
                     writes=['tA'])

                def evac_k(pb, t0, n):
                    P.op('dve', lambda e: e.tensor_copy(out=kraw[:, t0:t0 + n], in_=ps[pb][:, 0:n]), reads=[PS(pb)], writes=['kraw'])
                    P.op('dve', lambda e: e.tensor_tensor(out=kt[:, jc, t0:t0 + n], in0=ps[pb][:, 0:n], in1=tA[:, t0:t0 + n],
                                                          op=ALU.mult), reads=[PS(pb), 'tA'], writes=['kt'])
                proj_featmajor(wbuf, 256 + jc * 128, None, evac_k)
                P.op('dve', lambda e: e.tensor_tensor(out=tA[:].rearrange("p (c l) -> p c l", l=64),
                                                      in0=tB[:].rearrange("p (c l) -> p c l", l=64),
                                                      in1=bL.unsqueeze(2).to_broadcast([128, NCH, 64]), op=ALU.subtract),
                     reads=['tB', 'kt', 'tA'], writes=['tA'])
                P.op('act', lambda e: e.activation(out=tA[:], in_=tA[:], func=AF.Exp, scale=1.0 / 16), reads=['tA'], writes=['tA'])
                P.op('dve', lambda e: e.tensor_tensor(out=khT, in0=kraw[:], in1=tA[:], op=ALU.mult),
                     reads=['tA', 'kraw'], writes=['khT'])
                for i in range(NTT):
                    P.op('pe', lambda e, i=i: e.transpose(out=psb[1][:, 0:128], in_=khT[:, i * 128:(i + 1) * 128], identity=ident_b[:]),
                         reads=['khT', 'ident_b'], writes=[PS(1)])
                    P.op('dve', lambda e, i=i: e.tensor_copy(out=khTok[:, i, jc * 128:(jc + 1) * 128], in_=psb[1][:, 0:128]),
                         reads=[PS(1)], writes=['khTok'])
            P.barrier()
            P.op('pool', lambda e: e.memset(Sst[:], 0.0), writes=['Sst'])
            P.op('pool', lambda e: e.memset(SbfA[:], 0.0), writes=[('SbfA', 0), ('SbfA', 1)])
            P.op('pool', lambda e: e.memset(SbfB[:], 0.0), writes=[('SbfB', 0), ('SbfB', 1)])
            order = list(range(NCH)) if d == 0 else list(range(NCC - 1, -1, -1)) + list(range(NCH - 1, NCC - 1, -1))
            mask = masks[d]
            first_dir = (d == DIRS[0])

            def gl_geo(step):
                c = order[step]
                ti, p0 = c // 2, (c % 2) * 64
                return c, ti, slice(p0, p0 + 64), slice(c * 64, (c + 1) * 64), c >= NCC, 6 + step % 2, c % 2

            def gl_pre(step):
                c, ti, rows, toks, lat, pbD, hf = gl_geo(step)
                if lat:
                    for h in range(4):
                        hp = (h % 2) * 64
                        P.op('pe', lambda e, h=h, hp=hp: e.matmul(
                            ps[2 + h % 2][rows, (h // 2) * 64:(h // 2) * 64 + 64], lhsT=kt[hp:hp + 64, h // 2, toks],
                            rhs=qt[hp:hp + 64, h // 2, toks], start=True, stop=True), writes=[('psh', 2 + h % 2, hf)], inc=(h >= 2))
                for h in range(4):
                    P.op('pe', lambda e, h=h: e.matmul(
                        ps[pbD][(h % 2) * 64:(h % 2) * 64 + 64, (h // 2) * 128:(h // 2 + 1) * 128],
                        lhsT=khTok[rows, ti, h * 64:(h + 1) * 64], rhs=gv[rows, ti, h * 128:(h + 1) * 128], start=True, stop=True),
                        writes=[PS(pbD)], inc=(h == 3))
                if lat:
                    for par in range(2):
                        P.op('dve', lambda e, par=par: e.tensor_tensor(
                            out=Pt[rows, par, :, :], in0=ps[2 + par][rows, 0:128].rearrange("p (a c) -> p a c", c=64),
                            in1=mask[rows, :].unsqueeze(1).to_broadcast([64, 2, 64]), op=ALU.mult),
                            reads=[('psh', 2 + par, hf)], writes=[('Pt', hf, par)])

            def gl_main(step):
                c, ti, rows, toks, lat, pbD, hf = gl_geo(step)
                for pr in range(2):
                    P.op('dve', lambda e, pr=pr: e.scalar_tensor_tensor(
                        out=Sst[:, pr, :], in0=Sst[:, pr, :], scalar=ebL[:, pr, c:c + 1], in1=ps[pbD][:, pr * 128:(pr + 1) * 128],
                        op0=ALU.mult, op1=ALU.add), reads=['Sst', 'ebL', PS(pbD)], writes=['Sst'])
                P.op('act', lambda e: e.copy(out=SbfA[0:64, (step + 1) % 2], in_=Sst[0:64]), reads=['Sst'], writes=[('SbfA', (step + 1) % 2)])
                P.op('act', lambda e: e.copy(out=SbfB[64:128, (step + 1) % 2], in_=Sst[64:128]), reads=['Sst'], writes=[('SbfB', (step + 1) % 2)])

                if lat:
                    j = ti - NTC
                    for h in range(4):
                        par, pr = h % 2, h // 2
                        P.op('pe', lambda e, h=h, par=par, pr=pr: e.matmul(
                            ps[4][rows, h * 128:(h + 1) * 128], lhsT=Pt[rows, par, pr, :], rhs=gv[rows, ti, h * 128:(h + 1) * 128],
                            start=True, stop=False), reads=[('Pt', hf, par)], writes=[('psh', 4, hf)], inc=False)
                        P.op('pe', lambda e, h=h, par=par, pr=pr: e.matmul(
                            ps[4][rows, h * 128:(h + 1) * 128], lhsT=qt[:, pr, toks], rhs=(SbfA if par == 0 else SbfB)[:, step % 2, pr, :],
                            start=False, stop=True), reads=[('SbfA', step % 2), ('SbfB', step % 2)], writes=[('psh', 4, hf)], inc=(h == 3))
                    if first_dir:
                        P.op('act', lambda e: e.copy(out=Hacc[rows, j, :], in_=ps[4][rows, :]), reads=[('psh', 4, hf)],
                             writes=[('Hacc', j)])
                    else:
                        P.op('act', lambda e: e.copy(out=rms_scratch[rows, 0:512], in_=ps[4][rows, :]), reads=[('psh', 4, hf)],
                             writes=[('otmp', hf)])
                        P.op('pool', lambda e: e.tensor_tensor(out=Hacc[rows, j, :], in0=Hacc[rows, j, :],
                                                               in1=rms_scratch[rows, 0:512], op=ALU.add),
                             reads=[('otmp', hf), ('Hacc', j)], writes=[('Hacc', j)])
            gl_pre(0)
            for step in range(len(order)):
                if step + 1 < len(order):
                    gl_pre(step + 1)
                gl_main(step)
            P.barrier()
            if dbg and d == DIRS[0]:
                P.dma('sp', dbg['H1'][b], Hacc, writes=['dbgH1'])
                P.barrier()
        if dbg:
            P.dma('sp', dbg['H'][b], Hacc, writes=['dbgH'])
            P.dma('sp', dbg['qt'][b], qt[:], writes=['dbgqt'])
            P.dma('sp', dbg['kt'][b], kt[:], writes=['dbgkt'])
            P.dma('sp', dbg['gv'][b], gv[:], writes=['dbggv'])
            P.dma('sp', dbg['khTok'][b], khTok, writes=['dbgkh'])
            P.dma('sp', dbg['ebL'][b], ebL[:], writes=['dbgebl'])
            P.barrier()
        P.op('pool', lambda e: e.memset(ones_nt[:], 1.0), writes=['rm'])
        win_load(wbuf[:], 2576, 512, 'w_cur')
        P.dma('sp', gnbc[:], g['gng_d'].to_broadcast([128, 512]), writes=['gnbc'])

        def gm_front(j):
            tk = slice(TC + j * 128, TC + (j + 1) * 128)
            xh = xn[:, (j % 2) * 512:(j % 2) * 512 + 512]
            pb = 2 + j % 2
            for k in range(KC):
                P.op('pe', lambda e, k=k: e.matmul(ps[pb][:, :], lhsT=uT[:, k, tk], rhs=wbuf[:, k, :], start=(k == 0),
                                                   stop=(k == KC - 1)), reads=['w_cur'], writes=[PS(pb)], inc=(k == KC - 1))
            P.op('act', lambda e: e.activation(out=xh, in_=ps[pb][:, :], func=AF.Sigmoid), reads=[PS(pb)], writes=[('xnh', j % 2)])
            P.op('dve', lambda e: e.tensor_tensor(out=xh, in0=ps[pb][:, :], in1=xh, op=ALU.mult),
                 reads=[PS(pb), ('xnh', j % 2)], writes=[('xnh', j % 2)])

        def gm_back(j):
            xh = xn[:, (j % 2) * 512:(j % 2) * 512 + 512]
            per_head_norm_stats(j)
            P.op('dve', lambda e: e.tensor_tensor(out=rms_scratch[:, 0:512], in0=rms_scratch[:, 0:512], in1=gnbc[:], op=ALU.mult),
                 reads=['rms_scratch', 'gnbc'], writes=['rms_scratch'])
            P.op('dve', lambda e: e.tensor_tensor(out=g_tok(j), in0=rms_scratch[:, 0:512], in1=xh, op=ALU.mult),
                 reads=['rms_scratch', ('xnh', j % 2), ('Hacc', j)], writes=[('g_tok', j)])

        gm_front(0)
        for j in range(NLT):
            if j + 1 < NLT:
                gm_front(j + 1)
            gm_back(j)
        P.barrier()

    if dbg:
        P.dma('sp', dbg['uT'][b], uT[:], writes=['dbg_uT'])
        P.barrier()
    for j in range(NLT if KSTOP == '' else 0):
        for src_i, src in enumerate((a_tok[:, j, :], g_tok(j))):
            pb = src_i
            for q in range(4):
                P.op('pe', lambda e, q=q, src=src, pb=pb: e.transpose(out=psb[pb][:, q * 128:(q + 1) * 128],
                                                                      in_=src[:, q * 128:(q + 1) * 128], identity=ident_b[:]),
                     reads=['ident_b'], writes=[PS(pb)], inc=(q == 3))
            P.op('act' if src_i == 0 else 'dve',
                 (lambda e, j=j, pb=pb, src_i=src_i: e.copy(out=mT[:, 4 * src_i:4 * src_i + 4, j * 128:(j + 1) * 128],
                                                            in_=psb[pb][:, 0:512].rearrange("p (q c) -> p q c", c=128)))
                 if src_i == 0 else
                 (lambda e, j=j, pb=pb, src_i=src_i: e.tensor_copy(out=mT[:, 4 * src_i:4 * src_i + 4, j * 128:(j + 1) * 128],
                                                                   in_=psb[pb][:, 0:512].rearrange("p (q c) -> p q c", c=128))),
                 reads=[PS(pb)], writes=[('mT', j, src_i)])
    P.barrier()
    if dbg:
        P.dma('sp', dbg['mT'][b], mT, writes=['dbg_mT'])
        P.barrier()
    with nc.sbuf_tensor(S("wout"), [128, KC, D], BF16) as wout, nc.sbuf_tensor(S("xt2"), [128, 2, D], F32) as xt2, \
            nc.sbuf_tensor(S("pt2"), [128, 2, D], F32) as pt2:
        for half in range(2):
            P.dma('pool', wout[:, :, half * 512:(half + 1) * 512],
                  wout_d[:, half * 512:(half + 1) * 512].rearrange("(k p) c -> p k c", p=128), writes=['wout'])
        g['make_gbc'](b, 0)
        for j in range(NLT):
            bf = j % 2
            P.dma('sp', x_res[:, j, :], x_d[b, j * 128:(j + 1) * 128, :], writes=[('x_res', j)])
            P.dma('sp', pt2[:, bf, :], pe_d[j * 128:(j + 1) * 128, :], writes=[('pt2', bf)])
            P.op('pool', lambda e, j=j, bf=bf: e.tensor_tensor(out=x_res[:, j, :], in0=x_res[:, j, :], in1=pt2[:, bf, :], op=ALU.add),
                 reads=[('x_res', j), ('pt2', bf)], writes=[('x_res', j)])
            for half in range(2):
                pb = 2 + half
                hs = slice(half * 512, (half + 1) * 512)
                for k in range(KC):
                    P.op('pe', lambda e, k=k, j=j, pb=pb, hs=hs: e.matmul(ps[pb][:, :], lhsT=mT[:, k, j * 128:(j + 1) * 128],
                                                                          rhs=wout[:, k, hs], start=(k == 0), stop=(k == KC - 1)),
                         reads=['wout'], writes=[PS(pb)], inc=(k == KC - 1))
                P.op('dve', lambda e, pb=pb, hs=hs, bf=bf: e.tensor_tensor(out=xt2[:, bf, hs], in0=ps[pb][:, :], in1=gbc[:, hs],
                                                                           op=ALU.mult), reads=[PS(pb), 'gbc'], writes=[('xt2', bf, half)])
                if dbg:
                    pass
                P.op('pool', lambda e, j=j, hs=hs, bf=bf: e.tensor_tensor(out=x_res[:, j, hs], in0=x_res[:, j, hs], in1=xt2[:, bf, hs],
                                                                          op=ALU.add), reads=[('xt2', bf, half), ('x_res', j)],
                     writes=[('x_res', j)])
        P.barrier()
    st.close()


_CACHE = {}


def _get_nc(cfg_key):
    if cfg_key not in _CACHE:
        _CACHE[cfg_key] = build(Cfg(*cfg_key))
    return _CACHE[cfg_key]


def make_in_maps(inputs, n_cores, NB):
    f = lambda a: np.ascontiguousarray(np.asarray(a, dtype=np.float32))
    shared = {
        "c_ctx": f(inputs["c_ctx"]).reshape(1, D),
        "ada_w": f(inputs["ada_w"])[0], "ada_b": f(inputs["ada_b"]).reshape(1, -1),
        "norm1_g": f(inputs["norm1_g"]).reshape(1, D), "w_in": f(inputs["w_in"])[0],
        "ml_conv_w": f(inputs["ml_conv_w"])[0], "ml_conv_b": f(inputs["ml_conv_b"]).reshape(1, -1),
        "ml_gate_b": f(inputs["ml_gate_b"])[0], "ml_norm_g": f(inputs["ml_norm_g"]).reshape(1, -1),
        "gla_gate_w2": f(inputs["gla_gate_w2"])[0], "gla_gate_b": f(inputs["gla_gate_b"])[0],
        "gla_norm_g": f(inputs["gla_norm_g"]).reshape(1, -1), "w_out": f(inputs["w_out"])[0],
        "norm2_g": f(inputs["norm2_g"]).reshape(1, D), "router_w": f(inputs["router_w"])[0],
        "router_b": f(inputs["router_b"]).reshape(1, -1), "moe_w_gu": f(inputs["moe_w_gu"])[0],
        "moe_b_gu": f(inputs["moe_b_gu"])[0], "moe_w_down": f(inputs["moe_w_down"])[0],
        "moe_b_down": f(inputs["moe_b_down"])[0], "final_norm_g": f(inputs["final_norm_g"]).reshape(1, D),
    }
    x, c, ctx = f(inputs["x"]), f(inputs["c"]), f(inputs["ctx"])
    maps = []
    for i in range(n_cores):
        m = dict(shared)
        m["x"] = x[i * NB:(i + 1) * NB]
        m["c"] = c[i * NB:(i + 1) * NB]
        m["ctx"] = ctx[i * NB:(i + 1) * NB]
        maps.append(m)
    return maps


def kernel(**inputs):
    n_cores = 8
    B = inputs["x"].shape[0]
    NB = B // n_cores
    T = inputs["x"].shape[1]
    TC = inputs["ctx"].shape[1]
    E = inputs["router_w"].shape[-1]
    nc = _get_nc((NB, T, TC, E, False, 99))
    maps = make_in_maps(inputs, n_cores, NB)
    res = run_bass_kernel_spmd(nc, maps, core_ids=list(range(n_cores)))
    return np.concatenate([r["out"] for r in res.results], axis=0)
```

```python
import math
import os
import types
import numpy as np
from contextlib import ExitStack
import concourse.bass as bass
import concourse.mybir as mybir
from concourse.bass_utils import run_bass_kernel_spmd

F32 = mybir.dt.float32
BF16 = mybir.dt.bfloat16
AF = mybir.ActivationFunctionType
ALU = mybir.AluOpType
AX = mybir.AxisListType

D = 1024
KC = 8
EPS = 1e-6
LIM = 7.0
ALPHA = 1.702


class Cfg:
    def __init__(self, NB=4, T=2048, TC=256, E=32, debug=False, stages=99):
        self.NB, self.T, self.TC, self.E = NB, T, TC, E
        self.NT = T + TC
        self.NTT = self.NT // 128
        self.NTC = TC // 128
        self.NLT = T // 128
        self.NCH = self.NT // 64
        self.NCC = TC // 64
        self.debug = debug
        self.stages = stages


def _snapshot(fn):
    if fn is None or fn.__closure__ is None:
        return fn
    cells = []
    for c in fn.__closure__:
        try:
            cells.append(types.CellType(c.cell_contents))
        except ValueError:
            cells.append(c)
    return types.FunctionType(fn.__code__, fn.__globals__, fn.__name__, fn.__defaults__, tuple(cells))


class Prog:
    ENGS = ('pe', 'act', 'dve', 'pool', 'sp')

    def __init__(self, nc, st):
        self.nc = nc
        self.sem = {e: st.enter_context(nc.semaphore('sem_' + e)) for e in self.ENGS}
        self.cnt = dict.fromkeys(self.ENGS, 0)
        self.seen = {e: {} for e in self.ENGS}
        self.streams = {e: [] for e in self.ENGS}
        self.res = {}
        self.pend = {e: [] for e in self.ENGS}
        self.dsem, self.dcnt, self.drr = {}, {}, {}
        for q, n in (('sp', 14), ('pool', 10), ('act', 4)):
            self.dsem[q] = [st.enter_context(nc.semaphore(f'dma_{q}{i}')) for i in range(n)]
            self.dcnt[q] = [0] * n
            self.drr[q] = 0
        self.all_dma_events = []

    def _need(self, eng, ev, waits, raw):
        key, sem, val = ev
        if key == eng and not raw and eng == 'pe':
            return
        if val is None:
            raise RuntimeError(f'pending event consumed: {key} by {eng}')
        if self.seen[eng].get(key, 0) >= val:
            return
        if key in waits and waits[key][2] >= val:
            return
        waits[key] = (key, sem, val)

    def _deps(self, eng, reads, writes):
        waits = {}
        for r in reads:
            s = self.res.get(r)
            if s and s[0] is not None:
                self._need(eng, s[0], waits, True)
        for w in writes:
            s = self.res.get(w)
            if s:
                if s[0] is not None:
                    self._need(eng, s[0], waits, False)
                for ev in s[1].values():
                    self._need(eng, ev, waits, False)
        wl = list(waits.values())
        for key, sem, val in wl:
            self.seen[eng][key] = max(self.seen[eng].get(key, 0), val)
        return wl

    def _register(self, ev, reads, writes):
        for r in reads:
            s = self.res.setdefault(r, [None, {}])
            s[1][ev[0]] = ev
        for w in writes:
            self.res[w] = [ev, {}]

    def op(self, eng, fn, reads=(), writes=(), inc=True):
        fn = _snapshot(fn)
        wl = self._deps(eng, reads, writes)
        if inc:
            self.cnt[eng] += 1
            ev = [eng, self.sem[eng], self.cnt[eng]]
            for p in self.pend[eng]:
                p[2] = self.cnt[eng]
            self.pend[eng] = []
        else:
            ev = [eng, self.sem[eng], None]
            self.pend[eng].append(ev)
        self._register(ev, reads, writes)
        self.streams[eng].append((wl, fn, 'inc' if inc else None))

    def dma(self, q, out, in_, reads=(), writes=(), **kw):
        k = self.drr[q]
        self.drr[q] = (k + 1) % len(self.dsem[q])
        sem = self.dsem[q][k]
        key = ('d', q, k)
        wl = self._deps(q, reads, writes)
        prev = self.dcnt[q][k]
        if prev > 0 and self.seen[q].get(key, 0) < 16 * prev:
            wl.append((key, sem, 16 * prev))
            self.seen[q][key] = 16 * prev
        self.dcnt[q][k] += 1
        ev = [key, sem, 16 * self.dcnt[q][k]]
        self._register(ev, reads, writes)
        self.all_dma_events.append(ev)

        def fn(e, out=out, in_=in_, kw=kw, sem=sem):
            e.dma_start(out=out, in_=in_, **kw).then_inc(sem, 16)
        self.streams[q].append((wl, fn, 'dma'))

    def barrier(self):
        for e in self.ENGS:
            wl = []
            for o in self.ENGS:
                if self.cnt[o] > self.seen[e].get(o, 0):
                    if self.pend[o]:
                        raise RuntimeError('barrier with pending non-inc ops on ' + o)
                    wl.append((o, self.sem[o], self.cnt[o]))
                    self.seen[e][o] = self.cnt[o]
            for q in self.dsem:
                for k, c in enumerate(self.dcnt[q]):
                    key = ('d', q, k)
                    if c > 0 and self.seen[e].get(key, 0) < 16 * c:
                        wl.append((key, self.dsem[q][k], 16 * c))
                        self.seen[e][key] = 16 * c
            if wl:
                self.streams[e].append((wl, None, None))
        self.res = {}

    def emit(self, block):
        decos = {'pe': block.tensor, 'act': block.scalar, 'dve': block.vector,
                 'pool': block.gpsimd, 'sp': block.sync}
        for name in self.ENGS:
            stream = self.streams[name]
            sem_e = self.sem[name]

            def body(e, stream=stream, sem_e=sem_e):
                for wl, fn, kind in stream:
                    for key, sem, val in wl:
                        e.wait_ge(sem, val)
                    if fn is None:
                        continue
                    ins = fn(e)
                    if kind == 'inc':
                        ins.then_inc(sem_e, 1)
            decos[name](body)


def build(cfg):
    NB, T, TC, E = cfg.NB, cfg.T, cfg.TC, cfg.E
    NT, NTT, NTC, NLT, NCH, NCC = cfg.NT, cfg.NTT, cfg.NTC, cfg.NLT, cfg.NCH, cfg.NCC
    NBC = NB + 1
    nc = bass.Bass("TRN2", target_bir_lowering=False)

    def din(name, shape):
        return nc.dram_tensor(name, list(shape), F32, kind="ExternalInput").ap()

    x_d = din("x", [NB, T, D])
    c_d = din("c", [NB, D])
    ctx_d = din("ctx", [NB, TC, D])
    cctx_d = din("c_ctx", [1, D])
    adaw_d = din("ada_w", [D, 6 * D])
    adab_d = din("ada_b", [1, 6 * D])
    n1g_d = din("norm1_g", [1, D])
    win_d = din("w_in", [D, 3120])
    cw_d = din("ml_conv_w", [3, 512])
    cb_d = din("ml_conv_b", [1, 512])
    mgb_d = din("ml_gate_b", [4, 4])
    mng_d = din("ml_norm_g", [1, 512])
    gw2_d = din("gla_gate_w2", [2, 16, 256])
    ggb_d = din("gla_gate_b", [2, 256])
    gng_d = din("gla_norm_g", [1, 512])
    wout_d = din("w_out", [D, D])
    n2g_d = din("norm2_g", [1, D])
    rw_d = din("router_w", [D, E])
    rb_d = din("router_b", [1, E])
    wgu_d = din("moe_w_gu", [E, D, 2 * D])
    bgu_d = din("moe_b_gu", [E, 2 * D])
    wdn_d = din("moe_w_down", [E, D, D])
    bdn_d = din("moe_b_down", [E, D])
    fng_d = din("final_norm_g", [1, D])
    out_d = nc.dram_tensor("out", [NB, T, D], F32, kind="ExternalOutput").ap()
    pe_d = nc.dram_tensor("pe_scratch", [T, D], F32, kind="Internal").ap()
    gsc_d = nc.dram_tensor("gate_scratch", [NBC, 2, D], F32, kind="Internal").ap()
    dbg = {}
    if cfg.debug:
        dbg['xmid'] = nc.dram_tensor("dbg_xmid", [NB, T, D], F32, kind="ExternalOutput").ap()
        dbg['mT'] = nc.dram_tensor("dbg_mT", [NB, 128, KC, T], BF16, kind="ExternalOutput").ap()
        dbg['uT'] = nc.dram_tensor("dbg_uT", [NB, 128, KC, NT], BF16, kind="ExternalOutput").ap()
        dbg['H'] = nc.dram_tensor("dbg_H", [NB, 128, NLT, 512], F32, kind="ExternalOutput").ap()
        dbg['H1'] = nc.dram_tensor("dbg_H1", [NB, 128, NLT, 512], F32, kind="ExternalOutput").ap()
        dbg['HM'] = nc.dram_tensor("dbg_HM", [NB, 128, NLT, 512], F32, kind="ExternalOutput").ap()
        dbg['edT'] = nc.dram_tensor("dbg_edT", [NB, 128, NTT, 8], F32, kind="ExternalOutput").ap()
        dbg['wcB'] = nc.dram_tensor("dbg_wcB", [NB, 128, 2, NCH], F32, kind="ExternalOutput").ap()
        dbg['qkT'] = nc.dram_tensor("dbg_qkT", [NB, 128, 4, NT], BF16, kind="ExternalOutput").ap()
        dbg['vtok'] = nc.dram_tensor("dbg_vtok", [NB, 128, NTT, 4, 129], BF16, kind="ExternalOutput").ap()
        dbg['kTok'] = nc.dram_tensor("dbg_kTok", [NB, 128, NTT, 256], BF16, kind="ExternalOutput").ap()
        dbg['qt'] = nc.dram_tensor("dbg_qt", [NB, 128, 2, NT], BF16, kind="ExternalOutput").ap()
        dbg['kt'] = nc.dram_tensor("dbg_kt", [NB, 128, 2, NT], BF16, kind="ExternalOutput").ap()
        dbg['gv'] = nc.dram_tensor("dbg_gv", [NB, 128, NTT, 512], BF16, kind="ExternalOutput").ap()
        dbg['khTok'] = nc.dram_tensor("dbg_khTok", [NB, 128, NTT, 256], BF16, kind="ExternalOutput").ap()
        dbg['ebL'] = nc.dram_tensor("dbg_ebL", [NB, 128, 2, NCH], F32, kind="ExternalOutput").ap()

    st = ExitStack()
    P = Prog(nc, st)

    def sb(name, shape, dt=F32):
        return st.enter_context(nc.sbuf_tensor(name, list(shape), dt))

    ps = [st.enter_context(nc.psum_tensor(f"ps{i}", [128, 512], F32)) for i in range(8)]
    psb = [p[:].bitcast(BF16) for p in ps]

    def PS(i):
        return ('ps', i)

    ident_f = sb("ident_f", [128, 128])
    ident_b = sb("ident_b", [128, 128], BF16)
    ones_f = sb("ones_f", [128, 128])
    ones_nt = sb("ones_nt", [128, NT], BF16)
    maskF = sb("maskF", [128, 64])
    maskB = sb("maskB", [128, 64])
    selh = sb("selh", [4, 2, 2, 64])
    eps_c = sb("eps_c", [128, 1])
    one_c = sb("one_c", [128, 1])
    P.op('pool', lambda e: e.memset(one_c[:], 1.0), writes=['one_c'])
    P.op('pool', lambda e: e.memset(eps_c[:], EPS), writes=['eps_c'])
    P.op('pool', lambda e: e.memset(ones_f[:], 1.0), writes=['ones_f'])
    P.op('pool', lambda e: e.memset(ones_nt[:], 1.0), writes=['ones_nt'])
    P.op('pool', lambda e: e.affine_select(out=ident_f[:], in_=ones_f[:], pattern=[[-1, 128]],
                                           compare_op=ALU.is_equal, fill=0.0, base=0, channel_multiplier=1),
         reads=['ones_f'], writes=['ident_f'])
    P.op('dve', lambda e: e.tensor_copy(out=ident_b[:], in_=ident_f[:]), reads=['ident_f'], writes=['ident_b'])
    for half in range(2):
        sl = slice(half * 64, half * 64 + 64)
        P.op('pool', lambda e, sl=sl: e.affine_select(out=maskF[sl, :], in_=ones_f[sl, 0:64], pattern=[[1, 64]],
                                                      compare_op=ALU.is_ge, fill=0.0, base=0, channel_multiplier=-1),
             reads=['ones_f'], writes=['maskF'])
        P.op('pool', lambda e, sl=sl: e.affine_select(out=maskB[sl, :], in_=ones_f[sl, 0:64], pattern=[[-1, 64]],
                                                      compare_op=ALU.is_ge, fill=0.0, base=0, channel_multiplier=1),
             reads=['ones_f'], writes=['maskB'])
    P.op('pool', lambda e: e.affine_select(out=selh[:].rearrange("p a b c -> p (a b c)"), in_=ones_nt[0:4, 0:256],
                                           pattern=[[-2, 2], [-1, 2], [0, 64]], compare_op=ALU.is_equal, fill=0.0, base=0,
                                           channel_multiplier=1),
         reads=['ones_nt'], writes=['selh'])

    modT = sb("modT", [128, 4, KC, NBC])
    rw_sb = sb("rw_sb", [128, KC, E])
    rb_bc = sb("rb_bc", [128, E])
    bguT = sb("bguT", [128, 16, E])
    cw_sb = sb("cw_sb", [128, 4, 3])
    cb_sb = sb("cb_sb", [128, 4])
    mgb_sb = sb("mgb_sb", [4, 4])
    wg_sb = sb("wg_sb", [128, KC, 16], BF16)
    wlr_sb = sb("wlr_sb", [128, KC, 32], BF16)
    gw2_sb = sb("gw2_sb", [16, 2, 256], BF16)
    ggbT = sb("ggbT", [128, 2, 2])
    nc_allow = nc.allow_non_contiguous_dma(reason="tiny param layouts")
    st.enter_context(nc_allow)

    P.dma('sp', rw_sb[:], rw_d.rearrange("(k p) e -> p k e", p=128), writes=['rw_sb'])
    P.dma('sp', rb_bc[:], rb_d.to_broadcast([128, E]), writes=['rb_bc'])
    for q in range(4):
        P.dma('sp', cw_sb[:, q, :], cw_d[:, q * 128:(q + 1) * 128].rearrange("i p -> p i"), writes=['cw_sb'])
    P.dma('sp', cb_sb[:], cb_d.rearrange("o (q p) -> p (o q)", p=128), writes=['cb_sb'])
    P.dma('sp', mgb_sb[:], mgb_d.rearrange("g h -> h g"), writes=['mgb_sb'])
    P.dma('sp', ggbT[:], ggb_d.rearrange("z (j p) -> p z j", p=128), writes=['ggbT'])
    P.dma('pool', wg_sb[:], win_d[:, 1536:1552].rearrange("(k p) c -> p k c", p=128), writes=['wg_sb'])
    P.dma('pool', wlr_sb[:], win_d[:, 3088:3120].rearrange("(k p) c -> p k c", p=128), writes=['wlr_sb'])
    P.dma('pool', gw2_sb[:], gw2_d.rearrange("z r c -> r z c"), writes=['gw2_sb'])
    with nc.sbuf_tensor("bgu_rows", [E, 2 * D], F32) as bgu_rows:
        P.dma('sp', bgu_rows[:], bgu_d, writes=['bgu_rows'])
        for j in range(16):
            P.op('pe', lambda e, j=j: e.transpose(out=ps[0][:, j * E:(j + 1) * E], in_=bgu_rows[:, j * 128:(j + 1) * 128],
                                                  identity=ident_f[0:E, 0:E]), reads=['bgu_rows', 'ident_f'], writes=[PS(0)],
                 inc=(j == 15))
        P.op('dve', lambda e: e.tensor_copy(out=bguT[:].rearrange("p j e -> p (j e)"), in_=ps[0][:, 0:16 * E]),
             reads=[PS(0)], writes=['bguT'])
        P.op('dve', lambda e: e.tensor_scalar(out=bguT[:, 8:16, :], in0=bguT[:, 8:16, :], scalar1=1.0, scalar2=None, op0=ALU.add),
             reads=['bguT'], writes=['bguT'])
        P.barrier()
    P.op('dve', lambda e: e.tensor_scalar(out=ggbT[:], in0=ggbT[:], scalar1=-1.0, scalar2=None, op0=ALU.mult),
         reads=['ggbT'], writes=['ggbT'])

    with nc.sbuf_tensor("om", [128, 256], F32) as om, nc.sbuf_tensor("jf", [128, 256], F32) as jf, \
            nc.sbuf_tensor("pidx", [128, 2], F32) as pidx, nc.sbuf_tensor("arg", [128, 256], F32) as arg, \
            nc.sbuf_tensor("petile", [128, 2, D], F32) as petile, nc.sbuf_tensor("omr0", [128, 256], F32) as omr0, \
            nc.sbuf_tensor("argc", [128, 256], F32) as argc, nc.sbuf_tensor("argr", [128, 256], F32) as argr, \
            nc.sbuf_tensor("arg2", [128, 256], F32) as arg2:
        P.op('pool', lambda e: e.iota(out=jf[:], pattern=[[1, 256]], base=0, channel_multiplier=0,
                                      allow_small_or_imprecise_dtypes=True), writes=['jf'])
        P.op('act', lambda e: e.activation(out=om[:], in_=jf[:], func=AF.Exp, scale=-math.log(10000.0) / 256.0),
             reads=['jf'], writes=['om'])
        for half in range(2):
            sl = slice(half * 64, half * 64 + 64)
            P.op('pool', lambda e, sl=sl: e.iota(out=pidx[sl, 0:1], pattern=[[0, 1]], base=0, channel_multiplier=1,
                                                 allow_small_or_imprecise_dtypes=True), writes=['pidx'])
            P.op('pool', lambda e, sl=sl, half=half: e.memset(pidx[sl, 1:2], float(half)), writes=['pidx'])
        PI = math.pi

        def sincos(dst_sin, dst_cos, argap, rd):
            MAGIC = 12582912.0
            for dst, off in ((dst_sin, 0.0), (dst_cos, 0.5 * PI)):
                P.op('dve', lambda e, off=off: e.tensor_scalar(out=arg2[:], in0=argap, scalar1=off, scalar2=None, op0=ALU.add),
                     reads=rd, writes=['arg2'])
                P.op('dve', lambda e: e.tensor_scalar(out=arg[:], in0=arg2[:], scalar1=1.0 / (2 * PI), scalar2=MAGIC,
                                                      op0=ALU.mult, op1=ALU.add), reads=['arg2'], writes=['arg'])
                P.op('dve', lambda e: e.tensor_scalar(out=arg[:], in0=arg[:], scalar1=-MAGIC, scalar2=None, op0=ALU.add),
                     reads=['arg'], writes=['arg'])
                P.op('dve', lambda e: e.scalar_tensor_tensor(out=arg[:], in0=arg[:], scalar=-2 * PI, in1=arg2[:],
                                                             op0=ALU.mult, op1=ALU.add), reads=['arg', 'arg2'], writes=['arg'])
                P.op('dve', lambda e: e.tensor_scalar(out=arg[:], in0=arg[:], scalar1=-PI, scalar2=PI, op0=ALU.max, op1=ALU.min),
                     reads=['arg'], writes=['arg'])
                P.op('act', lambda e, dst=dst: e.activation(out=dst, in_=arg[:], func=AF.Sin), reads=['arg'],
                     writes=['petile'])
        P.op('dve', lambda e: e.tensor_scalar(out=omr0[:], in0=om[:], scalar1=pidx[:, 1:2], scalar2=None, op0=ALU.mult),
             reads=['om', 'pidx'], writes=['omr0'])
        P.op('dve', lambda e: e.tensor_scalar(out=argc[:], in0=om[:], scalar1=pidx[:, 0:1], scalar2=None, op0=ALU.mult),
             reads=['om', 'pidx'], writes=['argc'])
        for k in range(NLT):
            buf = k % 2
            if k < 2:
                sincos(petile[:, buf, 512:768], petile[:, buf, 768:1024], argc[:], ['argc'])
            P.op('dve', lambda e, k=k: e.scalar_tensor_tensor(out=argr[:], in0=om[:], scalar=float(2 * k), in1=omr0[:],
                                                              op0=ALU.mult, op1=ALU.add), reads=['om', 'omr0'], writes=['argr'])
            sincos(petile[:, buf, 0:256], petile[:, buf, 256:512], argr[:], ['argr'])
            P.dma('sp', pe_d[k * 128:(k + 1) * 128, :], petile[:, buf, :], reads=['petile'], writes=['pe_d'])
        P.barrier()

    with nc.sbuf_tensor("cin", [NBC, D], F32) as cin, nc.sbuf_tensor("csig", [NBC, D], F32) as csig, \
            nc.sbuf_tensor("scT", [128, KC, NBC], F32) as scT, nc.sbuf_tensor("adab", [1, 6 * D], F32) as adab, \
            nc.sbuf_tensor("modrows", [NBC, 6 * D], F32) as modrows, \
            nc.sbuf_tensor("adaw", [128, 2, KC, 512], F32) as adaw, \
            nc.sbuf_tensor("grows", [NBC, 2, D], F32) as grows, \
            nc.sbuf_tensor("n1bc", [NBC, D], F32) as n1bc, nc.sbuf_tensor("n2bc", [NBC, D], F32) as n2bc:
        P.dma('sp', n1bc[:], n1g_d.to_broadcast([NBC, D]), writes=['n1bc'])
        P.dma('sp', n2bc[:], n2g_d.to_broadcast([NBC, D]), writes=['n2bc'])
        P.dma('sp', cin[0:NB, :], c_d, writes=['cin'])
        P.dma('sp', cin[NB:NBC, :], cctx_d, writes=['cin'])
        P.dma('sp', adab[:], adab_d, writes=['adab'])
        P.op('act', lambda e: e.activation(out=csig[:], in_=cin[:], func=AF.Sigmoid), reads=['cin'], writes=['csig'])
        P.op('dve', lambda e: e.tensor_tensor(out=csig[:], in0=csig[:], in1=cin[:], op=ALU.mult),
             reads=['csig', 'cin'], writes=['csig'])
        for k in range(KC):
            P.op('pe', lambda e, k=k: e.transpose(out=ps[0][:, k * NBC:(k + 1) * NBC], in_=csig[:, k * 128:(k + 1) * 128],
                                                  identity=ident_f[0:NBC, 0:NBC]),
                 reads=['csig', 'ident_f'], writes=[PS(0)], inc=(k == KC - 1))
        P.op('dve', lambda e: e.tensor_copy(out=scT[:].rearrange("p k b -> p (k b)"), in_=ps[0][:, 0:KC * NBC]),
             reads=[PS(0)], writes=['scT'])
        for cg in range(12):
            buf = cg % 2
            P.dma('sp', adaw[:, buf], adaw_d[:, cg * 512:(cg + 1) * 512].rearrange("(k p) c -> p k c", p=128),
                  writes=[('adaw', buf)])
            pb = 1 + (cg % 2)
            for k in range(KC):
                P.op('pe', lambda e, k=k, buf=buf, pb=pb: e.matmul(ps[pb][0:NBC, :], lhsT=scT[:, k, :], rhs=adaw[:, buf, k, :],
                                                                   start=(k == 0), stop=False),
                     reads=['scT', ('adaw', buf)], writes=[PS(pb)], inc=False)
            P.op('pe', lambda e, cg=cg, pb=pb: e.matmul(ps[pb][0:NBC, :], lhsT=ones_f[0:1, 0:NBC],
                                                        rhs=adab[0:1, cg * 512:(cg + 1) * 512], start=False, stop=True),
                 reads=['ones_f', 'adab'], writes=[PS(pb)])
            P.op('act', lambda e, cg=cg, pb=pb: e.copy(out=modrows[:, cg * 512:(cg + 1) * 512], in_=ps[pb][0:NBC, :]),
                 reads=[PS(pb)], writes=['modrows'])
        P.op('dve', lambda e: e.scalar_tensor_tensor(out=grows[:, 0, :], in0=modrows[:, D:2 * D], scalar=1.0, in1=n1bc[:],
                                                     op0=ALU.add, op1=ALU.mult), reads=['modrows', 'n1bc'], writes=['grows'])
        P.op('dve', lambda e: e.scalar_tensor_tensor(out=grows[:, 1, :], in0=modrows[:, 4 * D:5 * D], scalar=1.0, in1=n2bc[:],
                                                     op0=ALU.add, op1=ALU.mult), reads=['modrows', 'n2bc'], writes=['grows'])
        srcs = [grows[:, 0, :], modrows[:, 0:D], grows[:, 1, :], modrows[:, 3 * D:4 * D]]
        for m in range(4):
            for k in range(KC):
                o = (m * KC + k) * NBC
                P.op('pe', lambda e, m=m, k=k, o=o: e.transpose(out=ps[3][:, o:o + NBC], in_=srcs[m][:, k * 128:(k + 1) * 128],
                                                                identity=ident_f[0:NBC, 0:NBC]),
                     reads=['grows', 'modrows', 'ident_f'], writes=[PS(3)], inc=(m == 3 and k == KC - 1))
        P.op('dve', lambda e: e.tensor_copy(out=modT[:].rearrange("p m k b -> p (m k b)"), in_=ps[3][:, 0:4 * KC * NBC]),
             reads=[PS(3)], writes=['modT'])
        P.dma('sp', gsc_d[:, 0, :], modrows[:, 2 * D:3 * D], reads=['modrows'], writes=['gsc_d'])
        P.dma('sp', gsc_d[:, 1, :], modrows[:, 5 * D:6 * D], reads=['modrows'], writes=['gsc_d'])
        P.barrier()

    def rms_to_featmajor(xt_ap, xt_res, dstT, tok0, gsel, bsel, scratch, xn, ssq, pbanks, extra_f32=None, extra_res='extra_f32'):
        P.op('act', lambda e: e.activation(out=scratch, in_=xt_ap, func=AF.Square, accum_out=ssq),
             reads=[xt_res], writes=['rms_scratch', 'ssq'])
        P.op('act', lambda e: e.activation(out=ssq, in_=ssq, func=AF.Sqrt, scale=1.0 / D, bias=eps_c[:]),
             reads=['ssq'], writes=['ssq'])
        P.op('dve', lambda e: e.reciprocal(out=ssq, in_=ssq), reads=['ssq'], writes=['ssq'])
        P.op('dve', lambda e: e.tensor_scalar(out=xn, in0=xt_ap, scalar1=ssq, scalar2=None, op0=ALU.mult),
             reads=['ssq', xt_res], writes=['xn'])
        for k in range(KC):
            pb = pbanks[k // 4]
            P.op('pe', lambda e, k=k, pb=pb: e.transpose(out=ps[pb][:, (k % 4) * 128:(k % 4 + 1) * 128],
                                                         in_=xn[:, k * 128:(k + 1) * 128], identity=ident_f[:]),
                 reads=['xn', 'ident_f'], writes=[PS(pb)], inc=(k % 4 == 3))
        for k in range(KC):
            pb = pbanks[k // 4]
            P.op('act', lambda e, k=k, pb=pb: e.activation(out=dstT[:, k, tok0:tok0 + 128],
                                                           in_=ps[pb][:, (k % 4) * 128:(k % 4 + 1) * 128], func=AF.Identity,
                                                           scale=modT[:, gsel, k, bsel:bsel + 1],
                                                           bias=modT[:, gsel + 1, k, bsel:bsel + 1]),
                 reads=[PS(pb), 'modT'], writes=[('dstT', tok0)])
            if extra_f32 is not None:
                P.op('act', lambda e, k=k, pb=pb: e.activation(out=extra_f32[:, k, :],
                                                               in_=ps[pb][:, (k % 4) * 128:(k % 4 + 1) * 128], func=AF.Identity,
                                                               scale=modT[:, gsel, k, bsel:bsel + 1],
                                                               bias=modT[:, gsel + 1, k, bsel:bsel + 1]),
                     reads=[PS(pb), 'modT'], writes=[extra_res])

    ARENA = max(NLT * D, NLT * 512 + max(3 * NT, NLT * 256 + NTT * 128 + NT // 2 + 64))
    arena = sb("arena", [128, ARENA])
    x_res = arena[:, 0:NLT * D].rearrange("p (j c) -> p j c", c=D)
    gbc = sb("gbc", [128, D])
    rms_scratch = sb("rms_scratch", [128, D])
    xn = sb("xn", [128, D])
    ssq = sb("ssq", [128, 1])

    def make_gbc(b, which):
        P.dma('sp', gbc[:], gsc_d[b:b + 1, which, :].to_broadcast([128, D]), writes=['gbc'])

    for b in range(NB):
        if cfg.stages >= 2:
            build_mixer(nc, P, cfg, b, dict(
                ps=ps, psb=psb, PS=PS, ident_f=ident_f, ident_b=ident_b, ones_f=ones_f, ones_nt=ones_nt, maskF=maskF,
                maskB=maskB, selh=selh, modT=modT, x_d=x_d, ctx_d=ctx_d, pe_d=pe_d, win_d=win_d, wout_d=wout_d,
                cw_sb=cw_sb, cb_sb=cb_sb, mgb_sb=mgb_sb, wg_sb=wg_sb, wlr_sb=wlr_sb, gw2_sb=gw2_sb, ggbT=ggbT,
                mng_d=mng_d, gng_d=gng_d, x_res=x_res, arena=arena, eps_c=eps_c, one_c=one_c, gbc=gbc,
                rms_scratch=rms_scratch, xn=xn, ssq=ssq,
                make_gbc=make_gbc, rms_to_featmajor=rms_to_featmajor, dbg=dbg))
        else:
            with nc.sbuf_tensor(f"pt0_{b}", [128, 2, D], F32) as pt0:
                for j in range(NLT):
                    P.dma('sp', x_res[:, j, :], x_d[b, j * 128:(j + 1) * 128, :], writes=[('x_res', j)])
                    P.dma('sp', pt0[:, j % 2, :], pe_d[j * 128:(j + 1) * 128, :], reads=['pe_d'], writes=[('pt0', j % 2)])
                    P.op('dve', lambda e, j=j: e.tensor_tensor(out=x_res[:, j, :], in0=x_res[:, j, :], in1=pt0[:, j % 2, :],
                                                               op=ALU.add), reads=[('x_res', j), ('pt0', j % 2)],
                         writes=[('x_res', j)])
                P.barrier()
        if cfg.debug:
            for j in range(NLT):
                P.dma('sp', dbg['xmid'][b, j * 128:(j + 1) * 128, :], x_res[:, j, :], reads=[('x_res', j)],
                      writes=[('dbgx', j)])
        build_moe(nc, P, cfg, b, dict(
            ps=ps, PS=PS, ident_f=ident_f, modT=modT, x_res=x_res, gbc=gbc, rms_scratch=rms_scratch, xn=xn, ssq=ssq,
            make_gbc=make_gbc, rms_to_featmajor=rms_to_featmajor, rw_sb=rw_sb, rb_bc=rb_bc, bdn_d=bdn_d, bguT=bguT,
            wgu_d=wgu_d, wdn_d=wdn_d, fng_d=fng_d, out_d=out_d, eps_c=eps_c))

    P.barrier()
    with nc.Block() as block:
        P.emit(block)
    st.close()
    return nc


def build_moe(nc, P, cfg, b, g):
    NB, T, E, NLT = cfg.NB, cfg.T, cfg.E, cfg.NLT
    ps, PS, x_res, gbc = g['ps'], g['PS'], g['x_res'], g['gbc']
    ident_f, modT = g['ident_f'], g['modT']
    rw_sb, rb_bc, bguT = g['rw_sb'], g['rb_bc'], g['bguT']
    wgu_d, wdn_d, fng_d, out_d = g['wgu_d'], g['wdn_d'], g['fng_d'], g['out_d']
    TG = 512 if T >= 512 else T
    NG = T // TG
    TPG = TG // 128
    mst = ExitStack()
    u2T = mst.enter_context(nc.sbuf_tensor(f"u2T_{b}", [128, KC, T], BF16))
    gates = mst.enter_context(nc.sbuf_tensor(f"gates_{b}", [128, NLT, E], F32))
    lg2 = mst.enter_context(nc.sbuf_tensor(f"lg_{b}", [128, 2, E], F32))
    top82 = mst.enter_context(nc.sbuf_tensor(f"top8_{b}", [128, 2, 8], F32))
    rsum2 = mst.enter_context(nc.sbuf_tensor(f"rsum_{b}", [128, 2, 2], F32))
    wgu = mst.enter_context(nc.sbuf_tensor(f"wgu_{b}", [128, 2, KC, 1024], BF16))
    wdn = mst.enter_context(nc.sbuf_tensor(f"wdn_{b}", [128, 2, 4, D], BF16))
    hT = mst.enter_context(nc.sbuf_tensor(f"hT_{b}", [128, 2, 4, TG], BF16))
    g1 = mst.enter_context(nc.sbuf_tensor(f"g1_{b}", [128, 2, TG], F32))
    t1 = mst.enter_context(nc.sbuf_tensor(f"t1_{b}", [128, 2, TG], F32))
    sl = mst.enter_context(nc.sbuf_tensor(f"sl_{b}", [128, 2, TG], F32))
    wdn32 = mst.enter_context(nc.sbuf_tensor(f"wdn32_{b}", [128, 4, D], F32))
    w32flat = wdn32[:].rearrange("p k c -> p (k c)")
    u2f = w32flat[:, 0:KC * 128].rearrange("p (k c) -> p k c", c=128)
    bdn_sb = w32flat[0:E, 1024:1024 + D]
    gatesT2 = [w32flat[0:E, 2048:2048 + 128], w32flat[0:E, 2304:2304 + 128]]
    if True:
        xn, ssq, rms_scratch = g['xn'], g['ssq'], g['rms_scratch']
        g['make_gbc'](b, 1)
        P.dma('sp', bdn_sb, g['bdn_d'], writes=['bdn_sb'])

        def load_wgu(step):
            ex_, fh_ = step // 2, step % 2
            buf_ = step % 2
            for part in range(2):
                c0 = part * 1024 + fh_ * 512
                P.dma('pool', wgu[:, buf_, :, part * 512:(part + 1) * 512],
                      wgu_d[ex_, :, c0:c0 + 512].rearrange("(k p) c -> p k c", p=128), writes=[('wgu', buf_)])

        load_wgu(0)
        load_wgu(1)
        u2fs = [u2f, w32flat[:, 3072:3072 + KC * 128].rearrange("p (k c) -> p k c", c=128)]

        def pro_front(j):
            g['rms_to_featmajor'](x_res[:, j, :], ('x_res', j), u2T, j * 128, 2, b, rms_scratch[:], xn[:], ssq[:], (0, 1),
                                  extra_f32=u2fs[j % 2], extra_res=('u2f', j % 2))

        def pro_back(j):
            u2f = u2fs[j % 2]
            si = j % 2
            br, b0_, b1_ = (2, 3, 4) if si == 0 else (5, 6, 7)
            bb = (b0_, b1_)
            lg = lg2[:, si, :]
            top8 = top82[:, si, :]
            rsum = rsum2[:, si, :]
            gatesT = gatesT2[si]
            LG, T8, RS, GT = ('lg', si), ('top8', si), ('rsum', si), ('gatesT', si)
            for k in range(KC):
                P.op('pe', lambda e, k=k: e.matmul(ps[br][:, 0:E], lhsT=u2f[:, k, :], rhs=rw_sb[:, k, :], start=(k == 0),
                                                   stop=(k == KC - 1)), reads=[('u2f', j % 2), 'rw_sb'], writes=[PS(br)],
                     inc=(k == KC - 1))
            yield
            P.op('dve', lambda e: e.tensor_tensor(out=lg, in0=ps[br][:, 0:E], in1=rb_bc[:], op=ALU.add),
                 reads=[PS(br), 'rb_bc'], writes=[LG])
            yield
            P.op('dve', lambda e: e.max(out=top8, in_=lg), reads=[LG], writes=[T8])
            yield
            P.op('dve', lambda e: e.tensor_scalar(out=rsum[:, 0:1], in0=top8[:, 0:1], scalar1=-1.0, scalar2=None, op0=ALU.mult),
                 reads=[T8], writes=[RS])
            yield
            P.op('act', lambda e, j=j: e.activation(out=gates[:, j, :], in_=lg, func=AF.Exp, bias=rsum[:, 0:1], scale=1.0),
                 reads=[LG, RS], writes=[('gates', j)])
            yield
            P.op('dve', lambda e: e.tensor_scalar(out=lg, in0=lg, scalar1=top8[:, 3:4], scalar2=None, op0=ALU.is_ge),
                 reads=[LG, T8, ('gates', j)], writes=[LG])
            yield
            P.op('dve', lambda e, j=j: e.tensor_tensor(out=gates[:, j, :], in0=gates[:, j, :], in1=lg, op=ALU.mult),
                 reads=[('gates', j), LG], writes=[('gates', j)])
            yield
            P.op('dve', lambda e, j=j: e.tensor_reduce(out=rsum[:, 1:2], in_=gates[:, j, :], axis=AX.X, op=ALU.add),
                 reads=[('gates', j)], writes=[RS])
            yield
            P.op('dve', lambda e: e.reciprocal(out=rsum[:, 1:2], in_=rsum[:, 1:2]), reads=[RS], writes=[RS])
            yield
            P.op('dve', lambda e, j=j: e.tensor_scalar(out=gates[:, j, :], in0=gates[:, j, :], scalar1=rsum[:, 1:2],
                                                       scalar2=None, op0=ALU.mult), reads=[('gates', j), RS],
                 writes=[('gates', j)])
            P.op('pe', lambda e, j=j: e.transpose(out=ps[br][0:E, 128:256], in_=gates[:, j, :], identity=ident_f[:]),
                 reads=[('gates', j), 'ident_f'], writes=[PS(br)])
            yield
            P.op('act', lambda e: e.copy(out=gatesT, in_=ps[br][0:E, 128:256]), reads=[PS(br)], writes=[GT])
            yield
            P.op('dve', lambda e, j=j: e.tensor_scalar(out=gates[:, j, :], in0=gates[:, j, :], scalar1=1.0 / ALPHA, scalar2=None,
                                                       op0=ALU.mult), reads=[('gates', j)], writes=[('gates', j)])
            for half in range(2):
                hs = slice(half * 512, (half + 1) * 512)
                P.op('pe', lambda e, hs=hs, half=half: e.matmul(ps[bb[half]][:, :], lhsT=gatesT, rhs=bdn_sb[:, hs],
                                                                start=True, stop=True), reads=[GT, 'bdn_sb'],
                     writes=[PS(bb[half])])
                tmpb = (g1 if si == 0 else t1)[:, half, :] if TG == 512 else rms_scratch[:, hs]
                tres = ('btmp', si, half) if TG == 512 else 'rms_scratch'
                P.op('dve', lambda e, half=half, hs=hs, tmpb=tmpb: e.tensor_tensor(out=tmpb, in0=ps[bb[half]][:, :], in1=gbc[:, hs],
                                                                                   op=ALU.mult), reads=[PS(bb[half]), 'gbc'],
                     writes=[tres])
                P.op('pool', lambda e, j=j, hs=hs, tmpb=tmpb: e.tensor_tensor(out=x_res[:, j, hs], in0=x_res[:, j, hs],
                                                                              in1=tmpb, op=ALU.add),
                     reads=[tres, ('x_res', j)], writes=[('x_res', j)])
            yield
        def fronts(js):
            for jj in js:
                if jj < NLT:
                    pro_front(jj)
                yield

        pro_front(0)
        if NLT > 1:
            pro_front(1)
        for j0 in range(0, NLT, 2):
            gens = [pro_back(j0)] + ([pro_back(j0 + 1)] if j0 + 1 < NLT else []) + [fronts([j0 + 2, j0 + 3])]
            while gens:
                for gnr in list(gens):
                    try:
                        next(gnr)
                    except StopIteration:
                        gens.remove(gnr)
        P.barrier()
        acc_i = 0

        def load_wdn(step):
            ex_, fh_ = step // 2, step % 2
            buf_ = step % 2
            P.dma('sp', wdn32[:], wdn_d[ex_, fh_ * 512:(fh_ + 1) * 512, :].rearrange("(k p) c -> p k c", p=128),
                  writes=['wdn32'])
            for k4 in range(4):
                P.op('pool', lambda e, k4=k4: e.tensor_tensor(out=wdn[:, buf_, k4, :], in0=wdn32[:, k4, :], in1=gbc[:], op=ALU.mult),
                     reads=['wdn32', 'gbc'], writes=[('wdn', buf_)])

        def load_weights(step):
            load_wgu(step)
            load_wdn(step)

        acc_box = [0]

        def emit_G(step, tg, gi):
            ex, fh = step // 2, step % 2
            buf = step % 2
            hb = gi % 2
            toks = slice(tg * TG, (tg + 1) * TG)
            for fc in range(4):
                fidx = fh * 4 + fc
                pg, pu = (fc % 2) * 2, 1 + (fc % 2) * 2
                for k in range(KC):
                    P.op('pe', lambda e, k=k, fc=fc, pg=pg: e.matmul(
                        ps[pg][:, 0:TG], lhsT=wgu[:, buf, k, fc * 128:(fc + 1) * 128], rhs=u2T[:, k, toks],
                        start=(k == 0), stop=(k == KC - 1)), reads=[('wgu', buf)], writes=[PS(pg)], inc=(k == KC - 1))
                for k in range(KC):
                    P.op('pe', lambda e, k=k, fc=fc, pu=pu: e.matmul(
                        ps[pu][:, 0:TG], lhsT=wgu[:, buf, k, 512 + fc * 128:512 + (fc + 1) * 128], rhs=u2T[:, k, toks],
                        start=(k == 0), stop=(k == KC - 1)), reads=[('wgu', buf)], writes=[PS(pu)], inc=(k == KC - 1))
                eb = fc % 2
                P.op('dve', lambda e, pg=pg, eb=eb, fidx=fidx: e.tensor_scalar(
                    out=g1[:, eb, :], in0=ps[pg][:, 0:TG], scalar1=bguT[:, fidx, ex:ex + 1], scalar2=LIM,
                    op0=ALU.add, op1=ALU.min), reads=[PS(pg), 'bguT'], writes=[('g1', eb)])
                P.op('act', lambda e, eb=eb: e.activation(out=sl[:, eb, :], in_=g1[:, eb, :], func=AF.Silu, scale=ALPHA),
                     reads=[('g1', eb)], writes=[('sl', eb)])
                P.op('dve', lambda e, pu=pu, eb=eb, fidx=fidx: e.tensor_scalar(
                    out=t1[:, eb, :], in0=ps[pu][:, 0:TG], scalar1=bguT[:, 8 + fidx, ex:ex + 1], scalar2=1.0 - LIM,
                    op0=ALU.add, op1=ALU.max), reads=[PS(pu), 'bguT'], writes=[('t1', eb)])
                P.op('dve', lambda e, eb=eb, fc=fc: e.scalar_tensor_tensor(
                    out=hT[:, hb, fc, :], in0=t1[:, eb, :], scalar=1.0 + LIM, in1=sl[:, eb, :],
                    op0=ALU.min, op1=ALU.mult), reads=[('t1', eb), ('sl', eb)], writes=[('hT', hb, fc)])

        def emit_D(step, tg, gi):
            ex = step // 2
            buf = step % 2
            hb = gi % 2
            for tt in range(TPG):
                j = tg * TPG + tt
                for half in range(2):
                    pb = 4 + ((tt * 2 + half) % 4)
                    for fc in range(4):
                        P.op('pe', lambda e, fc=fc, pb=pb, tt=tt, half=half: e.matmul(
                            ps[pb][:, :], lhsT=hT[:, hb, fc, tt * 128:(tt + 1) * 128],
                            rhs=wdn[:, buf, fc, half * 512:(half + 1) * 512], start=(fc == 0), stop=(fc == 3)),
                            reads=[('hT', hb, fc), ('wdn', buf)], writes=[PS(pb)], inc=(fc == 3))
                    hs = slice(half * 512, (half + 1) * 512)
                    P.op('dve', lambda e, pb=pb, hs=hs, j=j: e.scalar_tensor_tensor(
                        out=x_res[:, j, hs], in0=ps[pb][:, :], scalar=gates[:, j, ex:ex + 1], in1=x_res[:, j, hs],
                        op0=ALU.mult, op1=ALU.add), reads=[PS(pb), ('x_res', j)], writes=[('x_res', j)])

        items = [(st_, tg) for st_ in range(2 * E) for tg in range(NG)]
        load_wdn(0)
        load_wdn(1)
        emit_G(items[0][0], items[0][1], 0)
        for i, (st_, tg) in enumerate(items):
            if i + 1 < len(items):
                emit_G(items[i + 1][0], items[i + 1][1], i + 1)
            emit_D(st_, tg, i)
            if tg == NG - 1 and st_ + 2 < 2 * E:
                load_weights(st_ + 2)
        P.barrier()
        P.dma('sp', gbc[:], fng_d.to_broadcast([128, D]), writes=['gbc'])
        dbl = (TG == 512)
        xnF = [xn[:], g1[:].rearrange("p a c -> p (a c)") if dbl else xn[:]]
        scF = [rms_scratch[:], t1[:].rearrange("p a c -> p (a c)") if dbl else rms_scratch[:]]
        ssF = [ssq[:], sl[:, 0, 0:1] if dbl else ssq[:]]

        def fin_tile(j):
            sp = (j % 2) if dbl else 0
            xnb, scr, sq = xnF[sp], scF[sp], ssF[sp]
            XN, SQ, SC = ('xnF', sp), ('ssF', sp), ('scF', sp)
            P.op('act', lambda e: e.activation(out=scr, in_=x_res[:, j, :], func=AF.Square, accum_out=sq),
                 reads=[('x_res', j)], writes=[SC, SQ])
            yield
            P.op('act', lambda e: e.activation(out=sq, in_=sq, func=AF.Sqrt, scale=1.0 / D, bias=g['eps_c'][:]),
                 reads=[SQ], writes=[SQ])
            yield
            P.op('dve', lambda e: e.reciprocal(out=sq, in_=sq), reads=[SQ], writes=[SQ])
            yield
            P.op('dve', lambda e: e.tensor_scalar(out=xnb, in0=x_res[:, j, :], scalar1=sq, scalar2=None, op0=ALU.mult),
                 reads=[SQ, ('x_res', j)], writes=[XN])
            yield
            P.op('pool', lambda e: e.tensor_tensor(out=x_res[:, j, :], in0=xnb, in1=gbc[:], op=ALU.mult),
                 reads=[XN, 'gbc'], writes=[('x_res', j)])
            P.dma('sp', out_d[b, j * 128:(j + 1) * 128, :], x_res[:, j, :], reads=[('x_res', j)], writes=[('out', b, j)])
            yield

        nxt = 0
        active = []
        while nxt < NLT or active:
            while len(active) < (2 if dbl else 1) and nxt < NLT:
                active.append(fin_tile(nxt))
                nxt += 1
            for gnr in list(active):
                try:
                    next(gnr)
                except StopIteration:
                    active.remove(gnr)
        P.barrier()
    mst.close()


def build_mixer(nc, P, cfg, b, g):
    NB, T, TC = cfg.NB, cfg.T, cfg.TC
    NT, NTT, NTC, NLT, NCH, NCC = cfg.NT, cfg.NTT, cfg.NTC, cfg.NLT, cfg.NCH, cfg.NCC
    ps, psb, PS = g['ps'], g['psb'], g['PS']
    ident_f, ident_b, ones_f, ones_nt = g['ident_f'], g['ident_b'], g['ones_f'], g['ones_nt']
    masks = (g['maskF'], g['maskB'])
    selh, modT = g['selh'], g['modT']
    x_d, ctx_d, pe_d, win_d, wout_d = g['x_d'], g['ctx_d'], g['pe_d'], g['win_d'], g['wout_d']
    cw_sb, cb_sb, mgb_sb, wg_sb, wlr_sb, gw2_sb, ggbT = (g['cw_sb'], g['cb_sb'], g['mgb_sb'], g['wg_sb'], g['wlr_sb'],
                                                         g['gw2_sb'], g['ggbT'])
    arena, x_res, gbc = g['arena'], g['x_res'], g['gbc']
    rms_scratch, xn, ssq, eps_c, one_c = g['rms_scratch'], g['xn'], g['ssq'], g['eps_c'], g['one_c']
    dbg = g['dbg']
    groups = [(t0, min(512, NT - t0)) for t0 in range(0, NT, 512)]
    HOFF = NLT * 512
    Hacc = arena[:, 0:HOFF].rearrange("p (j c) -> p j c", c=512)
    gt_tiles = [arena[0:4, HOFF + i * NT:HOFF + (i + 1) * NT] for i in range(3)]

    def S(name):
        return f"{name}_{b}"

    def win_load(dst, c0, ncol, res):
        P.dma('pool', dst, win_d[:, c0:c0 + ncol].rearrange("(k p) c -> p k c", p=128), writes=[res])

    def proj_featmajor(w, col0, dst_fn, evac):
        for gi, (t0, n) in enumerate(groups):
            pb = 2 + gi % 2
            for k in range(KC):
                P.op('pe', lambda e, k=k, pb=pb, t0=t0, n=n: e.matmul(ps[pb][:, 0:n], lhsT=w[:, k, col0:col0 + 128],
                                                                      rhs=uT[:, k, t0:t0 + n], start=(k == 0), stop=(k == KC - 1)),
                     reads=['w_cur'], writes=[PS(pb)], inc=(k == KC - 1))
            evac(pb, t0, n)

    def per_head_norm_stats(j):
        P.op('dve', lambda e: e.tensor_tensor(out=rms_scratch[:, 0:512], in0=Hacc[:, j, :], in1=Hacc[:, j, :], op=ALU.mult),
             reads=[('Hacc', j)], writes=['rms_scratch'])
        P.op('dve', lambda e: e.tensor_reduce(out=hst[:, 0:4], in_=rms_scratch[:, 0:512].rearrange("p (h c) -> p h c", c=128),
                                              axis=AX.X, op=ALU.add), reads=['rms_scratch'], writes=['hst'])
        P.op('act', lambda e: e.activation(out=hst[:, 0:4], in_=hst[:, 0:4], func=AF.Sqrt, scale=1.0 / 128, bias=eps_c[:]),
             reads=['hst'], writes=['hst'])
        P.op('dve', lambda e: e.reciprocal(out=hst[:, 0:4], in_=hst[:, 0:4]), reads=['hst'], writes=['hst'])
        P.op('dve', lambda e: e.tensor_tensor(out=rms_scratch[:, 0:512].rearrange("p (h c) -> p h c", c=128),
                                              in0=Hacc[:, j, :].rearrange("p (h c) -> p h c", c=128),
                                              in1=hst[:, 0:4].unsqueeze(2).to_broadcast([128, 4, 128]), op=ALU.mult),
             reads=['hst', ('Hacc', j)], writes=['rms_scratch'])

    st = ExitStack()
    uT = st.enter_context(nc.sbuf_tensor(S("uT"), [128, KC, NT], BF16))
    mT = uT[:].rearrange("p k n -> p (k n)")[:, 0:KC * T].rearrange("p (k t) -> p k t", t=T)
    a_tok = arena[:, HOFF:HOFF + NLT * 256].bitcast(BF16).rearrange("p (j c) -> p j c", c=512)
    AO = HOFF + NLT * 256
    khTok = arena[:, AO:AO + NTT * 128].bitcast(BF16).rearrange("p (i c) -> p i c", c=256)
    khT = arena[:, AO + NTT * 128:AO + NTT * 128 + NT // 2].bitcast(BF16)

    def g_tok(j):
        return arena[:, j * 512:j * 512 + 256].bitcast(BF16)
    hst = st.enter_context(nc.sbuf_tensor(S("hst"), [128, 8], F32))
    gnbc = st.enter_context(nc.sbuf_tensor(S("gnbc"), [128, 512], F32))
    wbuf = st.enter_context(nc.sbuf_tensor(S("wbuf"), [128, KC, 512], BF16))
    Pt = st.enter_context(nc.sbuf_tensor(S("Pt"), [128, 2, 2, 64], BF16))
    dn = st.enter_context(nc.sbuf_tensor(S("dn"), [128, 8], F32))

    with nc.sbuf_tensor(S("xt"), [128, 2, D], F32) as xt, nc.sbuf_tensor(S("pt"), [128, 2, D], F32) as pt, \
            nc.sbuf_tensor(S("xnA"), [128, D], F32) as xnA, nc.sbuf_tensor(S("scrA"), [128, D], F32) as scrA, \
            nc.sbuf_tensor(S("ssqA"), [128, 2], F32) as ssqA:
        def stageA_tile(i):
            sp = i % 2
            xnb = xn[:] if sp == 0 else xnA[:]
            scr = rms_scratch[:] if sp == 0 else scrA[:]
            sq = ssqA[:, sp:sp + 1]
            pbanks = (0, 1) if sp == 0 else (2, 3)
            XT, XN, SQ, SC = ('xt', sp), ('xnA', sp), ('ssqA', sp), ('scrA', sp)
            if i < NTC:
                P.dma('sp', xt[:, sp, :], ctx_d[b, i * 128:(i + 1) * 128, :], writes=[XT])
                bsel = NB
            else:
                j = i - NTC
                P.dma('sp', xt[:, sp, :], x_d[b, j * 128:(j + 1) * 128, :], writes=[XT])
                P.dma('sp', pt[:, sp, :], pe_d[j * 128:(j + 1) * 128, :], writes=[('pt', sp)])
                P.op('pool', lambda e: e.tensor_tensor(out=xt[:, sp, :], in0=xt[:, sp, :], in1=pt[:, sp, :], op=ALU.add),
                     reads=[XT, ('pt', sp)], writes=[XT])
                bsel = b
            yield
            P.op('act', lambda e: e.activation(out=scr, in_=xt[:, sp, :], func=AF.Square, accum_out=sq), reads=[XT], writes=[SC, SQ])
            yield
            P.op('act', lambda e: e.activation(out=sq, in_=sq, func=AF.Sqrt, scale=1.0 / D, bias=eps_c[:]), reads=[SQ], writes=[SQ])
            yield
            P.op('dve', lambda e: e.reciprocal(out=sq, in_=sq), reads=[SQ], writes=[SQ])
            yield
            P.op('dve', lambda e: e.tensor_scalar(out=xnb, in0=xt[:, sp, :], scalar1=sq, scalar2=None, op0=ALU.mult),
                 reads=[SQ, XT], writes=[XN])
            yield
            for k in range(KC):
                pb = pbanks[k // 4]
                P.op('pe', lambda e, k=k, pb=pb: e.transpose(out=ps[pb][:, (k % 4) * 128:(k % 4 + 1) * 128],
                                                             in_=xnb[:, k * 128:(k + 1) * 128], identity=ident_f[:]),
                     reads=[XN, 'ident_f'], writes=[PS(pb)], inc=(k % 4 == 3))
            yield
            for k in range(KC):
                pb = pbanks[k // 4]
                P.op('act', lambda e, k=k, pb=pb: e.activation(out=uT[:, k, i * 128:(i + 1) * 128],
                                                               in_=ps[pb][:, (k % 4) * 128:(k % 4 + 1) * 128], func=AF.Identity,
                                                               scale=modT[:, 0, k, bsel:bsel + 1], bias=modT[:, 1, k, bsel:bsel + 1]),
                     reads=[PS(pb), 'modT'], writes=[('uT', i)])
                if k == 3:
                    yield
            yield

        nxt = 0
        active = []
        while nxt < NTT or active:
            while len(active) < 2 and nxt < NTT:
                active.append(stageA_tile(nxt))
                nxt += 1
            for gnr in list(active):
                try:
                    next(gnr)
                except StopIteration:
                    active.remove(gnr)
        P.barrier()

    mst = ExitStack()
    qkT = mst.enter_context(nc.sbuf_tensor(S("qkT"), [128, 4, NT], BF16))
    kTok = mst.enter_context(nc.sbuf_tensor(S("kTok"), [128, NTT, 256], BF16))
    if True:
        with nc.sbuf_tensor(S("zt"), [128, NT], F32) as zt, nc.sbuf_tensor(S("ot"), [128, NT], F32) as ot, \
                nc.sbuf_tensor(S("sg"), [128, NT], F32) as sg:
            win_load(wbuf[:], 0, 512, 'w_cur')
            for qc in range(4):
                proj_featmajor(wbuf, qc * 128, None,
                               lambda pb, t0, n: P.op('act', lambda e: e.copy(out=zt[:, t0:t0 + n], in_=ps[pb][:, 0:n]),
                                                      reads=[PS(pb)], writes=['zt']))
                P.op('dve', lambda e, qc=qc: e.tensor_scalar(out=ot[:], in0=zt[:], scalar1=cw_sb[:, qc, 1:2],
                                                             scalar2=cb_sb[:, qc:qc + 1], op0=ALU.mult, op1=ALU.add),
                     reads=['zt', 'cw_sb', 'cb_sb'], writes=['ot'])
                for lo, hi in ((0, TC), (TC, NT)):
                    P.op('dve', lambda e, qc=qc, lo=lo, hi=hi: e.scalar_tensor_tensor(
                        out=ot[:, lo + 1:hi], in0=zt[:, lo:hi - 1], scalar=cw_sb[:, qc, 0:1], in1=ot[:, lo + 1:hi],
                        op0=ALU.mult, op1=ALU.add), reads=['zt', 'ot'], writes=['ot'])
                    P.op('dve', lambda e, qc=qc, lo=lo, hi=hi: e.scalar_tensor_tensor(
                        out=ot[:, lo:hi - 1], in0=zt[:, lo + 1:hi], scalar=cw_sb[:, qc, 2:3], in1=ot[:, lo:hi - 1],
                        op0=ALU.mult, op1=ALU.add), reads=['zt', 'ot'], writes=['ot'])
                P.op('act', lambda e: e.activation(out=sg[:], in_=ot[:], func=AF.Sigmoid), reads=['ot'], writes=['sg'])
                P.op('dve', lambda e, qc=qc: e.scalar_tensor_tensor(out=qkT[:, qc, :], in0=ot[:], scalar=(0.125 if qc < 2 else 1.0),
                                                                    in1=sg[:], op0=ALU.mult, op1=ALU.mult),
                     reads=['ot', 'sg'], writes=['qkT'])
            P.barrier()
        vtok = mst.enter_context(nc.sbuf_tensor(S("vtok"), [128, NTT, 4, 129], BF16))
        kp = mst.enter_context(nc.sbuf_tensor(S("kp"), [128, NTT, 256], BF16))
        edT = mst.enter_context(nc.sbuf_tensor(S("edT"), [128, NTT, 8], F32))
        wcB = mst.enter_context(nc.sbuf_tensor(S("wcB"), [128, 2, NCH], F32))
        Chat = mst.enter_context(nc.sbuf_tensor(S("Chat"), [128, 2, 129], F32))
        CbfA = mst.enter_context(nc.sbuf_tensor(S("CbfA"), [128, 2, 2, 129], BF16))
        CbfB = mst.enter_context(nc.sbuf_tensor(S("CbfB"), [128, 2, 2, 129], BF16))
        Aend = mst.enter_context(nc.sbuf_tensor(S("Aend"), [4, 3, NCH], F32))
        for i in range(NTT):
            for kc in range(2):
                P.op('pe', lambda e, i=i, kc=kc: e.transpose(out=psb[1][:, kc * 128:(kc + 1) * 128],
                                                             in_=qkT[:, 2 + kc, i * 128:(i + 1) * 128], identity=ident_b[:]),
                     reads=['ident_b'], writes=[PS(1)], inc=(kc == 1))
            P.op('dve', lambda e, i=i: e.tensor_copy(out=kTok[:, i, :], in_=psb[1][:, 0:256]), reads=[PS(1)], writes=['kTok'])
        win_load(wbuf[:], 512, 512, 'w_cur')
        P.op('pool', lambda e: e.memset(vtok[:, :, :, 128:129], 1.0), writes=['vtok'])
        for i in range(NTT):
            pb = 2 + i % 2
            for k in range(KC):
                P.op('pe', lambda e, i=i, k=k, pb=pb: e.matmul(ps[pb][:, :], lhsT=uT[:, k, i * 128:(i + 1) * 128], rhs=wbuf[:, k, :],
                                                               start=(k == 0), stop=(k == KC - 1)),
                     reads=['w_cur'], writes=[PS(pb)], inc=(k == KC - 1))
            P.op('act', lambda e, i=i, pb=pb: e.copy(out=vtok[:, i, :, 0:128], in_=ps[pb][:, :].rearrange("p (h c) -> p h c", c=128)),
                 reads=[PS(pb)], writes=['vtok'])
        P.barrier()

        DIRS = [int(c) for c in os.environ.get('KDIRS', '01')]
        for d in DIRS:
            IGt, FPt, T3 = gt_tiles
            for gi, dst in ((2 * d, IGt), (2 * d + 1, FPt)):
                for gj, (t0, n) in enumerate(groups):
                    pb = 2 + gj % 2
                    for k in range(KC):
                        P.op('pe', lambda e, k=k, pb=pb, t0=t0, n=n, gi=gi: e.matmul(
                            ps[pb][0:4, 0:n], lhsT=wg_sb[:, k, gi * 4:(gi + 1) * 4], rhs=uT[:, k, t0:t0 + n],
                            start=(k == 0), stop=(k == KC - 1)), reads=['wg_sb'], writes=[PS(pb)], inc=(k == KC - 1))
                    P.op('act', lambda e, pb=pb, t0=t0, n=n, gi=gi, dst=dst: e.activation(
                        out=dst[:, t0:t0 + n], in_=ps[pb][0:4, 0:n], func=AF.Identity, bias=mgb_sb[:, gi:gi + 1], scale=1.0),
                        reads=[PS(pb), 'mgb_sb'], writes=[('gt', gi % 2)])
            P.op('act', lambda e: e.activation(out=FPt, in_=FPt, func=AF.Exp, scale=-1.0), reads=[('gt', 1)], writes=[('gt', 1)])
            P.op('act', lambda e: e.activation(out=FPt, in_=FPt, func=AF.Ln, bias=one_c[0:4, :], scale=1.0),
                 reads=[('gt', 1)], writes=[('gt', 1)])

            def scan(out_t, d0, d1, op0, op1, rd, wr):
                if d == 0:
                    P.op('dve', lambda e: e.tensor_tensor_scan(out=out_t[:, 0:NT], data0=d0[:, 0:NT], data1=d1[:, 0:NT], initial=0.0,
                                                               op0=op0, op1=op1), reads=rd, writes=wr)
                else:
                    P.op('dve', lambda e: e.tensor_tensor_scan(out=out_t[:, 0:TC][:, ::-1], data0=d0[:, 0:TC][:, ::-1],
                                                               data1=d1[:, 0:TC][:, ::-1], initial=0.0, op0=op0, op1=op1),
                         reads=rd, writes=wr)
                    P.op('dve', lambda e: e.tensor_tensor_scan(out=out_t[:, TC:NT][:, ::-1], data0=d0[:, TC:NT][:, ::-1],
                                                               data1=d1[:, TC:NT][:, ::-1], initial=out_t[:, 0:1], op0=op0, op1=op1),
                         reads=rd + wr, writes=wr)
            scan(T3, ones_nt[0:4, :], FPt, ALU.mult, ALU.add, [('gt', 1), 'ones_nt'], [('gt', 2)])
            P.op('dve', lambda e: e.tensor_tensor(out=IGt, in0=IGt, in1=T3, op=ALU.add), reads=[('gt', 0), ('gt', 2)],
                 writes=[('gt', 0)])
            scan(FPt, IGt, IGt, ALU.max, ALU.max, [('gt', 0)], [('gt', 1)])
            endcol = 63 if d == 0 else 0
            A3 = FPt.rearrange("p (c l) -> p c l", l=64)
            P.op('dve', lambda e: e.tensor_copy(out=Aend[:, 0, :], in_=A3[:, :, endcol]), reads=[('gt', 1)], writes=['Aend'])
            P.op('pool', lambda e: e.memset(Aend[:, 1, :], 0.0), writes=['Aprev'])
            if d == 0:
                P.op('dve', lambda e: e.tensor_copy(out=Aend[:, 1, 1:NCH], in_=Aend[:, 0, 0:NCH - 1]), reads=['Aend', 'Aprev'],
                     writes=['Aprev'])
            else:
                if NCC > 1:
                    P.op('dve', lambda e: e.tensor_copy(out=Aend[:, 1, 0:NCC - 1], in_=Aend[:, 0, 1:NCC]), reads=['Aend', 'Aprev'],
                         writes=['Aprev'])
                P.op('dve', lambda e: e.tensor_copy(out=Aend[:, 1, NCC:NCH - 1], in_=Aend[:, 0, NCC + 1:NCH]),
                     reads=['Aend', 'Aprev'], writes=['Aprev'])
                P.op('dve', lambda e: e.tensor_copy(out=Aend[:, 1, NCH - 1:NCH], in_=Aend[:, 0, 0:1]), reads=['Aend', 'Aprev'],
                     writes=['Aprev'])
            P.op('dve', lambda e: e.tensor_tensor(out=Aend[:, 2, :], in0=Aend[:, 1, :], in1=Aend[:, 0, :], op=ALU.subtract),
                 reads=['Aend', 'Aprev'], writes=['wc'])
            P.op('act', lambda e: e.activation(out=Aend[:, 2, :], in_=Aend[:, 2, :], func=AF.Exp), reads=['wc'], writes=['wc'])
            for ti_, nm in ((IGt, 0), (T3, 2)):
                P.op('dve', lambda e, ti_=ti_: e.tensor_tensor(out=ti_.rearrange("p (c l) -> p c l", l=64),
                                                               in0=ti_.rearrange("p (c l) -> p c l", l=64),
                                                               in1=Aend[:, 0, :].unsqueeze(2).to_broadcast([4, NCH, 64]),
                                                               op=ALU.subtract), reads=[('gt', nm), 'Aend'], writes=[('gt', nm)])
                P.op('act', lambda e, ti_=ti_: e.activation(out=ti_, in_=ti_, func=AF.Exp), reads=[('gt', nm)], writes=[('gt', nm)])
            for i in range(NTT):
                P.op('pe', lambda e, i=i: e.matmul(ps[2][:, i * 8:i * 8 + 4], lhsT=IGt[:, i * 128:(i + 1) * 128], rhs=ident_f[0:4, 0:4],
                                                   start=True, stop=True), reads=[('gt', 0), 'ident_f'], writes=[PS(2)], inc=False)
                P.op('pe', lambda e, i=i: e.matmul(ps[2][:, i * 8 + 4:i * 8 + 8], lhsT=T3[:, i * 128:(i + 1) * 128],
                                                   rhs=ident_f[0:4, 0:4], start=True, stop=True), reads=[('gt', 2), 'ident_f'],
                     writes=[PS(2)], inc=(i == NTT - 1))
            P.op('dve', lambda e: e.tensor_copy(out=edT[:].rearrange("p i c -> p (i c)"), in_=ps[2][:, 0:NTT * 8]), reads=[PS(2)],
                 writes=['edT'])
            for pr in range(2):
                P.op('pe', lambda e, pr=pr: e.matmul(ps[3][:, pr * NCH:(pr + 1) * NCH],
                                                     lhsT=selh[0:4, pr].rearrange("p a b -> p (a b)"), rhs=Aend[:, 2, :],
                                                     start=True, stop=True), reads=['selh', 'wc'], writes=[PS(3)], inc=(pr == 1))
            P.op('dve', lambda e: e.tensor_copy(out=wcB[:].rearrange("p a c -> p (a c)"), in_=ps[3][:, 0:2 * NCH]), reads=[PS(3)],
                 writes=['wcB'])
            P.op('dve', lambda e: e.tensor_tensor(out=kp[:].rearrange("p i (h c) -> p i h c", c=64),
                                                  in0=kTok[:].rearrange("p i (h c) -> p i h c", c=64),
                                                  in1=edT[:, :, 0:4].unsqueeze(3).to_broadcast([128, NTT, 4, 64]), op=ALU.mult),
                 reads=['kTok', 'edT'], writes=['kp'])
            P.barrier()
            P.op('pool', lambda e: e.memset(Chat[:], 0.0), writes=['Chat'])
            P.op('pool', lambda e: e.memset(CbfA[:], 0.0), writes=[('CbfA', 0), ('CbfA', 1)])
            P.op('pool', lambda e: e.memset(CbfB[:], 0.0), writes=[('CbfB', 0), ('CbfB', 1)])
            order = list(range(NCH)) if d == 0 else list(range(NCC - 1, -1, -1)) + list(range(NCH - 1, NCC - 1, -1))
            mask = masks[d]
            first_dir = (d == DIRS[0])

            def ml_geo(step):
                c = order[step]
                ti, p0 = c // 2, (c % 2) * 64
                return c, ti, slice(p0, p0 + 64), slice(c * 64, (c + 1) * 64), c >= NCC, 6 + step % 2, c % 2

            def ml_pre(step):
                c, ti, rows, toks, lat, pbD, hf = ml_geo(step)
                if lat:
                    for h in range(4):
                        hp = (h % 2) * 64
                        P.op('pe', lambda e, h=h, hp=hp: e.matmul(
                            ps[2 + h % 2][rows, (h // 2) * 64:(h // 2) * 64 + 64], lhsT=qkT[hp:hp + 64, 2 + h // 2, toks],
                            rhs=qkT[hp:hp + 64, h // 2, toks], start=True, stop=True), writes=[('psh', 2 + h % 2, hf)], inc=(h >= 2))
                for h in range(4):
                    P.op('pe', lambda e, h=h: e.matmul(
                        ps[pbD][(h % 2) * 64:(h % 2) * 64 + 64, (h // 2) * 129:(h // 2 + 1) * 129],
                        lhsT=kp[rows, ti, h * 64:(h + 1) * 64], rhs=vtok[rows, ti, h, :], start=True, stop=True),
                        reads=['kp'], writes=[PS(pbD)], inc=(h == 3))
                if lat:
                    for h in range(4):
                        par, hh = h % 2, h // 2
                        P.op('dve', lambda e, h=h, par=par, hh=hh: e.scalar_tensor_tensor(
                            out=Pt[rows, par, hh, :], in0=ps[2 + par][rows, hh * 64:(hh + 1) * 64], scalar=edT[rows, ti, h:h + 1],
                            in1=mask[rows, :], op0=ALU.mult, op1=ALU.mult), reads=[('psh', 2 + par, hf), 'edT'],
                            writes=[('Pt', hf, h)])

            def ml_main(step):
                c, ti, rows, toks, lat, pbD, hf = ml_geo(step)
                for pr in range(2):
                    P.op('dve', lambda e, pr=pr: e.scalar_tensor_tensor(
                        out=Chat[:, pr, :], in0=Chat[:, pr, :], scalar=wcB[:, pr, c:c + 1], in1=ps[pbD][:, pr * 129:(pr + 1) * 129],
                        op0=ALU.mult, op1=ALU.add), reads=['Chat', 'wcB', PS(pbD)], writes=['Chat'])
                if step + 1 < len(order):
                    cn = order[step + 1]
                    for pr in range(2):
                        P.op('act', lambda e, pr=pr: e.activation(out=CbfA[0:64, (step + 1) % 2, pr, :], in_=Chat[0:64, pr, :], func=AF.Identity,
                                                                  scale=wcB[0:64, pr, cn:cn + 1]),
                             reads=['Chat', 'wcB'], writes=[('CbfA', (step + 1) % 2)])
                        P.op('act', lambda e, pr=pr: e.activation(out=CbfB[64:128, (step + 1) % 2, pr, :], in_=Chat[64:128, pr, :], func=AF.Identity,
                                                                  scale=wcB[64:128, pr, cn:cn + 1]),
                             reads=['Chat', 'wcB'], writes=[('CbfB', (step + 1) % 2)])

                if lat:
                    j = ti - NTC
                    for h in range(4):
                        par, pr = h % 2, h // 2
                        P.op('pe', lambda e, h=h, par=par, pr=pr: e.matmul(
                            ps[4 + pr][rows, par * 129:(par + 1) * 129], lhsT=Pt[rows, par, pr, :], rhs=vtok[rows, ti, h, :],
                            start=True, stop=False), reads=[('Pt', hf, h)], writes=[('psh', 4 + pr, hf)], inc=False)
                        P.op('pe', lambda e, par=par, pr=pr: e.matmul(
                            ps[4 + pr][rows, par * 129:(par + 1) * 129], lhsT=qkT[:, pr, toks],
                            rhs=(CbfA if par == 0 else CbfB)[:, step % 2, pr, :], start=False, stop=True),
                            reads=[('CbfA', step % 2), ('CbfB', step % 2)], writes=[('psh', 4 + pr, hf)], inc=True)
                    for pr in range(2):
                        den = lambda pr=pr, rows=rows: ps[4 + pr][rows, 0:258].rearrange("p (a c) -> p a c", c=129)[:, :, 128]
                        P.op('dve', lambda e, pr=pr, den=den: e.tensor_tensor(
                            out=dn[rows, 2 * pr:2 * pr + 2], in0=den(), in1=edT[rows, ti, 4 + 2 * pr:6 + 2 * pr], op=ALU.max),
                            reads=[('psh', 4 + pr, hf), 'edT'], writes=[('dn', hf)])
                        P.op('dve', lambda e, pr=pr, den=den: e.scalar_tensor_tensor(
                            out=dn[rows, 2 * pr:2 * pr + 2], in0=den(), scalar=-1.0, in1=dn[rows, 2 * pr:2 * pr + 2],
                            op0=ALU.mult, op1=ALU.max), reads=[('psh', 4 + pr, hf), ('dn', hf)], writes=[('dn', hf)])
                    P.op('dve', lambda e: e.reciprocal(out=dn[rows, 4:8], in_=dn[rows, 0:4]), reads=[('dn', hf)],
                         writes=[('rd', hf)])
                    for h in range(4):
                        src = lambda h=h, rows=rows: ps[4 + h // 2][rows, (h % 2) * 129:(h % 2) * 129 + 128]
                        if first_dir:
                            P.op('dve', lambda e, h=h, src=src: e.tensor_scalar(
                                out=Hacc[rows, j, h * 128:(h + 1) * 128], in0=src(), scalar1=dn[rows, 4 + h:5 + h], scalar2=None,
                                op0=ALU.mult), reads=[('psh', 4 + h // 2, hf), ('rd', hf)], writes=[('Hacc', j)])
                        else:
                            P.op('dve', lambda e, h=h, src=src: e.scalar_tensor_tensor(
                                out=Hacc[rows, j, h * 128:(h + 1) * 128], in0=src(), scalar=dn[rows, 4 + h:5 + h],
                                in1=Hacc[rows, j, h * 128:(h + 1) * 128], op0=ALU.mult, op1=ALU.add),
                                reads=[('psh', 4 + h // 2, hf), ('rd', hf), ('Hacc', j)], writes=[('Hacc', j)])
            ml_pre(0)
            for step in range(len(order)):
                if step + 1 < len(order):
                    ml_pre(step + 1)
                ml_main(step)
            P.barrier()
        if dbg:
            P.dma('sp', dbg['HM'][b], Hacc, writes=['dbgHM'])
            P.dma('sp', dbg['edT'][b], edT[:], writes=['dbgedT'])
            P.dma('sp', dbg['wcB'][b], wcB[:], writes=['dbgwcB'])
            P.dma('sp', dbg['qkT'][b], qkT[:], writes=['dbgqkT'])
            P.dma('sp', dbg['vtok'][b], vtok[:], writes=['dbgvtok'])
            P.dma('sp', dbg['kTok'][b], kTok[:], writes=['dbgkTok'])
            P.barrier()
        win_load(wbuf[:], 1024, 512, 'w_cur')
        P.dma('sp', gnbc[:], g['mng_d'].to_broadcast([128, 512]), writes=['gnbc'])
        def mm_front(j):
            tk = slice(TC + j * 128, TC + (j + 1) * 128)
            xh = xn[:, (j % 2) * 512:(j % 2) * 512 + 512]
            pb = 2 + j % 2
            for k in range(KC):
                P.op('pe', lambda e, k=k: e.matmul(ps[pb][:, :], lhsT=uT[:, k, tk], rhs=wbuf[:, k, :], start=(k == 0),
                                                   stop=(k == KC - 1)), reads=['w_cur'], writes=[PS(pb)], inc=(k == KC - 1))
            P.op('act', lambda e: e.activation(out=xh, in_=ps[pb][:, :], func=AF.Sigmoid), reads=[PS(pb)], writes=[('xnh', j % 2)])

        def mm_back(j):
            xh = xn[:, (j % 2) * 512:(j % 2) * 512 + 512]
            per_head_norm_stats(j)
            P.op('dve', lambda e: e.tensor_tensor(out=rms_scratch[:, 0:512], in0=rms_scratch[:, 0:512], in1=gnbc[:], op=ALU.mult),
                 reads=['rms_scratch', 'gnbc'], writes=['rms_scratch'])
            P.op('dve', lambda e: e.tensor_tensor(out=a_tok[:, j, :], in0=rms_scratch[:, 0:512], in1=xh, op=ALU.mult),
                 reads=['rms_scratch', ('xnh', j % 2)], writes=[('a_tok', j)])

        mm_front(0)
        for j in range(NLT):
            if j + 1 < NLT:
                mm_front(j + 1)
            mm_back(j)
        P.barrier()

    mst.close()
    KSTOP = os.environ.get('KSTOP', '')
    rm = ones_nt
    with nc.sbuf_tensor(S("gv"), [128, NTT, 512], BF16) as gv, \
            nc.sbuf_tensor(S("qt"), [128, 2, NT], BF16) as qt, nc.sbuf_tensor(S("kt"), [128, 2, NT], BF16) as kt, \
            nc.sbuf_tensor(S("kraw"), [128, NT], BF16) as kraw, \
            nc.sbuf_tensor(S("lrT"), [16, NT], BF16) as lrT, \
            nc.sbuf_tensor(S("tA"), [128, NT], F32) as tA, nc.sbuf_tensor(S("tB"), [128, NT], F32) as tB, \
            nc.sbuf_tensor(S("ebL"), [128, 2, NCH], F32) as ebL, \
            nc.sbuf_tensor(S("Sst"), [128, 2, 128], F32) as Sst, nc.sbuf_tensor(S("SbfA"), [128, 2, 2, 128], BF16) as SbfA, \
            nc.sbuf_tensor(S("SbfB"), [128, 2, 2, 128], BF16) as SbfB:
        win_load(wbuf[:], 2064, 512, 'w_cur')
        for i in range(NTT):
            pb = 2 + i % 2
            for k in range(KC):
                P.op('pe', lambda e, i=i, k=k, pb=pb: e.matmul(ps[pb][:, :], lhsT=uT[:, k, i * 128:(i + 1) * 128], rhs=wbuf[:, k, :],
                                                               start=(k == 0), stop=(k == KC - 1)),
                     reads=['w_cur'], writes=[PS(pb)], inc=(k == KC - 1))
            P.op('act', lambda e, i=i, pb=pb: e.copy(out=gv[:, i, :], in_=ps[pb][:, :]), reads=[PS(pb)], writes=['gv'])
        P.barrier()
        DIRS = [int(c) for c in os.environ.get('KDIRS', '01')]
        if KSTOP == 'mlstm':
            DIRS = []
        for d in DIRS:
            endcol = 63 if d == 0 else 0
            win_load(wbuf[:], 1552, 512, 'w_cur')
            P.op('pool', lambda e: e.memset(rm[:], 1.0), writes=['rm'])
            zc = 0 if d == 0 else 63
            P.op('pool', lambda e: e.memset(rm[:].rearrange("p (c l) -> p c l", l=64)[:, :, zc:zc + 1], 0.0), writes=['rm'])
            for gj, (t0, n) in enumerate(groups):
                pb = 2 + gj % 2
                for k in range(KC):
                    P.op('pe', lambda e, k=k, pb=pb, t0=t0, n=n: e.matmul(
                        ps[pb][0:16, 0:n], lhsT=wlr_sb[:, k, d * 16:(d + 1) * 16], rhs=uT[:, k, t0:t0 + n],
                        start=(k == 0), stop=(k == KC - 1)), reads=['wlr_sb'], writes=[PS(pb)], inc=(k == KC - 1))
                P.op('act', lambda e, pb=pb, t0=t0, n=n: e.copy(out=lrT[:, t0:t0 + n], in_=ps[pb][0:16, 0:n]),
                     reads=[PS(pb)], writes=['lrT'])
            for jc in range(2):
                for gj, (t0, n) in enumerate(groups):
                    pb = 2 + gj % 2
                    P.op('pe', lambda e, pb=pb, t0=t0, n=n: e.matmul(
                        ps[pb][:, 0:n], lhsT=gw2_sb[:, d, jc * 128:(jc + 1) * 128], rhs=lrT[:, t0:t0 + n], start=True, stop=True),
                        reads=['gw2_sb', 'lrT'], writes=[PS(pb)])
                    P.op('act', lambda e, pb=pb, t0=t0, n=n: e.activation(
                        out=tA[:, t0:t0 + n], in_=ps[pb][:, 0:n], func=AF.Exp, scale=-1.0, bias=ggbT[:, d, jc:jc + 1]),
                        reads=[PS(pb), 'ggbT'], writes=['tA'])
                P.op('act', lambda e: e.activation(out=tA[:], in_=tA[:], func=AF.Ln, bias=one_c[:], scale=1.0), reads=['tA'],
                     writes=['tA'])
                if d == 0:
                    P.op('dve', lambda e: e.tensor_tensor_scan(out=tB[:], data0=rm[:], data1=tA[:], initial=0.0,
                                                               op0=ALU.mult, op1=ALU.add), reads=['tA', 'rm'], writes=['tB'])
                else:
                    P.op('dve', lambda e: e.tensor_tensor_scan(out=tB[:, ::-1], data0=rm[:, ::-1], data1=tA[:, ::-1], initial=0.0,
                                                               op0=ALU.mult, op1=ALU.add), reads=['tA', 'rm'], writes=['tB'])
                bL = tB[:].rearrange("p (c l) -> p c l", l=64)[:, :, endcol]
                P.op('act', lambda e: e.activation(out=ebL[:, jc, :], in_=bL, func=AF.Exp, scale=-1.0 / 16),
                     reads=['tB'], writes=['ebL'])
                P.op('act', lambda e: e.activation(out=tA[:], in_=tB[:], func=AF.Exp, scale=-1.0 / 16), reads=['tB', 'tA'],
                     writes=['tA'])
                proj_featmajor(wbuf, jc * 128, None,
                               lambda pb, t0, n: P.op('dve', lambda e: e.scalar_tensor_tensor(
                                   out=qt[:, jc, t0:t0 + n], in0=ps[pb][:, 0:n], scalar=0.125, in1=tA[:, t0:t0 + n],
                                   op0=ALU.mult, op1=ALU.mult), reads=[PS(pb), 'tA'], writes=['qt']))
                P.op('act', lambda e: e.activation(out=tA[:], in_=tB[:], func=AF.Exp, scale=1.0 / 16), reads=['tB', 'qt', 'tA'],
                     writes=['tA'])

                def evac_k(pb, t0, n):
                    P.op('dve', lambda e: e.tensor_copy(out=kraw[:, t0:t0 + n], in_=ps[pb][:, 0:n]), reads=[PS(pb)], writes=['kraw'])
                    P.op('dve', lambda e: e.tensor_tensor(out=kt[:, jc, t0:t0 + n], in0=ps[pb][:, 0:n], in1=tA[:, t0:t0 + n],
                                                          op=ALU.mult), reads=[PS(pb), 'tA'], writes=['kt'])
                proj_featmajor(wbuf, 256 + jc * 128, None, evac_k)
                P.op('dve', lambda e: e.tensor_tensor(out=tA[:].rearrange("p (c l) -> p c l", l=64),
                                                      in0=tB[:].rearrange("p (c l) -> p c l", l=64),
                                                      in1=bL.unsqueeze(2).to_broadcast([128, NCH, 64]), op=ALU.subtract),
                     reads=['tB', 'kt', 'tA'], writes=['tA'])
                P.op('act', lambda e: e.activation(out=tA[:], in_=tA[:], func=AF.Exp, scale=1.0 / 16), reads=['tA'], writes=['tA'])
                P.op('dve', lambda e: e.tensor_tensor(out=khT, in0=kraw[:], in1=tA[:], op=ALU.mult),
                     reads=['tA', 'kraw'], writes=['khT'])
                for i in range(NTT):
                    P.op('pe', lambda e, i=i: e.transpose(out=psb[1][:, 0:128], in_=khT[:, i * 128:(i + 1) * 128], identity=ident_b[:]),
                         reads=['khT', 'ident_b'], writes=[PS(1)])
                    P.op('dve', lambda e, i=i: e.tensor_copy(out=khTok[:, i, jc * 128:(jc + 1) * 128], in_=psb[1][:, 0:128]),
                         reads=[PS(1)], writes=['khTok'])
            P.barrier()
            P.op('pool', lambda e: e.memset(Sst[:], 0.0), writes=['Sst'])
            P.op('pool', lambda e: e.memset(SbfA[:], 0.0), writes=[('SbfA', 0), ('SbfA', 1)])
            P.op('pool', lambda e: e.memset(SbfB[:], 0.0), writes=[('SbfB', 0), ('SbfB', 1)])
            order = list(range(NCH)) if d == 0 else list(range(NCC - 1, -1, -1)) + list(range(NCH - 1, NCC - 1, -1))
            mask = masks[d]
            first_dir = (d == DIRS[0])

            def gl_geo(step):
                c = order[step]
                ti, p0 = c // 2, (c % 2) * 64
                return c, ti, slice(p0, p0 + 64), slice(c * 64, (c + 1) * 64), c >= NCC, 6 + step % 2, c % 2

            def gl_pre(step):
                c, ti, rows, toks, lat, pbD, hf = gl_geo(step)
                if lat:
                    for h in range(4):
                        hp = (h % 2) * 64
                        P.op('pe', lambda e, h=h, hp=hp: e.matmul(
                            ps[2 + h % 2][rows, (h // 2) * 64:(h // 2) * 64 + 64], lhsT=kt[hp:hp + 64, h // 2, toks],
                            rhs=qt[hp:hp + 64, h // 2, toks], start=True, stop=True), writes=[('psh', 2 + h % 2, hf)], inc=(h >= 2))
                for h in range(4):
                    P.op('pe', lambda e, h=h: e.matmul(
                        ps[pbD][(h % 2) * 64:(h % 2) * 64 + 64, (h // 2) * 128:(h // 2 + 1) * 128],
                        lhsT=khTok[rows, ti, h * 64:(h + 1) * 64], rhs=gv[rows, ti, h * 128:(h + 1) * 128], start=True, stop=True),
                        writes=[PS(pbD)], inc=(h == 3))
                if lat:
                    for par in range(2):
                        P.op('dve', lambda e, par=par: e.tensor_tensor(
                            out=Pt[rows, par, :, :], in0=ps[2 + par][rows, 0:128].rearrange("p (a c) -> p a c", c=64),
                            in1=mask[rows, :].unsqueeze(1).to_broadcast([64, 2, 64]), op=ALU.mult),
                            reads=[('psh', 2 + par, hf)], writes=[('Pt', hf, par)])

            def gl_main(step):
                c, ti, rows, toks, lat, pbD, hf = gl_geo(step)
                for pr in range(2):
                    P.op('dve', lambda e, pr=pr: e.scalar_tensor_tensor(
                        out=Sst[:, pr, :], in0=Sst[:, pr, :], scalar=ebL[:, pr, c:c + 1], in1=ps[pbD][:, pr * 128:(pr + 1) * 128],
                        op0=ALU.mult, op1=ALU.add), reads=['Sst', 'ebL', PS(pbD)], writes=['Sst'])
                P.op('act', lambda e: e.copy(out=SbfA[0:64, (step + 1) % 2], in_=Sst[0:64]), reads=['Sst'], writes=[('SbfA', (step + 1) % 2)])
                P.op('act', lambda e: e.copy(out=SbfB[64:128, (step + 1) % 2], in_=Sst[64:128]), reads=['Sst'], writes=[('SbfB', (step + 1) % 2)])

                if lat:
                    j = ti - NTC
                    for h in range(4):
                        par, pr = h % 2, h // 2
                        P.op('pe', lambda e, h=h, par=par, pr=pr: e.matmul(
                            ps[4][rows, h * 128:(h + 1) * 128], lhsT=Pt[rows, par, pr, :], rhs=gv[rows, ti, h * 128:(h + 1) * 128],
                            start=True, stop=False), reads=[('Pt', hf, par)], writes=[('psh', 4, hf)], inc=False)
                        P.op('pe', lambda e, h=h, par=par, pr=pr: e.matmul(
                            ps[4][rows, h * 128:(h + 1) * 128], lhsT=qt[:, pr, toks], rhs=(SbfA if par == 0 else SbfB)[:, step % 2, pr, :],
                            start=False, stop=True), reads=[('SbfA', step % 2), ('SbfB', step % 2)], writes=[('psh', 4, hf)], inc=(h == 3))
                    if first_dir:
                        P.op('act', lambda e: e.copy(out=Hacc[rows, j, :], in_=ps[4][rows, :]), reads=[('psh', 4, hf)],
                             writes=[('Hacc', j)])
                    else:
                        P.op('act', lambda e: e.copy(out=rms_scratch[rows, 0:512], in_=ps[4][rows, :]), reads=[('psh', 4, hf)],
                             writes=[('otmp', hf)])
                        P.op('pool', lambda e: e.tensor_tensor(out=Hacc[rows, j, :], in0=Hacc[rows, j, :],
                                                               in1=rms_scratch[rows, 0:512], op=ALU.add),
                             reads=[('otmp', hf), ('Hacc', j)], writes=[('Hacc', j)])
            gl_pre(0)
            for step in range(len(order)):
                if step + 1 < len(order):
                    gl_pre(step + 1)
                gl_main(step)
            P.barrier()
            if dbg and d == DIRS[0]:
                P.dma('sp', dbg['H1'][b], Hacc, writes=['dbgH1'])
                P.barrier()
        if dbg:
            P.dma('sp', dbg['H'][b], Hacc, writes=['dbgH'])
            P.dma('sp', dbg['qt'][b], qt[:], writes=['dbgqt'])
            P.dma('sp', dbg['kt'][b], kt[:], writes=['dbgkt'])
            P.dma('sp', dbg['gv'][b], gv[:], writes=['dbggv'])
            P.dma('sp', dbg['khTok'][b], khTok, writes=['dbgkh'])
            P.dma('sp', dbg['ebL'][b], ebL[:], writes=['dbgebl'])
            P.barrier()
        P.op('pool', lambda e: e.memset(ones_nt[:], 1.0), writes=['rm'])
        win_load(wbuf[:], 2576, 512, 'w_cur')
        P.dma('sp', gnbc[:], g['gng_d'].to_broadcast([128, 512]), writes=['gnbc'])

        def gm_front(j):
            tk = slice(TC + j * 128, TC + (j + 1) * 128)
            xh = xn[:, (j % 2) * 512:(j % 2) * 512 + 512]
            pb = 2 + j % 2
            for k in range(KC):
                P.op('pe', lambda e, k=k: e.matmul(ps[pb][:, :], lhsT=uT[:, k, tk], rhs=wbuf[:, k, :], start=(k == 0),
                                                   stop=(k == KC - 1)), reads=['w_cur'], writes=[PS(pb)], inc=(k == KC - 1))
            P.op('act', lambda e: e.activation(out=xh, in_=ps[pb][:, :], func=AF.Sigmoid), reads=[PS(pb)], writes=[('xnh', j % 2)])
            P.op('dve', lambda e: e.tensor_tensor(out=xh, in0=ps[pb][:, :], in1=xh, op=ALU.mult),
                 reads=[PS(pb), ('xnh', j % 2)], writes=[('xnh', j % 2)])

        def gm_back(j):
            xh = xn[:, (j % 2) * 512:(j % 2) * 512 + 512]
            per_head_norm_stats(j)
            P.op('dve', lambda e: e.tensor_tensor(out=rms_scratch[:, 0:512], in0=rms_scratch[:, 0:512], in1=gnbc[:], op=ALU.mult),
                 reads=['rms_scratch', 'gnbc'], writes=['rms_scratch'])
            P.op('dve', lambda e: e.tensor_tensor(out=g_tok(j), in0=rms_scratch[:, 0:512], in1=xh, op=ALU.mult),
                 reads=['rms_scratch', ('xnh', j % 2), ('Hacc', j)], writes=[('g_tok', j)])

        gm_front(0)
        for j in range(NLT):
            if j + 1 < NLT:
                gm_front(j + 1)
            gm_back(j)
        P.barrier()

    if dbg:
        P.dma('sp', dbg['uT'][b], uT[:], writes=['dbg_uT'])
        P.barrier()
    for j in range(NLT if KSTOP == '' else 0):
        for src_i, src in enumerate((a_tok[:, j, :], g_tok(j))):
            pb = src_i
            for q in range(4):
                P.op('pe', lambda e, q=q, src=src, pb=pb: e.transpose(out=psb[pb][:, q * 128:(q + 1) * 128],
                                                                      in_=src[:, q * 128:(q + 1) * 128], identity=ident_b[:]),
                     reads=['ident_b'], writes=[PS(pb)], inc=(q == 3))
            P.op('act' if src_i == 0 else 'dve',
                 (lambda e, j=j, pb=pb, src_i=src_i: e.copy(out=mT[:, 4 * src_i:4 * src_i + 4, j * 128:(j + 1) * 128],
                                                            in_=psb[pb][:, 0:512].rearrange("p (q c) -> p q c", c=128)))
                 if src_i == 0 else
                 (lambda e, j=j, pb=pb, src_i=src_i: e.tensor_copy(out=mT[:, 4 * src_i:4 * src_i + 4, j * 128:(j + 1) * 128],
                                                                   in_=psb[pb][:, 0:512].rearrange("p (q c) -> p q c", c=128))),
                 reads=[PS(pb)], writes=[('mT', j, src_i)])
    P.barrier()
    if dbg:
        P.dma('sp', dbg['mT'][b], mT, writes=['dbg_mT'])
        P.barrier()
    with nc.sbuf_tensor(S("wout"), [128, KC, D], BF16) as wout, nc.sbuf_tensor(S("xt2"), [128, 2, D], F32) as xt2, \
            nc.sbuf_tensor(S("pt2"), [128, 2, D], F32) as pt2:
        for half in range(2):
            P.dma('pool', wout[:, :, half * 512:(half + 1) * 512],
                  wout_d[:, half * 512:(half + 1) * 512].rearrange("(k p) c -> p k c", p=128), writes=['wout'])
        g['make_gbc'](b, 0)
        for j in range(NLT):
            bf = j % 2
            P.dma('sp', x_res[:, j, :], x_d[b, j * 128:(j + 1) * 128, :], writes=[('x_res', j)])
            P.dma('sp', pt2[:, bf, :], pe_d[j * 128:(j + 1) * 128, :], writes=[('pt2', bf)])
            P.op('pool', lambda e, j=j, bf=bf: e.tensor_tensor(out=x_res[:, j, :], in0=x_res[:, j, :], in1=pt2[:, bf, :], op=ALU.add),
                 reads=[('x_res', j), ('pt2', bf)], writes=[('x_res', j)])
            for half in range(2):
                pb = 2 + half
                hs = slice(half * 512, (half + 1) * 512)
                for k in range(KC):
                    P.op('pe', lambda e, k=k, j=j, pb=pb, hs=hs: e.matmul(ps[pb][:, :], lhsT=mT[:, k, j * 128:(j + 1) * 128],
                                                                          rhs=wout[:, k, hs], start=(k == 0), stop=(k == KC - 1)),
                         reads=['wout'], writes=[PS(pb)], inc=(k == KC - 1))
                P.op('dve', lambda e, pb=pb, hs=hs, bf=bf: e.tensor_tensor(out=xt2[:, bf, hs], in0=ps[pb][:, :], in1=gbc[:, hs],
                                                                           op=ALU.mult), reads=[PS(pb), 'gbc'], writes=[('xt2', bf, half)])
                if dbg:
                    pass
                P.op('pool', lambda e, j=j, hs=hs, bf=bf: e.tensor_tensor(out=x_res[:, j, hs], in0=x_res[:, j, hs], in1=xt2[:, bf, hs],
                                                                          op=ALU.add), reads=[('xt2', bf, half), ('x_res', j)],
                     writes=[('x_res', j)])
        P.barrier()
    st.close()


_CACHE = {}


def _get_nc(cfg_key):
    if cfg_key not in _CACHE:
        _CACHE[cfg_key] = build(Cfg(*cfg_key))
    return _CACHE[cfg_key]


def make_in_maps(inputs, n_cores, NB):
    f = lambda a: np.ascontiguousarray(np.asarray(a, dtype=np.float32))
    shared = {
        "c_ctx": f(inputs["c_ctx"]).reshape(1, D),
        "ada_w": f(inputs["ada_w"])[0], "ada_b": f(inputs["ada_b"]).reshape(1, -1),
        "norm1_g": f(inputs["norm1_g"]).reshape(1, D), "w_in": f(inputs["w_in"])[0],
        "ml_conv_w": f(inputs["ml_conv_w"])[0], "ml_conv_b": f(inputs["ml_conv_b"]).reshape(1, -1),
        "ml_gate_b": f(inputs["ml_gate_b"])[0], "ml_norm_g": f(inputs["ml_norm_g"]).reshape(1, -1),
        "gla_gate_w2": f(inputs["gla_gate_w2"])[0], "gla_gate_b": f(inputs["gla_gate_b"])[0],
        "gla_norm_g": f(inputs["gla_norm_g"]).reshape(1, -1), "w_out": f(inputs["w_out"])[0],
        "norm2_g": f(inputs["norm2_g"]).reshape(1, D), "router_w": f(inputs["router_w"])[0],
        "router_b": f(inputs["router_b"]).reshape(1, -1), "moe_w_gu": f(inputs["moe_w_gu"])[0],
        "moe_b_gu": f(inputs["moe_b_gu"])[0], "moe_w_down": f(inputs["moe_w_down"])[0],
        "moe_b_down": f(inputs["moe_b_down"])[0], "final_norm_g": f(inputs["final_norm_g"]).reshape(1, D),
    }
    x, c, ctx = f(inputs["x"]), f(inputs["c"]), f(inputs["ctx"])
    maps = []
    for i in range(n_cores):
        m = dict(shared)
        m["x"] = x[i * NB:(i + 1) * NB]
        m["c"] = c[i * NB:(i + 1) * NB]
        m["ctx"] = ctx[i * NB:(i + 1) * NB]
        maps.append(m)
    return maps


def kernel(**inputs):
    n_cores = 8
    B = inputs["x"].shape[0]
    NB = B // n_cores
    T = inputs["x"].shape[1]
    TC = inputs["ctx"].shape[1]
    E = inputs["router_w"].shape[-1]
    nc = _get_nc((NB, T, TC, E, False, 99))
    maps = make_in_maps(inputs, n_cores, NB)
    res = run_bass_kernel_spmd(nc, maps, core_ids=list(range(n_cores)))
    return np.concatenate([r["out"] for r in res.results], axis=0)
```
